# Optimizing a Trainium2 kernel written in Bass

```python
import jax
import jax.numpy as jnp
from jax import lax
import numpy as np


D_MODEL = 1024
BATCH = 32
SEQ = 256
DEPTH = 2
DEC_BATCH = 8
DEC_SEQ = 4096
PAST_LEN = 256

GRID_W = 64
POOL_GROUPS = 4
POOL_GROUP_WIDTH = 128
POOL_WIDTH = POOL_GROUPS * POOL_GROUP_WIDTH
POOL_WINDOWS = (2, 4, 8, 16)
LRU_WIDTH = D_MODEL
LRU_HEADS = 16
LRU_HEAD_DIM = LRU_WIDTH // LRU_HEADS
CONV_WIDTH = 4
CONV_LEFT = 2
LRU_C = 8.0
N_EXPERTS = 16
EXPERT_FF = 2048
CAPACITY_FACTOR = 2
N_MOD = 6
IN_WIDTH = POOL_WIDTH + LRU_WIDTH + 2 * D_MODEL
EPS = 1e-6

kernel_name = 'hybrid_pool_rglru_ecmoe_diffusion_step'


def _rmsnorm(x, g):
    x32 = x.astype(jnp.float32)
    y = x32 * lax.rsqrt(jnp.mean(x32 * x32, axis=-1, keepdims=True) + EPS)
    return (y * g.astype(jnp.float32)).astype(x.dtype)


def _multiscale_pool(p):
    n = p.shape[-2]
    p32 = p.astype(jnp.float32)
    cs = jnp.cumsum(p32, axis=-2)
    cs = jnp.concatenate([jnp.zeros_like(cs[..., :1, :]), cs], axis=-2)
    t = jnp.arange(n)
    outs = []
    for g, w in enumerate(POOL_WINDOWS):
        lo = jnp.clip(t - w // 2, 0, n)
        hi = jnp.clip(t + w // 2, 0, n)
        sl = slice(g * POOL_GROUP_WIDTH, (g + 1) * POOL_GROUP_WIDTH)
        seg = cs[..., sl]
        s = jnp.take(seg, hi, axis=-2) - jnp.take(seg, lo, axis=-2)
        cnt = (hi - lo).astype(jnp.float32)[:, None]
        outs.append(s / cnt - p32[..., sl])
    return jnp.concatenate(outs, axis=-1)


def _pool_mixer(p, w_group, scale, rows):
    bsz, n, ch = p.shape
    if rows is None:
        pooled = _multiscale_pool(p)
    else:
        pooled = _multiscale_pool(p.reshape(bsz, rows, GRID_W, ch)).reshape(bsz, n, ch)
    pg = pooled.reshape(bsz, n, POOL_GROUPS, POOL_GROUP_WIDTH)
    y = jnp.einsum('bngc,gcd->bngd', pg, w_group.astype(jnp.float32)).reshape(bsz, n, ch)
    return (y * scale.astype(jnp.float32)).astype(p.dtype)


def _centred_dwconv(v, w, b):
    n = v.shape[1]
    vp = jnp.pad(v, ((0, 0), (CONV_LEFT, CONV_WIDTH - 1 - CONV_LEFT), (0, 0)))
    out = b
    for k in range(CONV_WIDTH):
        out = out + vp[:, k:k + n] * w[k]
    return out


def _rglru_bidir(v, w_r, b_r, w_i, b_i, lam, h0):
    bsz, n, _ = v.shape
    v32 = v.astype(jnp.float32)
    vh = v32.reshape(bsz, n, LRU_HEADS, LRU_HEAD_DIM)

    def gate(w, bias):
        z = jnp.einsum('bnhd,zhde->zbnhe', vh, w.astype(jnp.float32)).reshape(2, bsz, n, LRU_WIDTH)
        return jax.nn.sigmoid(z + bias.astype(jnp.float32)[:, None, None, :])

    r = gate(w_r, b_r)
    i = gate(w_i, b_i)
    log_a = -LRU_C * r * jax.nn.softplus(-lam.astype(jnp.float32))[:, None, None, :]
    a = jnp.exp(log_a)
    u = jnp.sqrt(-jnp.expm1(2.0 * log_a)) * i * v32[None]
    a = jnp.stack([a[0], a[1][:, ::-1]])
    u = jnp.stack([u[0], u[1][:, ::-1]])

    def step(h, au):
        a_t, u_t = au
        h = a_t * h + u_t
        return h, h

    h_last, hs = lax.scan(step, h0, (jnp.moveaxis(a, 2, 0), jnp.moveaxis(u, 2, 0)))
    y = hs[:, 0] + hs[::-1, 1]
    return jnp.moveaxis(y, 0, 1).astype(v.dtype), h_last


def _token_mixer(h, lp, rows, h0):
    d = h.shape[-1]
    u = h @ lp['w_in']
    p = u[..., :POOL_WIDTH]
    v = u[..., POOL_WIDTH:POOL_WIDTH + LRU_WIDTH]
    gl = u[..., POOL_WIDTH + LRU_WIDTH:]
    y_pool = _pool_mixer(p, lp['pool_w'], lp['pool_scale'], rows)
    v = _centred_dwconv(v, lp['conv_w'], lp['conv_b'])
    y_lru, h_last = _rglru_bidir(v, lp['lru_wr'], lp['lru_br'], lp['lru_wi'], lp['lru_bi'], lp['lru_lambda'], h0)
    g = jax.nn.sigmoid(gl.astype(jnp.float32)).astype(h.dtype)
    merged = g[..., :d] * (y_pool @ lp['w_br_pool']) + g[..., d:] * (y_lru @ lp['w_br_lru'])
    return merged @ lp['w_out'], h_last


def _expert_choice_moe(h, router_w, w1, w3, w2):
    bsz, n, d = h.shape
    cap = CAPACITY_FACTOR * n // N_EXPERTS
    aff = jax.nn.softmax(jnp.einsum('bnd,de->bne', h.astype(jnp.float32), router_w.astype(jnp.float32)), axis=-1)
    gate, idx = lax.top_k(jnp.swapaxes(aff, 1, 2), cap)
    xs = jax.vmap(lambda hb, ib: hb[ib])(h, idx)
    hid = jax.nn.silu(jnp.einsum('becd,edf->becf', xs, w1)) * jnp.einsum('becd,edf->becf', xs, w3)
    out = jnp.einsum('becf,efd->becd', hid, w2) * gate[..., None].astype(h.dtype)
    y = jax.vmap(lambda ib, ob: jnp.zeros((n, d), ob.dtype).at[ib.reshape(-1)].add(ob.reshape(-1, d)))(idx, out)
    return y.astype(h.dtype)


def _layer(x, mod, lp, rows, h0):
    shift1, scale1, gate1, shift2, scale2, gate2 = jnp.split(mod.astype(x.dtype), N_MOD, axis=-1)
    hn = _rmsnorm(x, lp['norm1']) * (1.0 + scale1) + shift1
    mix, h_last = _token_mixer(hn, lp, rows, h0)
    x = x + gate1 * mix
    hn = _rmsnorm(x, lp['norm2']) * (1.0 + scale2) + shift2
    x = x + gate2 * _expert_choice_moe(hn, lp['router_w'], lp['exp_w1'], lp['exp_w3'], lp['exp_w2'])
    return x, h_last


def setup_inputs(seed: int = 0) -> dict:
    key = jax.random.key(seed)
    ks = jax.random.split(key, 32)
    f32 = jnp.float32
    D, P, R, H, Dh, E, F = D_MODEL, POOL_WIDTH, LRU_WIDTH, LRU_HEADS, LRU_HEAD_DIM, N_EXPERTS, EXPERT_FF

    def nrm(k, shape, s):
        return jax.random.normal(k, shape, f32) * s

    a0 = jax.random.uniform(ks[19], (DEPTH, 2, R), f32, 0.9, 0.999)
    return {
        'x_prompt': nrm(ks[0], (BATCH, SEQ, D), 1.0),
        'x_sample': nrm(ks[1], (DEC_BATCH, DEC_SEQ, D), 1.0),
        'state_lru': nrm(ks[2], (DEC_BATCH, DEPTH, 2, R), 0.5),
        'c': nrm(ks[3], (DEC_BATCH, D), 1.0),
        'c_ctx': nrm(ks[4], (D,), 1.0),
        'norm1_g': 1.0 + nrm(ks[5], (DEPTH, D), 0.02),
        'norm2_g': 1.0 + nrm(ks[6], (DEPTH, D), 0.02),
        'final_g': 1.0 + nrm(ks[7], (D,), 0.02),
        'w_mod': nrm(ks[8], (DEPTH, D, N_MOD * D), 0.5 * D ** -0.5),
        'b_mod': nrm(ks[9], (DEPTH, N_MOD * D), 0.02),
        'w_in': nrm(ks[10], (DEPTH, D, IN_WIDTH), D ** -0.5),
        'pool_w': nrm(ks[11], (DEPTH, POOL_GROUPS, POOL_GROUP_WIDTH, POOL_GROUP_WIDTH), POOL_GROUP_WIDTH ** -0.5),
        'pool_scale': 1.0 + nrm(ks[12], (DEPTH, P), 0.02),
        'conv_w': nrm(ks[13], (DEPTH, CONV_WIDTH, R), CONV_WIDTH ** -0.5),
        'conv_b': nrm(ks[14], (DEPTH, R), 0.02),
        'lru_wr': nrm(ks[15], (DEPTH, 2, H, Dh, Dh), Dh ** -0.5),
        'lru_br': nrm(ks[16], (DEPTH, 2, R), 0.02),
        'lru_wi': nrm(ks[17], (DEPTH, 2, H, Dh, Dh), Dh ** -0.5),
        'lru_bi': nrm(ks[18], (DEPTH, 2, R), 0.02),
        'lru_lambda': jnp.log(a0) - jnp.log1p(-a0),
        'w_br_pool': nrm(ks[20], (DEPTH, P, D), P ** -0.5),
        'w_br_lru': nrm(ks[21], (DEPTH, R, D), R ** -0.5),
        'w_out': nrm(ks[22], (DEPTH, D, D), D ** -0.5),
        'router_w': nrm(ks[23], (DEPTH, D, E), D ** -0.5),
        'exp_w1': nrm(ks[24], (DEPTH, E, D, F), D ** -0.5),
        'exp_w3': nrm(ks[25], (DEPTH, E, D, F), D ** -0.5),
        'exp_w2': nrm(ks[26], (DEPTH, E, F, D), F ** -0.5),
    }


def reference(x_prompt, x_sample, state_lru, c, c_ctx, norm1_g, norm2_g, final_g, w_mod, b_mod, w_in,
              pool_w, pool_scale, conv_w, conv_b, lru_wr, lru_br, lru_wi, lru_bi, lru_lambda,
              w_br_pool, w_br_lru, w_out, router_w, exp_w1, exp_w3, exp_w2):
    rows = x_sample.shape[1] // GRID_W
    silu_ctx = jax.nn.silu(c_ctx)
    silu_c = jax.nn.silu(c)
    xp = x_prompt
    xs = x_sample
    ctx_states = []
    for l in range(DEPTH):
        lp = {
            'norm1': norm1_g[l], 'norm2': norm2_g[l], 'w_in': w_in[l],
            'pool_w': pool_w[l], 'pool_scale': pool_scale[l],
            'conv_w': conv_w[l], 'conv_b': conv_b[l],
            'lru_wr': lru_wr[l], 'lru_br': lru_br[l], 'lru_wi': lru_wi[l], 'lru_bi': lru_bi[l],
            'lru_lambda': lru_lambda[l],
            'w_br_pool': w_br_pool[l], 'w_br_lru': w_br_lru[l], 'w_out': w_out[l],
            'router_w': router_w[l], 'exp_w1': exp_w1[l], 'exp_w3': exp_w3[l], 'exp_w2': exp_w2[l],
        }
        mod_ctx = silu_ctx @ w_mod[l] + b_mod[l]
        h0_ctx = jnp.zeros((2, xp.shape[0], LRU_WIDTH), jnp.float32)
        xp, h_last_ctx = _layer(xp, mod_ctx, lp, None, h0_ctx)
        ctx_states.append(jnp.swapaxes(h_last_ctx, 0, 1))
        mod_lat = (silu_c @ w_mod[l] + b_mod[l])[:, None, :]
        h0_lat = jnp.swapaxes(state_lru[:, l], 0, 1).astype(jnp.float32)
        xs, _ = _layer(xs, mod_lat, lp, rows, h0_lat)
    y_prompt = _rmsnorm(xp, final_g)
    y_sample = _rmsnorm(xs, final_g)
    new_state_lru = jnp.stack(ctx_states, axis=1).astype(x_prompt.dtype)
    return (y_prompt, y_sample, new_state_lru)
```

```python
from contextlib import ExitStack
import numpy as np
import concourse.bass as bass
import concourse.mybir as mybir
from concourse.bass_utils import run_bass_kernel_spmd

F32 = mybir.dt.float32
BF16 = mybir.dt.bfloat16
U32 = mybir.dt.uint32
I32 = mybir.dt.int32
AF = mybir.ActivationFunctionType
ALU = mybir.AluOpType
AX = mybir.AxisListType

COMPUTE = ("pe", "act", "dve", "pool")
NDMASEM = {"sp": 24, "actq": 4, "poolq": 24}


class Prog:
    def __init__(self, nc):
        self.nc = nc
        self.sems = {}
        self.cnt = {k: 0 for k in COMPUTE}
        self.dma_i = {k: 0 for k in NDMASEM}
        self.waited = {}
        self.last_w = {}
        self.readers = {}
        self.nops = 0
        self.inject = None
        self.inject_every = 2
        self._inj_n = 0
        self._in_inject = False

    def alloc(self, stack):
        nc = self.nc
        for k in COMPUTE:
            self.sems[k] = stack.enter_context(nc.semaphore("s_" + k))
        for q, n in NDMASEM.items():
            for i in range(n):
                self.sems[(q, i)] = stack.enter_context(nc.semaphore("d_%s_%d" % (q, i)))

    def _need(self, stream, ev, waits):
        if ev is None:
            return
        key, val = ev
        if key == stream and val > self.cnt[key]:
            return
        if self.waited.get((stream, key), 0) >= val:
            return
        if val > waits.get(key, 0):
            waits[key] = val

    def op(self, eng, fn, reads=(), writes=(), signal=True, dmaq=None):
        stream = eng
        waits = {}
        for r in reads:
            self._need(stream, self.last_w.get(r), waits)
        for w in writes:
            self._need(stream, self.last_w.get(w), waits)
            for ev in self.readers.get(w, ()):
                self._need(stream, ev, waits)
        if dmaq is not None:
            n = NDMASEM[dmaq]
            i = self.dma_i[dmaq]
            self.dma_i[dmaq] = i + 1
            key = (dmaq, i % n)
            val = 16 * (i // n + 1)
            if i >= n:
                self._need(stream, (key, val - 16), waits)
            ev = (key, val)
            inc = (key, 16)
        else:
            if signal:
                self.cnt[eng] += 1
                ev = (eng, self.cnt[eng])
                inc = (eng, 1)
            else:
                ev = (eng, self.cnt[eng] + 1)
                inc = None
        for key, val in waits.items():
            self.waited[(stream, key)] = max(self.waited.get((stream, key), 0), val)
        self._emit(stream, fn, list(waits.items()), inc)
        for r in reads:
            self.readers.setdefault(r, []).append(ev)
        for w in writes:
            self.last_w[w] = ev
            self.readers[w] = []
        self.nops += 1
        if eng == "dve" and self.inject is not None and not self._in_inject:
            self._inj_n += 1
            if self._inj_n % self.inject_every == 0:
                self._in_inject = True
                try:
                    next(self.inject)
                except StopIteration:
                    self.inject = None
                self._in_inject = False
        return ev

    def pe(self, fn, reads=(), writes=(), signal=True):
        return self.op("pe", fn, reads, writes, signal)

    def act(self, fn, reads=(), writes=()):
        return self.op("act", fn, reads, writes)

    def dve(self, fn, reads=(), writes=()):
        return self.op("dve", fn, reads, writes)

    def pool(self, fn, reads=(), writes=()):
        return self.op("pool", fn, reads, writes)

    def dma(self, out, in_, reads=(), writes=(), q="sp", **kw):
        stream = {"sp": "sp", "actq": "act", "poolq": "pool"}[q]
        return self.op(stream, lambda e: e.dma_start(out=out, in_=in_, **kw), reads, writes, dmaq=q)

    def _emit(self, stream, fn, waits, inc):
        nc = self.nc
        e = {"pe": nc.tensor, "act": nc.scalar, "dve": nc.vector, "pool": nc.gpsimd, "sp": nc.sync}[stream]
        for key, val in waits:
            e.wait_ge(self.sems[key], val)
        if fn is None:
            return
        ins = fn(e)
        if inc is not None:
            ins.then_inc(self.sems[inc[0]], inc[1])

    def barrier(self):
        evs = [(k, self.cnt[k]) for k in COMPUTE if self.cnt[k] > 0]
        for q, n in NDMASEM.items():
            i = self.dma_i[q]
            for j in range(max(0, i - n), i):
                evs.append(((q, j % n), 16 * (j // n + 1)))
        for stream in ("pe", "act", "dve", "pool", "sp"):
            waits = {}
            for ev in evs:
                self._need(stream, ev, waits)
            for key, val in waits.items():
                self.waited[(stream, key)] = max(self.waited.get((stream, key), 0), val)
            self._emit(stream, None, list(waits.items()), None)


class Rot:
    def __init__(self, items):
        self.items = list(items)
        self.i = 0

    def next(self):
        it = self.items[self.i % len(self.items)]
        self.i += 1
        return it


D = 1024
NTOK = 5120
NTILE = 10
TS = 512
VW = 5140
EPS = 1e-6
NEXP = 16
FF = 2048
DEPTH = 2


def build_nc(dbg=False, nlayers=DEPTH, stop_after=None):
    nc = bass.Bass("TRN2", target_bir_lowering=False)

    def din(name, shape, dt=F32):
        return nc.dram_tensor(name, list(shape), dt, kind="ExternalInput").ap()

    def dint(name, shape, dt=F32):
        return nc.dram_tensor(name, list(shape), dt, kind=("ExternalOutput" if dbg else "Internal")).ap()

    x_in = din("x_in", [NTOK, D])
    cvec = din("cvec", [2, D])
    h0s = din("h0s", [2, 2, D])
    norm1_g = din("norm1_g", [2, D])
    norm2_g = din("norm2_g", [2, D])
    final_g = din("final_g", [D])
    w_mod = din("w_mod", [2, D, 6 * D])
    b_mod = din("b_mod", [2, 6 * D])
    w_in = din("w_in", [2, D, 3584])
    pool_w = din("pool_w", [2, 4, 128, 128])
    pool_scale = din("pool_scale", [2, 512])
    conv_w = din("conv_w", [2, 4, D])
    conv_b = din("conv_b", [2, D])
    lru_wr = din("lru_wr", [2, 2, 16, 64, 64])
    lru_br = din("lru_br", [2, 2, D])
    lru_wi = din("lru_wi", [2, 2, 16, 64, 64])
    lru_bi = din("lru_bi", [2, 2, D])
    lru_lambda = din("lru_lambda", [2, 2, D])
    w_br_pool = din("w_br_pool", [2, 512, D])
    w_br_lru = din("w_br_lru", [2, D, D])
    w_out = din("w_out", [2, D, D])
    router_w = din("router_w", [2, D, NEXP])
    _need_exp = stop_after is None or tuple(stop_after)[0] == "moe" or tuple(stop_after)[1] > 0
    exp_w1 = din("exp_w1", [2, NEXP, D, FF]) if _need_exp else None
    exp_w3 = din("exp_w3", [2, NEXP, D, FF]) if _need_exp else None
    exp_w2 = din("exp_w2", [2, NEXP, FF, D]) if _need_exp else None

    y_out = nc.dram_tensor("y", [NTOK, D], F32, kind="ExternalOutput").ap()
    ns_out = nc.dram_tensor("ns", [4, 2, 2, D], F32, kind="ExternalOutput").ap()

    X = dint("Xs", [NTOK, D])
    HN2 = dint("HN2s", [NTOK, D], BF16)
    V = dint("Vs", [D, VW])
    VC = dint("VCs", [D, NTOK])
    HF = dint("HFs", [D, NTOK])
    MP = dint("MPs", [D, NTOK])
    G = dint("Gs", [D, NTOK], BF16)
    MOD = dint("MODs", [2, 2, 6 * D])
    SIDX = dint("SIDXs", [16, 512], I32)
    SGATE = dint("SGATEs", [16, 512])
    PIDX = dint("PIDXs", [64, 32], I32)
    PGATE = dint("PGATEs", [64, 32])
    OFFS = dint("OFFSs", [64])

    with ExitStack() as gst:
        P = Prog(nc)
        P.alloc(gst)
        PS = [gst.enter_context(nc.psum_tensor("PS%d" % k, [128, 1024], F32)) for k in range(4)]

        def bank(j):
            return PS[j // 2][:, (j % 2) * 512:(j % 2) * 512 + 512]

        _tn = [0]

        def T(st, name, shape, dt=F32):
            _tn[0] += 1
            return st.enter_context(nc.sbuf_tensor("%s_%d" % (name, _tn[0]), list(shape), dt))

        def fmview(ap2d):
            return ap2d.rearrange("(c p) w -> p c w", p=128)

        class _Stop(Exception):
            pass

        def phase_end(name, lyr):
            return stop_after is not None and tuple(stop_after) == (name, lyr)

        ident = T(gst, "ident", [128, 128])
        identb = T(gst, "identb", [128, 128], BF16)
        iot = T(gst, "iot", [128, 128], I32)
        epsT = T(gst, "epsT", [128, 1])
        zt = T(gst, "zt", [128, 8, 2])
        rowsb = [T(gst, "rows%d" % i, [128, 128]) for i in range(2)]
        rows_rot = Rot([0, 1])
        fm = [T(gst, "fm%d" % l, [128, 192]) for l in range(2)]
        h0fm = T(gst, "h0fm", [128, 32])
        nsfm = T(gst, "nsfm", [128, 128])
        hcar = T(gst, "hcar", [128, 8])

        P.pool(lambda e: e.iota(iot[:], pattern=[[1, 128]], base=0, channel_multiplier=-1), writes=["iot"])
        P.dve(lambda e: e.tensor_scalar(out=ident[:], in0=iot[:], scalar1=0.0, scalar2=None, op0=ALU.is_equal),
              reads=["iot"], writes=["ident"])
        P.dve(lambda e: e.tensor_copy(out=identb[:], in_=ident[:]), reads=["ident"], writes=["identb"])
        P.dve(lambda e: e.memset(epsT[:], EPS), writes=["epsT"])
        P.dve(lambda e: e.memset(zt[:], 0.0), writes=["zt"])
        P.dve(lambda e: e.memset(nsfm[:], 0.0), writes=["nsfm"])

        pads = []
        for q in range(4):
            pads += [q * 260, q * 260 + 258]
        pads += [1040, 5138]
        for a in pads:
            P.dma(fmview(V[:, a:a + 2]), zt[:], reads=["zt"], writes=[("Vpad", a)])

        ps_rot = Rot(range(8))

        def load_fm(dst, col0, vec_aps, rows_per=8):
            nv = len(vec_aps)
            nr = nv * rows_per
            ri = rows_rot.next()
            rb = rowsb[ri]
            for v, ap in enumerate(vec_aps):
                P.dma(rb[v * rows_per:(v + 1) * rows_per, :], ap.rearrange("(c p) -> c p", p=128),
                      writes=[("rows", ri)])
            j = ps_rot.next()
            pb = bank(j)
            P.pe(lambda e: e.transpose(out=pb[:, 0:nr], in_=rb[0:nr, :], identity=ident[0:nr, 0:nr]),
                 reads=[("rows", ri), "ident"], writes=[("ps", j)])
            P.dve(lambda e: e.tensor_copy(out=dst[:, col0:col0 + nr], in_=pb[:, 0:nr]),
                  reads=[("ps", j)], writes=[("fmc", id(dst))])

        def FM(l, blk, c):
            return fm[l][:, blk * 8 + c: blk * 8 + c + 1]

        def run_setup():
            if phase_end("setup", 0):
                return True
            with ExitStack() as st:
                cfm = T(st, "cfm", [128, 16])
                csil = T(st, "csil", [128, 16], BF16)
                bm = T(st, "bm", [2, 6 * D])
                modrow = T(st, "modrow", [2, 6 * D])
                wmb = [T(st, "wm%d" % i, [128, 8, 512], BF16) for i in range(3)]
                wm_rot = Rot(range(3))
                load_fm(cfm, 0, [cvec[0, :], cvec[1, :]])
                P.act(lambda e: e.activation(out=csil[:], in_=cfm[:], func=AF.Silu),
                      reads=[("fmc", id(cfm))], writes=["csil"])
                for l in range(DEPTH):
                    for r in range(2):
                        P.dma(bm[r:r + 1, :], b_mod[l:l + 1, :], writes=["bm"])
                    for j in range(12):
                        wi = wm_rot.next()
                        wm = wmb[wi]
                        P.dma(wm[:], fmview(w_mod[l][:, j * 512:(j + 1) * 512]), writes=[("wm", wi)], q="poolq")
                        pj = ps_rot.next()
                        pb = bank(pj)
                        for c in range(8):
                            P.pe(lambda e, c=c, pb=pb, wm=wm: e.matmul(pb[0:2, :], lhsT=csil[:, c:16:8], rhs=wm[:, c, :],
                                                                      start=(c == 0), stop=(c == 7)),
                                 reads=["csil", ("wm", wi)], writes=[("ps", pj)], signal=(c == 7))
                        P.dve(lambda e, j=j, pb=pb: e.tensor_tensor(out=modrow[0:2, j * 512:(j + 1) * 512], in0=pb[0:2, :],
                                                                     in1=bm[0:2, j * 512:(j + 1) * 512], op=ALU.add),
                              reads=[("ps", pj), "bm"], writes=["modrow"])
                    P.dma(MOD[l], modrow[:], reads=["modrow"], writes=[("MOD", l)])
                P.barrier()
            if phase_end("mod", 0):
                return True


            with ExitStack() as st:
                tmpf = T(st, "tmpf", [128, 16])
                for l in range(DEPTH):
                    vecs = [norm1_g[l, :], MOD[l, 0, D:2 * D], MOD[l, 0, 0:D], MOD[l, 1, D:2 * D], MOD[l, 1, 0:D],
                            conv_w[l, 0, :], conv_w[l, 1, :], conv_w[l, 2, :], conv_w[l, 3, :], conv_b[l, :],
                            lru_br[l, 0, :], lru_bi[l, 0, :], lru_lambda[l, 0, :],
                            lru_br[l, 1, :], lru_bi[l, 1, :], lru_lambda[l, 1, :]]
                    load_fm(fm[l], 0, vecs)
                    load_fm(fm[l], 128, [pool_scale[l, :]], rows_per=4)
                    fr = ("fmc", id(fm[l]))
                    f = fm[l]
                    for vi, (sc, a1) in enumerate([(1, 17), (3, 18)]):
                        P.dve(lambda e, sc=sc, a1=a1, f=f: e.scalar_tensor_tensor(
                            out=f[:, a1 * 8:a1 * 8 + 8], in0=f[:, sc * 8:sc * 8 + 8], scalar=1.0, in1=f[:, 0:8],
                            op0=ALU.add, op1=ALU.mult), reads=[fr], writes=[fr])
                    for d, lam in enumerate([12, 15]):
                        P.act(lambda e, lam=lam, f=f, d=d: e.activation(out=tmpf[:, d * 8:d * 8 + 8], in_=f[:, lam * 8:lam * 8 + 8],
                                                                        func=AF.Exp, scale=-1.0), reads=[fr], writes=["tmpf"])
                        P.act(lambda e, d=d: e.activation(out=tmpf[:, d * 8:d * 8 + 8], in_=tmpf[:, d * 8:d * 8 + 8],
                                                          func=AF.Ln, bias=1.0), reads=["tmpf"], writes=["tmpf"])
                        P.dve(lambda e, d=d, f=f: e.tensor_scalar(out=f[:, (19 + d) * 8:(19 + d) * 8 + 8], in0=tmpf[:, d * 8:d * 8 + 8],
                                                                  scalar1=-8.0, scalar2=None, op0=ALU.mult),
                              reads=["tmpf", fr], writes=[fr])
                        P.dve(lambda e, d=d, f=f: e.tensor_scalar(out=f[:, (21 + d) * 8:(21 + d) * 8 + 8], in0=tmpf[:, d * 8:d * 8 + 8],
                                                                  scalar1=-16.0, scalar2=None, op0=ALU.mult),
                              reads=["tmpf", fr], writes=[fr])
                load_fm(h0fm, 0, [h0s[0, 0, :], h0s[0, 1, :], h0s[1, 0, :], h0s[1, 1, :]])
                P.barrier()

        def run_layers():
          for _ in range(1):
            for l in range(nlayers):
                Xsrc = x_in if l == 0 else X
                f = fm[l]
                fr = ("fmc", id(f))

                with ExitStack() as st:
                    winb = T(st, "winb", [128, 8, 3584], BF16)
                    pwb = T(st, "pwb", [128, 4, 128], BF16)
                    wbpb = T(st, "wbpb", [128, 4, D], BF16)
                    for j in range(7):
                        P.dma(winb[:, :, j * 512:(j + 1) * 512], fmview(w_in[l][:, j * 512:(j + 1) * 512]),
                              writes=["winb"], q="poolq")
                    P.dma(pwb[:], pool_w[l].rearrange("g c d -> c g d"), writes=["pwb"], q="poolq")
                    P.dma(wbpb[:], w_br_pool[l].rearrange("(c p) d -> p c d", p=128), writes=["wbpb"], q="poolq")
                    xtb = [T(st, "xt%d" % i, [128, D]) for i in range(4)]
                    xnb = [T(st, "xn%d" % i, [128, D]) for i in range(2)]
                    junk = T(st, "junk", [128, D], BF16)
                    ssb = [T(st, "ss%d" % i, [128, 1]) for i in range(4)]
                    rsb = [T(st, "rs%d" % i, [128, 1]) for i in range(4)]
                    hnT = [T(st, "hnT%d" % i, [128, 8, TS], BF16) for i in range(2)]
                    ppad = [T(st, "ppad%d" % g, [128, 640]) for g in range(4)]
                    sA = [T(st, "sA%d" % g, [128, 640]) for g in range(4)]
                    sB = [T(st, "sB%d" % g, [128, 640]) for g in range(4)]
                    icnt = [T(st, "icnt%d" % k, [128, 4, TS]) for k in range(2)]
                    pooled = T(st, "pooled", [128, 4, TS], BF16)
                    ptmp = T(st, "ptmp", [128, TS])
                    ypool = T(st, "ypool", [128, 4, TS], BF16)
                    vst = [T(st, "vst%d" % i, [128, TS]) for i in range(3)]
                    gst_ = [T(st, "gst%d" % i, [128, TS], BF16) for i in range(3)]
                    gpb = [T(st, "gp%d" % i, [128, TS]) for i in range(2)]
                    mpst = [T(st, "mpst%d" % i, [128, TS]) for i in range(3)]
                    xt_rot, xn_rot, v_rot, g_rot, gp_rot, mp_rot = Rot(range(4)), Rot(range(2)), Rot(range(3)), Rot(range(3)), Rot(range(2)), Rot(range(3))
                    for g in range(4):
                        P.dve(lambda e, g=g: e.memset(ppad[g][:], 0.0), writes=[("ppad", g)])
                        P.pool(lambda e, g=g: e.memset(sA[g][:], 0.0), writes=[("sA", g)])
                        P.pool(lambda e, g=g: e.memset(sB[g][:], 0.0), writes=[("sB", g)])
                    for k, (nrow, L) in enumerate([(8, 64), (2, 256)]):
                        for g, w in enumerate((2, 4, 8, 16)):
                            iv = icnt[k][:, g, :].rearrange("p (r t) -> p r t", t=L)
                            P.dve(lambda e, iv=iv, w=w: e.memset(iv, 1.0 / w), writes=[("icnt", k)])
                            h = w // 2
                            for t in range(h):
                                c_lo = float(min(t + h, L) - max(t - h, 0))
                                tt = L - 1 - t
                                c_hi = float(min(tt + h, L) - max(tt - h, 0))
                                P.dve(lambda e, iv=iv, t=t, c_lo=c_lo: e.memset(iv[:, :, t:t + 1], 1.0 / c_lo), writes=[("icnt", k)])
                                if c_hi != float(w):
                                    P.dve(lambda e, iv=iv, tt=tt, c_hi=c_hi: e.memset(iv[:, :, tt:tt + 1], 1.0 / c_hi), writes=[("icnt", k)])

                    def geom(i):
                        return (2, 256, 272, 1) if i < 2 else (8, 64, 80, 0)

                    def p1_front(i):
                        vi = 0 if i < 2 else 1
                        A1 = 17 + vi
                        B1 = 2 if vi == 0 else 4
                        hb = i % 2
                        for pair in range(2):
                            pbs = []
                            for _ in range(4):
                                pbs.append(ps_rot.next())
                            for sj in range(2):
                                s = pair * 2 + sj
                                xi = xt_rot.next()
                                xt = xtb[xi]
                                r0 = i * TS + s * 128
                                P.dma(xt[:], Xsrc[r0:r0 + 128, :], writes=[("xt", xi)])
                                P.act(lambda e, xt=xt, xi=xi: e.activation(out=junk[:], in_=xt[:], func=AF.Square, accum_out=ssb[xi][:, 0:1]),
                                      reads=[("xt", xi)], writes=["junk", ("ss", xi)])
                                P.act(lambda e, xi=xi: e.activation(out=ssb[xi][:, 0:1], in_=ssb[xi][:, 0:1], func=AF.Sqrt,
                                                                    scale=1.0 / D, bias=epsT[:, 0:1]),
                                      reads=[("ss", xi), "epsT"], writes=[("ss", xi)])
                                P.dve(lambda e, xi=xi: e.reciprocal(out=rsb[xi][:, 0:1], in_=ssb[xi][:, 0:1]),
                                      reads=[("ss", xi)], writes=[("rs", xi)])
                                ni = xn_rot.next()
                                xn = xnb[ni]
                                P.dve(lambda e, xn=xn, xt=xt, xi=xi: e.tensor_scalar(out=xn[:], in0=xt[:], scalar1=rsb[xi][:, 0:1], scalar2=None, op0=ALU.mult),
                                      reads=[("xt", xi), ("rs", xi)], writes=[("xn", ni)])
                                for c in range(8):
                                    pj = pbs[c // 2]
                                    off = (c % 2) * 256 + sj * 128
                                    P.pe(lambda e, xn=xn, c=c, pj=pj, off=off: e.transpose(out=bank(pj)[:, off:off + 128], in_=xn[:, c * 128:(c + 1) * 128], identity=ident[:]),
                                         reads=[("xn", ni), "ident"], writes=[("ps", pj)], signal=(c == 7))
                            for c in range(8):
                                pj = pbs[c // 2]
                                off = (c % 2) * 256
                                P.act(lambda e, c=c, pj=pj, off=off, pair=pair, hb=hb: e.activation(
                                    out=hnT[hb][:, c, pair * 256:(pair + 1) * 256], in_=bank(pj)[:, off:off + 256], func=AF.Identity,
                                    scale=FM(l, A1, c), bias=FM(l, B1, c)),
                                    reads=[("ps", pj), fr], writes=[("hnT", hb)])

                    def win_mm(i, oc):
                        hb = i % 2
                        pj = ps_rot.next()
                        for kc in range(8):
                            P.pe(lambda e, kc=kc, pj=pj, oc=oc, hb=hb: e.matmul(bank(pj), lhsT=winb[:, kc, oc * 128:(oc + 1) * 128], rhs=hnT[hb][:, kc, :],
                                                                                 start=(kc == 0), stop=(kc == 7)),
                                 reads=["winb", ("hnT", hb)], writes=[("ps", pj)], signal=(kc == 7))
                        return pj

                    def p1_back(i):
                        nrow, L, stride, ik = geom(i)
                        W = nrow * stride
                        if i == 2:
                            for g in range(4):
                                P.dve(lambda e, g=g: e.memset(ppad[g][:], 0.0), writes=[("ppad", g)])
                        for g in range(4):
                            pj = win_mm(i, g)
                            dst = ppad[g][:, 0:W].rearrange("p (r t) -> p r t", t=stride)[:, :, 8:8 + L]
                            P.act(lambda e, dst=dst, pj=pj, L=L: e.activation(out=dst, in_=bank(pj).rearrange("p (r t) -> p r t", t=L), func=AF.Copy),
                                  reads=[("ps", pj)], writes=[("ppad", g)])
                        for g, w in enumerate((2, 4, 8, 16)):
                            src, sres = ppad[g], ("ppad", g)
                            bufs = [(sA[g], ("sA", g)), (sB[g], ("sB", g))]
                            dstb, dres = bufs[0]
                            P.pool(lambda e, dstb=dstb, src=src, W=W: e.tensor_tensor(out=dstb[:, 1:W], in0=src[:, 0:W - 1], in1=src[:, 1:W], op=ALU.add),
                                   reads=[sres], writes=[dres])
                            cur, cres = dstb, dres
                            sh = 1
                            nb = 1
                            ww = 2
                            while ww < w:
                                dstb, dres = bufs[nb % 2]
                                P.pool(lambda e, dstb=dstb, cur=cur, sh=sh, W=W: e.tensor_tensor(out=dstb[:, sh:W - sh], in0=cur[:, 0:W - 2 * sh], in1=cur[:, 2 * sh:W], op=ALU.add),
                                       reads=[cres], writes=[dres])
                                cur, cres = dstb, dres
                                nb += 1
                                sh *= 2
                                ww *= 2
                            sw = cur[:, 0:W].rearrange("p (r t) -> p r t", t=stride)[:, :, 8:8 + L]
                            pin = src[:, 0:W].rearrange("p (r t) -> p r t", t=stride)[:, :, 8:8 + L]
                            iv = icnt[ik][:, g, :].rearrange("p (r t) -> p r t", t=L)
                            pt = ptmp[:].rearrange("p (r t) -> p r t", t=L)
                            P.dve(lambda e, pt=pt, sw=sw, iv=iv: e.tensor_tensor(out=pt, in0=sw, in1=iv, op=ALU.mult),
                                  reads=[cres, ("icnt", ik)], writes=["ptmp"])
                            po = pooled[:, g, :].rearrange("p (r t) -> p r t", t=L)
                            P.dve(lambda e, po=po, pt=pt, pin=pin: e.tensor_tensor(out=po, in0=pt, in1=pin, op=ALU.subtract),
                                  reads=["ptmp", sres], writes=[("pooled", g)])
                        for c in range(8):
                            pj = win_mm(i, 4 + c)
                            vi_ = v_rot.next()
                            P.act(lambda e, vi_=vi_, pj=pj: e.activation(out=vst[vi_][:], in_=bank(pj), func=AF.Copy),
                                  reads=[("ps", pj)], writes=[("vst", vi_)])
                            rows = V[c * 128:(c + 1) * 128, :]
                            if i < 2:
                                a = (2 * i) * 260 + 2
                                dst = rows[:, a:a + 520].rearrange("p (s w) -> p s w", w=260)[:, :, 0:256]
                                srcv = vst[vi_][:].rearrange("p (s w) -> p s w", w=256)
                            else:
                                a = 1042 + (i - 2) * TS
                                dst = rows[:, a:a + TS]
                                srcv = vst[vi_][:]
                            P.dma(dst, srcv, reads=[("vst", vi_)], writes=[("V", i, c)])
                        for c in range(8):
                            pj = win_mm(i, 20 + c)
                            gi = g_rot.next()
                            P.act(lambda e, gi=gi, pj=pj: e.activation(out=gst_[gi][:], in_=bank(pj), func=AF.Sigmoid),
                                  reads=[("ps", pj)], writes=[("gst", gi)])
                            P.dma(G[c * 128:(c + 1) * 128, i * TS:(i + 1) * TS], gst_[gi][:], reads=[("gst", gi)], writes=[("G", i, c)])
                        for g in range(4):
                            pj = ps_rot.next()
                            P.pe(lambda e, g=g, pj=pj: e.matmul(bank(pj), lhsT=pwb[:, g, :], rhs=pooled[:, g, :], start=True, stop=True),
                                 reads=["pwb", ("pooled", g)], writes=[("ps", pj)])
                            P.act(lambda e, g=g, pj=pj: e.activation(out=ypool[:, g, :], in_=bank(pj), func=AF.Identity, scale=f[:, 128 + g:129 + g]),
                                  reads=[("ps", pj), fr], writes=[("ypool", g)])
                        for k in range(8):
                            pj = win_mm(i, 12 + k)
                            gi = gp_rot.next()
                            P.act(lambda e, gi=gi, pj=pj: e.activation(out=gpb[gi][:], in_=bank(pj), func=AF.Sigmoid),
                                  reads=[("ps", pj)], writes=[("gp", gi)])
                            pj2 = ps_rot.next()
                            for kc in range(4):
                                P.pe(lambda e, kc=kc, k=k, pj2=pj2: e.matmul(bank(pj2), lhsT=wbpb[:, kc, k * 128:(k + 1) * 128], rhs=ypool[:, kc, :],
                                                                              start=(kc == 0), stop=(kc == 3)),
                                     reads=["wbpb"] + [("ypool", g) for g in range(4)], writes=[("ps", pj2)], signal=(kc == 3))
                            mi = mp_rot.next()
                            P.dve(lambda e, mi=mi, pj2=pj2, gi=gi: e.tensor_tensor(out=mpst[mi][:], in0=bank(pj2), in1=gpb[gi][:], op=ALU.mult),
                                  reads=[("ps", pj2), ("gp", gi)], writes=[("mpst", mi)])
                            P.dma(MP[k * 128:(k + 1) * 128, i * TS:(i + 1) * TS], mpst[mi][:], reads=[("mpst", mi)], writes=[("MP", i, k)])

                    p1_front(0)
                    for i in range(NTILE):
                        if i + 1 < NTILE:
                            p1_front(i + 1)
                        p1_back(i)
                    P.barrier()
                if phase_end("p1", l):
                    return True

                lst = ExitStack()
                gw = T(lst, "gw", [128, 4, 8, 128], BF16)
                P.pool(lambda e: e.memset(gw[:], 0.0), writes=["gw"])
                for gi_, wsrc in enumerate((lru_wr, lru_wi)):
                    for d in range(2):
                        for hl in range(2):
                            src = wsrc[l, d].rearrange("(c two) dd ee -> two dd c ee", two=2)[hl]
                            P.dma(gw[hl * 64:(hl + 1) * 64, gi_ * 2 + d, :, hl * 64:(hl + 1) * 64], src, writes=["gw"], q="poolq")

                NG = 4

                def mk_sets(st):
                    return [(T(st, "vcb%d" % j, [128, TS], BF16), T(st, "rs_%d" % j, [128, TS]), T(st, "is_%d" % j, [128, TS]),
                             T(st, "sq_%d" % j, [128, TS])) for j in range(NG)]

                def lru_group(sets, d, cs, vc_aps, res_ins, nseq, inits_list, out_hs, res_outs, reverse):
                    n = len(cs)
                    br = 10 + 3 * d
                    for j in range(n):
                        P.act(lambda e, j=j: e.activation(out=sets[j][0][:], in_=vc_aps[j], func=AF.Copy),
                              reads=[res_ins[j]], writes=[("vcb", j)])
                    prs, pis = [], []
                    for j in range(n):
                        pr = ps_rot.next()
                        prs.append(pr)
                        P.pe(lambda e, j=j, pr=pr: e.matmul(bank(pr), lhsT=gw[:, 0 * 2 + d, cs[j], :], rhs=sets[j][0][:], start=True, stop=True),
                             reads=["gw", ("vcb", j)], writes=[("ps", pr)])
                    for j in range(n):
                        pi = ps_rot.next()
                        pis.append(pi)
                        P.pe(lambda e, j=j, pi=pi: e.matmul(bank(pi), lhsT=gw[:, 1 * 2 + d, cs[j], :], rhs=sets[j][0][:], start=True, stop=True),
                             reads=["gw", ("vcb", j)], writes=[("ps", pi)])
                    for j in range(n):
                        P.act(lambda e, j=j: e.activation(out=sets[j][1][:], in_=bank(prs[j]), func=AF.Sigmoid, bias=FM(l, br, cs[j])),
                              reads=[("ps", prs[j]), fr], writes=[("rs_", j)])
                    for j in range(n):
                        P.act(lambda e, j=j: e.activation(out=sets[j][2][:], in_=bank(pis[j]), func=AF.Sigmoid, bias=FM(l, br + 1, cs[j])),
                              reads=[("ps", pis[j]), fr], writes=[("is_", j)])
                    for j in range(n):
                        P.act(lambda e, j=j: e.activation(out=sets[j][3][:], in_=sets[j][1][:], func=AF.Exp, scale=FM(l, 21 + d, cs[j])),
                              reads=[("rs_", j), fr], writes=[("sq_", j)])
                    for j in range(n):
                        P.act(lambda e, j=j: e.activation(out=sets[j][1][:], in_=sets[j][1][:], func=AF.Exp, scale=FM(l, 19 + d, cs[j])),
                              reads=[("rs_", j), fr], writes=[("rs_", j)])
                    for j in range(n):
                        P.act(lambda e, j=j: e.activation(out=sets[j][3][:], in_=sets[j][3][:], func=AF.Sqrt, scale=-1.0, bias=1.0),
                              reads=[("sq_", j)], writes=[("sq_", j)])
                    for j in range(n):
                        P.pool(lambda e, j=j: e.tensor_tensor(out=sets[j][2][:], in0=sets[j][3][:], in1=sets[j][2][:], op=ALU.mult),
                               reads=[("sq_", j), ("is_", j)], writes=[("is_", j)])
                    for j in range(n):
                        P.dve(lambda e, j=j: e.tensor_tensor(out=sets[j][2][:], in0=sets[j][2][:], in1=vc_aps[j], op=ALU.mult),
                              reads=[("is_", j), res_ins[j]], writes=[("is_", j)])
                    L = TS // nseq
                    for j in range(n):
                        a_, u_, out_h = sets[j][1], sets[j][2], out_hs[j]
                        for s in range(nseq):
                            if reverse:
                                sl = slice((s + 1) * L - 1, (s * L - 1 if s > 0 else None), -1)
                            else:
                                sl = slice(s * L, (s + 1) * L)
                            init = inits_list[j][s]
                            rd = [("rs_", j), ("is_", j)] + ([] if isinstance(init, float) else ["hcar"])
                            P.dve(lambda e, out_h=out_h, a_=a_, u_=u_, sl=sl, init=init: e.tensor_tensor_scan(
                                out=out_h[:, sl], data0=a_[:, sl], data1=u_[:, sl], initial=init, op0=ALU.mult, op1=ALU.add),
                                reads=rd, writes=[res_outs[j]])

                with ExitStack() as st:
                    vtb = [T(st, "vt%d" % i, [128, 8, 520]) for i in range(2)]
                    vcs = [T(st, "vcs%d" % i, [128, TS]) for i in range(8)]
                    hfs = [T(st, "hfs%d" % i, [128, TS]) for i in range(8)]
                    sets = mk_sets(st)
                    vc_rot, hf_rot = Rot(range(8)), Rot(range(8))

                    def p2_load(i):
                        vt = vtb[i % 2]
                        if i < 2:
                            a = (2 * i) * 260
                            P.dma(vt[:], fmview(V[:, a:a + 520]), reads=[("V", i, c) for c in range(8)], writes=[("vt", i % 2)])
                        else:
                            a = 1040 + (i - 2) * TS
                            rd = [("V", ii, c) for ii in (i - 1, i, i + 1) if 2 <= ii < NTILE for c in range(8)]
                            P.dma(vt[:, :, 0:515], fmview(V[:, a:a + 515]), reads=rd, writes=[("vt", i % 2)])

                    p2_load(0)
                    for i in range(NTILE):
                        if i + 1 < NTILE:
                            p2_load(i + 1)
                        vt = vtb[i % 2]
                        nseq = 2 if i < 2 else 1
                        for g0 in range(0, 8, NG):
                            cs = list(range(g0, g0 + NG))
                            cis = [vc_rot.next() for _ in cs]
                            his = [hf_rot.next() for _ in cs]
                            vos, tapss = [], []
                            for j, c in enumerate(cs):
                                vc = vcs[cis[j]]
                                if i < 2:
                                    vin = vt[:, c, :].rearrange("p (s w) -> p s w", w=260)
                                    vos.append(vc[:].rearrange("p (s w) -> p s w", w=256))
                                    tapss.append([vin[:, :, k:k + 256] for k in range(4)])
                                else:
                                    vos.append(vc[:])
                                    tapss.append([vt[:, c, k:k + TS] for k in range(4)])
                            for j, c in enumerate(cs):
                                P.dve(lambda e, j=j, c=c: e.tensor_scalar(out=vos[j], in0=tapss[j][0], scalar1=FM(l, 5, c), scalar2=FM(l, 9, c),
                                                                         op0=ALU.mult, op1=ALU.add),
                                      reads=[("vt", i % 2), fr], writes=[("vcs", cis[j])])
                            for k in range(1, 4):
                                for j, c in enumerate(cs):
                                    P.dve(lambda e, j=j, c=c, k=k: e.scalar_tensor_tensor(out=vos[j], in0=tapss[j][k], scalar=FM(l, 5 + k, c), in1=vos[j],
                                                                                         op0=ALU.mult, op1=ALU.add),
                                          reads=[("vt", i % 2), fr, ("vcs", cis[j])], writes=[("vcs", cis[j])])
                            for j, c in enumerate(cs):
                                P.dma(VC[c * 128:(c + 1) * 128, i * TS:(i + 1) * TS], vcs[cis[j]][:], reads=[("vcs", cis[j])], writes=[("VC", i, c)])
                            inits_list = []
                            for c in cs:
                                if i < 2:
                                    inits_list.append([0.0, 0.0])
                                elif i == 2:
                                    inits_list.append([h0fm[:, (l * 2 + 0) * 8 + c:(l * 2 + 0) * 8 + c + 1]])
                                else:
                                    inits_list.append([hcar[:, c:c + 1]])
                            lru_group(sets, 0, cs, [vcs[ci][:] for ci in cis], [("vcs", ci) for ci in cis], nseq, inits_list,
                                      [hfs[hi] for hi in his], [("hfs", hi) for hi in his], False)
                            for j, c in enumerate(cs):
                                hf = hfs[his[j]]
                                if i >= 2:
                                    P.act(lambda e, hf=hf, c=c: e.activation(out=hcar[:, c:c + 1], in_=hf[:, TS - 1:TS], func=AF.Copy),
                                          reads=[("hfs", his[j])], writes=["hcar"])
                                else:
                                    for s in range(2):
                                        req = 2 * i + s
                                        col = ((l * 2 + 0) * 4 + req) * 8 + c
                                        P.act(lambda e, hf=hf, s=s, col=col: e.activation(out=nsfm[:, col:col + 1], in_=hf[:, (s + 1) * 256 - 1:(s + 1) * 256], func=AF.Copy),
                                              reads=[("hfs", his[j])], writes=["nsfm"])
                                P.dma(HF[c * 128:(c + 1) * 128, i * TS:(i + 1) * TS], hf[:], reads=[("hfs", his[j])], writes=[("HF", i, c)])
                    P.barrier()
                if phase_end("p2", l):
                    lst.close()
                    return True

                affS = T(lst, "affS", [16, 4096])
                affP = T(lst, "affP", [64, 256])
                P.dve(lambda e: e.memset(affP[:], 0.0), writes=["affP"])
                vS = T(lst, "vS", [16, 512])
                iS = T(lst, "iS", [16, 512], U32)

                def topk_gen(src, vals, idxs, niter, tag, srcres):
                    P.last_w[tag + "src"] = P.last_w.get(srcres)
                    for it in range(niter):
                        sl = slice(it * 8, it * 8 + 8)
                        P.dve(lambda e, sl=sl: e.max(out=vals[:, sl], in_=src), reads=[tag + "src"], writes=[tag + "v"])
                        P.dve(lambda e, sl=sl: e.max_index(out=idxs[:, sl], in_max=vals[:, sl], in_values=src), reads=[tag + "src", tag + "v"], writes=[tag + "i"])
                        if it + 1 < niter:
                            P.dve(lambda e, sl=sl: e.match_replace(out=src, in_to_replace=vals[:, sl], in_values=src, imm_value=-1.0),
                                  reads=[tag + "src", tag + "v"], writes=[tag + "src"])
                        yield
                with ExitStack() as st:
                    wblb = T(st, "wblb", [128, 8, D], BF16)
                    woutb = T(st, "woutb", [128, 8, D], BF16)
                    rwt = T(st, "rwt", [128, 8, NEXP])
                    for j in range(2):
                        P.dma(wblb[:, :, j * 512:(j + 1) * 512], fmview(w_br_lru[l][:, j * 512:(j + 1) * 512]), writes=["wblb"], q="poolq")
                        P.dma(woutb[:, :, j * 512:(j + 1) * 512], fmview(w_out[l][:, j * 512:(j + 1) * 512]), writes=["woutb"], q="poolq")
                    P.dma(rwt[:], fmview(router_w[l]), writes=["rwt"])
                    g1bc = T(st, "g1bc", [128, D])
                    a2bc = T(st, "a2bc", [128, D])
                    b2bc = T(st, "b2bc", [128, D])
                    g2n = T(st, "g2n", [128, D])
                    vcl = [T(st, "vcl%d" % i, [128, TS]) for i in range(4)]
                    hfl = [T(st, "hfl%d" % i, [128, TS]) for i in range(4)]
                    gl_ = [T(st, "gl%d" % i, [128, TS], BF16) for i in range(3)]
                    mpl = [T(st, "mpl%d" % i, [128, TS]) for i in range(3)]
                    hbs = [T(st, "hbs%d" % i, [128, TS]) for i in range(4)]
                    sets = mk_sets(st)
                    ylru2 = [T(st, "ylru%d" % i, [128, 8, TS], BF16) for i in range(2)]
                    merged = T(st, "merged", [128, 8, TS], BF16)
                    mtmp = [T(st, "mtmp%d" % i, [128, TS]) for i in range(2)]
                    xlb = [T(st, "xl%d" % i, [128, D]) for i in range(2)]
                    hn2 = [T(st, "hn2_%d" % i, [128, D]) for i in range(2)]
                    hn2b = [T(st, "hn2b%d" % i, [128, D], BF16) for i in range(2)]
                    hn2T = [T(st, "hn2T%d" % i, [128, 8, 128]) for i in range(2)]
                    junk = T(st, "junk3", [128, D], BF16)
                    sm = [T(st, "sm%d" % i, [128, 8]) for i in range(2)]
                    ex = [T(st, "ex%d" % i, [128, NEXP]) for i in range(2)]
                    affpad = [T(st, "affpad%d" % i, [128, 128]) for i in range(2)]
                    afst = [T(st, "afst%d" % i, [NEXP, 128]) for i in range(2)]
                    for k in range(2):
                        P.dve(lambda e, k=k: e.memset(affpad[k][:], 0.0), writes=[("affpad", k)])
                    vcl_rot, hfl_rot, gl_rot, mpl_rot, hb_rot, mt_rot = Rot(range(4)), Rot(range(4)), Rot(range(3)), Rot(range(3)), Rot(range(4)), Rot(range(2))

                    def load_bc(vi):
                        P.dma(g1bc[:], MOD[l, vi, 2 * D:3 * D].partition_broadcast(128), writes=["g1bc"])
                        P.dma(a2bc[:], MOD[l, vi, 4 * D:5 * D].partition_broadcast(128), writes=["a2bc"])
                        P.dma(b2bc[:], MOD[l, vi, 3 * D:4 * D].partition_broadcast(128), writes=["b2bc"])
                        P.dma(g2n[:], norm2_g[l, :].partition_broadcast(128), writes=["g2n"])
                        P.dve(lambda e: e.scalar_tensor_tensor(out=a2bc[:], in0=a2bc[:], scalar=1.0, in1=g2n[:], op0=ALU.add, op1=ALU.mult),
                              reads=["a2bc", "g2n"], writes=["a2bc"])

                    def p3_lru(i):
                        nseq = 2 if i < 2 else 1
                        cols = slice(i * TS, (i + 1) * TS)
                        ylru = ylru2[i % 2]
                        for g0 in range(0, 8, NG):
                            cs = list(range(g0, g0 + NG))
                            vis = [vcl_rot.next() for _ in cs]
                            his = [hfl_rot.next() for _ in cs]
                            bis = [hb_rot.next() for _ in cs]
                            for j, c in enumerate(cs):
                                rows = slice(c * 128, (c + 1) * 128)
                                P.dma(vcl[vis[j]][:], VC[rows, cols], reads=[("VC", i, c)], writes=[("vcl", vis[j])])
                                P.dma(hfl[his[j]][:], HF[rows, cols], reads=[("HF", i, c)], writes=[("hfl", his[j])])
                            inits_list = []
                            for c in cs:
                                if i < 2:
                                    inits_list.append([0.0, 0.0])
                                elif i == NTILE - 1:
                                    inits_list.append([h0fm[:, (l * 2 + 1) * 8 + c:(l * 2 + 1) * 8 + c + 1]])
                                else:
                                    inits_list.append([hcar[:, c:c + 1]])
                            lru_group(sets, 1, cs, [vcl[v][:] for v in vis], [("vcl", v) for v in vis], nseq, inits_list,
                                      [hbs[b] for b in bis], [("hbs", b) for b in bis], True)
                            for j, c in enumerate(cs):
                                hb = hbs[bis[j]]
                                if i >= 2:
                                    P.act(lambda e, hb=hb, c=c: e.activation(out=hcar[:, c:c + 1], in_=hb[:, 0:1], func=AF.Copy),
                                          reads=[("hbs", bis[j])], writes=["hcar"])
                                else:
                                    for s in range(2):
                                        req = 2 * i + s
                                        col = ((l * 2 + 1) * 4 + req) * 8 + c
                                        P.act(lambda e, hb=hb, s=s, col=col: e.activation(out=nsfm[:, col:col + 1], in_=hb[:, s * 256:s * 256 + 1], func=AF.Copy),
                                              reads=[("hbs", bis[j])], writes=["nsfm"])
                            for j, c in enumerate(cs):
                                P.pool(lambda e, j=j, c=c: e.tensor_tensor(out=ylru[:, c, :], in0=hbs[bis[j]][:], in1=hfl[his[j]][:], op=ALU.add),
                                       reads=[("hbs", bis[j]), ("hfl", his[j])], writes=[("ylru", i % 2, c)])

                    def p3_merge(i):
                        cols = slice(i * TS, (i + 1) * TS)
                        ylru = ylru2[i % 2]
                        for oc in range(8):
                            gi_, mi_ = gl_rot.next(), mpl_rot.next()
                            rows = slice(oc * 128, (oc + 1) * 128)
                            P.dma(gl_[gi_][:], G[rows, cols], reads=[("G", i, oc)], writes=[("gl", gi_)])
                            P.dma(mpl[mi_][:], MP[rows, cols], reads=[("MP", i, oc)], writes=[("mpl", mi_)])
                            pj = ps_rot.next()
                            for kc in range(8):
                                P.pe(lambda e, kc=kc, oc=oc, pj=pj: e.matmul(bank(pj), lhsT=wblb[:, kc, oc * 128:(oc + 1) * 128], rhs=ylru[:, kc, :],
                                                                              start=(kc == 0), stop=(kc == 7)),
                                     reads=["wblb"] + [("ylru", i % 2, k) for k in range(8)], writes=[("ps", pj)], signal=(kc == 7))
                            ti = mt_rot.next()
                            P.dve(lambda e, pj=pj, gi_=gi_, ti=ti: e.tensor_tensor(out=mtmp[ti][:], in0=bank(pj), in1=gl_[gi_][:], op=ALU.mult),
                                  reads=[("ps", pj), ("gl", gi_)], writes=[("mtmp", ti)])
                            P.pool(lambda e, oc=oc, mi_=mi_, ti=ti: e.tensor_tensor(out=merged[:, oc, :], in0=mtmp[ti][:], in1=mpl[mi_][:], op=ALU.add),
                                   reads=[("mtmp", ti), ("mpl", mi_)], writes=[("merged", oc)])

                    def p3_wout(i, ss):
                        K_ = list(range(len(ss)))
                        r0s = [i * TS + s * 128 for s in ss]
                        for k in K_:
                            P.dma(xlb[k][:], Xsrc[r0s[k]:r0s[k] + 128, :], writes=[("xl", k)])
                        for k in K_:
                            s = ss[k]
                            for half in range(2):
                                pj = ps_rot.next()
                                hs = slice(half * 512, (half + 1) * 512)
                                for kc in range(8):
                                    P.pe(lambda e, kc=kc, pj=pj, s=s, hs=hs: e.matmul(bank(pj), lhsT=merged[:, kc, s * 128:(s + 1) * 128], rhs=woutb[:, kc, hs],
                                                                                       start=(kc == 0), stop=(kc == 7)),
                                         reads=["woutb"] + [("merged", q) for q in range(8)], writes=[("ps", pj)], signal=(kc == 7))
                                P.dve(lambda e, pj=pj, hs=hs, k=k: e.tensor_tensor(out=hn2[k][:, hs], in0=bank(pj), in1=g1bc[:, hs], op=ALU.mult),
                                      reads=[("ps", pj), "g1bc"], writes=[("hn2", k)])
                                P.pool(lambda e, hs=hs, k=k: e.tensor_tensor(out=xlb[k][:, hs], in0=xlb[k][:, hs], in1=hn2[k][:, hs], op=ALU.add),
                                       reads=[("hn2", k), ("xl", k)], writes=[("xl", k)])
                        for k in K_:
                            P.dma(X[r0s[k]:r0s[k] + 128, :], xlb[k][:], reads=[("xl", k)], writes=[("X", i, ss[k])])
                        for k in K_:
                            P.act(lambda e, k=k: e.activation(out=junk[:], in_=xlb[k][:], func=AF.Square, accum_out=sm[k][:, 0:1]),
                                  reads=[("xl", k)], writes=[("sm0", k)])
                        for k in K_:
                            P.act(lambda e, k=k: e.activation(out=sm[k][:, 0:1], in_=sm[k][:, 0:1], func=AF.Sqrt, scale=1.0 / D, bias=epsT[:, 0:1]),
                                  reads=[("sm0", k), "epsT"], writes=[("sm0", k)])
                        for k in K_:
                            P.dve(lambda e, k=k: e.reciprocal(out=sm[k][:, 1:2], in_=sm[k][:, 0:1]), reads=[("sm0", k)], writes=[("sm1", k)])
                        for k in K_:
                            P.dve(lambda e, k=k: e.scalar_tensor_tensor(out=hn2[k][:], in0=xlb[k][:], scalar=sm[k][:, 1:2], in1=a2bc[:], op0=ALU.mult, op1=ALU.mult),
                                  reads=[("xl", k), ("sm1", k), "a2bc"], writes=[("hn2", k)])
                        for k in K_:
                            P.pool(lambda e, k=k: e.tensor_tensor(out=hn2[k][:], in0=hn2[k][:], in1=b2bc[:], op=ALU.add),
                                   reads=[("hn2", k), "b2bc"], writes=[("hn2", k)])
                        for k in K_:
                            P.act(lambda e, k=k: e.activation(out=hn2b[k][:], in_=hn2[k][:], func=AF.Copy),
                                  reads=[("hn2", k)], writes=[("hn2b", k)])
                            P.dma(HN2[r0s[k]:r0s[k] + 128, :], hn2b[k][:], reads=[("hn2b", k)], writes=[("HN2", i, ss[k])])
                        for k in K_:
                            for h2 in range(2):
                                pj = ps_rot.next()
                                for cc in range(4):
                                    c = h2 * 4 + cc
                                    P.pe(lambda e, c=c, cc=cc, pj=pj, k=k: e.transpose(out=bank(pj)[:, cc * 128:(cc + 1) * 128], in_=hn2[k][:, c * 128:(c + 1) * 128], identity=ident[:]),
                                         reads=[("hn2", k), "ident"], writes=[("ps", pj)], signal=(cc == 3))
                                P.act(lambda e, pj=pj, h2=h2, k=k: e.activation(out=hn2T[k][:, h2 * 4:(h2 + 1) * 4, :], in_=bank(pj).rearrange("p (c t) -> p c t", t=128), func=AF.Copy),
                                      reads=[("ps", pj)], writes=[("hn2T", k, h2)])
                        pjs = []
                        for k in K_:
                            pj = ps_rot.next()
                            pjs.append(pj)
                            for kc in range(8):
                                P.pe(lambda e, kc=kc, pj=pj, k=k: e.matmul(bank(pj)[:, 0:NEXP], lhsT=hn2T[k][:, kc, :], rhs=rwt[:, kc, :], start=(kc == 0), stop=(kc == 7)),
                                     reads=["rwt", ("hn2T", k, 0), ("hn2T", k, 1)], writes=[("ps", pj)], signal=(kc == 7))
                        for k in K_:
                            P.dve(lambda e, k=k: e.reduce_max(out=sm[k][:, 2:3], in_=bank(pjs[k])[:, 0:NEXP], axis=AX.X), reads=[("ps", pjs[k])], writes=[("sm2", k)])
                        for k in K_:
                            P.dve(lambda e, k=k: e.tensor_scalar(out=sm[k][:, 3:4], in0=sm[k][:, 2:3], scalar1=-1.0, scalar2=None, op0=ALU.mult), reads=[("sm2", k)], writes=[("sm3", k)])
                        for k in K_:
                            P.act(lambda e, k=k: e.activation(out=ex[k][:], in_=bank(pjs[k])[:, 0:NEXP], func=AF.Exp, bias=sm[k][:, 3:4], accum_out=sm[k][:, 4:5]),
                                  reads=[("ps", pjs[k]), ("sm3", k)], writes=[("ex", k), ("sm4", k)])
                        for k in K_:
                            P.dve(lambda e, k=k: e.reciprocal(out=sm[k][:, 5:6], in_=sm[k][:, 4:5]), reads=[("sm4", k)], writes=[("sm5", k)])
                        for k in K_:
                            P.dve(lambda e, k=k: e.tensor_scalar(out=affpad[k][:, 0:NEXP], in0=ex[k][:], scalar1=sm[k][:, 5:6], scalar2=None, op0=ALU.mult),
                                  reads=[("ex", k), ("sm5", k)], writes=[("affpad", k)])
                        pts = []
                        for k in K_:
                            pj = ps_rot.next()
                            pts.append(pj)
                            P.pe(lambda e, pj=pj, k=k: e.transpose(out=bank(pj)[:, 0:128], in_=affpad[k][:], identity=ident[:]),
                                 reads=[("affpad", k), "ident"], writes=[("ps", pj)])
                        for k in K_:
                            s = ss[k]
                            pj = pts[k]
                            if i < 2:
                                req = 2 * i + s // 2
                                tc0 = (s % 2) * 128
                                P.act(lambda e, pj=pj, k=k: e.activation(out=afst[k][0:NEXP, :], in_=bank(pj)[0:NEXP, 0:128], func=AF.Copy),
                                      reads=[("ps", pj)], writes=[("afst", k)])
                                P.dma(affP[NEXP * req:NEXP * req + NEXP, tc0:tc0 + 128], afst[k][0:NEXP, :], reads=[("afst", k)], writes=["affP"])
                            else:
                                tc0 = (i - 2) * TS + s * 128
                                P.act(lambda e, pj=pj, tc0=tc0: e.activation(out=affS[0:NEXP, tc0:tc0 + 128], in_=bank(pj)[0:NEXP, 0:128], func=AF.Copy),
                                      reads=[("ps", pj)], writes=["affS"])

                    load_bc(1)
                    order = list(range(NTILE - 1, -1, -1))
                    p3_lru(order[0])
                    for n_, i in enumerate(order):
                        if n_ + 1 < len(order):
                            p3_lru(order[n_ + 1])
                        if i == 1:
                            load_bc(0)
                        p3_merge(i)
                        p3_wout(i, [0, 1])
                        p3_wout(i, [2, 3])
                        if i == 2:
                            sgen = topk_gen(affS[:], vS, iS, 64, "S", "affS")
                            P.inject = sgen
                    P.inject = None
                    P.barrier()
                if phase_end("p3", l):
                    lst.close()
                    return True

                idxS = T(lst, "idxS", [128, NEXP, 4], I32)
                gatS = T(lst, "gatS", [128, NEXP, 4])
                idxP = T(lst, "idxP", [128, NEXP], I32)
                gatP = T(lst, "gatP", [128, NEXP])
                with ExitStack() as st:
                    fS = T(st, "fS", [16, 512])
                    iS2 = T(st, "iS2", [16, 512], I32)
                    wkP = T(st, "wkP", [64, 256])
                    vP = T(st, "vP", [64, 32])
                    iP = T(st, "iP", [64, 32], U32)
                    fP = T(st, "fP", [64, 32])
                    iP2 = T(st, "iP2", [64, 32], I32)
                    offi = T(st, "offi", [4, NEXP], I32)
                    offf = T(st, "offf", [4, NEXP])
                    offPf = T(st, "offPf", [64, 1])
                    P.pool(lambda e: e.iota(offi[:], pattern=[[0, NEXP]], base=0, channel_multiplier=256), writes=["offi"])
                    P.dve(lambda e: e.tensor_copy(out=offf[:], in_=offi[:]), reads=["offi"], writes=["offf"])
                    P.dma(OFFS.rearrange("(r e) -> r e", e=NEXP), offf[:], reads=["offf"], writes=["OFFS"])
                    P.dma(offPf[:], OFFS.rearrange("(p o) -> p o", o=1), reads=["OFFS"], writes=["offPf"])

                    def topk(src, wk, vals, idxs, niter, tag):
                        cur = src
                        cres = tag + "src"
                        for it in range(niter):
                            sl = slice(it * 8, it * 8 + 8)
                            P.dve(lambda e, cur=cur, sl=sl: e.max(out=vals[:, sl], in_=cur), reads=[cres], writes=[tag + "v"])
                            P.dve(lambda e, cur=cur, sl=sl: e.max_index(out=idxs[:, sl], in_max=vals[:, sl], in_values=cur), reads=[cres, tag + "v"], writes=[tag + "i"])
                            if it + 1 < niter:
                                P.dve(lambda e, cur=cur, sl=sl: e.match_replace(out=wk, in_to_replace=vals[:, sl], in_values=cur, imm_value=-1.0),
                                      reads=[cres, tag + "v"], writes=[tag + "wk"])
                                cur = wk
                                cres = tag + "wk"

                    P.last_w["Psrc"] = P.last_w.get("affP")
                    topk(affP[:], wkP[:], vP, iP, 4, "P")
                    P.dve(lambda e: e.tensor_copy(out=fP[:], in_=iP[:]), reads=["Pi"], writes=["fP"])
                    P.dve(lambda e: e.tensor_scalar(out=fP[:], in0=fP[:], scalar1=offPf[:, 0:1], scalar2=None, op0=ALU.add), reads=["fP", "offPf"], writes=["fP"])
                    P.dve(lambda e: e.tensor_copy(out=iP2[:], in_=fP[:]), reads=["fP"], writes=["iP2"])
                    P.dma(PIDX, iP2[:], reads=["iP2"], writes=["PIDX"])
                    P.dma(PGATE, vP[:], reads=["Pv"], writes=["PGATE"])
                    with nc.allow_non_contiguous_dma(reason="tiny index relayout"):
                        for r in range(4):
                            P.dma(idxP[32 * r:32 * r + 32, :], PIDX[NEXP * r:NEXP * r + NEXP, :].rearrange("e k -> k e"), reads=["PIDX"], writes=["idxP"])
                            P.dma(gatP[32 * r:32 * r + 32, :], PGATE[NEXP * r:NEXP * r + NEXP, :].rearrange("e k -> k e"), reads=["PGATE"], writes=["gatP"])
                    for _ in sgen:
                        pass
                    P.dve(lambda e: e.tensor_copy(out=fS[:], in_=iS[:]), reads=["Si"], writes=["fS"])
                    P.dve(lambda e: e.tensor_scalar(out=fS[:], in0=fS[:], scalar1=1024.0, scalar2=None, op0=ALU.add), reads=["fS"], writes=["fS"])
                    P.dve(lambda e: e.tensor_copy(out=iS2[:], in_=fS[:]), reads=["fS"], writes=["iS2"])
                    P.dma(SIDX, iS2[:], reads=["iS2"], writes=["SIDX"])
                    P.dma(SGATE, vS[:], reads=["Sv"], writes=["SGATE"])
                    with nc.allow_non_contiguous_dma(reason="tiny index relayout"):
                        P.dma(idxS[:], SIDX.rearrange("e (g p) -> p e g", p=128), reads=["SIDX"], writes=["idxS"])
                        P.dma(gatS[:], SGATE.rearrange("e (g p) -> p e g", p=128), reads=["SGATE"], writes=["gatS"])
                    P.barrier()
                if phase_end("route", l):
                    lst.close()
                    return True

                with ExitStack() as st:
                    g2bc = [T(st, "g2bc%d" % v, [128, D]) for v in range(2)]
                    for v in range(2):
                        P.dma(g2bc[v][:], MOD[l, v, 5 * D:6 * D].partition_broadcast(128), writes=[("g2bc", v)])
                    xg = [T(st, "xg%d" % g, [128, D], BF16) for g in range(5)]
                    xsT = T(st, "xsT", [128, 8, 640], BF16)
                    w1q = [T(st, "w1q%d" % i, [128, 8, 512], BF16) for i in range(2)]
                    w3q = [T(st, "w3q%d" % i, [128, 8, 512], BF16) for i in range(2)]
                    w2h = [T(st, "w2h%d" % i, [128, 16, 512], BF16) for i in range(2)]
                    hid = T(st, "hid", [128, 16, 640], BF16)
                    s1 = [T(st, "s1_%d" % i, [128, 640]) for i in range(2)]
                    osb = [T(st, "osb%d" % g, [128, D]) for g in range(5)]
                    wq_rot, w2_rot, s1_rot, hp_rot = Rot(range(2)), Rot(range(2)), Rot(range(2)), Rot(range(2))

                    def moe_load1(e_, q):
                        wi = wq_rot.next()
                        P.dma(w1q[wi][:], fmview(exp_w1[l, e_][:, q * 512:(q + 1) * 512]), writes=[("w1q", wi)], q="poolq")
                        P.dma(w3q[wi][:], fmview(exp_w3[l, e_][:, q * 512:(q + 1) * 512]), writes=[("w3q", wi)], q="poolq")
                        return wi

                    def moe_load2(e_, half):
                        wi = w2_rot.next()
                        P.dma(w2h[wi][:], exp_w2[l, e_][:, half * 512:(half + 1) * 512].rearrange("(c p) d -> p c d", p=128), writes=[("w2h", wi)], q="poolq")
                        return wi

                    def moe_gather(e_):
                        for g in range(5):
                            ia = idxS[:, e_, g:g + 1] if g < 4 else idxP[:, e_:e_ + 1]
                            ir = "idxS" if g < 4 else "idxP"
                            P.op("pool", lambda e, g=g, ia=ia: e.indirect_dma_start(out=xg[g][:], out_offset=None, in_=HN2,
                                                                                     in_offset=bass.IndirectOffsetOnAxis(ap=ia, axis=0)),
                                 reads=[ir], writes=[("xg", g)], dmaq="poolq")

                    def moe_xpose(e_):
                        for g in range(5):
                            pj = ps_rot.next()
                            pbb = bank(pj).bitcast(BF16)
                            for c in range(8):
                                P.pe(lambda e, g=g, c=c, pbb=pbb: e.transpose(out=pbb[:, c * 128:(c + 1) * 128], in_=xg[g][:, c * 128:(c + 1) * 128], identity=identb[:]),
                                     reads=[("xg", g), "identb"], writes=[("ps", pj)], signal=(c == 7))
                            P.act(lambda e, g=g, pbb=pbb: e.activation(out=xsT[:, :, g * 128:(g + 1) * 128], in_=pbb.rearrange("p (c t) -> p c t", t=128), func=AF.Copy),
                                  reads=[("ps", pj)], writes=["xsT"])

                    moe_gather(0)
                    w1i = moe_load1(0, 0)
                    moe_xpose(0)
                    for e_ in range(NEXP):
                        w2is = [None, None]
                        for q in range(4):
                            nxt = moe_load1(e_, q + 1) if q < 3 else None
                            if q == 1:
                                w2is[0] = moe_load2(e_, 0)
                            if q == 2:
                                w2is[1] = moe_load2(e_, 1)
                                if e_ + 1 < NEXP:
                                    moe_gather(e_ + 1)
                            for fc in range(4):
                                fcg = q * 4 + fc
                                hk = hp_rot.next()
                                H1, H3 = PS[2 * hk], PS[2 * hk + 1]
                                for (Hh, wt, wr) in ((H1, w1q[w1i], ("w1q", w1i)), (H3, w3q[w1i], ("w3q", w1i))):
                                    pidx = 2 * hk if Hh is H1 else 2 * hk + 1
                                    for kc in range(8):
                                        P.pe(lambda e, Hh=Hh, wt=wt, kc=kc, fc=fc: e.matmul(Hh[:, 0:512], lhsT=wt[:, kc, fc * 128:(fc + 1) * 128], rhs=xsT[:, kc, 0:512],
                                                                                             start=(kc == 0), stop=(kc == 7)),
                                             reads=[wr, "xsT"], writes=[("ps", 2 * pidx)], signal=False)
                                        P.pe(lambda e, Hh=Hh, wt=wt, kc=kc, fc=fc: e.matmul(Hh[:, 512:640], lhsT=wt[:, kc, fc * 128:(fc + 1) * 128], rhs=xsT[:, kc, 512:640],
                                                                                             start=(kc == 0), stop=(kc == 7)),
                                             reads=[wr, "xsT"], writes=[("ps", 2 * pidx + 1)], signal=(kc == 7))
                                si = s1_rot.next()
                                r1 = [("ps", 4 * hk), ("ps", 4 * hk + 1)]
                                r3 = [("ps", 4 * hk + 2), ("ps", 4 * hk + 3)]
                                P.act(lambda e, H1=H1, si=si: e.activation(out=s1[si][:], in_=H1[:, 0:640], func=AF.Silu),
                                      reads=r1, writes=[("s1", si)])
                                P.dve(lambda e, H3=H3, si=si, fcg=fcg: e.tensor_tensor(out=hid[:, fcg, :], in0=H3[:, 0:640], in1=s1[si][:], op=ALU.mult),
                                      reads=r3 + [("s1", si)], writes=[("hid", fcg)])
                            w1i = nxt
                        if e_ + 1 < NEXP:
                            w1i = moe_load1(e_ + 1, 0)
                        for half in range(2):
                            w2i = w2is[half]
                            hs = slice(half * 512, (half + 1) * 512)
                            for g in range(5):
                                pj = ps_rot.next()
                                for fcg in range(16):
                                    P.pe(lambda e, pj=pj, fcg=fcg, g=g, w2i=w2i: e.matmul(bank(pj), lhsT=hid[:, fcg, g * 128:(g + 1) * 128], rhs=w2h[w2i][:, fcg, :],
                                                                                           start=(fcg == 0), stop=(fcg == 15)),
                                         reads=[("w2h", w2i)] + [("hid", k) for k in range(16)], writes=[("ps", pj)], signal=(fcg == 15))
                                ga = gatS[:, e_, g:g + 1] if g < 4 else gatP[:, e_:e_ + 1]
                                gr = "gatS" if g < 4 else "gatP"
                                v_ = 1 if g < 4 else 0
                                P.dve(lambda e, pj=pj, g=g, ga=ga, hs=hs, v_=v_: e.scalar_tensor_tensor(out=osb[g][:, hs], in0=bank(pj), scalar=ga, in1=g2bc[v_][:, hs],
                                                                                                        op0=ALU.mult, op1=ALU.mult),
                                      reads=[("ps", pj), gr, ("g2bc", v_)], writes=[("osb", g)])
                            if half == 0 and e_ + 1 < NEXP:
                                moe_xpose(e_ + 1)
                        for g in range(5):
                            ia = idxS[:, e_, g:g + 1] if g < 4 else idxP[:, e_:e_ + 1]
                            ir = "idxS" if g < 4 else "idxP"
                            P.op("pool", lambda e, g=g, ia=ia: e.indirect_dma_start(out=X, out_offset=bass.IndirectOffsetOnAxis(ap=ia, axis=0),
                                                                                     in_=osb[g][:], in_offset=None, compute_op=ALU.add),
                                 reads=[ir, ("osb", g)], writes=["Xall"], dmaq="poolq")
                    P.barrier()
                if phase_end("moe", l):
                    lst.close()
                    return True
                lst.close()

        def run_final():
            with ExitStack() as st:
                fgbc = T(st, "fgbc", [128, D])
                P.dma(fgbc[:], final_g.partition_broadcast(128), writes=["fgbc"])
                xf = [T(st, "xf%d" % i, [128, D]) for i in range(3)]
                yf = [T(st, "yf%d" % i, [128, D]) for i in range(3)]
                junk = T(st, "junkf", [128, D], BF16)
                sf = [T(st, "sf%d" % i, [128, 2]) for i in range(3)]
                for t in range(NTOK // 128):
                    bi = t % 3
                    P.dma(xf[bi][:], X[t * 128:(t + 1) * 128, :], writes=[("xf", bi)])
                    P.act(lambda e, bi=bi: e.activation(out=junk[:], in_=xf[bi][:], func=AF.Square, accum_out=sf[bi][:, 0:1]),
                          reads=[("xf", bi)], writes=["junkf", ("sf", bi)])
                    P.act(lambda e, bi=bi: e.activation(out=sf[bi][:, 0:1], in_=sf[bi][:, 0:1], func=AF.Sqrt, scale=1.0 / D, bias=epsT[:, 0:1]),
                          reads=[("sf", bi), "epsT"], writes=[("sf", bi)])
                    P.dve(lambda e, bi=bi: e.reciprocal(out=sf[bi][:, 1:2], in_=sf[bi][:, 0:1]), reads=[("sf", bi)], writes=[("sf1", bi)])
                    P.dve(lambda e, bi=bi: e.scalar_tensor_tensor(out=yf[bi][:], in0=xf[bi][:], scalar=sf[bi][:, 1:2], in1=fgbc[:], op0=ALU.mult, op1=ALU.mult),
                          reads=[("xf", bi), ("sf1", bi), "fgbc"], writes=[("yf", bi)])
                    P.dma(y_out[t * 128:(t + 1) * 128, :], yf[bi][:], reads=[("yf", bi)], writes=[("y", t)])
                pj = ps_rot.next()
                P.pe(lambda e: e.transpose(out=bank(pj)[:, 0:128], in_=nsfm[:], identity=ident[:]), reads=["nsfm", "ident"], writes=[("ps", pj)])
                nsr = T(st, "nsr", [128, 128])
                P.dve(lambda e: e.tensor_copy(out=nsr[:], in_=bank(pj)[:, 0:128]), reads=[("ps", pj)], writes=["nsr"])
                for l in range(2):
                    for d in range(2):
                        for r in range(4):
                            r0 = ((l * 2 + d) * 4 + r) * 8
                            P.dma(ns_out[r, l, d, :].rearrange("(c p) -> c p", p=128), nsr[r0:r0 + 8, :], reads=["nsr"], writes=[("ns", l, d, r)])
                P.barrier()
        if not run_setup():
            if not phase_end("fm", 0):
                run_layers()
        P.barrier()
        run_final()
    nc._prog_stats = (dict(P.cnt), dict(P.dma_i), P.nops)
    return nc


_NC_CACHE = {}


def kernel(x_prompt, x_sample, state_lru, c, c_ctx, norm1_g, norm2_g, final_g, w_mod, b_mod, w_in,
           pool_w, pool_scale, conv_w, conv_b, lru_wr, lru_br, lru_wi, lru_bi, lru_lambda,
           w_br_pool, w_br_lru, w_out, router_w, exp_w1, exp_w3, exp_w2):
    f = lambda a: np.ascontiguousarray(np.asarray(a, dtype=np.float32))
    if "nc" not in _NC_CACHE:
        _NC_CACHE["nc"] = build_nc()
    nc = _NC_CACHE["nc"]
    shared = dict(norm1_g=f(norm1_g), norm2_g=f(norm2_g), final_g=f(final_g), w_mod=f(w_mod), b_mod=f(b_mod),
                  w_in=f(w_in), pool_w=f(pool_w), pool_scale=f(pool_scale), conv_w=f(conv_w), conv_b=f(conv_b),
                  lru_wr=f(lru_wr), lru_br=f(lru_br), lru_wi=f(lru_wi), lru_bi=f(lru_bi), lru_lambda=f(lru_lambda),
                  w_br_pool=f(w_br_pool), w_br_lru=f(w_br_lru), w_out=f(w_out), router_w=f(router_w),
                  exp_w1=f(exp_w1), exp_w3=f(exp_w3), exp_w2=f(exp_w2))
    xp, xs, sl, cc, cx = f(x_prompt), f(x_sample), f(state_lru), f(c), f(c_ctx)
    in_maps = []
    for k in range(8):
        m = dict(shared)
        m["x_in"] = np.concatenate([xp[4 * k:4 * k + 4].reshape(1024, D), xs[k]], axis=0)
        m["cvec"] = np.stack([cx, cc[k]], axis=0)
        m["h0s"] = np.ascontiguousarray(sl[k])
        in_maps.append(m)
    res = run_bass_kernel_spmd(nc, in_maps, core_ids=list(range(8)))
    y_prompt = np.zeros((32, 256, D), np.float32)
    y_sample = np.zeros((8, 4096, D), np.float32)
    ns = np.zeros((32, 2, 2, D), np.float32)
    for k in range(8):
        r = res.results[k]
        y_prompt[4 * k:4 * k + 4] = r["y"][0:1024].reshape(4, 256, D)
        y_sample[k] = r["y"][1024:]
        ns[4 * k:4 * k + 4] = r["ns"]
    return (y_prompt, y_sample, ns)
```

```python
from contextlib import ExitStack
import numpy as np
import concourse.bass as bass
import concourse.mybir as mybir
from concourse.bass_utils import run_bass_kernel_spmd

F32 = mybir.dt.float32
BF16 = mybir.dt.bfloat16
U32 = mybir.dt.uint32
I32 = mybir.dt.int32
AF = mybir.ActivationFunctionType
ALU = mybir.AluOpType
AX = mybir.AxisListType

COMPUTE = ("pe", "act", "dve", "pool")
NDMASEM = {"sp": 24, "actq": 12, "poolq": 24}


class Prog:
    def __init__(self, nc):
        self.nc = nc
        self.sems = {}
        self.cnt = {k: 0 for k in COMPUTE}
        self.dma_i = {k: 0 for k in NDMASEM}
        self.waited = {}
        self.last_w = {}
        self.readers = {}
        self.nops = 0
        self.inject = None
        self.inject_every = 2
        self._inj_n = 0
        self._in_inject = False

    def alloc(self, stack):
        nc = self.nc
        for k in COMPUTE:
            self.sems[k] = stack.enter_context(nc.semaphore("s_" + k))
        for q, n in NDMASEM.items():
            for i in range(n):
                self.sems[(q, i)] = stack.enter_context(nc.semaphore("d_%s_%d" % (q, i)))

    def _need(self, stream, ev, waits):
        if ev is None:
            return
        key, val = ev
        if key == stream and val > self.cnt[key]:
            return
        if self.waited.get((stream, key), 0) >= val:
            return
        if val > waits.get(key, 0):
            waits[key] = val

    def op(self, eng, fn, reads=(), writes=(), signal=True, dmaq=None):
        stream = eng
        waits = {}
        for r in reads:
            self._need(stream, self.last_w.get(r), waits)
        for w in writes:
            self._need(stream, self.last_w.get(w), waits)
            for ev in self.readers.get(w, ()):
                self._need(stream, ev, waits)
        if dmaq is not None:
            n = NDMASEM[dmaq]
            i = self.dma_i[dmaq]
            self.dma_i[dmaq] = i + 1
            key = (dmaq, i % n)
            val = 16 * (i // n + 1)
            if i >= n:
                self._need(stream, (key, val - 16), waits)
            ev = (key, val)
            inc = (key, 16)
        else:
            if signal:
                self.cnt[eng] += 1
                ev = (eng, self.cnt[eng])
                inc = (eng, 1)
            else:
                ev = (eng, self.cnt[eng] + 1)
                inc = None
        for key, val in waits.items():
            self.waited[(stream, key)] = max(self.waited.get((stream, key), 0), val)
        self._emit(stream, fn, list(waits.items()), inc)
        for r in reads:
            self.readers.setdefault(r, []).append(ev)
        for w in writes:
            self.last_w[w] = ev
            self.readers[w] = []
        self.nops += 1
        if eng == "dve" and self.inject is not None and not self._in_inject:
            self._inj_n += 1
            if self._inj_n % self.inject_every == 0:
                self._in_inject = True
                try:
                    next(self.inject)
                except StopIteration:
                    self.inject = None
                self._in_inject = False
        return ev

    def pe(self, fn, reads=(), writes=(), signal=True):
        return self.op("pe", fn, reads, writes, signal)

    def act(self, fn, reads=(), writes=()):
        return self.op("act", fn, reads, writes)

    def dve(self, fn, reads=(), writes=()):
        return self.op("dve", fn, reads, writes)

    def pool(self, fn, reads=(), writes=()):
        return self.op("pool", fn, reads, writes)

    def dma(self, out, in_, reads=(), writes=(), q="sp", **kw):
        stream = {"sp": "sp", "actq": "act", "poolq": "pool"}[q]
        return self.op(stream, lambda e: e.dma_start(out=out, in_=in_, **kw), reads, writes, dmaq=q)

    def _emit(self, stream, fn, waits, inc):
        nc = self.nc
        e = {"pe": nc.tensor, "act": nc.scalar, "dve": nc.vector, "pool": nc.gpsimd, "sp": nc.sync}[stream]
        for key, val in waits:
            e.wait_ge(self.sems[key], val)
        if fn is None:
            return
        ins = fn(e)
        if inc is not None:
            ins.then_inc(self.sems[inc[0]], inc[1])

    def barrier(self):
        evs = [(k, self.cnt[k]) for k in COMPUTE if self.cnt[k] > 0]
        for q, n in NDMASEM.items():
            i = self.dma_i[q]
            for j in range(max(0, i - n), i):
                evs.append(((q, j % n), 16 * (j // n + 1)))
        for stream in ("pe", "act", "dve", "pool", "sp"):
            waits = {}
            for ev in evs:
                self._need(stream, ev, waits)
            for key, val in waits.items():
                self.waited[(stream, key)] = max(self.waited.get((stream, key), 0), val)
            self._emit(stream, None, list(waits.items()), None)


class Rot:
    def __init__(self, items):
        self.items = list(items)
        self.i = 0

    def next(self):
        it = self.items[self.i % len(self.items)]
        self.i += 1
        return it


D = 1024
NTOK = 5120
NTILE = 10
TS = 512
VW = 5140
EPS = 1e-6
NEXP = 16
FF = 2048
DEPTH = 2


def build_nc(dbg=False, nlayers=DEPTH, stop_after=None):
    nc = bass.Bass("TRN2", target_bir_lowering=False)

    def din(name, shape, dt=F32):
        return nc.dram_tensor(name, list(shape), dt, kind="ExternalInput").ap()

    def dint(name, shape, dt=F32):
        return nc.dram_tensor(name, list(shape), dt, kind=("ExternalOutput" if dbg else "Internal")).ap()

    x_in = din("x_in", [NTOK, D])
    cvec = din("cvec", [2, D])
    h0s = din("h0s", [2, 2, D])
    norm1_g = din("norm1_g", [2, D])
    norm2_g = din("norm2_g", [2, D])
    final_g = din("final_g", [D])
    w_mod = din("w_mod", [2, D, 6 * D])
    b_mod = din("b_mod", [2, 6 * D])
    w_in = din("w_in", [2, D, 3584])
    pool_w = din("pool_w", [2, 4, 128, 128])
    pool_scale = din("pool_scale", [2, 512])
    conv_w = din("conv_w", [2, 4, D])
    conv_b = din("conv_b", [2, D])
    lru_wr = din("lru_wr", [2, 2, 16, 64, 64])
    lru_br = din("lru_br", [2, 2, D])
    lru_wi = din("lru_wi", [2, 2, 16, 64, 64])
    lru_bi = din("lru_bi", [2, 2, D])
    lru_lambda = din("lru_lambda", [2, 2, D])
    w_br_pool = din("w_br_pool", [2, 512, D])
    w_br_lru = din("w_br_lru", [2, D, D])
    w_out = din("w_out", [2, D, D])
    router_w = din("router_w", [2, D, NEXP])
    _need_exp = stop_after is None or tuple(stop_after)[0] == "moe" or tuple(stop_after)[1] > 0
    exp_w1 = din("exp_w1", [2, NEXP, D, FF]) if _need_exp else None
    exp_w3 = din("exp_w3", [2, NEXP, D, FF]) if _need_exp else None
    exp_w2 = din("exp_w2", [2, NEXP, FF, D]) if _need_exp else None

    y_out = nc.dram_tensor("y", [NTOK, D], F32, kind="ExternalOutput").ap()
    ns_out = nc.dram_tensor("ns", [4, 2, 2, D], F32, kind="ExternalOutput").ap()

    X = dint("Xs", [NTOK, D])
    HN2 = dint("HN2s", [NTOK, D], BF16)
    V = dint("Vs", [D, VW])
    VC = dint("VCs", [D, NTOK])
    HF = dint("HFs", [D, NTOK])
    MP = dint("MPs", [D, NTOK])
    G = dint("Gs", [D, NTOK], BF16)
    MOD = dint("MODs", [2, 2, 6 * D])
    SIDX = dint("SIDXs", [16, 512], I32)
    SGATE = dint("SGATEs", [16, 512])
    PIDX = dint("PIDXs", [64, 32], I32)
    PGATE = dint("PGATEs", [64, 32])
    OFFS = dint("OFFSs", [64])

    with ExitStack() as gst:
        P = Prog(nc)
        P.alloc(gst)
        PS = [gst.enter_context(nc.psum_tensor("PS%d" % k, [128, 1024], F32)) for k in range(4)]

        def bank(j):
            return PS[j // 2][:, (j % 2) * 512:(j % 2) * 512 + 512]

        _tn = [0]

        def T(st, name, shape, dt=F32):
            _tn[0] += 1
            return st.enter_context(nc.sbuf_tensor("%s_%d" % (name, _tn[0]), list(shape), dt))

        def fmview(ap2d):
            return ap2d.rearrange("(c p) w -> p c w", p=128)

        class _Stop(Exception):
            pass

        def phase_end(name, lyr):
            return stop_after is not None and tuple(stop_after) == (name, lyr)

        ident = T(gst, "ident", [128, 128])
        identb = T(gst, "identb", [128, 128], BF16)
        iot = T(gst, "iot", [128, 128], I32)
        epsT = T(gst, "epsT", [128, 1])
        zt = T(gst, "zt", [128, 8, 2])
        rowsb = [T(gst, "rows%d" % i, [128, 128]) for i in range(2)]
        rows_rot = Rot([0, 1])
        fm = [T(gst, "fm%d" % l, [128, 192]) for l in range(2)]
        h0fm = T(gst, "h0fm", [128, 32])
        nsfm = T(gst, "nsfm", [128, 128])
        hcar = T(gst, "hcar", [128, 8])

        P.pool(lambda e: e.iota(iot[:], pattern=[[1, 128]], base=0, channel_multiplier=-1), writes=["iot"])
        P.dve(lambda e: e.tensor_scalar(out=ident[:], in0=iot[:], scalar1=0.0, scalar2=None, op0=ALU.is_equal),
              reads=["iot"], writes=["ident"])
        P.dve(lambda e: e.tensor_copy(out=identb[:], in_=ident[:]), reads=["ident"], writes=["identb"])
        P.dve(lambda e: e.memset(epsT[:], EPS), writes=["epsT"])
        P.dve(lambda e: e.memset(zt[:], 0.0), writes=["zt"])
        P.dve(lambda e: e.memset(nsfm[:], 0.0), writes=["nsfm"])

        pads = []
        for q in range(4):
            pads += [q * 260, q * 260 + 258]
        pads += [1040, 5138]
        for a in pads:
            P.dma(fmview(V[:, a:a + 2]), zt[:], reads=["zt"], writes=[("Vpad", a)])

        ps_rot = Rot(range(8))

        def load_fm(dst, col0, vec_aps, rows_per=8):
            nv = len(vec_aps)
            nr = nv * rows_per
            ri = rows_rot.next()
            rb = rowsb[ri]
            for v, ap in enumerate(vec_aps):
                P.dma(rb[v * rows_per:(v + 1) * rows_per, :], ap.rearrange("(c p) -> c p", p=128),
                      writes=[("rows", ri)])
            j = ps_rot.next()
            pb = bank(j)
            P.pe(lambda e: e.transpose(out=pb[:, 0:nr], in_=rb[0:nr, :], identity=ident[0:nr, 0:nr]),
                 reads=[("rows", ri), "ident"], writes=[("ps", j)])
            P.dve(lambda e: e.tensor_copy(out=dst[:, col0:col0 + nr], in_=pb[:, 0:nr]),
                  reads=[("ps", j)], writes=[("fmc", id(dst))])

        def FM(l, blk, c):
            return fm[l][:, blk * 8 + c: blk * 8 + c + 1]

        def run_setup():
            if phase_end("setup", 0):
                return True
            with ExitStack() as st:
                cfm = T(st, "cfm", [128, 16])
                csil = T(st, "csil", [128, 16], BF16)
                bm = T(st, "bm", [2, 6 * D])
                modrow = T(st, "modrow", [2, 6 * D])
                wmb = [T(st, "wm%d" % i, [128, 8, 512], BF16) for i in range(3)]
                wm_rot = Rot(range(3))
                load_fm(cfm, 0, [cvec[0, :], cvec[1, :]])
                P.act(lambda e: e.activation(out=csil[:], in_=cfm[:], func=AF.Silu),
                      reads=[("fmc", id(cfm))], writes=["csil"])
                for l in range(DEPTH):
                    for r in range(2):
                        P.dma(bm[r:r + 1, :], b_mod[l:l + 1, :], writes=["bm"])
                    for j in range(12):
                        wi = wm_rot.next()
                        wm = wmb[wi]
                        P.dma(wm[:], fmview(w_mod[l][:, j * 512:(j + 1) * 512]), writes=[("wm", wi)], q="poolq")
                        pj = ps_rot.next()
                        pb = bank(pj)
                        for c in range(8):
                            P.pe(lambda e, c=c, pb=pb, wm=wm: e.matmul(pb[0:2, :], lhsT=csil[:, c:16:8], rhs=wm[:, c, :],
                                                                      start=(c == 0), stop=(c == 7)),
                                 reads=["csil", ("wm", wi)], writes=[("ps", pj)], signal=(c == 7))
                        P.dve(lambda e, j=j, pb=pb: e.tensor_tensor(out=modrow[0:2, j * 512:(j + 1) * 512], in0=pb[0:2, :],
                                                                     in1=bm[0:2, j * 512:(j + 1) * 512], op=ALU.add),
                              reads=[("ps", pj), "bm"], writes=["modrow"])
                    P.dma(MOD[l], modrow[:], reads=["modrow"], writes=[("MOD", l)])
                P.barrier()
            if phase_end("mod", 0):
                return True


            with ExitStack() as st:
                tmpf = T(st, "tmpf", [128, 16])
                for l in range(DEPTH):
                    vecs = [norm1_g[l, :], MOD[l, 0, D:2 * D], MOD[l, 0, 0:D], MOD[l, 1, D:2 * D], MOD[l, 1, 0:D],
                            conv_w[l, 0, :], conv_w[l, 1, :], conv_w[l, 2, :], conv_w[l, 3, :], conv_b[l, :],
                            lru_br[l, 0, :], lru_bi[l, 0, :], lru_lambda[l, 0, :],
                            lru_br[l, 1, :], lru_bi[l, 1, :], lru_lambda[l, 1, :]]
                    load_fm(fm[l], 0, vecs)
                    load_fm(fm[l], 128, [pool_scale[l, :]], rows_per=4)
                    fr = ("fmc", id(fm[l]))
                    f = fm[l]
                    for vi, (sc, a1) in enumerate([(1, 17), (3, 18)]):
                        P.dve(lambda e, sc=sc, a1=a1, f=f: e.scalar_tensor_tensor(
                            out=f[:, a1 * 8:a1 * 8 + 8], in0=f[:, sc * 8:sc * 8 + 8], scalar=1.0, in1=f[:, 0:8],
                            op0=ALU.add, op1=ALU.mult), reads=[fr], writes=[fr])
                    for d, lam in enumerate([12, 15]):
                        P.act(lambda e, lam=lam, f=f, d=d: e.activation(out=tmpf[:, d * 8:d * 8 + 8], in_=f[:, lam * 8:lam * 8 + 8],
                                                                        func=AF.Exp, scale=-1.0), reads=[fr], writes=["tmpf"])
                        P.act(lambda e, d=d: e.activation(out=tmpf[:, d * 8:d * 8 + 8], in_=tmpf[:, d * 8:d * 8 + 8],
                                                          func=AF.Ln, bias=1.0), reads=["tmpf"], writes=["tmpf"])
                        P.dve(lambda e, d=d, f=f: e.tensor_scalar(out=f[:, (19 + d) * 8:(19 + d) * 8 + 8], in0=tmpf[:, d * 8:d * 8 + 8],
                                                                  scalar1=-8.0, scalar2=None, op0=ALU.mult),
                              reads=["tmpf", fr], writes=[fr])
                        P.dve(lambda e, d=d, f=f: e.tensor_scalar(out=f[:, (21 + d) * 8:(21 + d) * 8 + 8], in0=tmpf[:, d * 8:d * 8 + 8],
                                                                  scalar1=-16.0, scalar2=None, op0=ALU.mult),
                              reads=["tmpf", fr], writes=[fr])
                load_fm(h0fm, 0, [h0s[0, 0, :], h0s[0, 1, :], h0s[1, 0, :], h0s[1, 1, :]])
                P.barrier()

        def run_layers():
          for _ in range(1):
            for l in range(nlayers):
                Xsrc = x_in if l == 0 else X
                f = fm[l]
                fr = ("fmc", id(f))

                with ExitStack() as st:
                    winb = T(st, "winb", [128, 8, 3584], BF16)
                    pwb = T(st, "pwb", [128, 4, 128], BF16)
                    wbpb = T(st, "wbpb", [128, 4, D], BF16)
                    for j in range(7):
                        P.dma(winb[:, :, j * 512:(j + 1) * 512], fmview(w_in[l][:, j * 512:(j + 1) * 512]),
                              writes=["winb"], q="poolq")
                    P.dma(pwb[:], pool_w[l].rearrange("g c d -> c g d"), writes=["pwb"], q="poolq")
                    P.dma(wbpb[:], w_br_pool[l].rearrange("(c p) d -> p c d", p=128), writes=["wbpb"], q="poolq")
                    xtb = [T(st, "xt%d" % i, [128, D]) for i in range(4)]
                    xnb = [T(st, "xn%d" % i, [128, D]) for i in range(2)]
                    junk = T(st, "junk", [128, D], BF16)
                    ssb = [T(st, "ss%d" % i, [128, 1]) for i in range(4)]
                    rsb = [T(st, "rs%d" % i, [128, 1]) for i in range(4)]
                    hnT = [T(st, "hnT%d" % i, [128, 8, TS], BF16) for i in range(2)]
                    ppad = [T(st, "ppad%d" % g, [128, 640]) for g in range(4)]
                    sA = [T(st, "sA%d" % g, [128, 640]) for g in range(4)]
                    sB = [T(st, "sB%d" % g, [128, 640]) for g in range(4)]
                    icnt = [T(st, "icnt%d" % k, [128, 4, TS]) for k in range(2)]
                    pooled = T(st, "pooled", [128, 4, TS], BF16)
                    ptmp = T(st, "ptmp", [128, TS])
                    ypool = T(st, "ypool", [128, 4, TS], BF16)
                    vst = [T(st, "vst%d" % i, [128, TS]) for i in range(3)]
                    gst_ = [T(st, "gst%d" % i, [128, TS], BF16) for i in range(3)]
                    gpb = [T(st, "gp%d" % i, [128, TS]) for i in range(2)]
                    mpst = [T(st, "mpst%d" % i, [128, TS]) for i in range(3)]
                    xt_rot, xn_rot, v_rot, g_rot, gp_rot, mp_rot = Rot(range(4)), Rot(range(2)), Rot(range(3)), Rot(range(3)), Rot(range(2)), Rot(range(3))
                    for g in range(4):
                        P.dve(lambda e, g=g: e.memset(ppad[g][:], 0.0), writes=[("ppad", g)])
                        P.pool(lambda e, g=g: e.memset(sA[g][:], 0.0), writes=[("sA", g)])
                        P.pool(lambda e, g=g: e.memset(sB[g][:], 0.0), writes=[("sB", g)])
                    for k, (nrow, L) in enumerate([(8, 64), (2, 256)]):
                        for g, w in enumerate((2, 4, 8, 16)):
                            iv = icnt[k][:, g, :].rearrange("p (r t) -> p r t", t=L)
                            P.dve(lambda e, iv=iv, w=w: e.memset(iv, 1.0 / w), writes=[("icnt", k)])
                            h = w // 2
                            for t in range(h):
                                c_lo = float(min(t + h, L) - max(t - h, 0))
                                tt = L - 1 - t
                                c_hi = float(min(tt + h, L) - max(tt - h, 0))
                                P.dve(lambda e, iv=iv, t=t, c_lo=c_lo: e.memset(iv[:, :, t:t + 1], 1.0 / c_lo), writes=[("icnt", k)])
                                if c_hi != float(w):
                                    P.dve(lambda e, iv=iv, tt=tt, c_hi=c_hi: e.memset(iv[:, :, tt:tt + 1], 1.0 / c_hi), writes=[("icnt", k)])

                    def geom(i):
                        return (2, 256, 272, 1) if i < 2 else (8, 64, 80, 0)

                    def p1_front(i):
                        vi = 0 if i < 2 else 1
                        A1 = 17 + vi
                        B1 = 2 if vi == 0 else 4
                        hb = i % 2
                        for pair in range(2):
                            pbs = []
                            for _ in range(4):
                                pbs.append(ps_rot.next())
                            for sj in range(2):
                                s = pair * 2 + sj
                                xi = xt_rot.next()
                                xt = xtb[xi]
                                r0 = i * TS + s * 128
                                P.dma(xt[:], Xsrc[r0:r0 + 128, :], writes=[("xt", xi)])
                                P.act(lambda e, xt=xt, xi=xi: e.activation(out=junk[:], in_=xt[:], func=AF.Square, accum_out=ssb[xi][:, 0:1]),
                                      reads=[("xt", xi)], writes=["junk", ("ss", xi)])
                                P.act(lambda e, xi=xi: e.activation(out=ssb[xi][:, 0:1], in_=ssb[xi][:, 0:1], func=AF.Sqrt,
                                                                    scale=1.0 / D, bias=epsT[:, 0:1]),
                                      reads=[("ss", xi), "epsT"], writes=[("ss", xi)])
                                P.dve(lambda e, xi=xi: e.reciprocal(out=rsb[xi][:, 0:1], in_=ssb[xi][:, 0:1]),
                                      reads=[("ss", xi)], writes=[("rs", xi)])
                                ni = xn_rot.next()
                                xn = xnb[ni]
                                P.dve(lambda e, xn=xn, xt=xt, xi=xi: e.tensor_scalar(out=xn[:], in0=xt[:], scalar1=rsb[xi][:, 0:1], scalar2=None, op0=ALU.mult),
                                      reads=[("xt", xi), ("rs", xi)], writes=[("xn", ni)])
                                for c in range(8):
                                    pj = pbs[c // 2]
                                    off = (c % 2) * 256 + sj * 128
                                    P.pe(lambda e, xn=xn, c=c, pj=pj, off=off: e.transpose(out=bank(pj)[:, off:off + 128], in_=xn[:, c * 128:(c + 1) * 128], identity=ident[:]),
                                         reads=[("xn", ni), "ident"], writes=[("ps", pj)], signal=(c == 7))
                            for c in range(8):
                                pj = pbs[c // 2]
                                off = (c % 2) * 256
                                P.act(lambda e, c=c, pj=pj, off=off, pair=pair, hb=hb: e.activation(
                                    out=hnT[hb][:, c, pair * 256:(pair + 1) * 256], in_=bank(pj)[:, off:off + 256], func=AF.Identity,
                                    scale=FM(l, A1, c), bias=FM(l, B1, c)),
                                    reads=[("ps", pj), fr], writes=[("hnT", hb)])

                    def win_mm(i, oc):
                        hb = i % 2
                        pj = ps_rot.next()
                        for kc in range(8):
                            P.pe(lambda e, kc=kc, pj=pj, oc=oc, hb=hb: e.matmul(bank(pj), lhsT=winb[:, kc, oc * 128:(oc + 1) * 128], rhs=hnT[hb][:, kc, :],
                                                                                 start=(kc == 0), stop=(kc == 7)),
                                 reads=["winb", ("hnT", hb)], writes=[("ps", pj)], signal=(kc == 7))
                        return pj

                    def p1_back(i):
                        nrow, L, stride, ik = geom(i)
                        W = nrow * stride
                        if i == 2:
                            for g in range(4):
                                P.dve(lambda e, g=g: e.memset(ppad[g][:], 0.0), writes=[("ppad", g)])
                        for g in range(4):
                            pj = win_mm(i, g)
                            dst = ppad[g][:, 0:W].rearrange("p (r t) -> p r t", t=stride)[:, :, 8:8 + L]
                            P.act(lambda e, dst=dst, pj=pj, L=L: e.activation(out=dst, in_=bank(pj).rearrange("p (r t) -> p r t", t=L), func=AF.Copy),
                                  reads=[("ps", pj)], writes=[("ppad", g)])
                        for g, w in enumerate((2, 4, 8, 16)):
                            src, sres = ppad[g], ("ppad", g)
                            bufs = [(sA[g], ("sA", g)), (sB[g], ("sB", g))]
                            dstb, dres = bufs[0]
                            P.pool(lambda e, dstb=dstb, src=src, W=W: e.tensor_tensor(out=dstb[:, 1:W], in0=src[:, 0:W - 1], in1=src[:, 1:W], op=ALU.add),
                                   reads=[sres], writes=[dres])
                            cur, cres = dstb, dres
                            sh = 1
                            nb = 1
                            ww = 2
                            while ww < w:
                                dstb, dres = bufs[nb % 2]
                                P.pool(lambda e, dstb=dstb, cur=cur, sh=sh, W=W: e.tensor_tensor(out=dstb[:, sh:W - sh], in0=cur[:, 0:W - 2 * sh], in1=cur[:, 2 * sh:W], op=ALU.add),
                                       reads=[cres], writes=[dres])
                                cur, cres = dstb, dres
                                nb += 1
                                sh *= 2
                                ww *= 2
                            sw = cur[:, 0:W].rearrange("p (r t) -> p r t", t=stride)[:, :, 8:8 + L]
                            pin = src[:, 0:W].rearrange("p (r t) -> p r t", t=stride)[:, :, 8:8 + L]
                            iv = icnt[ik][:, g, :].rearrange("p (r t) -> p r t", t=L)
                            pt = ptmp[:].rearrange("p (r t) -> p r t", t=L)
                            P.dve(lambda e, pt=pt, sw=sw, iv=iv: e.tensor_tensor(out=pt, in0=sw, in1=iv, op=ALU.mult),
                                  reads=[cres, ("icnt", ik)], writes=["ptmp"])
                            po = pooled[:, g, :].rearrange("p (r t) -> p r t", t=L)
                            P.dve(lambda e, po=po, pt=pt, pin=pin: e.tensor_tensor(out=po, in0=pt, in1=pin, op=ALU.subtract),
                                  reads=["ptmp", sres], writes=[("pooled", g)])
                        for c in range(8):
                            pj = win_mm(i, 4 + c)
                            vi_ = v_rot.next()
                            P.act(lambda e, vi_=vi_, pj=pj: e.activation(out=vst[vi_][:], in_=bank(pj), func=AF.Copy),
                                  reads=[("ps", pj)], writes=[("vst", vi_)])
                            rows = V[c * 128:(c + 1) * 128, :]
                            if i < 2:
                                a = (2 * i) * 260 + 2
                                dst = rows[:, a:a + 520].rearrange("p (s w) -> p s w", w=260)[:, :, 0:256]
                                srcv = vst[vi_][:].rearrange("p (s w) -> p s w", w=256)
                            else:
                                a = 1042 + (i - 2) * TS
                                dst = rows[:, a:a + TS]
                                srcv = vst[vi_][:]
                            P.dma(dst, srcv, reads=[("vst", vi_)], writes=[("V", i, c)], q="actq")
                        for c in range(8):
                            pj = win_mm(i, 20 + c)
                            gi = g_rot.next()
                            P.act(lambda e, gi=gi, pj=pj: e.activation(out=gst_[gi][:], in_=bank(pj), func=AF.Sigmoid),
                                  reads=[("ps", pj)], writes=[("gst", gi)])
                            P.dma(G[c * 128:(c + 1) * 128, i * TS:(i + 1) * TS], gst_[gi][:], reads=[("gst", gi)], writes=[("G", i, c)], q="actq")
                        for g in range(4):
                            pj = ps_rot.next()
                            P.pe(lambda e, g=g, pj=pj: e.matmul(bank(pj), lhsT=pwb[:, g, :], rhs=pooled[:, g, :], start=True, stop=True),
                                 reads=["pwb", ("pooled", g)], writes=[("ps", pj)])
                            P.act(lambda e, g=g, pj=pj: e.activation(out=ypool[:, g, :], in_=bank(pj), func=AF.Identity, scale=f[:, 128 + g:129 + g]),
                                  reads=[("ps", pj), fr], writes=[("ypool", g)])
                        for k in range(8):
                            pj = win_mm(i, 12 + k)
                            gi = gp_rot.next()
                            P.act(lambda e, gi=gi, pj=pj: e.activation(out=gpb[gi][:], in_=bank(pj), func=AF.Sigmoid),
                                  reads=[("ps", pj)], writes=[("gp", gi)])
                            pj2 = ps_rot.next()
                            for kc in range(4):
                                P.pe(lambda e, kc=kc, k=k, pj2=pj2: e.matmul(bank(pj2), lhsT=wbpb[:, kc, k * 128:(k + 1) * 128], rhs=ypool[:, kc, :],
                                                                              start=(kc == 0), stop=(kc == 3)),
                                     reads=["wbpb"] + [("ypool", g) for g in range(4)], writes=[("ps", pj2)], signal=(kc == 3))
                            mi = mp_rot.next()
                            P.dve(lambda e, mi=mi, pj2=pj2, gi=gi: e.tensor_tensor(out=mpst[mi][:], in0=bank(pj2), in1=gpb[gi][:], op=ALU.mult),
                                  reads=[("ps", pj2), ("gp", gi)], writes=[("mpst", mi)])
                            P.dma(MP[k * 128:(k + 1) * 128, i * TS:(i + 1) * TS], mpst[mi][:], reads=[("mpst", mi)], writes=[("MP", i, k)], q="poolq")

                    p1_front(0)
                    for i in range(NTILE):
                        if i + 1 < NTILE:
                            p1_front(i + 1)
                        p1_back(i)
                    P.barrier()
                if phase_end("p1", l):
                    return True

                lst = ExitStack()
                gw = T(lst, "gw", [128, 4, 8, 128], BF16)
                P.pool(lambda e: e.memset(gw[:], 0.0), writes=["gw"])
                for gi_, wsrc in enumerate((lru_wr, lru_wi)):
                    for d in range(2):
                        for hl in range(2):
                            src = wsrc[l, d].rearrange("(c two) dd ee -> two dd c ee", two=2)[hl]
                            P.dma(gw[hl * 64:(hl + 1) * 64, gi_ * 2 + d, :, hl * 64:(hl + 1) * 64], src, writes=["gw"], q="poolq")

                NG = 4

                def mk_sets(st, n=NG):
                    return [(T(st, "vcb%d" % j, [128, TS], BF16), T(st, "rs_%d" % j, [128, TS]), T(st, "is_%d" % j, [128, TS]),
                             T(st, "sq_%d" % j, [128, TS])) for j in range(n)]

                def lru_group(sets, d, cs, vc_aps, res_ins, nseq, inits_list, out_hs, res_outs, reverse):
                    n = len(cs)
                    br = 10 + 3 * d
                    for j in range(n):
                        P.act(lambda e, j=j: e.activation(out=sets[j][0][:], in_=vc_aps[j], func=AF.Copy),
                              reads=[res_ins[j]], writes=[("vcb", j)])
                    prs, pis = {}, {}
                    for h0 in range(0, n, 4):
                        js = list(range(h0, min(h0 + 4, n)))
                        for j in js:
                            pr = ps_rot.next()
                            prs[j] = pr
                            P.pe(lambda e, j=j, pr=pr: e.matmul(bank(pr), lhsT=gw[:, 0 * 2 + d, cs[j], :], rhs=sets[j][0][:], start=True, stop=True),
                                 reads=["gw", ("vcb", j)], writes=[("ps", pr)])
                        for j in js:
                            pi = ps_rot.next()
                            pis[j] = pi
                            P.pe(lambda e, j=j, pi=pi: e.matmul(bank(pi), lhsT=gw[:, 1 * 2 + d, cs[j], :], rhs=sets[j][0][:], start=True, stop=True),
                                 reads=["gw", ("vcb", j)], writes=[("ps", pi)])
                        for j in js:
                            P.act(lambda e, j=j: e.activation(out=sets[j][1][:], in_=bank(prs[j]), func=AF.Sigmoid, bias=FM(l, br, cs[j])),
                                  reads=[("ps", prs[j]), fr], writes=[("rs_", j)])
                        for j in js:
                            P.act(lambda e, j=j: e.activation(out=sets[j][2][:], in_=bank(pis[j]), func=AF.Sigmoid, bias=FM(l, br + 1, cs[j])),
                                  reads=[("ps", pis[j]), fr], writes=[("is_", j)])
                    for j in range(n):
                        P.act(lambda e, j=j: e.activation(out=sets[j][3][:], in_=sets[j][1][:], func=AF.Exp, scale=FM(l, 21 + d, cs[j])),
                              reads=[("rs_", j), fr], writes=[("sq_", j)])
                    for j in range(n):
                        P.act(lambda e, j=j: e.activation(out=sets[j][1][:], in_=sets[j][1][:], func=AF.Exp, scale=FM(l, 19 + d, cs[j])),
                              reads=[("rs_", j), fr], writes=[("rs_", j)])
                    for j in range(n):
                        P.act(lambda e, j=j: e.activation(out=sets[j][3][:], in_=sets[j][3][:], func=AF.Sqrt, scale=-1.0, bias=1.0),
                              reads=[("sq_", j)], writes=[("sq_", j)])
                    for j in range(n):
                        P.dve(lambda e, j=j: e.tensor_tensor(out=sets[j][2][:], in0=sets[j][3][:], in1=sets[j][2][:], op=ALU.mult),
                              reads=[("sq_", j), ("is_", j)], writes=[("is_", j)])
                    for j in range(n):
                        P.dve(lambda e, j=j: e.tensor_tensor(out=sets[j][2][:], in0=sets[j][2][:], in1=vc_aps[j], op=ALU.mult),
                              reads=[("is_", j), res_ins[j]], writes=[("is_", j)])
                    L = TS // nseq
                    for j in range(n):
                        a_, u_, out_h = sets[j][1], sets[j][2], out_hs[j]
                        for s in range(nseq):
                            if reverse:
                                sl = slice((s + 1) * L - 1, (s * L - 1 if s > 0 else None), -1)
                            else:
                                sl = slice(s * L, (s + 1) * L)
                            init = inits_list[j][s]
                            rd = [("rs_", j), ("is_", j)] + ([] if isinstance(init, float) else ["hcar"])
                            P.dve(lambda e, out_h=out_h, a_=a_, u_=u_, sl=sl, init=init: e.tensor_tensor_scan(
                                out=out_h[:, sl], data0=a_[:, sl], data1=u_[:, sl], initial=init, op0=ALU.mult, op1=ALU.add),
                                reads=rd, writes=[res_outs[j]])

                with ExitStack() as st:
                    vtb = [T(st, "vt%d" % i, [128, 8, 520]) for i in range(3)]
                    vcs = [T(st, "vcs%d" % i, [128, TS]) for i in range(16)]
                    hfs = [T(st, "hfs%d" % i, [128, TS]) for i in range(12)]
                    sets = mk_sets(st, 8)
                    vc_rot, hf_rot = Rot(range(16)), Rot(range(12))

                    def p2_load(i):
                        vt = vtb[i % 3]
                        if i < 2:
                            a = (2 * i) * 260
                            P.dma(vt[:], fmview(V[:, a:a + 520]), reads=[("V", i, c) for c in range(8)], writes=[("vt", i % 3)])
                        else:
                            a = 1040 + (i - 2) * TS
                            rd = [("V", ii, c) for ii in (i - 1, i, i + 1) if 2 <= ii < NTILE for c in range(8)]
                            P.dma(vt[:, :, 0:515], fmview(V[:, a:a + 515]), reads=rd, writes=[("vt", i % 3)])

                    tile_state = {}

                    def p2_conv(i):
                        vt = vtb[i % 3]
                        cs = list(range(8))
                        cis = [vc_rot.next() for _ in cs]
                        vos, tapss = [], []
                        for j, c in enumerate(cs):
                            vc = vcs[cis[j]]
                            if i < 2:
                                vin = vt[:, c, :].rearrange("p (s w) -> p s w", w=260)
                                vos.append(vc[:].rearrange("p (s w) -> p s w", w=256))
                                tapss.append([vin[:, :, k:k + 256] for k in range(4)])
                            else:
                                vos.append(vc[:])
                                tapss.append([vt[:, c, k:k + TS] for k in range(4)])
                        for j, c in enumerate(cs):
                            P.dve(lambda e, j=j, c=c: e.tensor_scalar(out=vos[j], in0=tapss[j][0], scalar1=FM(l, 5, c), scalar2=FM(l, 9, c),
                                                                     op0=ALU.mult, op1=ALU.add),
                                  reads=[("vt", i % 3), fr], writes=[("vcs", cis[j])])
                        for k in range(1, 4):
                            for j, c in enumerate(cs):
                                P.dve(lambda e, j=j, c=c, k=k: e.scalar_tensor_tensor(out=vos[j], in0=tapss[j][k], scalar=FM(l, 5 + k, c), in1=vos[j],
                                                                                     op0=ALU.mult, op1=ALU.add),
                                      reads=[("vt", i % 3), fr, ("vcs", cis[j])], writes=[("vcs", cis[j])])
                        for j, c in enumerate(cs):
                            P.dma(VC[c * 128:(c + 1) * 128, i * TS:(i + 1) * TS], vcs[cis[j]][:], reads=[("vcs", cis[j])], writes=[("VC", i, c)], q="poolq")
                        tile_state[i] = cis

                    def p2_lru(i):
                        cis = tile_state.pop(i)
                        cs = list(range(8))
                        nseq = 2 if i < 2 else 1
                        his = [hf_rot.next() for _ in cs]
                        inits_list = []
                        for c in cs:
                            if i < 2:
                                inits_list.append([0.0, 0.0])
                            elif i == 2:
                                inits_list.append([h0fm[:, (l * 2 + 0) * 8 + c:(l * 2 + 0) * 8 + c + 1]])
                            else:
                                inits_list.append([hcar[:, c:c + 1]])
                        lru_group(sets, 0, cs, [vcs[ci][:] for ci in cis], [("vcs", ci) for ci in cis], nseq, inits_list,
                                  [hfs[hi] for hi in his], [("hfs", hi) for hi in his], False)
                        for j, c in enumerate(cs):
                            hf = hfs[his[j]]
                            if i >= 2:
                                P.act(lambda e, hf=hf, c=c: e.activation(out=hcar[:, c:c + 1], in_=hf[:, TS - 1:TS], func=AF.Copy),
                                      reads=[("hfs", his[j])], writes=["hcar"])
                            else:
                                for s in range(2):
                                    req = 2 * i + s
                                    col = ((l * 2 + 0) * 4 + req) * 8 + c
                                    P.act(lambda e, hf=hf, s=s, col=col: e.activation(out=nsfm[:, col:col + 1], in_=hf[:, (s + 1) * 256 - 1:(s + 1) * 256], func=AF.Copy),
                                          reads=[("hfs", his[j])], writes=["nsfm"])
                            P.dma(HF[c * 128:(c + 1) * 128, i * TS:(i + 1) * TS], hf[:], reads=[("hfs", his[j])], writes=[("HF", i, c)], q="poolq")

                    p2_load(0)
                    p2_load(1)
                    p2_conv(0)
                    for i in range(NTILE):
                        if i + 2 < NTILE:
                            p2_load(i + 2)
                        if i + 1 < NTILE:
                            p2_conv(i + 1)
                        p2_lru(i)
                    P.barrier()
                if phase_end("p2", l):
                    lst.close()
                    return True

                affS = T(lst, "affS", [16, 4096])
                affP = T(lst, "affP", [64, 256])
                P.dve(lambda e: e.memset(affP[:], 0.0), writes=["affP"])
                vS = T(lst, "vS", [16, 512])
                iS = T(lst, "iS", [16, 512], U32)

                def topk_gen(src, vals, idxs, niter, tag, srcres):
                    P.last_w[tag + "src"] = P.last_w.get(srcres)
                    for it in range(niter):
                        sl = slice(it * 8, it * 8 + 8)
                        P.dve(lambda e, sl=sl: e.max(out=vals[:, sl], in_=src), reads=[tag + "src"], writes=[tag + "v"])
                        P.dve(lambda e, sl=sl: e.max_index(out=idxs[:, sl], in_max=vals[:, sl], in_values=src), reads=[tag + "src", tag + "v"], writes=[tag + "i"])
                        if it + 1 < niter:
                            P.dve(lambda e, sl=sl: e.match_replace(out=src, in_to_replace=vals[:, sl], in_values=src, imm_value=-1.0),
                                  reads=[tag + "src", tag + "v"], writes=[tag + "src"])
                        yield
                with ExitStack() as st:
                    wblb = T(st, "wblb", [128, 8, D], BF16)
                    woutb = T(st, "woutb", [128, 8, D], BF16)
                    rwt = T(st, "rwt", [128, 8, NEXP])
                    for j in range(2):
                        P.dma(wblb[:, :, j * 512:(j + 1) * 512], fmview(w_br_lru[l][:, j * 512:(j + 1) * 512]), writes=["wblb"], q="poolq")
                        P.dma(woutb[:, :, j * 512:(j + 1) * 512], fmview(w_out[l][:, j * 512:(j + 1) * 512]), writes=["woutb"], q="poolq")
                    P.dma(rwt[:], fmview(router_w[l]), writes=["rwt"])
                    g1bc = T(st, "g1bc", [128, D])
                    a2bc = T(st, "a2bc", [128, D])
                    b2bc = T(st, "b2bc", [128, D])
                    g2n = T(st, "g2n", [128, D])
                    vcl = [T(st, "vcl%d" % i, [128, TS]) for i in range(4)]
                    hfl = [T(st, "hfl%d" % i, [128, TS]) for i in range(4)]
                    gl_ = [T(st, "gl%d" % i, [128, TS], BF16) for i in range(3)]
                    mpl = [T(st, "mpl%d" % i, [128, TS]) for i in range(3)]
                    hbs = [T(st, "hbs%d" % i, [128, TS]) for i in range(4)]
                    sets = mk_sets(st)
                    ylru2 = [T(st, "ylru%d" % i, [128, 8, TS], BF16) for i in range(2)]
                    merged = T(st, "merged", [128, 8, TS], BF16)
                    mtmp = [T(st, "mtmp%d" % i, [128, TS]) for i in range(2)]
                    xlb = [T(st, "xl%d" % i, [128, D]) for i in range(2)]
                    hn2 = [T(st, "hn2_%d" % i, [128, D]) for i in range(2)]
                    hn2b = [T(st, "hn2b%d" % i, [128, D], BF16) for i in range(2)]
                    hn2T = [T(st, "hn2T%d" % i, [128, 8, 128]) for i in range(2)]
                    junk = T(st, "junk3", [128, D], BF16)
                    sm = [T(st, "sm%d" % i, [128, 8]) for i in range(2)]
                    ex = [T(st, "ex%d" % i, [128, NEXP]) for i in range(2)]
                    affpad = [T(st, "affpad%d" % i, [128, 128]) for i in range(2)]
                    afst = [T(st, "afst%d" % i, [NEXP, 128]) for i in range(2)]
                    for k in range(2):
                        P.dve(lambda e, k=k: e.memset(affpad[k][:], 0.0), writes=[("affpad", k)])
                    vcl_rot, hfl_rot, gl_rot, mpl_rot, hb_rot, mt_rot = Rot(range(4)), Rot(range(4)), Rot(range(3)), Rot(range(3)), Rot(range(4)), Rot(range(2))

                    def load_bc(vi):
                        P.dma(g1bc[:], MOD[l, vi, 2 * D:3 * D].partition_broadcast(128), writes=["g1bc"])
                        P.dma(a2bc[:], MOD[l, vi, 4 * D:5 * D].partition_broadcast(128), writes=["a2bc"])
                        P.dma(b2bc[:], MOD[l, vi, 3 * D:4 * D].partition_broadcast(128), writes=["b2bc"])
                        P.dma(g2n[:], norm2_g[l, :].partition_broadcast(128), writes=["g2n"])
                        P.dve(lambda e: e.scalar_tensor_tensor(out=a2bc[:], in0=a2bc[:], scalar=1.0, in1=g2n[:], op0=ALU.add, op1=ALU.mult),
                              reads=["a2bc", "g2n"], writes=["a2bc"])

                    def p3_lru(i, groups=(0, NG)):
                        nseq = 2 if i < 2 else 1
                        cols = slice(i * TS, (i + 1) * TS)
                        ylru = ylru2[i % 2]
                        for g0 in groups:
                            cs = list(range(g0, g0 + NG))
                            vis = [vcl_rot.next() for _ in cs]
                            his = [hfl_rot.next() for _ in cs]
                            bis = [hb_rot.next() for _ in cs]
                            for j, c in enumerate(cs):
                                rows = slice(c * 128, (c + 1) * 128)
                                P.dma(vcl[vis[j]][:], VC[rows, cols], reads=[("VC", i, c)], writes=[("vcl", vis[j])])
                                P.dma(hfl[his[j]][:], HF[rows, cols], reads=[("HF", i, c)], writes=[("hfl", his[j])])
                            inits_list = []
                            for c in cs:
                                if i < 2:
                                    inits_list.append([0.0, 0.0])
                                elif i == NTILE - 1:
                                    inits_list.append([h0fm[:, (l * 2 + 1) * 8 + c:(l * 2 + 1) * 8 + c + 1]])
                                else:
                                    inits_list.append([hcar[:, c:c + 1]])
                            lru_group(sets, 1, cs, [vcl[v][:] for v in vis], [("vcl", v) for v in vis], nseq, inits_list,
                                      [hbs[b] for b in bis], [("hbs", b) for b in bis], True)
                            for j, c in enumerate(cs):
                                hb = hbs[bis[j]]
                                if i >= 2:
                                    P.act(lambda e, hb=hb, c=c: e.activation(out=hcar[:, c:c + 1], in_=hb[:, 0:1], func=AF.Copy),
                                          reads=[("hbs", bis[j])], writes=["hcar"])
                                else:
                                    for s in range(2):
                                        req = 2 * i + s
                                        col = ((l * 2 + 1) * 4 + req) * 8 + c
                                        P.act(lambda e, hb=hb, s=s, col=col: e.activation(out=nsfm[:, col:col + 1], in_=hb[:, s * 256:s * 256 + 1], func=AF.Copy),
                                              reads=[("hbs", bis[j])], writes=["nsfm"])
                            for j, c in enumerate(cs):
                                P.pool(lambda e, j=j, c=c: e.tensor_tensor(out=ylru[:, c, :], in0=hbs[bis[j]][:], in1=hfl[his[j]][:], op=ALU.add),
                                       reads=[("hbs", bis[j]), ("hfl", his[j])], writes=[("ylru", i % 2, c)])

                    def p3_merge(i):
                        cols = slice(i * TS, (i + 1) * TS)
                        ylru = ylru2[i % 2]
                        for oc in range(8):
                            gi_, mi_ = gl_rot.next(), mpl_rot.next()
                            rows = slice(oc * 128, (oc + 1) * 128)
                            P.dma(gl_[gi_][:], G[rows, cols], reads=[("G", i, oc)], writes=[("gl", gi_)])
                            P.dma(mpl[mi_][:], MP[rows, cols], reads=[("MP", i, oc)], writes=[("mpl", mi_)])
                            pj = ps_rot.next()
                            for kc in range(8):
                                P.pe(lambda e, kc=kc, oc=oc, pj=pj: e.matmul(bank(pj), lhsT=wblb[:, kc, oc * 128:(oc + 1) * 128], rhs=ylru[:, kc, :],
                                                                              start=(kc == 0), stop=(kc == 7)),
                                     reads=["wblb"] + [("ylru", i % 2, k) for k in range(8)], writes=[("ps", pj)], signal=(kc == 7))
                            ti = mt_rot.next()
                            P.dve(lambda e, pj=pj, gi_=gi_, ti=ti: e.tensor_tensor(out=mtmp[ti][:], in0=bank(pj), in1=gl_[gi_][:], op=ALU.mult),
                                  reads=[("ps", pj), ("gl", gi_)], writes=[("mtmp", ti)])
                            P.pool(lambda e, oc=oc, mi_=mi_, ti=ti: e.tensor_tensor(out=merged[:, oc, :], in0=mtmp[ti][:], in1=mpl[mi_][:], op=ALU.add),
                                   reads=[("mtmp", ti), ("mpl", mi_)], writes=[("merged", oc)])

                    def p3_wout(i, ss):
                        K_ = list(range(len(ss)))
                        r0s = [i * TS + s * 128 for s in ss]
                        for k in K_:
                            P.dma(xlb[k][:], Xsrc[r0s[k]:r0s[k] + 128, :], writes=[("xl", k)])
                        for k in K_:
                            s = ss[k]
                            for half in range(2):
                                pj = ps_rot.next()
                                hs = slice(half * 512, (half + 1) * 512)
                                for kc in range(8):
                                    P.pe(lambda e, kc=kc, pj=pj, s=s, hs=hs: e.matmul(bank(pj), lhsT=merged[:, kc, s * 128:(s + 1) * 128], rhs=woutb[:, kc, hs],
                                                                                       start=(kc == 0), stop=(kc == 7)),
                                         reads=["woutb"] + [("merged", q) for q in range(8)], writes=[("ps", pj)], signal=(kc == 7))
                                P.dve(lambda e, pj=pj, hs=hs, k=k: e.tensor_tensor(out=hn2[k][:, hs], in0=bank(pj), in1=g1bc[:, hs], op=ALU.mult),
                                      reads=[("ps", pj), "g1bc"], writes=[("hn2", k)])
                                P.pool(lambda e, hs=hs, k=k: e.tensor_tensor(out=xlb[k][:, hs], in0=xlb[k][:, hs], in1=hn2[k][:, hs], op=ALU.add),
                                       reads=[("hn2", k), ("xl", k)], writes=[("xl", k)])
                        for k in K_:
                            P.dma(X[r0s[k]:r0s[k] + 128, :], xlb[k][:], reads=[("xl", k)], writes=[("X", i, ss[k])], q="poolq")
                        for k in K_:
                            P.act(lambda e, k=k: e.activation(out=junk[:], in_=xlb[k][:], func=AF.Square, accum_out=sm[k][:, 0:1]),
                                  reads=[("xl", k)], writes=[("sm0", k)])
                        for k in K_:
                            P.act(lambda e, k=k: e.activation(out=sm[k][:, 0:1], in_=sm[k][:, 0:1], func=AF.Sqrt, scale=1.0 / D, bias=epsT[:, 0:1]),
                                  reads=[("sm0", k), "epsT"], writes=[("sm0", k)])
                        for k in K_:
                            P.dve(lambda e, k=k: e.reciprocal(out=sm[k][:, 1:2], in_=sm[k][:, 0:1]), reads=[("sm0", k)], writes=[("sm1", k)])
                        for k in K_:
                            P.dve(lambda e, k=k: e.scalar_tensor_tensor(out=hn2[k][:], in0=xlb[k][:], scalar=sm[k][:, 1:2], in1=a2bc[:], op0=ALU.mult, op1=ALU.mult),
                                  reads=[("xl", k), ("sm1", k), "a2bc"], writes=[("hn2", k)])
                        for k in K_:
                            P.pool(lambda e, k=k: e.tensor_tensor(out=hn2[k][:], in0=hn2[k][:], in1=b2bc[:], op=ALU.add),
                                   reads=[("hn2", k), "b2bc"], writes=[("hn2", k)])
                        for k in K_:
                            P.act(lambda e, k=k: e.activation(out=hn2b[k][:], in_=hn2[k][:], func=AF.Copy),
                                  reads=[("hn2", k)], writes=[("hn2b", k)])
                            P.dma(HN2[r0s[k]:r0s[k] + 128, :], hn2b[k][:], reads=[("hn2b", k)], writes=[("HN2", i, ss[k])], q="actq")
                        for k in K_:
                            for h2 in range(2):
                                pj = ps_rot.next()
                                for cc in range(4):
                                    c = h2 * 4 + cc
                                    P.pe(lambda e, c=c, cc=cc, pj=pj, k=k: e.transpose(out=bank(pj)[:, cc * 128:(cc + 1) * 128], in_=hn2[k][:, c * 128:(c + 1) * 128], identity=ident[:]),
                                         reads=[("hn2", k), "ident"], writes=[("ps", pj)], signal=(cc == 3))
                                P.act(lambda e, pj=pj, h2=h2, k=k: e.activation(out=hn2T[k][:, h2 * 4:(h2 + 1) * 4, :], in_=bank(pj).rearrange("p (c t) -> p c t", t=128), func=AF.Copy),
                                      reads=[("ps", pj)], writes=[("hn2T", k, h2)])
                        pjs = []
                        for k in K_:
                            pj = ps_rot.next()
                            pjs.append(pj)
                            for kc in range(8):
                                P.pe(lambda e, kc=kc, pj=pj, k=k: e.matmul(bank(pj)[:, 0:NEXP], lhsT=hn2T[k][:, kc, :], rhs=rwt[:, kc, :], start=(kc == 0), stop=(kc == 7)),
                                     reads=["rwt", ("hn2T", k, 0), ("hn2T", k, 1)], writes=[("ps", pj)], signal=(kc == 7))
                        for k in K_:
                            P.dve(lambda e, k=k: e.reduce_max(out=sm[k][:, 2:3], in_=bank(pjs[k])[:, 0:NEXP], axis=AX.X), reads=[("ps", pjs[k])], writes=[("sm2", k)])
                        for k in K_:
                            P.dve(lambda e, k=k: e.tensor_scalar(out=sm[k][:, 3:4], in0=sm[k][:, 2:3], scalar1=-1.0, scalar2=None, op0=ALU.mult), reads=[("sm2", k)], writes=[("sm3", k)])
                        for k in K_:
                            P.act(lambda e, k=k: e.activation(out=ex[k][:], in_=bank(pjs[k])[:, 0:NEXP], func=AF.Exp, bias=sm[k][:, 3:4], accum_out=sm[k][:, 4:5]),
                                  reads=[("ps", pjs[k]), ("sm3", k)], writes=[("ex", k), ("sm4", k)])
                        for k in K_:
                            P.dve(lambda e, k=k: e.reciprocal(out=sm[k][:, 5:6], in_=sm[k][:, 4:5]), reads=[("sm4", k)], writes=[("sm5", k)])
                        for k in K_:
                            P.dve(lambda e, k=k: e.tensor_scalar(out=affpad[k][:, 0:NEXP], in0=ex[k][:], scalar1=sm[k][:, 5:6], scalar2=None, op0=ALU.mult),
                                  reads=[("ex", k), ("sm5", k)], writes=[("affpad", k)])
                        pts = []
                        for k in K_:
                            pj = ps_rot.next()
                            pts.append(pj)
                            P.pe(lambda e, pj=pj, k=k: e.transpose(out=bank(pj)[:, 0:128], in_=affpad[k][:], identity=ident[:]),
                                 reads=[("affpad", k), "ident"], writes=[("ps", pj)])
                        for k in K_:
                            s = ss[k]
                            pj = pts[k]
                            if i < 2:
                                req = 2 * i + s // 2
                                tc0 = (s % 2) * 128
                                P.act(lambda e, pj=pj, k=k: e.activation(out=afst[k][0:NEXP, :], in_=bank(pj)[0:NEXP, 0:128], func=AF.Copy),
                                      reads=[("ps", pj)], writes=[("afst", k)])
                                P.dma(affP[NEXP * req:NEXP * req + NEXP, tc0:tc0 + 128], afst[k][0:NEXP, :], reads=[("afst", k)], writes=["affP"], q="actq")
                            else:
                                tc0 = (i - 2) * TS + s * 128
                                P.act(lambda e, pj=pj, tc0=tc0: e.activation(out=affS[0:NEXP, tc0:tc0 + 128], in_=bank(pj)[0:NEXP, 0:128], func=AF.Copy),
                                      reads=[("ps", pj)], writes=["affS"])

                    load_bc(1)
                    order = list(range(NTILE - 1, -1, -1))
                    p3_lru(order[0])
                    for n_, i in enumerate(order):
                        nx = order[n_ + 1] if n_ + 1 < len(order) else None
                        if nx is not None:
                            p3_lru(nx, (0,))
                        if i == 1:
                            load_bc(0)
                        p3_merge(i)
                        if nx is not None:
                            p3_lru(nx, (NG,))
                        p3_wout(i, [0, 1])
                        p3_wout(i, [2, 3])
                        if i == 2:
                            sgen = topk_gen(affS[:], vS, iS, 64, "S", "affS")
                            P.inject = sgen
                    P.inject = None
                    P.barrier()
                if phase_end("p3", l):
                    lst.close()
                    return True

                idxS = T(lst, "idxS", [128, NEXP, 4], I32)
                gatS = T(lst, "gatS", [128, NEXP, 4])
                idxP = T(lst, "idxP", [128, NEXP], I32)
                gatP = T(lst, "gatP", [128, NEXP])
                with ExitStack() as st:
                    fS = T(st, "fS", [16, 512])
                    iS2 = T(st, "iS2", [16, 512], I32)
                    wkP = T(st, "wkP", [64, 256])
                    vP = T(st, "vP", [64, 32])
                    iP = T(st, "iP", [64, 32], U32)
                    fP = T(st, "fP", [64, 32])
                    iP2 = T(st, "iP2", [64, 32], I32)
                    offi = T(st, "offi", [4, NEXP], I32)
                    offf = T(st, "offf", [4, NEXP])
                    offPf = T(st, "offPf", [64, 1])
                    P.pool(lambda e: e.iota(offi[:], pattern=[[0, NEXP]], base=0, channel_multiplier=256), writes=["offi"])
                    P.dve(lambda e: e.tensor_copy(out=offf[:], in_=offi[:]), reads=["offi"], writes=["offf"])
                    P.dma(OFFS.rearrange("(r e) -> r e", e=NEXP), offf[:], reads=["offf"], writes=["OFFS"])
                    P.dma(offPf[:], OFFS.rearrange("(p o) -> p o", o=1), reads=["OFFS"], writes=["offPf"])

                    def topk(src, wk, vals, idxs, niter, tag):
                        cur = src
                        cres = tag + "src"
                        for it in range(niter):
                            sl = slice(it * 8, it * 8 + 8)
                            P.dve(lambda e, cur=cur, sl=sl: e.max(out=vals[:, sl], in_=cur), reads=[cres], writes=[tag + "v"])
                            P.dve(lambda e, cur=cur, sl=sl: e.max_index(out=idxs[:, sl], in_max=vals[:, sl], in_values=cur), reads=[cres, tag + "v"], writes=[tag + "i"])
                            if it + 1 < niter:
                                P.dve(lambda e, cur=cur, sl=sl: e.match_replace(out=wk, in_to_replace=vals[:, sl], in_values=cur, imm_value=-1.0),
                                      reads=[cres, tag + "v"], writes=[tag + "wk"])
                                cur = wk
                                cres = tag + "wk"

                    P.last_w["Psrc"] = P.last_w.get("affP")
                    topk(affP[:], wkP[:], vP, iP, 4, "P")
                    P.dve(lambda e: e.tensor_copy(out=fP[:], in_=iP[:]), reads=["Pi"], writes=["fP"])
                    P.dve(lambda e: e.tensor_scalar(out=fP[:], in0=fP[:], scalar1=offPf[:, 0:1], scalar2=None, op0=ALU.add), reads=["fP", "offPf"], writes=["fP"])
                    P.dve(lambda e: e.tensor_copy(out=iP2[:], in_=fP[:]), reads=["fP"], writes=["iP2"])
                    P.dma(PIDX, iP2[:], reads=["iP2"], writes=["PIDX"])
                    P.dma(PGATE, vP[:], reads=["Pv"], writes=["PGATE"])
                    with nc.allow_non_contiguous_dma(reason="tiny index relayout"):
                        for r in range(4):
                            P.dma(idxP[32 * r:32 * r + 32, :], PIDX[NEXP * r:NEXP * r + NEXP, :].rearrange("e k -> k e"), reads=["PIDX"], writes=["idxP"])
                            P.dma(gatP[32 * r:32 * r + 32, :], PGATE[NEXP * r:NEXP * r + NEXP, :].rearrange("e k -> k e"), reads=["PGATE"], writes=["gatP"])
                    for _ in sgen:
                        pass
                    P.dve(lambda e: e.tensor_copy(out=fS[:], in_=iS[:]), reads=["Si"], writes=["fS"])
                    P.dve(lambda e: e.tensor_scalar(out=fS[:], in0=fS[:], scalar1=1024.0, scalar2=None, op0=ALU.add), reads=["fS"], writes=["fS"])
                    P.dve(lambda e: e.tensor_copy(out=iS2[:], in_=fS[:]), reads=["fS"], writes=["iS2"])
                    P.dma(SIDX, iS2[:], reads=["iS2"], writes=["SIDX"])
                    P.dma(SGATE, vS[:], reads=["Sv"], writes=["SGATE"])
                    with nc.allow_non_contiguous_dma(reason="tiny index relayout"):
                        P.dma(idxS[:], SIDX.rearrange("e (g p) -> p e g", p=128), reads=["SIDX"], writes=["idxS"])
                        P.dma(gatS[:], SGATE.rearrange("e (g p) -> p e g", p=128), reads=["SGATE"], writes=["gatS"])
                    P.barrier()
                if phase_end("route", l):
                    lst.close()
                    return True

                with ExitStack() as st:
                    g2bc = [T(st, "g2bc%d" % v, [128, D]) for v in range(2)]
                    for v in range(2):
                        P.dma(g2bc[v][:], MOD[l, v, 5 * D:6 * D].partition_broadcast(128), writes=[("g2bc", v)])
                    xg = [T(st, "xg%d" % g, [128, D], BF16) for g in range(5)]
                    xsT = T(st, "xsT", [128, 8, 640], BF16)
                    w1q = [T(st, "w1q%d" % i, [128, 8, 512], BF16) for i in range(2)]
                    w3q = [T(st, "w3q%d" % i, [128, 8, 512], BF16) for i in range(2)]
                    w2h = [T(st, "w2h%d" % i, [128, 16, 512], BF16) for i in range(2)]
                    hid = T(st, "hid", [128, 16, 640], BF16)
                    s1 = [T(st, "s1_%d" % i, [128, 640]) for i in range(2)]
                    osb = [T(st, "osb%d" % g, [128, D]) for g in range(5)]
                    wq_rot, w2_rot, s1_rot, hp_rot = Rot(range(2)), Rot(range(2)), Rot(range(2)), Rot(range(2))

                    def moe_load1(e_, q):
                        wi = wq_rot.next()
                        P.dma(w1q[wi][:], fmview(exp_w1[l, e_][:, q * 512:(q + 1) * 512]), writes=[("w1q", wi)], q="poolq")
                        P.dma(w3q[wi][:], fmview(exp_w3[l, e_][:, q * 512:(q + 1) * 512]), writes=[("w3q", wi)], q="poolq")
                        return wi

                    def moe_load2(e_, half):
                        wi = w2_rot.next()
                        P.dma(w2h[wi][:], exp_w2[l, e_][:, half * 512:(half + 1) * 512].rearrange("(c p) d -> p c d", p=128), writes=[("w2h", wi)], q="poolq")
                        return wi

                    def moe_gather(e_):
                        for g in range(5):
                            ia = idxS[:, e_, g:g + 1] if g < 4 else idxP[:, e_:e_ + 1]
                            ir = "idxS" if g < 4 else "idxP"
                            P.op("pool", lambda e, g=g, ia=ia: e.indirect_dma_start(out=xg[g][:], out_offset=None, in_=HN2,
                                                                                     in_offset=bass.IndirectOffsetOnAxis(ap=ia, axis=0)),
                                 reads=[ir], writes=[("xg", g)], dmaq="poolq")

                    def moe_xpose(e_):
                        for g in range(5):
                            pj = ps_rot.next()
                            pbb = bank(pj).bitcast(BF16)
                            for c in range(8):
                                P.pe(lambda e, g=g, c=c, pbb=pbb: e.transpose(out=pbb[:, c * 128:(c + 1) * 128], in_=xg[g][:, c * 128:(c + 1) * 128], identity=identb[:]),
                                     reads=[("xg", g), "identb"], writes=[("ps", pj)], signal=(c == 7))
                            P.act(lambda e, g=g, pbb=pbb: e.activation(out=xsT[:, :, g * 128:(g + 1) * 128], in_=pbb.rearrange("p (c t) -> p c t", t=128), func=AF.Copy),
                                  reads=[("ps", pj)], writes=["xsT"])

                    moe_gather(0)
                    w1i = moe_load1(0, 0)
                    moe_xpose(0)
                    for e_ in range(NEXP):
                        w2is = [None, None]
                        for q in range(4):
                            nxt = moe_load1(e_, q + 1) if q < 3 else None
                            if q == 1:
                                w2is[0] = moe_load2(e_, 0)
                            if q == 2:
                                w2is[1] = moe_load2(e_, 1)
                                if e_ + 1 < NEXP:
                                    moe_gather(e_ + 1)
                            for fc in range(4):
                                fcg = q * 4 + fc
                                hk = hp_rot.next()
                                H1, H3 = PS[2 * hk], PS[2 * hk + 1]
                                for (Hh, wt, wr) in ((H1, w1q[w1i], ("w1q", w1i)), (H3, w3q[w1i], ("w3q", w1i))):
                                    pidx = 2 * hk if Hh is H1 else 2 * hk + 1
                                    for kc in range(8):
                                        P.pe(lambda e, Hh=Hh, wt=wt, kc=kc, fc=fc: e.matmul(Hh[:, 0:512], lhsT=wt[:, kc, fc * 128:(fc + 1) * 128], rhs=xsT[:, kc, 0:512],
                                                                                             start=(kc == 0), stop=(kc == 7)),
                                             reads=[wr, "xsT"], writes=[("ps", 2 * pidx)], signal=False)
                                        P.pe(lambda e, Hh=Hh, wt=wt, kc=kc, fc=fc: e.matmul(Hh[:, 512:640], lhsT=wt[:, kc, fc * 128:(fc + 1) * 128], rhs=xsT[:, kc, 512:640],
                                                                                             start=(kc == 0), stop=(kc == 7)),
                                             reads=[wr, "xsT"], writes=[("ps", 2 * pidx + 1)], signal=(kc == 7))
                                si = s1_rot.next()
                                r1 = [("ps", 4 * hk), ("ps", 4 * hk + 1)]
                                r3 = [("ps", 4 * hk + 2), ("ps", 4 * hk + 3)]
                                P.act(lambda e, H1=H1, si=si: e.activation(out=s1[si][:], in_=H1[:, 0:640], func=AF.Silu),
                                      reads=r1, writes=[("s1", si)])
                                P.dve(lambda e, H3=H3, si=si, fcg=fcg: e.tensor_tensor(out=hid[:, fcg, :], in0=H3[:, 0:640], in1=s1[si][:], op=ALU.mult),
                                      reads=r3 + [("s1", si)], writes=[("hid", fcg)])
                            w1i = nxt
                        if e_ + 1 < NEXP:
                            w1i = moe_load1(e_ + 1, 0)
                        for half in range(2):
                            w2i = w2is[half]
                            hs = slice(half * 512, (half + 1) * 512)
                            for g in range(5):
                                pj = ps_rot.next()
                                for fcg in range(16):
                                    P.pe(lambda e, pj=pj, fcg=fcg, g=g, w2i=w2i: e.matmul(bank(pj), lhsT=hid[:, fcg, g * 128:(g + 1) * 128], rhs=w2h[w2i][:, fcg, :],
                                                                                           start=(fcg == 0), stop=(fcg == 15)),
                                         reads=[("w2h", w2i)] + [("hid", k) for k in range(16)], writes=[("ps", pj)], signal=(fcg == 15))
                                ga = gatS[:, e_, g:g + 1] if g < 4 else gatP[:, e_:e_ + 1]
                                gr = "gatS" if g < 4 else "gatP"
                                v_ = 1 if g < 4 else 0
                                P.dve(lambda e, pj=pj, g=g, ga=ga, hs=hs, v_=v_: e.scalar_tensor_tensor(out=osb[g][:, hs], in0=bank(pj), scalar=ga, in1=g2bc[v_][:, hs],
                                                                                                        op0=ALU.mult, op1=ALU.mult),
                                      reads=[("ps", pj), gr, ("g2bc", v_)], writes=[("osb", g)])
                            if half == 0 and e_ + 1 < NEXP:
                                moe_xpose(e_ + 1)
                        for g in range(5):
                            ia = idxS[:, e_, g:g + 1] if g < 4 else idxP[:, e_:e_ + 1]
                            ir = "idxS" if g < 4 else "idxP"
                            P.op("pool", lambda e, g=g, ia=ia: e.indirect_dma_start(out=X, out_offset=bass.IndirectOffsetOnAxis(ap=ia, axis=0),
                                                                                     in_=osb[g][:], in_offset=None, compute_op=ALU.add),
                                 reads=[ir, ("osb", g)], writes=["Xall"], dmaq="poolq")
                    P.barrier()
                if phase_end("moe", l):
                    lst.close()
                    return True
                lst.close()

        def run_final():
            with ExitStack() as st:
                fgbc = T(st, "fgbc", [128, D])
                P.dma(fgbc[:], final_g.partition_broadcast(128), writes=["fgbc"])
                xf = [T(st, "xf%d" % i, [128, D]) for i in range(3)]
                yf = [T(st, "yf%d" % i, [128, D]) for i in range(3)]
                junk = T(st, "junkf", [128, D], BF16)
                sf = [T(st, "sf%d" % i, [128, 2]) for i in range(3)]
                for t in range(NTOK // 128):
                    bi = t % 3
                    P.dma(xf[bi][:], X[t * 128:(t + 1) * 128, :], writes=[("xf", bi)])
                    P.act(lambda e, bi=bi: e.activation(out=junk[:], in_=xf[bi][:], func=AF.Square, accum_out=sf[bi][:, 0:1]),
                          reads=[("xf", bi)], writes=["junkf", ("sf", bi)])
                    P.act(lambda e, bi=bi: e.activation(out=sf[bi][:, 0:1], in_=sf[bi][:, 0:1], func=AF.Sqrt, scale=1.0 / D, bias=epsT[:, 0:1]),
                          reads=[("sf", bi), "epsT"], writes=[("sf", bi)])
                    P.dve(lambda e, bi=bi: e.reciprocal(out=sf[bi][:, 1:2], in_=sf[bi][:, 0:1]), reads=[("sf", bi)], writes=[("sf1", bi)])
                    P.dve(lambda e, bi=bi: e.scalar_tensor_tensor(out=yf[bi][:], in0=xf[bi][:], scalar=sf[bi][:, 1:2], in1=fgbc[:], op0=ALU.mult, op1=ALU.mult),
                          reads=[("xf", bi), ("sf1", bi), "fgbc"], writes=[("yf", bi)])
                    P.dma(y_out[t * 128:(t + 1) * 128, :], yf[bi][:], reads=[("yf", bi)], writes=[("y", t)], q="poolq")
                pj = ps_rot.next()
                P.pe(lambda e: e.transpose(out=bank(pj)[:, 0:128], in_=nsfm[:], identity=ident[:]), reads=["nsfm", "ident"], writes=[("ps", pj)])
                nsr = T(st, "nsr", [128, 128])
                P.dve(lambda e: e.tensor_copy(out=nsr[:], in_=bank(pj)[:, 0:128]), reads=[("ps", pj)], writes=["nsr"])
                for l in range(2):
                    for d in range(2):
                        for r in range(4):
                            r0 = ((l * 2 + d) * 4 + r) * 8
                            P.dma(ns_out[r, l, d, :].rearrange("(c p) -> c p", p=128), nsr[r0:r0 + 8, :], reads=["nsr"], writes=[("ns", l, d, r)])
                P.barrier()
        if not run_setup():
            if not phase_end("fm", 0):
                run_layers()
        P.barrier()
        run_final()
    nc._prog_stats = (dict(P.cnt), dict(P.dma_i), P.nops)
    return nc


_NC_CACHE = {}


def kernel(x_prompt, x_sample, state_lru, c, c_ctx, norm1_g, norm2_g, final_g, w_mod, b_mod, w_in,
           pool_w, pool_scale, conv_w, conv_b, lru_wr, lru_br, lru_wi, lru_bi, lru_lambda,
           w_br_pool, w_br_lru, w_out, router_w, exp_w1, exp_w3, exp_w2):
    f = lambda a: np.ascontiguousarray(np.asarray(a, dtype=np.float32))
    if "nc" not in _NC_CACHE:
        _NC_CACHE["nc"] = build_nc()
    nc = _NC_CACHE["nc"]
    shared = dict(norm1_g=f(norm1_g), norm2_g=f(norm2_g), final_g=f(final_g), w_mod=f(w_mod), b_mod=f(b_mod),
                  w_in=f(w_in), pool_w=f(pool_w), pool_scale=f(pool_scale), conv_w=f(conv_w), conv_b=f(conv_b),
                  lru_wr=f(lru_wr), lru_br=f(lru_br), lru_wi=f(lru_wi), lru_bi=f(lru_bi), lru_lambda=f(lru_lambda),
                  w_br_pool=f(w_br_pool), w_br_lru=f(w_br_lru), w_out=f(w_out), router_w=f(router_w),
                  exp_w1=f(exp_w1), exp_w3=f(exp_w3), exp_w2=f(exp_w2))
    xp, xs, sl, cc, cx = f(x_prompt), f(x_sample), f(state_lru), f(c), f(c_ctx)
    in_maps = []
    for k in range(8):
        m = dict(shared)
        m["x_in"] = np.concatenate([xp[4 * k:4 * k + 4].reshape(1024, D), xs[k]], axis=0)
        m["cvec"] = np.stack([cx, cc[k]], axis=0)
        m["h0s"] = np.ascontiguousarray(sl[k])
        in_maps.append(m)
    res = run_bass_kernel_spmd(nc, in_maps, core_ids=list(range(8)))
    y_prompt = np.zeros((32, 256, D), np.float32)
    y_sample = np.zeros((8, 4096, D), np.float32)
    ns = np.zeros((32, 2, 2, D), np.float32)
    for k in range(8):
        r = res.results[k]
        y_prompt[4 * k:4 * k + 4] = r["y"][0:1024].reshape(4, 256, D)
        y_sample[k] = r["y"][1024:]
        ns[4 * k:4 * k + 4] = r["ns"]
    return (y_prompt, y_sample, ns)
```

```python
from contextlib import ExitStack
import numpy as np
import concourse.bass as bass
import concourse.mybir as mybir
from concourse.bass_utils import run_bass_kernel_spmd

F32 = mybir.dt.float32
BF16 = mybir.dt.bfloat16
U32 = mybir.dt.uint32
I32 = mybir.dt.int32
AF = mybir.ActivationFunctionType
ALU = mybir.AluOpType
AX = mybir.AxisListType

COMPUTE = ("pe", "act", "dve", "pool")
NDMASEM = {"sp": 24, "actq": 12, "poolq": 24}


class Prog:
    def __init__(self, nc):
        self.nc = nc
        self.sems = {}
        self.cnt = {k: 0 for k in COMPUTE}
        self.dma_i = {k: 0 for k in NDMASEM}
        self.waited = {}
        self.last_w = {}
        self.readers = {}
        self.nops = 0
        self.inject = None
        self.inject_every = 2
        self._inj_n = 0
        self._in_inject = False

    def alloc(self, stack):
        nc = self.nc
        for k in COMPUTE:
            self.sems[k] = stack.enter_context(nc.semaphore("s_" + k))
        for q, n in NDMASEM.items():
            for i in range(n):
                self.sems[(q, i)] = stack.enter_context(nc.semaphore("d_%s_%d" % (q, i)))

    def _need(self, stream, ev, waits):
        if ev is None:
            return
        key, val = ev
        if key == stream and val > self.cnt[key]:
            return
        if self.waited.get((stream, key), 0) >= val:
            return
        if val > waits.get(key, 0):
            waits[key] = val

    def op(self, eng, fn, reads=(), writes=(), signal=True, dmaq=None):
        stream = eng
        waits = {}
        for r in reads:
            self._need(stream, self.last_w.get(r), waits)
        for w in writes:
            self._need(stream, self.last_w.get(w), waits)
            for ev in self.readers.get(w, ()):
                self._need(stream, ev, waits)
        if dmaq is not None:
            n = NDMASEM[dmaq]
            i = self.dma_i[dmaq]
            self.dma_i[dmaq] = i + 1
            key = (dmaq, i % n)
            val = 16 * (i // n + 1)
            if i >= n:
                self._need(stream, (key, val - 16), waits)
            ev = (key, val)
            inc = (key, 16)
        else:
            if signal:
                self.cnt[eng] += 1
                ev = (eng, self.cnt[eng])
                inc = (eng, 1)
            else:
                ev = (eng, self.cnt[eng] + 1)
                inc = None
        for key, val in waits.items():
            self.waited[(stream, key)] = max(self.waited.get((stream, key), 0), val)
        self._emit(stream, fn, list(waits.items()), inc)
        for r in reads:
            self.readers.setdefault(r, []).append(ev)
        for w in writes:
            self.last_w[w] = ev
            self.readers[w] = []
        self.nops += 1
        if eng == "dve" and self.inject is not None and not self._in_inject:
            self._inj_n += 1
            if self._inj_n % self.inject_every == 0:
                self._in_inject = True
                try:
                    next(self.inject)
                except StopIteration:
                    self.inject = None
                self._in_inject = False
        return ev

    def pe(self, fn, reads=(), writes=(), signal=True):
        return self.op("pe", fn, reads, writes, signal)

    def act(self, fn, reads=(), writes=()):
        return self.op("act", fn, reads, writes)

    def dve(self, fn, reads=(), writes=()):
        return self.op("dve", fn, reads, writes)

    def pool(self, fn, reads=(), writes=()):
        return self.op("pool", fn, reads, writes)

    def dma(self, out, in_, reads=(), writes=(), q="sp", **kw):
        stream = {"sp": "sp", "actq": "act", "poolq": "pool"}[q]
        return self.op(stream, lambda e: e.dma_start(out=out, in_=in_, **kw), reads, writes, dmaq=q)

    def _emit(self, stream, fn, waits, inc):
        nc = self.nc
        e = {"pe": nc.tensor, "act": nc.scalar, "dve": nc.vector, "pool": nc.gpsimd, "sp": nc.sync}[stream]
        for key, val in waits:
            e.wait_ge(self.sems[key], val)
        if fn is None:
            return
        ins = fn(e)
        if inc is not None:
            ins.then_inc(self.sems[inc[0]], inc[1])

    def barrier(self):
        evs = [(k, self.cnt[k]) for k in COMPUTE if self.cnt[k] > 0]
        for q, n in NDMASEM.items():
            i = self.dma_i[q]
            for j in range(max(0, i - n), i):
                evs.append(((q, j % n), 16 * (j // n + 1)))
        for stream in ("pe", "act", "dve", "pool", "sp"):
            waits = {}
            for ev in evs:
                self._need(stream, ev, waits)
            for key, val in waits.items():
                self.waited[(stream, key)] = max(self.waited.get((stream, key), 0), val)
            self._emit(stream, None, list(waits.items()), None)


class Rot:
    def __init__(self, items):
        self.items = list(items)
        self.i = 0

    def next(self):
        it = self.items[self.i % len(self.items)]
        self.i += 1
        return it


D = 1024
NTOK = 5120
NTILE = 10
TS = 512
VW = 5140
EPS = 1e-6
NEXP = 16
FF = 2048
DEPTH = 2


def build_nc(dbg=False, nlayers=DEPTH, stop_after=None):
    nc = bass.Bass("TRN2", target_bir_lowering=False)

    def din(name, shape, dt=F32):
        return nc.dram_tensor(name, list(shape), dt, kind="ExternalInput").ap()

    def dint(name, shape, dt=F32):
        return nc.dram_tensor(name, list(shape), dt, kind=("ExternalOutput" if dbg else "Internal")).ap()

    x_in = din("x_in", [NTOK, D])
    cvec = din("cvec", [2, D])
    h0s = din("h0s", [2, 2, D])
    norm1_g = din("norm1_g", [2, D])
    norm2_g = din("norm2_g", [2, D])
    final_g = din("final_g", [D])
    w_mod = din("w_mod", [2, D, 6 * D])
    b_mod = din("b_mod", [2, 6 * D])
    w_in = din("w_in", [2, D, 3584])
    pool_w = din("pool_w", [2, 4, 128, 128])
    pool_scale = din("pool_scale", [2, 512])
    conv_w = din("conv_w", [2, 4, D])
    conv_b = din("conv_b", [2, D])
    lru_wr = din("lru_wr", [2, 2, 16, 64, 64])
    lru_br = din("lru_br", [2, 2, D])
    lru_wi = din("lru_wi", [2, 2, 16, 64, 64])
    lru_bi = din("lru_bi", [2, 2, D])
    lru_lambda = din("lru_lambda", [2, 2, D])
    w_br_pool = din("w_br_pool", [2, 512, D])
    w_br_lru = din("w_br_lru", [2, D, D])
    w_out = din("w_out", [2, D, D])
    router_w = din("router_w", [2, D, NEXP])
    _need_exp = stop_after is None or tuple(stop_after)[0] == "moe" or tuple(stop_after)[1] > 0
    exp_w1 = din("exp_w1", [2, NEXP, D, FF]) if _need_exp else None
    exp_w3 = din("exp_w3", [2, NEXP, D, FF]) if _need_exp else None
    exp_w2 = din("exp_w2", [2, NEXP, FF, D]) if _need_exp else None

    y_out = nc.dram_tensor("y", [NTOK, D], F32, kind="ExternalOutput").ap()
    ns_out = nc.dram_tensor("ns", [4, 2, 2, D], F32, kind="ExternalOutput").ap()

    X = dint("Xs", [NTOK, D])
    HN2 = dint("HN2s", [NTOK, D], BF16)
    V = dint("Vs", [D, VW])
    VC = dint("VCs", [D, NTOK])
    HF = dint("HFs", [D, NTOK])
    MP = dint("MPs", [D, NTOK])
    G = dint("Gs", [D, NTOK], BF16)
    MG = dint("MGs", [D, NTOK], BF16)
    MOD = dint("MODs", [2, 2, 6 * D])
    SIDX = dint("SIDXs", [16, 512], I32)
    SGATE = dint("SGATEs", [16, 512])
    PIDX = dint("PIDXs", [64, 32], I32)
    PGATE = dint("PGATEs", [64, 32])
    OFFS = dint("OFFSs", [64])

    with ExitStack() as gst:
        P = Prog(nc)
        P.alloc(gst)
        PS = [gst.enter_context(nc.psum_tensor("PS%d" % k, [128, 1024], F32)) for k in range(4)]

        def bank(j):
            return PS[j // 2][:, (j % 2) * 512:(j % 2) * 512 + 512]

        _tn = [0]

        def T(st, name, shape, dt=F32):
            _tn[0] += 1
            return st.enter_context(nc.sbuf_tensor("%s_%d" % (name, _tn[0]), list(shape), dt))

        def fmview(ap2d):
            return ap2d.rearrange("(c p) w -> p c w", p=128)

        class _Stop(Exception):
            pass

        def phase_end(name, lyr):
            return stop_after is not None and tuple(stop_after) == (name, lyr)

        ident = T(gst, "ident", [128, 128])
        identb = T(gst, "identb", [128, 128], BF16)
        iot = T(gst, "iot", [128, 128], I32)
        epsT = T(gst, "epsT", [128, 1])
        zt = T(gst, "zt", [128, 8, 2])
        rowsb = [T(gst, "rows%d" % i, [128, 128]) for i in range(2)]
        rows_rot = Rot([0, 1])
        fm = [T(gst, "fm%d" % l, [128, 192]) for l in range(2)]
        h0fm = T(gst, "h0fm", [128, 32])
        nsfm = T(gst, "nsfm", [128, 128])
        hcar = T(gst, "hcar", [128, 8])

        P.pool(lambda e: e.iota(iot[:], pattern=[[1, 128]], base=0, channel_multiplier=-1), writes=["iot"])
        P.dve(lambda e: e.tensor_scalar(out=ident[:], in0=iot[:], scalar1=0.0, scalar2=None, op0=ALU.is_equal),
              reads=["iot"], writes=["ident"])
        P.dve(lambda e: e.tensor_copy(out=identb[:], in_=ident[:]), reads=["ident"], writes=["identb"])
        P.dve(lambda e: e.memset(epsT[:], EPS), writes=["epsT"])
        P.dve(lambda e: e.memset(zt[:], 0.0), writes=["zt"])
        P.dve(lambda e: e.memset(nsfm[:], 0.0), writes=["nsfm"])

        pads = []
        for q in range(4):
            pads += [q * 260, q * 260 + 258]
        pads += [1040, 5138]
        for a in pads:
            P.dma(fmview(V[:, a:a + 2]), zt[:], reads=["zt"], writes=[("Vpad", a)])

        ps_rot = Rot(range(8))

        def load_fm(dst, col0, vec_aps, rows_per=8):
            nv = len(vec_aps)
            nr = nv * rows_per
            ri = rows_rot.next()
            rb = rowsb[ri]
            for v, ap in enumerate(vec_aps):
                P.dma(rb[v * rows_per:(v + 1) * rows_per, :], ap.rearrange("(c p) -> c p", p=128),
                      writes=[("rows", ri)])
            j = ps_rot.next()
            pb = bank(j)
            P.pe(lambda e: e.transpose(out=pb[:, 0:nr], in_=rb[0:nr, :], identity=ident[0:nr, 0:nr]),
                 reads=[("rows", ri), "ident"], writes=[("ps", j)])
            P.dve(lambda e: e.tensor_copy(out=dst[:, col0:col0 + nr], in_=pb[:, 0:nr]),
                  reads=[("ps", j)], writes=[("fmc", id(dst))])

        def FM(l, blk, c):
            return fm[l][:, blk * 8 + c: blk * 8 + c + 1]

        def run_setup():
            if phase_end("setup", 0):
                return True
            with ExitStack() as st:
                cfm = T(st, "cfm", [128, 16])
                csil = T(st, "csil", [128, 16], BF16)
                bm = T(st, "bm", [2, 6 * D])
                modrow = T(st, "modrow", [2, 6 * D])
                wmb = [T(st, "wm%d" % i, [128, 8, 512], BF16) for i in range(3)]
                wm_rot = Rot(range(3))
                load_fm(cfm, 0, [cvec[0, :], cvec[1, :]])
                P.act(lambda e: e.activation(out=csil[:], in_=cfm[:], func=AF.Silu),
                      reads=[("fmc", id(cfm))], writes=["csil"])
                for l in range(DEPTH):
                    for r in range(2):
                        P.dma(bm[r:r + 1, :], b_mod[l:l + 1, :], writes=["bm"])
                    for j in range(12):
                        wi = wm_rot.next()
                        wm = wmb[wi]
                        P.dma(wm[:], fmview(w_mod[l][:, j * 512:(j + 1) * 512]), writes=[("wm", wi)], q="poolq")
                        pj = ps_rot.next()
                        pb = bank(pj)
                        for c in range(8):
                            P.pe(lambda e, c=c, pb=pb, wm=wm: e.matmul(pb[0:2, :], lhsT=csil[:, c:16:8], rhs=wm[:, c, :],
                                                                      start=(c == 0), stop=(c == 7)),
                                 reads=["csil", ("wm", wi)], writes=[("ps", pj)], signal=(c == 7))
                        P.dve(lambda e, j=j, pb=pb: e.tensor_tensor(out=modrow[0:2, j * 512:(j + 1) * 512], in0=pb[0:2, :],
                                                                     in1=bm[0:2, j * 512:(j + 1) * 512], op=ALU.add),
                              reads=[("ps", pj), "bm"], writes=["modrow"])
                    P.dma(MOD[l], modrow[:], reads=["modrow"], writes=[("MOD", l)])
                P.barrier()
            if phase_end("mod", 0):
                return True


            with ExitStack() as st:
                tmpf = T(st, "tmpf", [128, 16])
                for l in range(DEPTH):
                    vecs = [norm1_g[l, :], MOD[l, 0, D:2 * D], MOD[l, 0, 0:D], MOD[l, 1, D:2 * D], MOD[l, 1, 0:D],
                            conv_w[l, 0, :], conv_w[l, 1, :], conv_w[l, 2, :], conv_w[l, 3, :], conv_b[l, :],
                            lru_br[l, 0, :], lru_bi[l, 0, :], lru_lambda[l, 0, :],
                            lru_br[l, 1, :], lru_bi[l, 1, :], lru_lambda[l, 1, :]]
                    load_fm(fm[l], 0, vecs)
                    load_fm(fm[l], 128, [pool_scale[l, :]], rows_per=4)
                    fr = ("fmc", id(fm[l]))
                    f = fm[l]
                    for vi, (sc, a1) in enumerate([(1, 17), (3, 18)]):
                        P.dve(lambda e, sc=sc, a1=a1, f=f: e.scalar_tensor_tensor(
                            out=f[:, a1 * 8:a1 * 8 + 8], in0=f[:, sc * 8:sc * 8 + 8], scalar=1.0, in1=f[:, 0:8],
                            op0=ALU.add, op1=ALU.mult), reads=[fr], writes=[fr])
                    for d, lam in enumerate([12, 15]):
                        P.act(lambda e, lam=lam, f=f, d=d: e.activation(out=tmpf[:, d * 8:d * 8 + 8], in_=f[:, lam * 8:lam * 8 + 8],
                                                                        func=AF.Exp, scale=-1.0), reads=[fr], writes=["tmpf"])
                        P.act(lambda e, d=d: e.activation(out=tmpf[:, d * 8:d * 8 + 8], in_=tmpf[:, d * 8:d * 8 + 8],
                                                          func=AF.Ln, bias=1.0), reads=["tmpf"], writes=["tmpf"])
                        P.dve(lambda e, d=d, f=f: e.tensor_scalar(out=f[:, (19 + d) * 8:(19 + d) * 8 + 8], in0=tmpf[:, d * 8:d * 8 + 8],
                                                                  scalar1=-8.0, scalar2=None, op0=ALU.mult),
                              reads=["tmpf", fr], writes=[fr])
                        P.dve(lambda e, d=d, f=f: e.tensor_scalar(out=f[:, (21 + d) * 8:(21 + d) * 8 + 8], in0=tmpf[:, d * 8:d * 8 + 8],
                                                                  scalar1=-16.0, scalar2=None, op0=ALU.mult),
                              reads=["tmpf", fr], writes=[fr])
                load_fm(h0fm, 0, [h0s[0, 0, :], h0s[0, 1, :], h0s[1, 0, :], h0s[1, 1, :]])
                P.barrier()

        def run_layers():
          for _ in range(1):
            for l in range(nlayers):
                Xsrc = x_in if l == 0 else X
                f = fm[l]
                fr = ("fmc", id(f))

                with ExitStack() as st:
                    winb = T(st, "winb", [128, 8, 3584], BF16)
                    pwb = T(st, "pwb", [128, 4, 128], BF16)
                    wbpb = T(st, "wbpb", [128, 4, D], BF16)
                    for j in range(7):
                        P.dma(winb[:, :, j * 512:(j + 1) * 512], fmview(w_in[l][:, j * 512:(j + 1) * 512]),
                              writes=["winb"], q="poolq")
                    P.dma(pwb[:], pool_w[l].rearrange("g c d -> c g d"), writes=["pwb"], q="poolq")
                    P.dma(wbpb[:], w_br_pool[l].rearrange("(c p) d -> p c d", p=128), writes=["wbpb"], q="poolq")
                    xtb = [T(st, "xt%d" % i, [128, D]) for i in range(4)]
                    xnb = [T(st, "xn%d" % i, [128, D]) for i in range(2)]
                    junk = T(st, "junk", [128, D], BF16)
                    ssb = [T(st, "ss%d" % i, [128, 1]) for i in range(4)]
                    rsb = [T(st, "rs%d" % i, [128, 1]) for i in range(4)]
                    hnT = [T(st, "hnT%d" % i, [128, 8, TS], BF16) for i in range(2)]
                    ppad = [T(st, "ppad%d" % g, [128, 640]) for g in range(4)]
                    sA = [T(st, "sA%d" % g, [128, 640]) for g in range(4)]
                    sB = [T(st, "sB%d" % g, [128, 640]) for g in range(4)]
                    icnt = [T(st, "icnt%d" % k, [128, 4, TS]) for k in range(2)]
                    pooled = T(st, "pooled", [128, 4, TS], BF16)
                    ptmp = T(st, "ptmp", [128, TS])
                    ypool = T(st, "ypool", [128, 4, TS], BF16)
                    vst = [T(st, "vst%d" % i, [128, TS]) for i in range(3)]
                    gst_ = [T(st, "gst%d" % i, [128, TS], BF16) for i in range(3)]
                    gpb = [T(st, "gp%d" % i, [128, TS]) for i in range(2)]
                    mpst = [T(st, "mpst%d" % i, [128, TS]) for i in range(3)]
                    xt_rot, xn_rot, v_rot, g_rot, gp_rot, mp_rot = Rot(range(4)), Rot(range(2)), Rot(range(3)), Rot(range(3)), Rot(range(2)), Rot(range(3))
                    for g in range(4):
                        P.dve(lambda e, g=g: e.memset(ppad[g][:], 0.0), writes=[("ppad", g)])
                        P.pool(lambda e, g=g: e.memset(sA[g][:], 0.0), writes=[("sA", g)])
                        P.pool(lambda e, g=g: e.memset(sB[g][:], 0.0), writes=[("sB", g)])
                    for k, (nrow, L) in enumerate([(8, 64), (2, 256)]):
                        for g, w in enumerate((2, 4, 8, 16)):
                            iv = icnt[k][:, g, :].rearrange("p (r t) -> p r t", t=L)
                            P.dve(lambda e, iv=iv, w=w: e.memset(iv, 1.0 / w), writes=[("icnt", k)])
                            h = w // 2
                            for t in range(h):
                                c_lo = float(min(t + h, L) - max(t - h, 0))
                                tt = L - 1 - t
                                c_hi = float(min(tt + h, L) - max(tt - h, 0))
                                P.dve(lambda e, iv=iv, t=t, c_lo=c_lo: e.memset(iv[:, :, t:t + 1], 1.0 / c_lo), writes=[("icnt", k)])
                                if c_hi != float(w):
                                    P.dve(lambda e, iv=iv, tt=tt, c_hi=c_hi: e.memset(iv[:, :, tt:tt + 1], 1.0 / c_hi), writes=[("icnt", k)])

                    def geom(i):
                        return (2, 256, 272, 1) if i < 2 else (8, 64, 80, 0)

                    def p1_front(i):
                        vi = 0 if i < 2 else 1
                        A1 = 17 + vi
                        B1 = 2 if vi == 0 else 4
                        hb = i % 2
                        for pair in range(2):
                            pbs = []
                            for _ in range(4):
                                pbs.append(ps_rot.next())
                            for sj in range(2):
                                s = pair * 2 + sj
                                xi = xt_rot.next()
                                xt = xtb[xi]
                                r0 = i * TS + s * 128
                                P.dma(xt[:], Xsrc[r0:r0 + 128, :], writes=[("xt", xi)])
                                P.act(lambda e, xt=xt, xi=xi: e.activation(out=junk[:], in_=xt[:], func=AF.Square, accum_out=ssb[xi][:, 0:1]),
                                      reads=[("xt", xi)], writes=["junk", ("ss", xi)])
                                P.act(lambda e, xi=xi: e.activation(out=ssb[xi][:, 0:1], in_=ssb[xi][:, 0:1], func=AF.Sqrt,
                                                                    scale=1.0 / D, bias=epsT[:, 0:1]),
                                      reads=[("ss", xi), "epsT"], writes=[("ss", xi)])
                                P.dve(lambda e, xi=xi: e.reciprocal(out=rsb[xi][:, 0:1], in_=ssb[xi][:, 0:1]),
                                      reads=[("ss", xi)], writes=[("rs", xi)])
                                ni = xn_rot.next()
                                xn = xnb[ni]
                                P.dve(lambda e, xn=xn, xt=xt, xi=xi: e.tensor_scalar(out=xn[:], in0=xt[:], scalar1=rsb[xi][:, 0:1], scalar2=None, op0=ALU.mult),
                                      reads=[("xt", xi), ("rs", xi)], writes=[("xn", ni)])
                                for c in range(8):
                                    pj = pbs[c // 2]
                                    off = (c % 2) * 256 + sj * 128
                                    P.pe(lambda e, xn=xn, c=c, pj=pj, off=off: e.transpose(out=bank(pj)[:, off:off + 128], in_=xn[:, c * 128:(c + 1) * 128], identity=ident[:]),
                                         reads=[("xn", ni), "ident"], writes=[("ps", pj)], signal=(c == 7))
                            for c in range(8):
                                pj = pbs[c // 2]
                                off = (c % 2) * 256
                                P.act(lambda e, c=c, pj=pj, off=off, pair=pair, hb=hb: e.activation(
                                    out=hnT[hb][:, c, pair * 256:(pair + 1) * 256], in_=bank(pj)[:, off:off + 256], func=AF.Identity,
                                    scale=FM(l, A1, c), bias=FM(l, B1, c)),
                                    reads=[("ps", pj), fr], writes=[("hnT", hb)])

                    def win_mm(i, oc):
                        hb = i % 2
                        pj = ps_rot.next()
                        for kc in range(8):
                            P.pe(lambda e, kc=kc, pj=pj, oc=oc, hb=hb: e.matmul(bank(pj), lhsT=winb[:, kc, oc * 128:(oc + 1) * 128], rhs=hnT[hb][:, kc, :],
                                                                                 start=(kc == 0), stop=(kc == 7)),
                                 reads=["winb", ("hnT", hb)], writes=[("ps", pj)], signal=(kc == 7))
                        return pj

                    def p1_back(i):
                        nrow, L, stride, ik = geom(i)
                        W = nrow * stride
                        if i == 2:
                            for g in range(4):
                                P.dve(lambda e, g=g: e.memset(ppad[g][:], 0.0), writes=[("ppad", g)])
                        for g in range(4):
                            pj = win_mm(i, g)
                            dst = ppad[g][:, 0:W].rearrange("p (r t) -> p r t", t=stride)[:, :, 8:8 + L]
                            P.act(lambda e, dst=dst, pj=pj, L=L: e.activation(out=dst, in_=bank(pj).rearrange("p (r t) -> p r t", t=L), func=AF.Copy),
                                  reads=[("ps", pj)], writes=[("ppad", g)])
                        for g, w in enumerate((2, 4, 8, 16)):
                            src, sres = ppad[g], ("ppad", g)
                            bufs = [(sA[g], ("sA", g)), (sB[g], ("sB", g))]
                            dstb, dres = bufs[0]
                            P.pool(lambda e, dstb=dstb, src=src, W=W: e.tensor_tensor(out=dstb[:, 1:W], in0=src[:, 0:W - 1], in1=src[:, 1:W], op=ALU.add),
                                   reads=[sres], writes=[dres])
                            cur, cres = dstb, dres
                            sh = 1
                            nb = 1
                            ww = 2
                            while ww < w:
                                dstb, dres = bufs[nb % 2]
                                P.pool(lambda e, dstb=dstb, cur=cur, sh=sh, W=W: e.tensor_tensor(out=dstb[:, sh:W - sh], in0=cur[:, 0:W - 2 * sh], in1=cur[:, 2 * sh:W], op=ALU.add),
                                       reads=[cres], writes=[dres])
                                cur, cres = dstb, dres
                                nb += 1
                                sh *= 2
                                ww *= 2
                            sw = cur[:, 0:W].rearrange("p (r t) -> p r t", t=stride)[:, :, 8:8 + L]
                            pin = src[:, 0:W].rearrange("p (r t) -> p r t", t=stride)[:, :, 8:8 + L]
                            iv = icnt[ik][:, g, :].rearrange("p (r t) -> p r t", t=L)
                            pt = ptmp[:].rearrange("p (r t) -> p r t", t=L)
                            P.dve(lambda e, pt=pt, sw=sw, iv=iv: e.tensor_tensor(out=pt, in0=sw, in1=iv, op=ALU.mult),
                                  reads=[cres, ("icnt", ik)], writes=["ptmp"])
                            po = pooled[:, g, :].rearrange("p (r t) -> p r t", t=L)
                            P.dve(lambda e, po=po, pt=pt, pin=pin: e.tensor_tensor(out=po, in0=pt, in1=pin, op=ALU.subtract),
                                  reads=["ptmp", sres], writes=[("pooled", g)])
                        for c in range(8):
                            pj = win_mm(i, 4 + c)
                            vi_ = v_rot.next()
                            P.act(lambda e, vi_=vi_, pj=pj: e.activation(out=vst[vi_][:], in_=bank(pj), func=AF.Copy),
                                  reads=[("ps", pj)], writes=[("vst", vi_)])
                            rows = V[c * 128:(c + 1) * 128, :]
                            if i < 2:
                                a = (2 * i) * 260 + 2
                                dst = rows[:, a:a + 520].rearrange("p (s w) -> p s w", w=260)[:, :, 0:256]
                                srcv = vst[vi_][:].rearrange("p (s w) -> p s w", w=256)
                            else:
                                a = 1042 + (i - 2) * TS
                                dst = rows[:, a:a + TS]
                                srcv = vst[vi_][:]
                            P.dma(dst, srcv, reads=[("vst", vi_)], writes=[("V", i, c)], q="actq")
                        for c in range(8):
                            pj = win_mm(i, 20 + c)
                            gi = g_rot.next()
                            P.act(lambda e, gi=gi, pj=pj: e.activation(out=gst_[gi][:], in_=bank(pj), func=AF.Sigmoid),
                                  reads=[("ps", pj)], writes=[("gst", gi)])
                            P.dma(G[c * 128:(c + 1) * 128, i * TS:(i + 1) * TS], gst_[gi][:], reads=[("gst", gi)], writes=[("G", i, c)], q="actq")
                        for g in range(4):
                            pj = ps_rot.next()
                            P.pe(lambda e, g=g, pj=pj: e.matmul(bank(pj), lhsT=pwb[:, g, :], rhs=pooled[:, g, :], start=True, stop=True),
                                 reads=["pwb", ("pooled", g)], writes=[("ps", pj)])
                            P.act(lambda e, g=g, pj=pj: e.activation(out=ypool[:, g, :], in_=bank(pj), func=AF.Identity, scale=f[:, 128 + g:129 + g]),
                                  reads=[("ps", pj), fr], writes=[("ypool", g)])
                        for k in range(8):
                            pj = win_mm(i, 12 + k)
                            gi = gp_rot.next()
                            P.act(lambda e, gi=gi, pj=pj: e.activation(out=gpb[gi][:], in_=bank(pj), func=AF.Sigmoid),
                                  reads=[("ps", pj)], writes=[("gp", gi)])
                            pj2 = ps_rot.next()
                            for kc in range(4):
                                P.pe(lambda e, kc=kc, k=k, pj2=pj2: e.matmul(bank(pj2), lhsT=wbpb[:, kc, k * 128:(k + 1) * 128], rhs=ypool[:, kc, :],
                                                                              start=(kc == 0), stop=(kc == 3)),
                                     reads=["wbpb"] + [("ypool", g) for g in range(4)], writes=[("ps", pj2)], signal=(kc == 3))
                            mi = mp_rot.next()
                            P.dve(lambda e, mi=mi, pj2=pj2, gi=gi: e.tensor_tensor(out=mpst[mi][:], in0=bank(pj2), in1=gpb[gi][:], op=ALU.mult),
                                  reads=[("ps", pj2), ("gp", gi)], writes=[("mpst", mi)])
                            P.dma(MP[k * 128:(k + 1) * 128, i * TS:(i + 1) * TS], mpst[mi][:], reads=[("mpst", mi)], writes=[("MP", i, k)], q="poolq")

                    p1_front(0)
                    for i in range(NTILE):
                        if i + 1 < NTILE:
                            p1_front(i + 1)
                        p1_back(i)
                    P.barrier()
                if phase_end("p1", l):
                    return True

                lst = ExitStack()
                gw = T(lst, "gw", [128, 4, 8, 128], BF16)
                P.pool(lambda e: e.memset(gw[:], 0.0), writes=["gw"])
                for gi_, wsrc in enumerate((lru_wr, lru_wi)):
                    for d in range(2):
                        for hl in range(2):
                            src = wsrc[l, d].rearrange("(c two) dd ee -> two dd c ee", two=2)[hl]
                            P.dma(gw[hl * 64:(hl + 1) * 64, gi_ * 2 + d, :, hl * 64:(hl + 1) * 64], src, writes=["gw"], q="poolq")

                NG = 4

                def mk_sets(st, n=NG):
                    return [(T(st, "vcb%d" % j, [128, TS], BF16), T(st, "rs_%d" % j, [128, TS]), T(st, "is_%d" % j, [128, TS]),
                             T(st, "sq_%d" % j, [128, TS])) for j in range(n)]

                def lru_group(sets, d, cs, vc_aps, res_ins, nseq, inits_list, out_hs, res_outs, reverse):
                    n = len(cs)
                    br = 10 + 3 * d
                    for j in range(n):
                        P.act(lambda e, j=j: e.activation(out=sets[j][0][:], in_=vc_aps[j], func=AF.Copy),
                              reads=[res_ins[j]], writes=[("vcb", j)])
                    prs, pis = {}, {}
                    for h0 in range(0, n, 4):
                        js = list(range(h0, min(h0 + 4, n)))
                        for j in js:
                            pr = ps_rot.next()
                            prs[j] = pr
                            P.pe(lambda e, j=j, pr=pr: e.matmul(bank(pr), lhsT=gw[:, 0 * 2 + d, cs[j], :], rhs=sets[j][0][:], start=True, stop=True),
                                 reads=["gw", ("vcb", j)], writes=[("ps", pr)])
                        for j in js:
                            pi = ps_rot.next()
                            pis[j] = pi
                            P.pe(lambda e, j=j, pi=pi: e.matmul(bank(pi), lhsT=gw[:, 1 * 2 + d, cs[j], :], rhs=sets[j][0][:], start=True, stop=True),
                                 reads=["gw", ("vcb", j)], writes=[("ps", pi)])
                        for j in js:
                            P.act(lambda e, j=j: e.activation(out=sets[j][1][:], in_=bank(prs[j]), func=AF.Sigmoid, bias=FM(l, br, cs[j])),
                                  reads=[("ps", prs[j]), fr], writes=[("rs_", j)])
                        for j in js:
                            P.act(lambda e, j=j: e.activation(out=sets[j][2][:], in_=bank(pis[j]), func=AF.Sigmoid, bias=FM(l, br + 1, cs[j])),
                                  reads=[("ps", pis[j]), fr], writes=[("is_", j)])
                    for j in range(n):
                        P.act(lambda e, j=j: e.activation(out=sets[j][3][:], in_=sets[j][1][:], func=AF.Exp, scale=FM(l, 21 + d, cs[j])),
                              reads=[("rs_", j), fr], writes=[("sq_", j)])
                    for j in range(n):
                        P.act(lambda e, j=j: e.activation(out=sets[j][1][:], in_=sets[j][1][:], func=AF.Exp, scale=FM(l, 19 + d, cs[j])),
                              reads=[("rs_", j), fr], writes=[("rs_", j)])
                    for j in range(n):
                        P.act(lambda e, j=j: e.activation(out=sets[j][3][:], in_=sets[j][3][:], func=AF.Sqrt, scale=-1.0, bias=1.0),
                              reads=[("sq_", j)], writes=[("sq_", j)])
                    for j in range(n):
                        P.dve(lambda e, j=j: e.tensor_tensor(out=sets[j][2][:], in0=sets[j][3][:], in1=sets[j][2][:], op=ALU.mult),
                              reads=[("sq_", j), ("is_", j)], writes=[("is_", j)])
                    for j in range(n):
                        P.dve(lambda e, j=j: e.tensor_tensor(out=sets[j][2][:], in0=sets[j][2][:], in1=vc_aps[j], op=ALU.mult),
                              reads=[("is_", j), res_ins[j]], writes=[("is_", j)])
                    L = TS // nseq
                    for j in range(n):
                        a_, u_, out_h = sets[j][1], sets[j][2], out_hs[j]
                        for s in range(nseq):
                            if reverse:
                                sl = slice((s + 1) * L - 1, (s * L - 1 if s > 0 else None), -1)
                            else:
                                sl = slice(s * L, (s + 1) * L)
                            init = inits_list[j][s]
                            rd = [("rs_", j), ("is_", j)] + ([] if isinstance(init, float) else ["hcar"])
                            P.dve(lambda e, out_h=out_h, a_=a_, u_=u_, sl=sl, init=init: e.tensor_tensor_scan(
                                out=out_h[:, sl], data0=a_[:, sl], data1=u_[:, sl], initial=init, op0=ALU.mult, op1=ALU.add),
                                reads=rd, writes=[res_outs[j]])

                with ExitStack() as st:
                    vtb = [T(st, "vt%d" % i, [128, 8, 520]) for i in range(3)]
                    vcs = [T(st, "vcs%d" % i, [128, TS]) for i in range(16)]
                    hfs = [T(st, "hfs%d" % i, [128, TS]) for i in range(12)]
                    sets = mk_sets(st, 8)
                    vc_rot, hf_rot = Rot(range(16)), Rot(range(12))

                    def p2_load(i):
                        vt = vtb[i % 3]
                        if i < 2:
                            a = (2 * i) * 260
                            P.dma(vt[:], fmview(V[:, a:a + 520]), reads=[("V", i, c) for c in range(8)], writes=[("vt", i % 3)])
                        else:
                            a = 1040 + (i - 2) * TS
                            rd = [("V", ii, c) for ii in (i - 1, i, i + 1) if 2 <= ii < NTILE for c in range(8)]
                            P.dma(vt[:, :, 0:515], fmview(V[:, a:a + 515]), reads=rd, writes=[("vt", i % 3)])

                    tile_state = {}

                    def p2_conv(i):
                        vt = vtb[i % 3]
                        cs = list(range(8))
                        cis = [vc_rot.next() for _ in cs]
                        vos, tapss = [], []
                        for j, c in enumerate(cs):
                            vc = vcs[cis[j]]
                            if i < 2:
                                vin = vt[:, c, :].rearrange("p (s w) -> p s w", w=260)
                                vos.append(vc[:].rearrange("p (s w) -> p s w", w=256))
                                tapss.append([vin[:, :, k:k + 256] for k in range(4)])
                            else:
                                vos.append(vc[:])
                                tapss.append([vt[:, c, k:k + TS] for k in range(4)])
                        for j, c in enumerate(cs):
                            P.dve(lambda e, j=j, c=c: e.tensor_scalar(out=vos[j], in0=tapss[j][0], scalar1=FM(l, 5, c), scalar2=FM(l, 9, c),
                                                                     op0=ALU.mult, op1=ALU.add),
                                  reads=[("vt", i % 3), fr], writes=[("vcs", cis[j])])
                        for k in range(1, 4):
                            for j, c in enumerate(cs):
                                P.dve(lambda e, j=j, c=c, k=k: e.scalar_tensor_tensor(out=vos[j], in0=tapss[j][k], scalar=FM(l, 5 + k, c), in1=vos[j],
                                                                                     op0=ALU.mult, op1=ALU.add),
                                      reads=[("vt", i % 3), fr, ("vcs", cis[j])], writes=[("vcs", cis[j])])
                        for j, c in enumerate(cs):
                            P.dma(VC[c * 128:(c + 1) * 128, i * TS:(i + 1) * TS], vcs[cis[j]][:], reads=[("vcs", cis[j])], writes=[("VC", i, c)], q="poolq")
                        tile_state[i] = cis

                    def p2_lru(i):
                        cis = tile_state.pop(i)
                        cs = list(range(8))
                        nseq = 2 if i < 2 else 1
                        his = [hf_rot.next() for _ in cs]
                        inits_list = []
                        for c in cs:
                            if i < 2:
                                inits_list.append([0.0, 0.0])
                            elif i == 2:
                                inits_list.append([h0fm[:, (l * 2 + 0) * 8 + c:(l * 2 + 0) * 8 + c + 1]])
                            else:
                                inits_list.append([hcar[:, c:c + 1]])
                        lru_group(sets, 0, cs, [vcs[ci][:] for ci in cis], [("vcs", ci) for ci in cis], nseq, inits_list,
                                  [hfs[hi] for hi in his], [("hfs", hi) for hi in his], False)
                        for j, c in enumerate(cs):
                            hf = hfs[his[j]]
                            if i >= 2:
                                P.dve(lambda e, hf=hf, c=c: e.tensor_copy(out=hcar[:, c:c + 1], in_=hf[:, TS - 1:TS]),
                                      reads=[("hfs", his[j])], writes=["hcar"])
                            else:
                                for s in range(2):
                                    req = 2 * i + s
                                    col = ((l * 2 + 0) * 4 + req) * 8 + c
                                    P.dve(lambda e, hf=hf, s=s, col=col: e.tensor_copy(out=nsfm[:, col:col + 1], in_=hf[:, (s + 1) * 256 - 1:(s + 1) * 256]),
                                          reads=[("hfs", his[j])], writes=["nsfm"])
                            P.dma(HF[c * 128:(c + 1) * 128, i * TS:(i + 1) * TS], hf[:], reads=[("hfs", his[j])], writes=[("HF", i, c)], q="poolq")

                    p2_load(0)
                    p2_load(1)
                    p2_conv(0)
                    for i in range(NTILE):
                        if i + 2 < NTILE:
                            p2_load(i + 2)
                        if i + 1 < NTILE:
                            p2_conv(i + 1)
                        p2_lru(i)
                    P.barrier()
                if phase_end("p2", l):
                    lst.close()
                    return True

                def topk_gen(src, vals, idxs, niter, tag, srcres):
                    P.last_w[tag + "src"] = P.last_w.get(srcres)
                    for it in range(niter):
                        sl = slice(it * 8, it * 8 + 8)
                        P.dve(lambda e, sl=sl: e.max(out=vals[:, sl], in_=src), reads=[tag + "src"], writes=[tag + "v"])
                        P.dve(lambda e, sl=sl: e.max_index(out=idxs[:, sl], in_max=vals[:, sl], in_values=src), reads=[tag + "src", tag + "v"], writes=[tag + "i"])
                        if it + 1 < niter:
                            P.dve(lambda e, sl=sl: e.match_replace(out=src, in_to_replace=vals[:, sl], in_values=src, imm_value=-1.0),
                                  reads=[tag + "src", tag + "v"], writes=[tag + "src"])
                        yield

                if True:
                    def load_bc(vi):
                        P.dma(g1bc[:], MOD[l, vi, 2 * D:3 * D].partition_broadcast(128), writes=["g1bc"])
                        P.dma(a2bc[:], MOD[l, vi, 4 * D:5 * D].partition_broadcast(128), writes=["a2bc"])
                        P.dma(b2bc[:], MOD[l, vi, 3 * D:4 * D].partition_broadcast(128), writes=["b2bc"])
                        P.dma(g2n[:], norm2_g[l, :].partition_broadcast(128), writes=["g2n"])
                        P.dve(lambda e: e.scalar_tensor_tensor(out=a2bc[:], in0=a2bc[:], scalar=1.0, in1=g2n[:], op0=ALU.add, op1=ALU.mult),
                              reads=["a2bc", "g2n"], writes=["a2bc"])

                    def p3_lru(i, groups=(0,), gsz=8):
                        nseq = 2 if i < 2 else 1
                        cols = slice(i * TS, (i + 1) * TS)
                        ylru = ylru2[i % 2]
                        for g0 in groups:
                            cs = list(range(g0, g0 + gsz))
                            vis = [vcl_rot.next() for _ in cs]
                            his = [hfl_rot.next() for _ in cs]
                            bis = [hb_rot.next() for _ in cs]
                            for j, c in enumerate(cs):
                                rows = slice(c * 128, (c + 1) * 128)
                                P.dma(vcl[vis[j]][:], VC[rows, cols], reads=[("VC", i, c)], writes=[("vcl", vis[j])])
                            for j, c in enumerate(cs):
                                rows = slice(c * 128, (c + 1) * 128)
                                P.dma(hfl[his[j]][:], HF[rows, cols], reads=[("HF", i, c)], writes=[("hfl", his[j])])
                            inits_list = []
                            for c in cs:
                                if i < 2:
                                    inits_list.append([0.0, 0.0])
                                elif i == NTILE - 1:
                                    inits_list.append([h0fm[:, (l * 2 + 1) * 8 + c:(l * 2 + 1) * 8 + c + 1]])
                                else:
                                    inits_list.append([hcar[:, c:c + 1]])
                            lru_group(sets, 1, cs, [vcl[v][:] for v in vis], [("vcl", v) for v in vis], nseq, inits_list,
                                      [hbs[b] for b in bis], [("hbs", b) for b in bis], True)
                            for j, c in enumerate(cs):
                                hb = hbs[bis[j]]
                                if i >= 2:
                                    P.dve(lambda e, hb=hb, c=c: e.tensor_copy(out=hcar[:, c:c + 1], in_=hb[:, 0:1]),
                                          reads=[("hbs", bis[j])], writes=["hcar"])
                                else:
                                    for s in range(2):
                                        req = 2 * i + s
                                        col = ((l * 2 + 1) * 4 + req) * 8 + c
                                        P.dve(lambda e, hb=hb, s=s, col=col: e.tensor_copy(out=nsfm[:, col:col + 1], in_=hb[:, s * 256:s * 256 + 1]),
                                              reads=[("hbs", bis[j])], writes=["nsfm"])
                            for j, c in enumerate(cs):
                                P.pool(lambda e, j=j, c=c: e.tensor_tensor(out=ylru[:, c, :], in0=hbs[bis[j]][:], in1=hfl[his[j]][:], op=ALU.add),
                                       reads=[("hbs", bis[j]), ("hfl", his[j])], writes=[("ylru", i % 2, c)])

                    def p3_merge(i):
                        cols = slice(i * TS, (i + 1) * TS)
                        ylru = ylru2[i % 2]
                        merged = mergedb[i % 2]
                        for oc in range(8):
                            gi_, mi_ = gl_rot.next(), mpl_rot.next()
                            rows = slice(oc * 128, (oc + 1) * 128)
                            P.dma(gl_[gi_][:], G[rows, cols], reads=[("G", i, oc)], writes=[("gl", gi_)])
                            P.dma(mpl[mi_][:], MP[rows, cols], reads=[("MP", i, oc)], writes=[("mpl", mi_)])
                            pj = ps_rot.next()
                            for kc in range(8):
                                P.pe(lambda e, kc=kc, oc=oc, pj=pj: e.matmul(bank(pj), lhsT=wblb[:, kc, oc * 128:(oc + 1) * 128], rhs=ylru[:, kc, :],
                                                                              start=(kc == 0), stop=(kc == 7)),
                                     reads=["wblb"] + [("ylru", i % 2, k) for k in range(8)], writes=[("ps", pj)], signal=(kc == 7))
                            ti = mt_rot.next()
                            P.dve(lambda e, pj=pj, gi_=gi_, ti=ti: e.tensor_tensor(out=mtmp[ti][:], in0=bank(pj), in1=gl_[gi_][:], op=ALU.mult),
                                  reads=[("ps", pj), ("gl", gi_)], writes=[("mtmp", ti)])
                            P.pool(lambda e, oc=oc, mi_=mi_, ti=ti: e.tensor_tensor(out=merged[:, oc, :], in0=mtmp[ti][:], in1=mpl[mi_][:], op=ALU.add),
                                   reads=[("mtmp", ti), ("mpl", mi_)], writes=[("merged", i % 2, oc)])
                        P.dma(fmview(MG[:, cols]), merged[:], reads=[("merged", i % 2, oc) for oc in range(8)], writes=[("MG", i)], q="poolq")

                    def p3_wout(i, ss):
                        K_ = list(range(len(ss)))
                        r0s = [i * TS + s * 128 for s in ss]
                        merged = mgl[i % 2]
                        for k in K_:
                            P.dma(xlb[k][:], Xsrc[r0s[k]:r0s[k] + 128, :], writes=[("xl", k)])
                        for k in K_:
                            s = ss[k]
                            for half in range(2):
                                pj = ps_rot.next()
                                hs = slice(half * 512, (half + 1) * 512)
                                for kc in range(8):
                                    P.pe(lambda e, kc=kc, pj=pj, s=s, hs=hs: e.matmul(bank(pj), lhsT=merged[:, kc, s * 128:(s + 1) * 128], rhs=woutb[:, kc, hs],
                                                                                       start=(kc == 0), stop=(kc == 7)),
                                         reads=["woutb", ("mgl", i % 2)], writes=[("ps", pj)], signal=(kc == 7))
                                P.dve(lambda e, pj=pj, hs=hs, k=k: e.tensor_tensor(out=hn2[k][:, hs], in0=bank(pj), in1=g1bc[:, hs], op=ALU.mult),
                                      reads=[("ps", pj), "g1bc"], writes=[("hn2", k)])
                                P.pool(lambda e, hs=hs, k=k: e.tensor_tensor(out=xlb[k][:, hs], in0=xlb[k][:, hs], in1=hn2[k][:, hs], op=ALU.add),
                                       reads=[("hn2", k), ("xl", k)], writes=[("xl", k)])
                        for k in K_:
                            P.dma(X[r0s[k]:r0s[k] + 128, :], xlb[k][:], reads=[("xl", k)], writes=[("X", i, ss[k])], q="poolq")
                        for k in K_:
                            P.act(lambda e, k=k: e.activation(out=junk[:], in_=xlb[k][:], func=AF.Square, accum_out=sm[k][:, 0:1]),
                                  reads=[("xl", k)], writes=[("sm0", k)])
                        for k in K_:
                            P.act(lambda e, k=k: e.activation(out=sm[k][:, 0:1], in_=sm[k][:, 0:1], func=AF.Sqrt, scale=1.0 / D, bias=epsT[:, 0:1]),
                                  reads=[("sm0", k), "epsT"], writes=[("sm0", k)])
                        for k in K_:
                            P.dve(lambda e, k=k: e.reciprocal(out=sm[k][:, 1:2], in_=sm[k][:, 0:1]), reads=[("sm0", k)], writes=[("sm1", k)])
                        for k in K_:
                            P.dve(lambda e, k=k: e.scalar_tensor_tensor(out=hn2[k][:], in0=xlb[k][:], scalar=sm[k][:, 1:2], in1=a2bc[:], op0=ALU.mult, op1=ALU.mult),
                                  reads=[("xl", k), ("sm1", k), "a2bc"], writes=[("hn2", k)])
                        for k in K_:
                            P.pool(lambda e, k=k: e.tensor_tensor(out=hn2[k][:], in0=hn2[k][:], in1=b2bc[:], op=ALU.add),
                                   reads=[("hn2", k), "b2bc"], writes=[("hn2", k)])
                        for k in K_:
                            P.act(lambda e, k=k: e.activation(out=hn2b[k][:], in_=hn2[k][:], func=AF.Copy),
                                  reads=[("hn2", k)], writes=[("hn2b", k)])
                            P.dma(HN2[r0s[k]:r0s[k] + 128, :], hn2b[k][:], reads=[("hn2b", k)], writes=[("HN2", i, ss[k])], q="actq")
                        for k in K_:
                            for h2 in range(2):
                                pj = ps_rot.next()
                                for cc in range(4):
                                    c = h2 * 4 + cc
                                    P.pe(lambda e, c=c, cc=cc, pj=pj, k=k: e.transpose(out=bank(pj)[:, cc * 128:(cc + 1) * 128], in_=hn2[k][:, c * 128:(c + 1) * 128], identity=ident[:]),
                                         reads=[("hn2", k), "ident"], writes=[("ps", pj)], signal=(cc == 3))
                                P.act(lambda e, pj=pj, h2=h2, k=k: e.activation(out=hn2T[k][:, h2 * 4:(h2 + 1) * 4, :], in_=bank(pj).rearrange("p (c t) -> p c t", t=128), func=AF.Copy),
                                      reads=[("ps", pj)], writes=[("hn2T", k, h2)])
                        pjs = []
                        for k in K_:
                            pj = ps_rot.next()
                            pjs.append(pj)
                            for kc in range(8):
                                P.pe(lambda e, kc=kc, pj=pj, k=k: e.matmul(bank(pj)[:, 0:NEXP], lhsT=hn2T[k][:, kc, :], rhs=rwt[:, kc, :], start=(kc == 0), stop=(kc == 7)),
                                     reads=["rwt", ("hn2T", k, 0), ("hn2T", k, 1)], writes=[("ps", pj)], signal=(kc == 7))
                        for k in K_:
                            P.dve(lambda e, k=k: e.reduce_max(out=sm[k][:, 2:3], in_=bank(pjs[k])[:, 0:NEXP], axis=AX.X), reads=[("ps", pjs[k])], writes=[("sm2", k)])
                        for k in K_:
                            P.dve(lambda e, k=k: e.tensor_scalar(out=sm[k][:, 3:4], in0=sm[k][:, 2:3], scalar1=-1.0, scalar2=None, op0=ALU.mult), reads=[("sm2", k)], writes=[("sm3", k)])
                        for k in K_:
                            P.act(lambda e, k=k: e.activation(out=ex[k][:], in_=bank(pjs[k])[:, 0:NEXP], func=AF.Exp, bias=sm[k][:, 3:4], accum_out=sm[k][:, 4:5]),
                                  reads=[("ps", pjs[k]), ("sm3", k)], writes=[("ex", k), ("sm4", k)])
                        for k in K_:
                            P.dve(lambda e, k=k: e.reciprocal(out=sm[k][:, 5:6], in_=sm[k][:, 4:5]), reads=[("sm4", k)], writes=[("sm5", k)])
                        for k in K_:
                            P.dve(lambda e, k=k: e.tensor_scalar(out=affpad[k][:, 0:NEXP], in0=ex[k][:], scalar1=sm[k][:, 5:6], scalar2=None, op0=ALU.mult),
                                  reads=[("ex", k), ("sm5", k)], writes=[("affpad", k)])
                        pts = []
                        for k in K_:
                            pj = ps_rot.next()
                            pts.append(pj)
                            P.pe(lambda e, pj=pj, k=k: e.transpose(out=bank(pj)[:, 0:128], in_=affpad[k][:], identity=ident[:]),
                                 reads=[("affpad", k), "ident"], writes=[("ps", pj)])
                        for k in K_:
                            s = ss[k]
                            pj = pts[k]
                            if i < 2:
                                req = 2 * i + s // 2
                                tc0 = (s % 2) * 128
                                P.act(lambda e, pj=pj, k=k: e.activation(out=afst[k][0:NEXP, :], in_=bank(pj)[0:NEXP, 0:128], func=AF.Copy),
                                      reads=[("ps", pj)], writes=[("afst", k)])
                                P.dma(affP[NEXP * req:NEXP * req + NEXP, tc0:tc0 + 128], afst[k][0:NEXP, :], reads=[("afst", k)], writes=["affP"], q="actq")
                            else:
                                tc0 = (i - 2) * TS + s * 128
                                P.act(lambda e, pj=pj, tc0=tc0: e.activation(out=affS[0:NEXP, tc0:tc0 + 128], in_=bank(pj)[0:NEXP, 0:128], func=AF.Copy),
                                      reads=[("ps", pj)], writes=["affS"])

                order = list(range(NTILE - 1, -1, -1))
                NW = 4
                with ExitStack() as st:
                    wblb = T(st, "wblb", [128, 8, D], BF16)
                    for j in range(2):
                        P.dma(wblb[:, :, j * 512:(j + 1) * 512], fmview(w_br_lru[l][:, j * 512:(j + 1) * 512]), writes=["wblb"], q="poolq")
                    vcl = [T(st, "vcl%d" % i, [128, TS]) for i in range(16)]
                    hfl = [T(st, "hfl%d" % i, [128, TS]) for i in range(12)]
                    gl_ = [T(st, "gl%d" % i, [128, TS], BF16) for i in range(3)]
                    mpl = [T(st, "mpl%d" % i, [128, TS]) for i in range(3)]
                    hbs = [T(st, "hbs%d" % i, [128, TS]) for i in range(8)]
                    sets = mk_sets(st, 8)
                    ylru2 = [T(st, "ylru%d" % i, [128, 8, TS], BF16) for i in range(2)]
                    mergedb = [T(st, "merged%d" % i, [128, 8, TS], BF16) for i in range(2)]
                    mtmp = [T(st, "mtmp%d" % i, [128, TS]) for i in range(2)]
                    vcl_rot, hfl_rot, gl_rot, mpl_rot, hb_rot, mt_rot = Rot(range(16)), Rot(range(12)), Rot(range(3)), Rot(range(3)), Rot(range(8)), Rot(range(2))
                    p3_lru(order[0])
                    for n_, i in enumerate(order):
                        nx = order[n_ + 1] if n_ + 1 < len(order) else None
                        if nx is not None:
                            p3_lru(nx)
                        p3_merge(i)
                    P.barrier()
                if phase_end("p3a", l):
                    lst.close()
                    return True
                affS = T(lst, "affS", [16, 4096])
                affP = T(lst, "affP", [64, 256])
                P.dve(lambda e: e.memset(affP[:], 0.0), writes=["affP"])
                vS = T(lst, "vS", [16, 512])
                iS = T(lst, "iS", [16, 512], U32)
                with ExitStack() as st:
                    woutb = T(st, "woutb", [128, 8, D], BF16)
                    rwt = T(st, "rwt", [128, 8, NEXP])
                    for j in range(2):
                        P.dma(woutb[:, :, j * 512:(j + 1) * 512], fmview(w_out[l][:, j * 512:(j + 1) * 512]), writes=["woutb"], q="poolq")
                    P.dma(rwt[:], fmview(router_w[l]), writes=["rwt"])
                    g1bc = T(st, "g1bc", [128, D])
                    a2bc = T(st, "a2bc", [128, D])
                    b2bc = T(st, "b2bc", [128, D])
                    g2n = T(st, "g2n", [128, D])
                    mgl = [T(st, "mgl%d" % i, [128, 8, TS], BF16) for i in range(2)]
                    xlb = [T(st, "xl%d" % i, [128, D]) for i in range(NW)]
                    hn2 = [T(st, "hn2_%d" % i, [128, D]) for i in range(NW)]
                    hn2b = [T(st, "hn2b%d" % i, [128, D], BF16) for i in range(NW)]
                    hn2T = [T(st, "hn2T%d" % i, [128, 8, 128]) for i in range(NW)]
                    junk = T(st, "junk3", [128, D], BF16)
                    sm = [T(st, "sm%d" % i, [128, 8]) for i in range(NW)]
                    ex = [T(st, "ex%d" % i, [128, NEXP]) for i in range(NW)]
                    affpad = [T(st, "affpad%d" % i, [128, 128]) for i in range(NW)]
                    afst = [T(st, "afst%d" % i, [NEXP, 128]) for i in range(NW)]
                    for k in range(NW):
                        P.dve(lambda e, k=k: e.memset(affpad[k][:], 0.0), writes=[("affpad", k)])

                    def mg_load(i):
                        P.dma(mgl[i % 2][:], fmview(MG[:, i * TS:(i + 1) * TS]), reads=[("MG", i)], writes=[("mgl", i % 2)])

                    load_bc(1)
                    mg_load(order[0])
                    for n_, i in enumerate(order):
                        nx = order[n_ + 1] if n_ + 1 < len(order) else None
                        if nx is not None:
                            mg_load(nx)
                        if i == 1:
                            load_bc(0)
                        p3_wout(i, [0, 1, 2, 3])
                        if i == 2:
                            sgen = topk_gen(affS[:], vS, iS, 64, "S", "affS")
                            P.inject = sgen
                    P.inject = None
                    P.barrier()
                if phase_end("p3", l):
                    lst.close()
                    return True

                idxS = T(lst, "idxS", [128, NEXP, 4], I32)
                gatS = T(lst, "gatS", [128, NEXP, 4])
                idxP = T(lst, "idxP", [128, NEXP], I32)
                gatP = T(lst, "gatP", [128, NEXP])
                with ExitStack() as st:
                    fS = T(st, "fS", [16, 512])
                    iS2 = T(st, "iS2", [16, 512], I32)
                    wkP = T(st, "wkP", [64, 256])
                    vP = T(st, "vP", [64, 32])
                    iP = T(st, "iP", [64, 32], U32)
                    fP = T(st, "fP", [64, 32])
                    iP2 = T(st, "iP2", [64, 32], I32)
                    offi = T(st, "offi", [4, NEXP], I32)
                    offf = T(st, "offf", [4, NEXP])
                    offPf = T(st, "offPf", [64, 1])
                    P.pool(lambda e: e.iota(offi[:], pattern=[[0, NEXP]], base=0, channel_multiplier=256), writes=["offi"])
                    P.dve(lambda e: e.tensor_copy(out=offf[:], in_=offi[:]), reads=["offi"], writes=["offf"])
                    P.dma(OFFS.rearrange("(r e) -> r e", e=NEXP), offf[:], reads=["offf"], writes=["OFFS"])
                    P.dma(offPf[:], OFFS.rearrange("(p o) -> p o", o=1), reads=["OFFS"], writes=["offPf"])

                    def topk(src, wk, vals, idxs, niter, tag):
                        cur = src
                        cres = tag + "src"
                        for it in range(niter):
                            sl = slice(it * 8, it * 8 + 8)
                            P.dve(lambda e, cur=cur, sl=sl: e.max(out=vals[:, sl], in_=cur), reads=[cres], writes=[tag + "v"])
                            P.dve(lambda e, cur=cur, sl=sl: e.max_index(out=idxs[:, sl], in_max=vals[:, sl], in_values=cur), reads=[cres, tag + "v"], writes=[tag + "i"])
                            if it + 1 < niter:
                                P.dve(lambda e, cur=cur, sl=sl: e.match_replace(out=wk, in_to_replace=vals[:, sl], in_values=cur, imm_value=-1.0),
                                      reads=[cres, tag + "v"], writes=[tag + "wk"])
                                cur = wk
                                cres = tag + "wk"

                    P.last_w["Psrc"] = P.last_w.get("affP")
                    topk(affP[:], wkP[:], vP, iP, 4, "P")
                    P.dve(lambda e: e.tensor_copy(out=fP[:], in_=iP[:]), reads=["Pi"], writes=["fP"])
                    P.dve(lambda e: e.tensor_scalar(out=fP[:], in0=fP[:], scalar1=offPf[:, 0:1], scalar2=None, op0=ALU.add), reads=["fP", "offPf"], writes=["fP"])
                    P.dve(lambda e: e.tensor_copy(out=iP2[:], in_=fP[:]), reads=["fP"], writes=["iP2"])
                    P.dma(PIDX, iP2[:], reads=["iP2"], writes=["PIDX"])
                    P.dma(PGATE, vP[:], reads=["Pv"], writes=["PGATE"])
                    with nc.allow_non_contiguous_dma(reason="tiny index relayout"):
                        for r in range(4):
                            P.dma(idxP[32 * r:32 * r + 32, :], PIDX[NEXP * r:NEXP * r + NEXP, :].rearrange("e k -> k e"), reads=["PIDX"], writes=["idxP"])
                            P.dma(gatP[32 * r:32 * r + 32, :], PGATE[NEXP * r:NEXP * r + NEXP, :].rearrange("e k -> k e"), reads=["PGATE"], writes=["gatP"])
                    for _ in sgen:
                        pass
                    P.dve(lambda e: e.tensor_copy(out=fS[:], in_=iS[:]), reads=["Si"], writes=["fS"])
                    P.dve(lambda e: e.tensor_scalar(out=fS[:], in0=fS[:], scalar1=1024.0, scalar2=None, op0=ALU.add), reads=["fS"], writes=["fS"])
                    P.dve(lambda e: e.tensor_copy(out=iS2[:], in_=fS[:]), reads=["fS"], writes=["iS2"])
                    P.dma(SIDX, iS2[:], reads=["iS2"], writes=["SIDX"])
                    P.dma(SGATE, vS[:], reads=["Sv"], writes=["SGATE"])
                    with nc.allow_non_contiguous_dma(reason="tiny index relayout"):
                        P.dma(idxS[:], SIDX.rearrange("e (g p) -> p e g", p=128), reads=["SIDX"], writes=["idxS"])
                        P.dma(gatS[:], SGATE.rearrange("e (g p) -> p e g", p=128), reads=["SGATE"], writes=["gatS"])
                    P.barrier()
                if phase_end("route", l):
                    lst.close()
                    return True

                with ExitStack() as st:
                    g2bc = [T(st, "g2bc%d" % v, [128, D]) for v in range(2)]
                    for v in range(2):
                        P.dma(g2bc[v][:], MOD[l, v, 5 * D:6 * D].partition_broadcast(128), writes=[("g2bc", v)])
                    xg = [T(st, "xg%d" % g, [128, D], BF16) for g in range(5)]
                    xsT = T(st, "xsT", [128, 8, 640], BF16)
                    w1q = [T(st, "w1q%d" % i, [128, 8, 512], BF16) for i in range(2)]
                    w3q = [T(st, "w3q%d" % i, [128, 8, 512], BF16) for i in range(2)]
                    w2h = [T(st, "w2h%d" % i, [128, 16, 512], BF16) for i in range(2)]
                    hid = T(st, "hid", [128, 16, 640], BF16)
                    s1 = [T(st, "s1_%d" % i, [128, 640]) for i in range(2)]
                    osb = [T(st, "osb%d" % g, [128, D]) for g in range(5)]
                    wq_rot, w2_rot, s1_rot, hp_rot = Rot(range(2)), Rot(range(2)), Rot(range(2)), Rot(range(2))

                    def moe_load1(e_, q):
                        wi = wq_rot.next()
                        P.dma(w1q[wi][:], fmview(exp_w1[l, e_][:, q * 512:(q + 1) * 512]), writes=[("w1q", wi)], q="poolq")
                        P.dma(w3q[wi][:], fmview(exp_w3[l, e_][:, q * 512:(q + 1) * 512]), writes=[("w3q", wi)], q="poolq")
                        return wi

                    def moe_load2(e_, half):
                        wi = w2_rot.next()
                        P.dma(w2h[wi][:], exp_w2[l, e_][:, half * 512:(half + 1) * 512].rearrange("(c p) d -> p c d", p=128), writes=[("w2h", wi)], q="poolq")
                        return wi

                    def moe_gather(e_):
                        for g in range(5):
                            ia = idxS[:, e_, g:g + 1] if g < 4 else idxP[:, e_:e_ + 1]
                            ir = "idxS" if g < 4 else "idxP"
                            P.op("pool", lambda e, g=g, ia=ia: e.indirect_dma_start(out=xg[g][:], out_offset=None, in_=HN2,
                                                                                     in_offset=bass.IndirectOffsetOnAxis(ap=ia, axis=0)),
                                 reads=[ir], writes=[("xg", g)], dmaq="poolq")

                    def moe_xpose(e_):
                        for g in range(5):
                            pj = ps_rot.next()
                            pbb = bank(pj).bitcast(BF16)
                            for c in range(8):
                                P.pe(lambda e, g=g, c=c, pbb=pbb: e.transpose(out=pbb[:, c * 128:(c + 1) * 128], in_=xg[g][:, c * 128:(c + 1) * 128], identity=identb[:]),
                                     reads=[("xg", g), "identb"], writes=[("ps", pj)], signal=(c == 7))
                            P.act(lambda e, g=g, pbb=pbb: e.activation(out=xsT[:, :, g * 128:(g + 1) * 128], in_=pbb.rearrange("p (c t) -> p c t", t=128), func=AF.Copy),
                                  reads=[("ps", pj)], writes=["xsT"])

                    moe_gather(0)
                    w1i = moe_load1(0, 0)
                    moe_xpose(0)
                    for e_ in range(NEXP):
                        w2is = [None, None]
                        for q in range(4):
                            nxt = moe_load1(e_, q + 1) if q < 3 else None
                            if q == 1:
                                w2is[0] = moe_load2(e_, 0)
                            if q == 2:
                                w2is[1] = moe_load2(e_, 1)
                                if e_ + 1 < NEXP:
                                    moe_gather(e_ + 1)
                            for fc in range(4):
                                fcg = q * 4 + fc
                                hk = hp_rot.next()
                                H1, H3 = PS[2 * hk], PS[2 * hk + 1]
                                for (Hh, wt, wr) in ((H1, w1q[w1i], ("w1q", w1i)), (H3, w3q[w1i], ("w3q", w1i))):
                                    pidx = 2 * hk if Hh is H1 else 2 * hk + 1
                                    for kc in range(8):
                                        P.pe(lambda e, Hh=Hh, wt=wt, kc=kc, fc=fc: e.matmul(Hh[:, 0:512], lhsT=wt[:, kc, fc * 128:(fc + 1) * 128], rhs=xsT[:, kc, 0:512],
                                                                                             start=(kc == 0), stop=(kc == 7)),
                                             reads=[wr, "xsT"], writes=[("ps", 2 * pidx)], signal=False)
                                        P.pe(lambda e, Hh=Hh, wt=wt, kc=kc, fc=fc: e.matmul(Hh[:, 512:640], lhsT=wt[:, kc, fc * 128:(fc + 1) * 128], rhs=xsT[:, kc, 512:640],
                                                                                             start=(kc == 0), stop=(kc == 7)),
                                             reads=[wr, "xsT"], writes=[("ps", 2 * pidx + 1)], signal=(kc == 7))
                                si = s1_rot.next()
                                r1 = [("ps", 4 * hk), ("ps", 4 * hk + 1)]
                                r3 = [("ps", 4 * hk + 2), ("ps", 4 * hk + 3)]
                                P.act(lambda e, H1=H1, si=si: e.activation(out=s1[si][:], in_=H1[:, 0:640], func=AF.Silu),
                                      reads=r1, writes=[("s1", si)])
                                P.dve(lambda e, H3=H3, si=si, fcg=fcg: e.tensor_tensor(out=hid[:, fcg, :], in0=H3[:, 0:640], in1=s1[si][:], op=ALU.mult),
                                      reads=r3 + [("s1", si)], writes=[("hid", fcg)])
                            w1i = nxt
                        if e_ + 1 < NEXP:
                            w1i = moe_load1(e_ + 1, 0)
                        for half in range(2):
                            w2i = w2is[half]
                            hs = slice(half * 512, (half + 1) * 512)
                            for g in range(5):
                                pj = ps_rot.next()
                                for fcg in range(16):
                                    P.pe(lambda e, pj=pj, fcg=fcg, g=g, w2i=w2i: e.matmul(bank(pj), lhsT=hid[:, fcg, g * 128:(g + 1) * 128], rhs=w2h[w2i][:, fcg, :],
                                                                                           start=(fcg == 0), stop=(fcg == 15)),
                                         reads=[("w2h", w2i)] + [("hid", k) for k in range(16)], writes=[("ps", pj)], signal=(fcg == 15))
                                ga = gatS[:, e_, g:g + 1] if g < 4 else gatP[:, e_:e_ + 1]
                                gr = "gatS" if g < 4 else "gatP"
                                v_ = 1 if g < 4 else 0
                                P.dve(lambda e, pj=pj, g=g, ga=ga, hs=hs, v_=v_: e.scalar_tensor_tensor(out=osb[g][:, hs], in0=bank(pj), scalar=ga, in1=g2bc[v_][:, hs],
                                                                                                        op0=ALU.mult, op1=ALU.mult),
                                      reads=[("ps", pj), gr, ("g2bc", v_)], writes=[("osb", g)])
                            if half == 0 and e_ + 1 < NEXP:
                                moe_xpose(e_ + 1)
                        for g in range(5):
                            ia = idxS[:, e_, g:g + 1] if g < 4 else idxP[:, e_:e_ + 1]
                            ir = "idxS" if g < 4 else "idxP"
                            P.op("pool", lambda e, g=g, ia=ia: e.indirect_dma_start(out=X, out_offset=bass.IndirectOffsetOnAxis(ap=ia, axis=0),
                                                                                     in_=osb[g][:], in_offset=None, compute_op=ALU.add),
                                 reads=[ir, ("osb", g)], writes=["Xall"], dmaq="poolq")
                    P.barrier()
                if phase_end("moe", l):
                    lst.close()
                    return True
                lst.close()

        def run_final():
            with ExitStack() as st:
                fgbc = T(st, "fgbc", [128, D])
                P.dma(fgbc[:], final_g.partition_broadcast(128), writes=["fgbc"])
                xf = [T(st, "xf%d" % i, [128, D]) for i in range(3)]
                yf = [T(st, "yf%d" % i, [128, D]) for i in range(3)]
                junk = T(st, "junkf", [128, D], BF16)
                sf = [T(st, "sf%d" % i, [128, 2]) for i in range(3)]
                for t in range(NTOK // 128):
                    bi = t % 3
                    P.dma(xf[bi][:], X[t * 128:(t + 1) * 128, :], writes=[("xf", bi)])
                    P.act(lambda e, bi=bi: e.activation(out=junk[:], in_=xf[bi][:], func=AF.Square, accum_out=sf[bi][:, 0:1]),
                          reads=[("xf", bi)], writes=["junkf", ("sf", bi)])
                    P.act(lambda e, bi=bi: e.activation(out=sf[bi][:, 0:1], in_=sf[bi][:, 0:1], func=AF.Sqrt, scale=1.0 / D, bias=epsT[:, 0:1]),
                          reads=[("sf", bi), "epsT"], writes=[("sf", bi)])
                    P.dve(lambda e, bi=bi: e.reciprocal(out=sf[bi][:, 1:2], in_=sf[bi][:, 0:1]), reads=[("sf", bi)], writes=[("sf1", bi)])
                    P.dve(lambda e, bi=bi: e.scalar_tensor_tensor(out=yf[bi][:], in0=xf[bi][:], scalar=sf[bi][:, 1:2], in1=fgbc[:], op0=ALU.mult, op1=ALU.mult),
                          reads=[("xf", bi), ("sf1", bi), "fgbc"], writes=[("yf", bi)])
                    P.dma(y_out[t * 128:(t + 1) * 128, :], yf[bi][:], reads=[("yf", bi)], writes=[("y", t)], q="poolq")
                pj = ps_rot.next()
                P.pe(lambda e: e.transpose(out=bank(pj)[:, 0:128], in_=nsfm[:], identity=ident[:]), reads=["nsfm", "ident"], writes=[("ps", pj)])
                nsr = T(st, "nsr", [128, 128])
                P.dve(lambda e: e.tensor_copy(out=nsr[:], in_=bank(pj)[:, 0:128]), reads=[("ps", pj)], writes=["nsr"])
                for l in range(2):
                    for d in range(2):
                        for r in range(4):
                            r0 = ((l * 2 + d) * 4 + r) * 8
                            P.dma(ns_out[r, l, d, :].rearrange("(c p) -> c p", p=128), nsr[r0:r0 + 8, :], reads=["nsr"], writes=[("ns", l, d, r)])
                P.barrier()
        if not run_setup():
            if not phase_end("fm", 0):
                run_layers()
        P.barrier()
        run_final()
    nc._prog_stats = (dict(P.cnt), dict(P.dma_i), P.nops)
    return nc


_NC_CACHE = {}


def kernel(x_prompt, x_sample, state_lru, c, c_ctx, norm1_g, norm2_g, final_g, w_mod, b_mod, w_in,
           pool_w, pool_scale, conv_w, conv_b, lru_wr, lru_br, lru_wi, lru_bi, lru_lambda,
           w_br_pool, w_br_lru, w_out, router_w, exp_w1, exp_w3, exp_w2):
    f = lambda a: np.ascontiguousarray(np.asarray(a, dtype=np.float32))
    if "nc" not in _NC_CACHE:
        _NC_CACHE["nc"] = build_nc()
    nc = _NC_CACHE["nc"]
    shared = dict(norm1_g=f(norm1_g), norm2_g=f(norm2_g), final_g=f(final_g), w_mod=f(w_mod), b_mod=f(b_mod),
                  w_in=f(w_in), pool_w=f(pool_w), pool_scale=f(pool_scale), conv_w=f(conv_w), conv_b=f(conv_b),
                  lru_wr=f(lru_wr), lru_br=f(lru_br), lru_wi=f(lru_wi), lru_bi=f(lru_bi), lru_lambda=f(lru_lambda),
                  w_br_pool=f(w_br_pool), w_br_lru=f(w_br_lru), w_out=f(w_out), router_w=f(router_w),
                  exp_w1=f(exp_w1), exp_w3=f(exp_w3), exp_w2=f(exp_w2))
    xp, xs, sl, cc, cx = f(x_prompt), f(x_sample), f(state_lru), f(c), f(c_ctx)
    in_maps = []
    for k in range(8):
        m = dict(shared)
        m["x_in"] = np.concatenate([xp[4 * k:4 * k + 4].reshape(1024, D), xs[k]], axis=0)
        m["cvec"] = np.stack([cx, cc[k]], axis=0)
        m["h0s"] = np.ascontiguousarray(sl[k])
        in_maps.append(m)
    res = run_bass_kernel_spmd(nc, in_maps, core_ids=list(range(8)))
    y_prompt = np.zeros((32, 256, D), np.float32)
    y_sample = np.zeros((8, 4096, D), np.float32)
    ns = np.zeros((32, 2, 2, D), np.float32)
    for k in range(8):
        r = res.results[k]
        y_prompt[4 * k:4 * k + 4] = r["y"][0:1024].reshape(4, 256, D)
        y_sample[k] = r["y"][1024:]
        ns[4 * k:4 * k + 4] = r["ns"]
    return (y_prompt, y_sample, ns)
```

```python
from contextlib import ExitStack
import numpy as np
import concourse.bass as bass
import concourse.mybir as mybir
from concourse.bass_utils import run_bass_kernel_spmd

F32 = mybir.dt.float32
BF16 = mybir.dt.bfloat16
U32 = mybir.dt.uint32
I32 = mybir.dt.int32
AF = mybir.ActivationFunctionType
ALU = mybir.AluOpType
AX = mybir.AxisListType

COMPUTE = ("pe", "act", "dve", "pool")
NDMASEM = {"sp": 24, "actq": 12, "poolq": 24}


class Prog:
    def __init__(self, nc):
        self.nc = nc
        self.sems = {}
        self.cnt = {k: 0 for k in COMPUTE}
        self.dma_i = {k: 0 for k in NDMASEM}
        self.waited = {}
        self.last_w = {}
        self.readers = {}
        self.nops = 0
        self.inject = None
        self.inject_every = 1
        self._inj_n = 0
        self._in_inject = False

    def alloc(self, stack):
        nc = self.nc
        for k in COMPUTE:
            self.sems[k] = stack.enter_context(nc.semaphore("s_" + k))
        for q, n in NDMASEM.items():
            for i in range(n):
                self.sems[(q, i)] = stack.enter_context(nc.semaphore("d_%s_%d" % (q, i)))

    def _need(self, stream, ev, waits):
        if ev is None:
            return
        key, val = ev
        if key == stream and val > self.cnt[key]:
            return
        if self.waited.get((stream, key), 0) >= val:
            return
        if val > waits.get(key, 0):
            waits[key] = val

    def op(self, eng, fn, reads=(), writes=(), signal=True, dmaq=None):
        stream = eng
        waits = {}
        for r in reads:
            self._need(stream, self.last_w.get(r), waits)
        for w in writes:
            self._need(stream, self.last_w.get(w), waits)
            for ev in self.readers.get(w, ()):
                self._need(stream, ev, waits)
        if dmaq is not None:
            n = NDMASEM[dmaq]
            i = self.dma_i[dmaq]
            self.dma_i[dmaq] = i + 1
            key = (dmaq, i % n)
            val = 16 * (i // n + 1)
            if i >= n:
                self._need(stream, (key, val - 16), waits)
            ev = (key, val)
            inc = (key, 16)
        else:
            if signal:
                self.cnt[eng] += 1
                ev = (eng, self.cnt[eng])
                inc = (eng, 1)
            else:
                ev = (eng, self.cnt[eng] + 1)
                inc = None
        for key, val in waits.items():
            self.waited[(stream, key)] = max(self.waited.get((stream, key), 0), val)
        self._emit(stream, fn, list(waits.items()), inc)
        for r in reads:
            self.readers.setdefault(r, []).append(ev)
        for w in writes:
            self.last_w[w] = ev
            self.readers[w] = []
        self.nops += 1
        if eng == "dve" and self.inject is not None and not self._in_inject:
            self._inj_n += 1
            if self._inj_n % self.inject_every == 0:
                self._in_inject = True
                try:
                    next(self.inject)
                except StopIteration:
                    self.inject = None
                self._in_inject = False
        return ev

    def pe(self, fn, reads=(), writes=(), signal=True):
        return self.op("pe", fn, reads, writes, signal)

    def act(self, fn, reads=(), writes=()):
        return self.op("act", fn, reads, writes)

    def dve(self, fn, reads=(), writes=()):
        return self.op("dve", fn, reads, writes)

    def pool(self, fn, reads=(), writes=()):
        return self.op("pool", fn, reads, writes)

    def dma(self, out, in_, reads=(), writes=(), q="sp", **kw):
        stream = {"sp": "sp", "actq": "act", "poolq": "pool"}[q]
        return self.op(stream, lambda e: e.dma_start(out=out, in_=in_, **kw), reads, writes, dmaq=q)

    def _emit(self, stream, fn, waits, inc):
        nc = self.nc
        e = {"pe": nc.tensor, "act": nc.scalar, "dve": nc.vector, "pool": nc.gpsimd, "sp": nc.sync}[stream]
        for key, val in waits:
            e.wait_ge(self.sems[key], val)
        if fn is None:
            return
        ins = fn(e)
        if inc is not None:
            ins.then_inc(self.sems[inc[0]], inc[1])

    def barrier(self):
        evs = [(k, self.cnt[k]) for k in COMPUTE if self.cnt[k] > 0]
        for q, n in NDMASEM.items():
            i = self.dma_i[q]
            for j in range(max(0, i - n), i):
                evs.append(((q, j % n), 16 * (j // n + 1)))
        for stream in ("pe", "act", "dve", "pool", "sp"):
            waits = {}
            for ev in evs:
                self._need(stream, ev, waits)
            for key, val in waits.items():
                self.waited[(stream, key)] = max(self.waited.get((stream, key), 0), val)
            self._emit(stream, None, list(waits.items()), None)


class Rot:
    def __init__(self, items):
        self.items = list(items)
        self.i = 0

    def next(self):
        it = self.items[self.i % len(self.items)]
        self.i += 1
        return it


D = 1024
NTOK = 5120
NTILE = 10
TS = 512
VW = 5140
EPS = 1e-6
NEXP = 16
FF = 2048
DEPTH = 2


def build_nc(dbg=False, nlayers=DEPTH, stop_after=None):
    nc = bass.Bass("TRN2", target_bir_lowering=False)

    def din(name, shape, dt=F32):
        return nc.dram_tensor(name, list(shape), dt, kind="ExternalInput").ap()

    def dint(name, shape, dt=F32):
        return nc.dram_tensor(name, list(shape), dt, kind=("ExternalOutput" if dbg else "Internal")).ap()

    x_in = din("x_in", [NTOK, D])
    cvec = din("cvec", [2, D])
    h0s = din("h0s", [2, 2, D])
    norm1_g = din("norm1_g", [2, D])
    norm2_g = din("norm2_g", [2, D])
    final_g = din("final_g", [D])
    w_mod = din("w_mod", [2, D, 6 * D])
    b_mod = din("b_mod", [2, 6 * D])
    w_in = din("w_in", [2, D, 3584])
    pool_w = din("pool_w", [2, 4, 128, 128])
    pool_scale = din("pool_scale", [2, 512])
    conv_w = din("conv_w", [2, 4, D])
    conv_b = din("conv_b", [2, D])
    lru_wr = din("lru_wr", [2, 2, 16, 64, 64])
    lru_br = din("lru_br", [2, 2, D])
    lru_wi = din("lru_wi", [2, 2, 16, 64, 64])
    lru_bi = din("lru_bi", [2, 2, D])
    lru_lambda = din("lru_lambda", [2, 2, D])
    w_br_pool = din("w_br_pool", [2, 512, D])
    w_br_lru = din("w_br_lru", [2, D, D])
    w_out = din("w_out", [2, D, D])
    router_w = din("router_w", [2, D, NEXP])
    _need_exp = stop_after is None or tuple(stop_after)[0] == "moe" or tuple(stop_after)[1] > 0
    exp_w1 = din("exp_w1", [2, NEXP, D, FF]) if _need_exp else None
    exp_w3 = din("exp_w3", [2, NEXP, D, FF]) if _need_exp else None
    exp_w2 = din("exp_w2", [2, NEXP, FF, D]) if _need_exp else None

    y_out = nc.dram_tensor("y", [NTOK, D], F32, kind="ExternalOutput").ap()
    ns_out = nc.dram_tensor("ns", [4, 2, 2, D], F32, kind="ExternalOutput").ap()

    X = dint("Xs", [NTOK, D])
    HN2 = dint("HN2s", [NTOK, D], BF16)
    V = dint("Vs", [D, VW])
    VC = dint("VCs", [D, NTOK])
    HF = dint("HFs", [D, NTOK])
    MP = dint("MPs", [D, NTOK])
    G = dint("Gs", [D, NTOK], BF16)
    MG = dint("MGs", [D, NTOK], BF16)
    MOD = dint("MODs", [2, 2, 6 * D])
    SIDX = dint("SIDXs", [16, 512], I32)
    SGATE = dint("SGATEs", [16, 512])
    PIDX = dint("PIDXs", [64, 32], I32)
    PGATE = dint("PGATEs", [64, 32])
    OFFS = dint("OFFSs", [64])

    with ExitStack() as gst:
        P = Prog(nc)
        P.alloc(gst)
        PS = [gst.enter_context(nc.psum_tensor("PS%d" % k, [128, 1024], F32)) for k in range(4)]

        def bank(j):
            return PS[j // 2][:, (j % 2) * 512:(j % 2) * 512 + 512]

        _tn = [0]

        def T(st, name, shape, dt=F32):
            _tn[0] += 1
            return st.enter_context(nc.sbuf_tensor("%s_%d" % (name, _tn[0]), list(shape), dt))

        def fmview(ap2d):
            return ap2d.rearrange("(c p) w -> p c w", p=128)

        class _Stop(Exception):
            pass

        def phase_end(name, lyr):
            return stop_after is not None and tuple(stop_after) == (name, lyr)

        ident = T(gst, "ident", [128, 128])
        identb = T(gst, "identb", [128, 128], BF16)
        iot = T(gst, "iot", [128, 128], I32)
        epsT = T(gst, "epsT", [128, 1])
        zt = T(gst, "zt", [128, 8, 2])
        rowsb = [T(gst, "rows%d" % i, [128, 128]) for i in range(2)]
        rows_rot = Rot([0, 1])
        fm = [T(gst, "fm%d" % l, [128, 192]) for l in range(2)]
        h0fm = T(gst, "h0fm", [128, 32])
        nsfm = T(gst, "nsfm", [128, 128])
        hcar = T(gst, "hcar", [128, 8])

        P.pool(lambda e: e.iota(iot[:], pattern=[[1, 128]], base=0, channel_multiplier=-1), writes=["iot"])
        P.dve(lambda e: e.tensor_scalar(out=ident[:], in0=iot[:], scalar1=0.0, scalar2=None, op0=ALU.is_equal),
              reads=["iot"], writes=["ident"])
        P.dve(lambda e: e.tensor_copy(out=identb[:], in_=ident[:]), reads=["ident"], writes=["identb"])
        P.dve(lambda e: e.memset(epsT[:], EPS), writes=["epsT"])
        P.dve(lambda e: e.memset(zt[:], 0.0), writes=["zt"])
        P.dve(lambda e: e.memset(nsfm[:], 0.0), writes=["nsfm"])

        pads = []
        for q in range(4):
            pads += [q * 260, q * 260 + 258]
        pads += [1040, 5138]
        for a in pads:
            P.dma(fmview(V[:, a:a + 2]), zt[:], reads=["zt"], writes=[("Vpad", a)])

        ps_rot = Rot(range(8))

        def load_fm(dst, col0, vec_aps, rows_per=8):
            nv = len(vec_aps)
            nr = nv * rows_per
            ri = rows_rot.next()
            rb = rowsb[ri]
            for v, ap in enumerate(vec_aps):
                P.dma(rb[v * rows_per:(v + 1) * rows_per, :], ap.rearrange("(c p) -> c p", p=128),
                      writes=[("rows", ri)])
            j = ps_rot.next()
            pb = bank(j)
            P.pe(lambda e: e.transpose(out=pb[:, 0:nr], in_=rb[0:nr, :], identity=ident[0:nr, 0:nr]),
                 reads=[("rows", ri), "ident"], writes=[("ps", j)])
            P.dve(lambda e: e.tensor_copy(out=dst[:, col0:col0 + nr], in_=pb[:, 0:nr]),
                  reads=[("ps", j)], writes=[("fmc", id(dst))])

        def FM(l, blk, c):
            return fm[l][:, blk * 8 + c: blk * 8 + c + 1]

        def run_setup():
            if phase_end("setup", 0):
                return True
            with ExitStack() as st:
                cfm = T(st, "cfm", [128, 16])
                csil = T(st, "csil", [128, 16], BF16)
                bm = T(st, "bm", [2, 6 * D])
                modrow = T(st, "modrow", [2, 6 * D])
                wmb = [T(st, "wm%d" % i, [128, 8, 512], BF16) for i in range(3)]
                wm_rot = Rot(range(3))
                load_fm(cfm, 0, [cvec[0, :], cvec[1, :]])
                P.act(lambda e: e.activation(out=csil[:], in_=cfm[:], func=AF.Silu),
                      reads=[("fmc", id(cfm))], writes=["csil"])
                for l in range(DEPTH):
                    for r in range(2):
                        P.dma(bm[r:r + 1, :], b_mod[l:l + 1, :], writes=["bm"])
                    for j in range(12):
                        wi = wm_rot.next()
                        wm = wmb[wi]
                        P.dma(wm[:], fmview(w_mod[l][:, j * 512:(j + 1) * 512]), writes=[("wm", wi)], q="poolq")
                        pj = ps_rot.next()
                        pb = bank(pj)
                        for c in range(8):
                            P.pe(lambda e, c=c, pb=pb, wm=wm: e.matmul(pb[0:2, :], lhsT=csil[:, c:16:8], rhs=wm[:, c, :],
                                                                      start=(c == 0), stop=(c == 7)),
                                 reads=["csil", ("wm", wi)], writes=[("ps", pj)], signal=(c == 7))
                        P.dve(lambda e, j=j, pb=pb: e.tensor_tensor(out=modrow[0:2, j * 512:(j + 1) * 512], in0=pb[0:2, :],
                                                                     in1=bm[0:2, j * 512:(j + 1) * 512], op=ALU.add),
                              reads=[("ps", pj), "bm"], writes=["modrow"])
                    P.dma(MOD[l], modrow[:], reads=["modrow"], writes=[("MOD", l)])
                P.barrier()
            if phase_end("mod", 0):
                return True


            with ExitStack() as st:
                tmpf = T(st, "tmpf", [128, 16])
                for l in range(DEPTH):
                    vecs = [norm1_g[l, :], MOD[l, 0, D:2 * D], MOD[l, 0, 0:D], MOD[l, 1, D:2 * D], MOD[l, 1, 0:D],
                            conv_w[l, 0, :], conv_w[l, 1, :], conv_w[l, 2, :], conv_w[l, 3, :], conv_b[l, :],
                            lru_br[l, 0, :], lru_bi[l, 0, :], lru_lambda[l, 0, :],
                            lru_br[l, 1, :], lru_bi[l, 1, :], lru_lambda[l, 1, :]]
                    load_fm(fm[l], 0, vecs)
                    load_fm(fm[l], 128, [pool_scale[l, :]], rows_per=4)
                    fr = ("fmc", id(fm[l]))
                    f = fm[l]
                    for vi, (sc, a1) in enumerate([(1, 17), (3, 18)]):
                        P.dve(lambda e, sc=sc, a1=a1, f=f: e.scalar_tensor_tensor(
                            out=f[:, a1 * 8:a1 * 8 + 8], in0=f[:, sc * 8:sc * 8 + 8], scalar=1.0, in1=f[:, 0:8],
                            op0=ALU.add, op1=ALU.mult), reads=[fr], writes=[fr])
                    for d, lam in enumerate([12, 15]):
                        P.act(lambda e, lam=lam, f=f, d=d: e.activation(out=tmpf[:, d * 8:d * 8 + 8], in_=f[:, lam * 8:lam * 8 + 8],
                                                                        func=AF.Exp, scale=-1.0), reads=[fr], writes=["tmpf"])
                        P.act(lambda e, d=d: e.activation(out=tmpf[:, d * 8:d * 8 + 8], in_=tmpf[:, d * 8:d * 8 + 8],
                                                          func=AF.Ln, bias=1.0), reads=["tmpf"], writes=["tmpf"])
                        P.dve(lambda e, d=d, f=f: e.tensor_scalar(out=f[:, (19 + d) * 8:(19 + d) * 8 + 8], in0=tmpf[:, d * 8:d * 8 + 8],
                                                                  scalar1=-8.0, scalar2=None, op0=ALU.mult),
                              reads=["tmpf", fr], writes=[fr])
                        P.dve(lambda e, d=d, f=f: e.tensor_scalar(out=f[:, (21 + d) * 8:(21 + d) * 8 + 8], in0=tmpf[:, d * 8:d * 8 + 8],
                                                                  scalar1=-16.0, scalar2=None, op0=ALU.mult),
                              reads=["tmpf", fr], writes=[fr])
                load_fm(h0fm, 0, [h0s[0, 0, :], h0s[0, 1, :], h0s[1, 0, :], h0s[1, 1, :]])
                P.barrier()

        def run_layers():
          for _ in range(1):
            for l in range(nlayers):
                Xsrc = x_in if l == 0 else X
                f = fm[l]
                fr = ("fmc", id(f))

                with ExitStack() as st:
                    winb = T(st, "winb", [128, 8, 3584], BF16)
                    pwb = T(st, "pwb", [128, 4, 128], BF16)
                    wbpb = T(st, "wbpb", [128, 4, D], BF16)
                    for j in range(7):
                        P.dma(winb[:, :, j * 512:(j + 1) * 512], fmview(w_in[l][:, j * 512:(j + 1) * 512]),
                              writes=["winb"], q="poolq")
                    P.dma(pwb[:], pool_w[l].rearrange("g c d -> c g d"), writes=["pwb"], q="poolq")
                    P.dma(wbpb[:], w_br_pool[l].rearrange("(c p) d -> p c d", p=128), writes=["wbpb"], q="poolq")
                    xtb = [T(st, "xt%d" % i, [128, D]) for i in range(4)]
                    xnb = [T(st, "xn%d" % i, [128, D]) for i in range(2)]
                    junk = T(st, "junk", [128, D], BF16)
                    ssb = [T(st, "ss%d" % i, [128, 1]) for i in range(4)]
                    rsb = [T(st, "rs%d" % i, [128, 1]) for i in range(4)]
                    hnT = [T(st, "hnT%d" % i, [128, 8, TS], BF16) for i in range(2)]
                    ppad = [T(st, "ppad%d" % g, [128, 640]) for g in range(4)]
                    sA = [T(st, "sA%d" % g, [128, 640]) for g in range(4)]
                    sB = [T(st, "sB%d" % g, [128, 640]) for g in range(4)]
                    icnt = [T(st, "icnt%d" % k, [128, 4, TS]) for k in range(2)]
                    pooled = T(st, "pooled", [128, 4, TS], BF16)
                    ptmp = T(st, "ptmp", [128, TS])
                    ypool = T(st, "ypool", [128, 4, TS], BF16)
                    vst = [T(st, "vst%d" % i, [128, TS]) for i in range(3)]
                    gst_ = [T(st, "gst%d" % i, [128, TS], BF16) for i in range(3)]
                    gpb = [T(st, "gp%d" % i, [128, TS]) for i in range(2)]
                    mpst = [T(st, "mpst%d" % i, [128, TS]) for i in range(3)]
                    xt_rot, xn_rot, v_rot, g_rot, gp_rot, mp_rot = Rot(range(4)), Rot(range(2)), Rot(range(3)), Rot(range(3)), Rot(range(2)), Rot(range(3))
                    for g in range(4):
                        P.dve(lambda e, g=g: e.memset(ppad[g][:], 0.0), writes=[("ppad", g)])
                        P.pool(lambda e, g=g: e.memset(sA[g][:], 0.0), writes=[("sA", g)])
                        P.pool(lambda e, g=g: e.memset(sB[g][:], 0.0), writes=[("sB", g)])
                    for k, (nrow, L) in enumerate([(8, 64), (2, 256)]):
                        for g, w in enumerate((2, 4, 8, 16)):
                            iv = icnt[k][:, g, :].rearrange("p (r t) -> p r t", t=L)
                            P.dve(lambda e, iv=iv, w=w: e.memset(iv, 1.0 / w), writes=[("icnt", k)])
                            h = w // 2
                            for t in range(h):
                                c_lo = float(min(t + h, L) - max(t - h, 0))
                                tt = L - 1 - t
                                c_hi = float(min(tt + h, L) - max(tt - h, 0))
                                P.dve(lambda e, iv=iv, t=t, c_lo=c_lo: e.memset(iv[:, :, t:t + 1], 1.0 / c_lo), writes=[("icnt", k)])
                                if c_hi != float(w):
                                    P.dve(lambda e, iv=iv, tt=tt, c_hi=c_hi: e.memset(iv[:, :, tt:tt + 1], 1.0 / c_hi), writes=[("icnt", k)])

                    def geom(i):
                        return (2, 256, 272, 1) if i < 2 else (8, 64, 80, 0)

                    def p1_front(i):
                        vi = 0 if i < 2 else 1
                        A1 = 17 + vi
                        B1 = 2 if vi == 0 else 4
                        hb = i % 2
                        for pair in range(2):
                            pbs = []
                            for _ in range(4):
                                pbs.append(ps_rot.next())
                            for sj in range(2):
                                s = pair * 2 + sj
                                xi = xt_rot.next()
                                xt = xtb[xi]
                                r0 = i * TS + s * 128
                                P.dma(xt[:], Xsrc[r0:r0 + 128, :], writes=[("xt", xi)])
                                P.act(lambda e, xt=xt, xi=xi: e.activation(out=junk[:], in_=xt[:], func=AF.Square, accum_out=ssb[xi][:, 0:1]),
                                      reads=[("xt", xi)], writes=["junk", ("ss", xi)])
                                P.act(lambda e, xi=xi: e.activation(out=ssb[xi][:, 0:1], in_=ssb[xi][:, 0:1], func=AF.Sqrt,
                                                                    scale=1.0 / D, bias=epsT[:, 0:1]),
                                      reads=[("ss", xi), "epsT"], writes=[("ss", xi)])
                                P.dve(lambda e, xi=xi: e.reciprocal(out=rsb[xi][:, 0:1], in_=ssb[xi][:, 0:1]),
                                      reads=[("ss", xi)], writes=[("rs", xi)])
                                ni = xn_rot.next()
                                xn = xnb[ni]
                                P.dve(lambda e, xn=xn, xt=xt, xi=xi: e.tensor_scalar(out=xn[:], in0=xt[:], scalar1=rsb[xi][:, 0:1], scalar2=None, op0=ALU.mult),
                                      reads=[("xt", xi), ("rs", xi)], writes=[("xn", ni)])
                                for c in range(8):
                                    pj = pbs[c // 2]
                                    off = (c % 2) * 256 + sj * 128
                                    P.pe(lambda e, xn=xn, c=c, pj=pj, off=off: e.transpose(out=bank(pj)[:, off:off + 128], in_=xn[:, c * 128:(c + 1) * 128], identity=ident[:]),
                                         reads=[("xn", ni), "ident"], writes=[("ps", pj)], signal=(c == 7))
                            for c in range(8):
                                pj = pbs[c // 2]
                                off = (c % 2) * 256
                                P.act(lambda e, c=c, pj=pj, off=off, pair=pair, hb=hb: e.activation(
                                    out=hnT[hb][:, c, pair * 256:(pair + 1) * 256], in_=bank(pj)[:, off:off + 256], func=AF.Identity,
                                    scale=FM(l, A1, c), bias=FM(l, B1, c)),
                                    reads=[("ps", pj), fr], writes=[("hnT", hb)])

                    def win_mm(i, oc):
                        hb = i % 2
                        pj = ps_rot.next()
                        for kc in range(8):
                            P.pe(lambda e, kc=kc, pj=pj, oc=oc, hb=hb: e.matmul(bank(pj), lhsT=winb[:, kc, oc * 128:(oc + 1) * 128], rhs=hnT[hb][:, kc, :],
                                                                                 start=(kc == 0), stop=(kc == 7)),
                                 reads=["winb", ("hnT", hb)], writes=[("ps", pj)], signal=(kc == 7))
                        return pj

                    def p1_back(i):
                        nrow, L, stride, ik = geom(i)
                        W = nrow * stride
                        if i == 2:
                            for g in range(4):
                                P.dve(lambda e, g=g: e.memset(ppad[g][:], 0.0), writes=[("ppad", g)])
                        for g in range(4):
                            pj = win_mm(i, g)
                            dst = ppad[g][:, 0:W].rearrange("p (r t) -> p r t", t=stride)[:, :, 8:8 + L]
                            P.act(lambda e, dst=dst, pj=pj, L=L: e.activation(out=dst, in_=bank(pj).rearrange("p (r t) -> p r t", t=L), func=AF.Copy),
                                  reads=[("ps", pj)], writes=[("ppad", g)])
                        for g, w in enumerate((2, 4, 8, 16)):
                            src, sres = ppad[g], ("ppad", g)
                            bufs = [(sA[g], ("sA", g)), (sB[g], ("sB", g))]
                            dstb, dres = bufs[0]
                            P.pool(lambda e, dstb=dstb, src=src, W=W: e.tensor_tensor(out=dstb[:, 1:W], in0=src[:, 0:W - 1], in1=src[:, 1:W], op=ALU.add),
                                   reads=[sres], writes=[dres])
                            cur, cres = dstb, dres
                            sh = 1
                            nb = 1
                            ww = 2
                            while ww < w:
                                dstb, dres = bufs[nb % 2]
                                P.pool(lambda e, dstb=dstb, cur=cur, sh=sh, W=W: e.tensor_tensor(out=dstb[:, sh:W - sh], in0=cur[:, 0:W - 2 * sh], in1=cur[:, 2 * sh:W], op=ALU.add),
                                       reads=[cres], writes=[dres])
                                cur, cres = dstb, dres
                                nb += 1
                                sh *= 2
                                ww *= 2
                            sw = cur[:, 0:W].rearrange("p (r t) -> p r t", t=stride)[:, :, 8:8 + L]
                            pin = src[:, 0:W].rearrange("p (r t) -> p r t", t=stride)[:, :, 8:8 + L]
                            iv = icnt[ik][:, g, :].rearrange("p (r t) -> p r t", t=L)
                            pt = ptmp[:].rearrange("p (r t) -> p r t", t=L)
                            P.dve(lambda e, pt=pt, sw=sw, iv=iv: e.tensor_tensor(out=pt, in0=sw, in1=iv, op=ALU.mult),
                                  reads=[cres, ("icnt", ik)], writes=["ptmp"])
                            po = pooled[:, g, :].rearrange("p (r t) -> p r t", t=L)
                            P.dve(lambda e, po=po, pt=pt, pin=pin: e.tensor_tensor(out=po, in0=pt, in1=pin, op=ALU.subtract),
                                  reads=["ptmp", sres], writes=[("pooled", g)])
                        for c in range(8):
                            pj = win_mm(i, 4 + c)
                            vi_ = v_rot.next()
                            P.act(lambda e, vi_=vi_, pj=pj: e.activation(out=vst[vi_][:], in_=bank(pj), func=AF.Copy),
                                  reads=[("ps", pj)], writes=[("vst", vi_)])
                            rows = V[c * 128:(c + 1) * 128, :]
                            if i < 2:
                                a = (2 * i) * 260 + 2
                                dst = rows[:, a:a + 520].rearrange("p (s w) -> p s w", w=260)[:, :, 0:256]
                                srcv = vst[vi_][:].rearrange("p (s w) -> p s w", w=256)
                            else:
                                a = 1042 + (i - 2) * TS
                                dst = rows[:, a:a + TS]
                                srcv = vst[vi_][:]
                            P.dma(dst, srcv, reads=[("vst", vi_)], writes=[("V", i, c)], q="actq")
                        for c in range(8):
                            pj = win_mm(i, 20 + c)
                            gi = g_rot.next()
                            P.act(lambda e, gi=gi, pj=pj: e.activation(out=gst_[gi][:], in_=bank(pj), func=AF.Sigmoid),
                                  reads=[("ps", pj)], writes=[("gst", gi)])
                            P.dma(G[c * 128:(c + 1) * 128, i * TS:(i + 1) * TS], gst_[gi][:], reads=[("gst", gi)], writes=[("G", i, c)], q="actq")
                        for g in range(4):
                            pj = ps_rot.next()
                            P.pe(lambda e, g=g, pj=pj: e.matmul(bank(pj), lhsT=pwb[:, g, :], rhs=pooled[:, g, :], start=True, stop=True),
                                 reads=["pwb", ("pooled", g)], writes=[("ps", pj)])
                            P.act(lambda e, g=g, pj=pj: e.activation(out=ypool[:, g, :], in_=bank(pj), func=AF.Identity, scale=f[:, 128 + g:129 + g]),
                                  reads=[("ps", pj), fr], writes=[("ypool", g)])
                        for k in range(8):
                            pj = win_mm(i, 12 + k)
                            gi = gp_rot.next()
                            P.act(lambda e, gi=gi, pj=pj: e.activation(out=gpb[gi][:], in_=bank(pj), func=AF.Sigmoid),
                                  reads=[("ps", pj)], writes=[("gp", gi)])
                            pj2 = ps_rot.next()
                            for kc in range(4):
                                P.pe(lambda e, kc=kc, k=k, pj2=pj2: e.matmul(bank(pj2), lhsT=wbpb[:, kc, k * 128:(k + 1) * 128], rhs=ypool[:, kc, :],
                                                                              start=(kc == 0), stop=(kc == 3)),
                                     reads=["wbpb"] + [("ypool", g) for g in range(4)], writes=[("ps", pj2)], signal=(kc == 3))
                            mi = mp_rot.next()
                            P.dve(lambda e, mi=mi, pj2=pj2, gi=gi: e.tensor_tensor(out=mpst[mi][:], in0=bank(pj2), in1=gpb[gi][:], op=ALU.mult),
                                  reads=[("ps", pj2), ("gp", gi)], writes=[("mpst", mi)])
                            P.dma(MP[k * 128:(k + 1) * 128, i * TS:(i + 1) * TS], mpst[mi][:], reads=[("mpst", mi)], writes=[("MP", i, k)], q="poolq")

                    p1_front(0)
                    for i in range(NTILE):
                        if i + 1 < NTILE:
                            p1_front(i + 1)
                        p1_back(i)
                    P.barrier()
                if phase_end("p1", l):
                    return True

                lst = ExitStack()
                gw = T(lst, "gw", [128, 4, 8, 128], BF16)
                P.pool(lambda e: e.memset(gw[:], 0.0), writes=["gw"])
                for gi_, wsrc in enumerate((lru_wr, lru_wi)):
                    for d in range(2):
                        for hl in range(2):
                            src = wsrc[l, d].rearrange("(c two) dd ee -> two dd c ee", two=2)[hl]
                            P.dma(gw[hl * 64:(hl + 1) * 64, gi_ * 2 + d, :, hl * 64:(hl + 1) * 64], src, writes=["gw"], q="poolq")

                NG = 4

                def mk_sets(st, n=NG):
                    return [(T(st, "vcb%d" % j, [128, TS], BF16), T(st, "rs_%d" % j, [128, TS]), T(st, "is_%d" % j, [128, TS]),
                             T(st, "sq_%d" % j, [128, TS])) for j in range(n)]

                def lru_group(sets, d, cs, vc_aps, res_ins, nseq, inits_list, out_hs, res_outs, reverse):
                    n = len(cs)
                    br = 10 + 3 * d
                    for j in range(n):
                        P.act(lambda e, j=j: e.activation(out=sets[j][0][:], in_=vc_aps[j], func=AF.Copy),
                              reads=[res_ins[j]], writes=[("vcb", j)])
                    prs, pis = {}, {}
                    for h0 in range(0, n, 4):
                        js = list(range(h0, min(h0 + 4, n)))
                        for j in js:
                            pr = ps_rot.next()
                            prs[j] = pr
                            P.pe(lambda e, j=j, pr=pr: e.matmul(bank(pr), lhsT=gw[:, 0 * 2 + d, cs[j], :], rhs=sets[j][0][:], start=True, stop=True),
                                 reads=["gw", ("vcb", j)], writes=[("ps", pr)])
                        for j in js:
                            pi = ps_rot.next()
                            pis[j] = pi
                            P.pe(lambda e, j=j, pi=pi: e.matmul(bank(pi), lhsT=gw[:, 1 * 2 + d, cs[j], :], rhs=sets[j][0][:], start=True, stop=True),
                                 reads=["gw", ("vcb", j)], writes=[("ps", pi)])
                        for j in js:
                            P.act(lambda e, j=j: e.activation(out=sets[j][1][:], in_=bank(prs[j]), func=AF.Sigmoid, bias=FM(l, br, cs[j])),
                                  reads=[("ps", prs[j]), fr], writes=[("rs_", j)])
                        for j in js:
                            P.act(lambda e, j=j: e.activation(out=sets[j][2][:], in_=bank(pis[j]), func=AF.Sigmoid, bias=FM(l, br + 1, cs[j])),
                                  reads=[("ps", pis[j]), fr], writes=[("is_", j)])
                    for j in range(n):
                        P.act(lambda e, j=j: e.activation(out=sets[j][3][:], in_=sets[j][1][:], func=AF.Exp, scale=FM(l, 21 + d, cs[j])),
                              reads=[("rs_", j), fr], writes=[("sq_", j)])
                    for j in range(n):
                        P.act(lambda e, j=j: e.activation(out=sets[j][1][:], in_=sets[j][1][:], func=AF.Exp, scale=FM(l, 19 + d, cs[j])),
                              reads=[("rs_", j), fr], writes=[("rs_", j)])
                    for j in range(n):
                        P.act(lambda e, j=j: e.activation(out=sets[j][3][:], in_=sets[j][3][:], func=AF.Sqrt, scale=-1.0, bias=1.0),
                              reads=[("sq_", j)], writes=[("sq_", j)])
                    for j in range(n):
                        P.dve(lambda e, j=j: e.tensor_tensor(out=sets[j][2][:], in0=sets[j][3][:], in1=sets[j][2][:], op=ALU.mult),
                              reads=[("sq_", j), ("is_", j)], writes=[("is_", j)])
                    for j in range(n):
                        P.dve(lambda e, j=j: e.tensor_tensor(out=sets[j][2][:], in0=sets[j][2][:], in1=vc_aps[j], op=ALU.mult),
                              reads=[("is_", j), res_ins[j]], writes=[("is_", j)])
                    L = TS // nseq
                    for j in range(n):
                        a_, u_, out_h = sets[j][1], sets[j][2], out_hs[j]
                        for s in range(nseq):
                            if reverse:
                                sl = slice((s + 1) * L - 1, (s * L - 1 if s > 0 else None), -1)
                            else:
                                sl = slice(s * L, (s + 1) * L)
                            init = inits_list[j][s]
                            rd = [("rs_", j), ("is_", j)] + ([] if isinstance(init, float) else ["hcar"])
                            P.dve(lambda e, out_h=out_h, a_=a_, u_=u_, sl=sl, init=init: e.tensor_tensor_scan(
                                out=out_h[:, sl], data0=a_[:, sl], data1=u_[:, sl], initial=init, op0=ALU.mult, op1=ALU.add),
                                reads=rd, writes=[res_outs[j]])

                with ExitStack() as st:
                    vtb = [T(st, "vt%d" % i, [128, 8, 520]) for i in range(3)]
                    vcs = [T(st, "vcs%d" % i, [128, TS]) for i in range(16)]
                    hfs = [T(st, "hfs%d" % i, [128, TS]) for i in range(12)]
                    sets = mk_sets(st, 8)
                    vc_rot, hf_rot = Rot(range(16)), Rot(range(12))

                    def p2_load(i):
                        vt = vtb[i % 3]
                        if i < 2:
                            a = (2 * i) * 260
                            P.dma(vt[:], fmview(V[:, a:a + 520]), reads=[("V", i, c) for c in range(8)], writes=[("vt", i % 3)])
                        else:
                            a = 1040 + (i - 2) * TS
                            rd = [("V", ii, c) for ii in (i - 1, i, i + 1) if 2 <= ii < NTILE for c in range(8)]
                            P.dma(vt[:, :, 0:515], fmview(V[:, a:a + 515]), reads=rd, writes=[("vt", i % 3)])

                    tile_state = {}

                    def p2_conv(i):
                        vt = vtb[i % 3]
                        cs = list(range(8))
                        cis = [vc_rot.next() for _ in cs]
                        vos, tapss = [], []
                        for j, c in enumerate(cs):
                            vc = vcs[cis[j]]
                            if i < 2:
                                vin = vt[:, c, :].rearrange("p (s w) -> p s w", w=260)
                                vos.append(vc[:].rearrange("p (s w) -> p s w", w=256))
                                tapss.append([vin[:, :, k:k + 256] for k in range(4)])
                            else:
                                vos.append(vc[:])
                                tapss.append([vt[:, c, k:k + TS] for k in range(4)])
                        for j, c in enumerate(cs):
                            P.dve(lambda e, j=j, c=c: e.tensor_scalar(out=vos[j], in0=tapss[j][0], scalar1=FM(l, 5, c), scalar2=FM(l, 9, c),
                                                                     op0=ALU.mult, op1=ALU.add),
                                  reads=[("vt", i % 3), fr], writes=[("vcs", cis[j])])
                        for k in range(1, 4):
                            for j, c in enumerate(cs):
                                P.dve(lambda e, j=j, c=c, k=k: e.scalar_tensor_tensor(out=vos[j], in0=tapss[j][k], scalar=FM(l, 5 + k, c), in1=vos[j],
                                                                                     op0=ALU.mult, op1=ALU.add),
                                      reads=[("vt", i % 3), fr, ("vcs", cis[j])], writes=[("vcs", cis[j])])
                        for j, c in enumerate(cs):
                            P.dma(VC[c * 128:(c + 1) * 128, i * TS:(i + 1) * TS], vcs[cis[j]][:], reads=[("vcs", cis[j])], writes=[("VC", i, c)], q="poolq")
                        tile_state[i] = cis

                    def p2_lru(i):
                        cis = tile_state.pop(i)
                        cs = list(range(8))
                        nseq = 2 if i < 2 else 1
                        his = [hf_rot.next() for _ in cs]
                        inits_list = []
                        for c in cs:
                            if i < 2:
                                inits_list.append([0.0, 0.0])
                            elif i == 2:
                                inits_list.append([h0fm[:, (l * 2 + 0) * 8 + c:(l * 2 + 0) * 8 + c + 1]])
                            else:
                                inits_list.append([hcar[:, c:c + 1]])
                        lru_group(sets, 0, cs, [vcs[ci][:] for ci in cis], [("vcs", ci) for ci in cis], nseq, inits_list,
                                  [hfs[hi] for hi in his], [("hfs", hi) for hi in his], False)
                        for j, c in enumerate(cs):
                            hf = hfs[his[j]]
                            if i >= 2:
                                P.dve(lambda e, hf=hf, c=c: e.tensor_copy(out=hcar[:, c:c + 1], in_=hf[:, TS - 1:TS]),
                                      reads=[("hfs", his[j])], writes=["hcar"])
                            else:
                                for s in range(2):
                                    req = 2 * i + s
                                    col = ((l * 2 + 0) * 4 + req) * 8 + c
                                    P.dve(lambda e, hf=hf, s=s, col=col: e.tensor_copy(out=nsfm[:, col:col + 1], in_=hf[:, (s + 1) * 256 - 1:(s + 1) * 256]),
                                          reads=[("hfs", his[j])], writes=["nsfm"])
                            P.dma(HF[c * 128:(c + 1) * 128, i * TS:(i + 1) * TS], hf[:], reads=[("hfs", his[j])], writes=[("HF", i, c)], q="poolq")

                    p2_load(0)
                    p2_load(1)
                    p2_conv(0)
                    for i in range(NTILE):
                        if i + 2 < NTILE:
                            p2_load(i + 2)
                        if i + 1 < NTILE:
                            p2_conv(i + 1)
                        p2_lru(i)
                    P.barrier()
                if phase_end("p2", l):
                    lst.close()
                    return True

                def topk_gen(src, vals, idxs, niter, tag, srcres):
                    P.last_w[tag + "src"] = P.last_w.get(srcres)
                    for it in range(niter):
                        sl = slice(it * 8, it * 8 + 8)
                        P.dve(lambda e, sl=sl: e.max(out=vals[:, sl], in_=src), reads=[tag + "src"], writes=[tag + "v"])
                        P.dve(lambda e, sl=sl: e.max_index(out=idxs[:, sl], in_max=vals[:, sl], in_values=src), reads=[tag + "src", tag + "v"], writes=[tag + "i"])
                        if it + 1 < niter:
                            P.dve(lambda e, sl=sl: e.match_replace(out=src, in_to_replace=vals[:, sl], in_values=src, imm_value=-1.0),
                                  reads=[tag + "src", tag + "v"], writes=[tag + "src"])
                        yield

                if True:
                    def load_bc(vi):
                        P.dma(g1bc[:], MOD[l, vi, 2 * D:3 * D].partition_broadcast(128), writes=["g1bc"])
                        P.dma(a2bc[:], MOD[l, vi, 4 * D:5 * D].partition_broadcast(128), writes=["a2bc"])
                        P.dma(b2bc[:], MOD[l, vi, 3 * D:4 * D].partition_broadcast(128), writes=["b2bc"])
                        P.dma(g2n[:], norm2_g[l, :].partition_broadcast(128), writes=["g2n"])
                        P.dve(lambda e: e.scalar_tensor_tensor(out=a2bc[:], in0=a2bc[:], scalar=1.0, in1=g2n[:], op0=ALU.add, op1=ALU.mult),
                              reads=["a2bc", "g2n"], writes=["a2bc"])

                    def p3_lru(i, groups=(0,), gsz=8):
                        nseq = 2 if i < 2 else 1
                        cols = slice(i * TS, (i + 1) * TS)
                        ylru = ylru2[i % 2]
                        for g0 in groups:
                            cs = list(range(g0, g0 + gsz))
                            vis = [vcl_rot.next() for _ in cs]
                            his = [hfl_rot.next() for _ in cs]
                            bis = [hb_rot.next() for _ in cs]
                            for j, c in enumerate(cs):
                                rows = slice(c * 128, (c + 1) * 128)
                                P.dma(vcl[vis[j]][:], VC[rows, cols], reads=[("VC", i, c)], writes=[("vcl", vis[j])])
                            for j, c in enumerate(cs):
                                rows = slice(c * 128, (c + 1) * 128)
                                P.dma(hfl[his[j]][:], HF[rows, cols], reads=[("HF", i, c)], writes=[("hfl", his[j])])
                            inits_list = []
                            for c in cs:
                                if i < 2:
                                    inits_list.append([0.0, 0.0])
                                elif i == NTILE - 1:
                                    inits_list.append([h0fm[:, (l * 2 + 1) * 8 + c:(l * 2 + 1) * 8 + c + 1]])
                                else:
                                    inits_list.append([hcar[:, c:c + 1]])
                            lru_group(sets, 1, cs, [vcl[v][:] for v in vis], [("vcl", v) for v in vis], nseq, inits_list,
                                      [hbs[b] for b in bis], [("hbs", b) for b in bis], True)
                            for j, c in enumerate(cs):
                                hb = hbs[bis[j]]
                                if i >= 2:
                                    P.dve(lambda e, hb=hb, c=c: e.tensor_copy(out=hcar[:, c:c + 1], in_=hb[:, 0:1]),
                                          reads=[("hbs", bis[j])], writes=["hcar"])
                                else:
                                    for s in range(2):
                                        req = 2 * i + s
                                        col = ((l * 2 + 1) * 4 + req) * 8 + c
                                        P.dve(lambda e, hb=hb, s=s, col=col: e.tensor_copy(out=nsfm[:, col:col + 1], in_=hb[:, s * 256:s * 256 + 1]),
                                              reads=[("hbs", bis[j])], writes=["nsfm"])
                            for j, c in enumerate(cs):
                                P.pool(lambda e, j=j, c=c: e.tensor_tensor(out=ylru[:, c, :], in0=hbs[bis[j]][:], in1=hfl[his[j]][:], op=ALU.add),
                                       reads=[("hbs", bis[j]), ("hfl", his[j])], writes=[("ylru", i % 2, c)])

                    def p3_merge(i):
                        cols = slice(i * TS, (i + 1) * TS)
                        ylru = ylru2[i % 2]
                        merged = mergedb[i % 2]
                        for oc in range(8):
                            gi_, mi_ = gl_rot.next(), mpl_rot.next()
                            rows = slice(oc * 128, (oc + 1) * 128)
                            P.dma(gl_[gi_][:], G[rows, cols], reads=[("G", i, oc)], writes=[("gl", gi_)])
                            P.dma(mpl[mi_][:], MP[rows, cols], reads=[("MP", i, oc)], writes=[("mpl", mi_)])
                            pj = ps_rot.next()
                            for kc in range(8):
                                P.pe(lambda e, kc=kc, oc=oc, pj=pj: e.matmul(bank(pj), lhsT=wblb[:, kc, oc * 128:(oc + 1) * 128], rhs=ylru[:, kc, :],
                                                                              start=(kc == 0), stop=(kc == 7)),
                                     reads=["wblb"] + [("ylru", i % 2, k) for k in range(8)], writes=[("ps", pj)], signal=(kc == 7))
                            ti = mt_rot.next()
                            P.dve(lambda e, pj=pj, gi_=gi_, ti=ti: e.tensor_tensor(out=mtmp[ti][:], in0=bank(pj), in1=gl_[gi_][:], op=ALU.mult),
                                  reads=[("ps", pj), ("gl", gi_)], writes=[("mtmp", ti)])
                            P.pool(lambda e, oc=oc, mi_=mi_, ti=ti: e.tensor_tensor(out=merged[:, oc, :], in0=mtmp[ti][:], in1=mpl[mi_][:], op=ALU.add),
                                   reads=[("mtmp", ti), ("mpl", mi_)], writes=[("merged", i % 2, oc)])
                        P.dma(fmview(MG[:, cols]), merged[:], reads=[("merged", i % 2, oc) for oc in range(8)], writes=[("MG", i)], q="poolq")

                    def p3_wout(i, ss):
                        K_ = list(range(len(ss)))
                        r0s = [i * TS + s * 128 for s in ss]
                        merged = mgl[i % 2]
                        for k in K_:
                            P.dma(xlb[k][:], Xsrc[r0s[k]:r0s[k] + 128, :], writes=[("xl", k)])
                        for k in K_:
                            s = ss[k]
                            for half in range(2):
                                pj = ps_rot.next()
                                hs = slice(half * 512, (half + 1) * 512)
                                for kc in range(8):
                                    P.pe(lambda e, kc=kc, pj=pj, s=s, hs=hs: e.matmul(bank(pj), lhsT=merged[:, kc, s * 128:(s + 1) * 128], rhs=woutb[:, kc, hs],
                                                                                       start=(kc == 0), stop=(kc == 7)),
                                         reads=["woutb", ("mgl", i % 2)], writes=[("ps", pj)], signal=(kc == 7))
                                P.dve(lambda e, pj=pj, hs=hs, k=k: e.tensor_tensor(out=hn2[k][:, hs], in0=bank(pj), in1=g1bc[:, hs], op=ALU.mult),
                                      reads=[("ps", pj), "g1bc"], writes=[("hn2", k)])
                                P.pool(lambda e, hs=hs, k=k: e.tensor_tensor(out=xlb[k][:, hs], in0=xlb[k][:, hs], in1=hn2[k][:, hs], op=ALU.add),
                                       reads=[("hn2", k), ("xl", k)], writes=[("xl", k)])
                        for k in K_:
                            P.dma(X[r0s[k]:r0s[k] + 128, :], xlb[k][:], reads=[("xl", k)], writes=[("X", i, ss[k])], q="poolq")
                        for k in K_:
                            P.act(lambda e, k=k: e.activation(out=junk[:], in_=xlb[k][:], func=AF.Square, accum_out=sm[k][:, 0:1]),
                                  reads=[("xl", k)], writes=[("sm0", k)])
                        for k in K_:
                            P.act(lambda e, k=k: e.activation(out=sm[k][:, 0:1], in_=sm[k][:, 0:1], func=AF.Sqrt, scale=1.0 / D, bias=epsT[:, 0:1]),
                                  reads=[("sm0", k), "epsT"], writes=[("sm0", k)])
                        for k in K_:
                            P.dve(lambda e, k=k: e.reciprocal(out=sm[k][:, 1:2], in_=sm[k][:, 0:1]), reads=[("sm0", k)], writes=[("sm1", k)])
                        for k in K_:
                            P.dve(lambda e, k=k: e.scalar_tensor_tensor(out=hn2[k][:], in0=xlb[k][:], scalar=sm[k][:, 1:2], in1=a2bc[:], op0=ALU.mult, op1=ALU.mult),
                                  reads=[("xl", k), ("sm1", k), "a2bc"], writes=[("hn2", k)])
                        for k in K_:
                            P.pool(lambda e, k=k: e.tensor_tensor(out=hn2[k][:], in0=hn2[k][:], in1=b2bc[:], op=ALU.add),
                                   reads=[("hn2", k), "b2bc"], writes=[("hn2", k)])
                        for k in K_:
                            P.act(lambda e, k=k: e.activation(out=hn2b[k][:], in_=hn2[k][:], func=AF.Copy),
                                  reads=[("hn2", k)], writes=[("hn2b", k)])
                            P.dma(HN2[r0s[k]:r0s[k] + 128, :], hn2b[k][:], reads=[("hn2b", k)], writes=[("HN2", i, ss[k])], q="actq")
                        for k in K_:
                            for h2 in range(2):
                                pj = ps_rot.next()
                                for cc in range(4):
                                    c = h2 * 4 + cc
                                    P.pe(lambda e, c=c, cc=cc, pj=pj, k=k: e.transpose(out=bank(pj)[:, cc * 128:(cc + 1) * 128], in_=hn2[k][:, c * 128:(c + 1) * 128], identity=ident[:]),
                                         reads=[("hn2", k), "ident"], writes=[("ps", pj)], signal=(cc == 3))
                                P.act(lambda e, pj=pj, h2=h2, k=k: e.activation(out=hn2T[k][:, h2 * 4:(h2 + 1) * 4, :], in_=bank(pj).rearrange("p (c t) -> p c t", t=128), func=AF.Copy),
                                      reads=[("ps", pj)], writes=[("hn2T", k, h2)])
                        pjs = []
                        for k in K_:
                            pj = ps_rot.next()
                            pjs.append(pj)
                            for kc in range(8):
                                P.pe(lambda e, kc=kc, pj=pj, k=k: e.matmul(bank(pj)[:, 0:NEXP], lhsT=hn2T[k][:, kc, :], rhs=rwt[:, kc, :], start=(kc == 0), stop=(kc == 7)),
                                     reads=["rwt", ("hn2T", k, 0), ("hn2T", k, 1)], writes=[("ps", pj)], signal=(kc == 7))
                        for k in K_:
                            P.dve(lambda e, k=k: e.reduce_max(out=sm[k][:, 2:3], in_=bank(pjs[k])[:, 0:NEXP], axis=AX.X), reads=[("ps", pjs[k])], writes=[("sm2", k)])
                        for k in K_:
                            P.dve(lambda e, k=k: e.tensor_scalar(out=sm[k][:, 3:4], in0=sm[k][:, 2:3], scalar1=-1.0, scalar2=None, op0=ALU.mult), reads=[("sm2", k)], writes=[("sm3", k)])
                        for k in K_:
                            P.act(lambda e, k=k: e.activation(out=ex[k][:], in_=bank(pjs[k])[:, 0:NEXP], func=AF.Exp, bias=sm[k][:, 3:4], accum_out=sm[k][:, 4:5]),
                                  reads=[("ps", pjs[k]), ("sm3", k)], writes=[("ex", k), ("sm4", k)])
                        for k in K_:
                            P.dve(lambda e, k=k: e.reciprocal(out=sm[k][:, 5:6], in_=sm[k][:, 4:5]), reads=[("sm4", k)], writes=[("sm5", k)])
                        for k in K_:
                            P.dve(lambda e, k=k: e.tensor_scalar(out=affpad[k][:, 0:NEXP], in0=ex[k][:], scalar1=sm[k][:, 5:6], scalar2=None, op0=ALU.mult),
                                  reads=[("ex", k), ("sm5", k)], writes=[("affpad", k)])
                        pts = []
                        for k in K_:
                            pj = ps_rot.next()
                            pts.append(pj)
                            P.pe(lambda e, pj=pj, k=k: e.transpose(out=bank(pj)[:, 0:128], in_=affpad[k][:], identity=ident[:]),
                                 reads=[("affpad", k), "ident"], writes=[("ps", pj)])
                        for k in K_:
                            s = ss[k]
                            pj = pts[k]
                            if i < 2:
                                req = 2 * i + s // 2
                                tc0 = (s % 2) * 128
                                P.act(lambda e, pj=pj, k=k: e.activation(out=afst[k][0:NEXP, :], in_=bank(pj)[0:NEXP, 0:128], func=AF.Copy),
                                      reads=[("ps", pj)], writes=[("afst", k)])
                                P.dma(affP[NEXP * req:NEXP * req + NEXP, tc0:tc0 + 128], afst[k][0:NEXP, :], reads=[("afst", k)], writes=["affP"], q="actq")
                            else:
                                tc0 = (i - 2) * TS + s * 128
                                P.act(lambda e, pj=pj, tc0=tc0: e.activation(out=affS[0:NEXP, tc0:tc0 + 128], in_=bank(pj)[0:NEXP, 0:128], func=AF.Copy),
                                      reads=[("ps", pj)], writes=["affS"])

                order = list(range(NTILE - 1, -1, -1))
                NW = 4
                with ExitStack() as st:
                    wblb = T(st, "wblb", [128, 8, D], BF16)
                    for j in range(2):
                        P.dma(wblb[:, :, j * 512:(j + 1) * 512], fmview(w_br_lru[l][:, j * 512:(j + 1) * 512]), writes=["wblb"], q="poolq")
                    vcl = [T(st, "vcl%d" % i, [128, TS]) for i in range(16)]
                    hfl = [T(st, "hfl%d" % i, [128, TS]) for i in range(12)]
                    gl_ = [T(st, "gl%d" % i, [128, TS], BF16) for i in range(3)]
                    mpl = [T(st, "mpl%d" % i, [128, TS]) for i in range(3)]
                    hbs = [T(st, "hbs%d" % i, [128, TS]) for i in range(8)]
                    sets = mk_sets(st, 8)
                    ylru2 = [T(st, "ylru%d" % i, [128, 8, TS], BF16) for i in range(2)]
                    mergedb = [T(st, "merged%d" % i, [128, 8, TS], BF16) for i in range(2)]
                    mtmp = [T(st, "mtmp%d" % i, [128, TS]) for i in range(2)]
                    vcl_rot, hfl_rot, gl_rot, mpl_rot, hb_rot, mt_rot = Rot(range(16)), Rot(range(12)), Rot(range(3)), Rot(range(3)), Rot(range(8)), Rot(range(2))
                    p3_lru(order[0])
                    for n_, i in enumerate(order):
                        nx = order[n_ + 1] if n_ + 1 < len(order) else None
                        if nx is not None:
                            p3_lru(nx)
                        p3_merge(i)
                    P.barrier()
                if phase_end("p3a", l):
                    lst.close()
                    return True
                affS = T(lst, "affS", [16, 4096])
                affP = T(lst, "affP", [64, 256])
                P.dve(lambda e: e.memset(affP[:], 0.0), writes=["affP"])
                vS = T(lst, "vS", [16, 512])
                iS = T(lst, "iS", [16, 512], U32)
                with ExitStack() as st:
                    woutb = T(st, "woutb", [128, 8, D], BF16)
                    rwt = T(st, "rwt", [128, 8, NEXP])
                    for j in range(2):
                        P.dma(woutb[:, :, j * 512:(j + 1) * 512], fmview(w_out[l][:, j * 512:(j + 1) * 512]), writes=["woutb"], q="poolq")
                    P.dma(rwt[:], fmview(router_w[l]), writes=["rwt"])
                    g1bc = T(st, "g1bc", [128, D])
                    a2bc = T(st, "a2bc", [128, D])
                    b2bc = T(st, "b2bc", [128, D])
                    g2n = T(st, "g2n", [128, D])
                    mgl = [T(st, "mgl%d" % i, [128, 8, TS], BF16) for i in range(2)]
                    xlb = [T(st, "xl%d" % i, [128, D]) for i in range(NW)]
                    hn2 = [T(st, "hn2_%d" % i, [128, D]) for i in range(NW)]
                    hn2b = [T(st, "hn2b%d" % i, [128, D], BF16) for i in range(NW)]
                    hn2T = [T(st, "hn2T%d" % i, [128, 8, 128]) for i in range(NW)]
                    junk = T(st, "junk3", [128, D], BF16)
                    sm = [T(st, "sm%d" % i, [128, 8]) for i in range(NW)]
                    ex = [T(st, "ex%d" % i, [128, NEXP]) for i in range(NW)]
                    affpad = [T(st, "affpad%d" % i, [128, 128]) for i in range(NW)]
                    afst = [T(st, "afst%d" % i, [NEXP, 128]) for i in range(NW)]
                    for k in range(NW):
                        P.dve(lambda e, k=k: e.memset(affpad[k][:], 0.0), writes=[("affpad", k)])

                    def mg_load(i):
                        P.dma(mgl[i % 2][:], fmview(MG[:, i * TS:(i + 1) * TS]), reads=[("MG", i)], writes=[("mgl", i % 2)])

                    load_bc(1)
                    mg_load(order[0])
                    for n_, i in enumerate(order):
                        nx = order[n_ + 1] if n_ + 1 < len(order) else None
                        if nx is not None:
                            mg_load(nx)
                        if i == 1:
                            load_bc(0)
                        p3_wout(i, [0, 1, 2, 3])
                        if i == 2:
                            sgen = topk_gen(affS[:], vS, iS, 64, "S", "affS")
                            P.inject = sgen
                    P.inject = None
                    P.barrier()
                if phase_end("p3", l):
                    lst.close()
                    return True

                idxS = T(lst, "idxS", [128, NEXP, 4], I32)
                gatS = T(lst, "gatS", [128, NEXP, 4])
                idxP = T(lst, "idxP", [128, NEXP], I32)
                gatP = T(lst, "gatP", [128, NEXP])
                with ExitStack() as st:
                    fS = T(st, "fS", [16, 512])
                    iS2 = T(st, "iS2", [16, 512], I32)
                    wkP = T(st, "wkP", [64, 256])
                    vP = T(st, "vP", [64, 32])
                    iP = T(st, "iP", [64, 32], U32)
                    fP = T(st, "fP", [64, 32])
                    iP2 = T(st, "iP2", [64, 32], I32)
                    offi = T(st, "offi", [4, NEXP], I32)
                    offf = T(st, "offf", [4, NEXP])
                    offPf = T(st, "offPf", [64, 1])
                    P.pool(lambda e: e.iota(offi[:], pattern=[[0, NEXP]], base=0, channel_multiplier=256), writes=["offi"])
                    P.dve(lambda e: e.tensor_copy(out=offf[:], in_=offi[:]), reads=["offi"], writes=["offf"])
                    P.dma(OFFS.rearrange("(r e) -> r e", e=NEXP), offf[:], reads=["offf"], writes=["OFFS"])
                    P.dma(offPf[:], OFFS.rearrange("(p o) -> p o", o=1), reads=["OFFS"], writes=["offPf"])

                    def topk(src, wk, vals, idxs, niter, tag):
                        cur = src
                        cres = tag + "src"
                        for it in range(niter):
                            sl = slice(it * 8, it * 8 + 8)
                            P.dve(lambda e, cur=cur, sl=sl: e.max(out=vals[:, sl], in_=cur), reads=[cres], writes=[tag + "v"])
                            P.dve(lambda e, cur=cur, sl=sl: e.max_index(out=idxs[:, sl], in_max=vals[:, sl], in_values=cur), reads=[cres, tag + "v"], writes=[tag + "i"])
                            if it + 1 < niter:
                                P.dve(lambda e, cur=cur, sl=sl: e.match_replace(out=wk, in_to_replace=vals[:, sl], in_values=cur, imm_value=-1.0),
                                      reads=[cres, tag + "v"], writes=[tag + "wk"])
                                cur = wk
                                cres = tag + "wk"

                    P.last_w["Psrc"] = P.last_w.get("affP")
                    topk(affP[:], wkP[:], vP, iP, 4, "P")
                    P.dve(lambda e: e.tensor_copy(out=fP[:], in_=iP[:]), reads=["Pi"], writes=["fP"])
                    P.dve(lambda e: e.tensor_scalar(out=fP[:], in0=fP[:], scalar1=offPf[:, 0:1], scalar2=None, op0=ALU.add), reads=["fP", "offPf"], writes=["fP"])
                    P.dve(lambda e: e.tensor_copy(out=iP2[:], in_=fP[:]), reads=["fP"], writes=["iP2"])
                    P.dma(PIDX, iP2[:], reads=["iP2"], writes=["PIDX"])
                    P.dma(PGATE, vP[:], reads=["Pv"], writes=["PGATE"])
                    with nc.allow_non_contiguous_dma(reason="tiny index relayout"):
                        for r in range(4):
                            P.dma(idxP[32 * r:32 * r + 32, :], PIDX[NEXP * r:NEXP * r + NEXP, :].rearrange("e k -> k e"), reads=["PIDX"], writes=["idxP"])
                            P.dma(gatP[32 * r:32 * r + 32, :], PGATE[NEXP * r:NEXP * r + NEXP, :].rearrange("e k -> k e"), reads=["PGATE"], writes=["gatP"])
                    for _ in sgen:
                        pass
                    P.dve(lambda e: e.tensor_copy(out=fS[:], in_=iS[:]), reads=["Si"], writes=["fS"])
                    P.dve(lambda e: e.tensor_scalar(out=fS[:], in0=fS[:], scalar1=1024.0, scalar2=None, op0=ALU.add), reads=["fS"], writes=["fS"])
                    P.dve(lambda e: e.tensor_copy(out=iS2[:], in_=fS[:]), reads=["fS"], writes=["iS2"])
                    P.dma(SIDX, iS2[:], reads=["iS2"], writes=["SIDX"])
                    P.dma(SGATE, vS[:], reads=["Sv"], writes=["SGATE"])
                    with nc.allow_non_contiguous_dma(reason="tiny index relayout"):
                        P.dma(idxS[:], SIDX.rearrange("e (g p) -> p e g", p=128), reads=["SIDX"], writes=["idxS"])
                        P.dma(gatS[:], SGATE.rearrange("e (g p) -> p e g", p=128), reads=["SGATE"], writes=["gatS"])
                    P.barrier()
                if phase_end("route", l):
                    lst.close()
                    return True

                with ExitStack() as st:
                    g2bc = [T(st, "g2bc%d" % v, [128, D]) for v in range(2)]
                    for v in range(2):
                        P.dma(g2bc[v][:], MOD[l, v, 5 * D:6 * D].partition_broadcast(128), writes=[("g2bc", v)])
                    xg = [T(st, "xg%d" % g, [128, D], BF16) for g in range(5)]
                    xsT = T(st, "xsT", [128, 8, 640], BF16)
                    w1q = [T(st, "w1q%d" % i, [128, 8, 512], BF16) for i in range(2)]
                    w3q = [T(st, "w3q%d" % i, [128, 8, 512], BF16) for i in range(2)]
                    w2h = [T(st, "w2h%d" % i, [128, 16, 512], BF16) for i in range(2)]
                    hid = T(st, "hid", [128, 16, 640], BF16)
                    s1 = [T(st, "s1_%d" % i, [128, 640]) for i in range(2)]
                    osb = [T(st, "osb%d" % g, [128, D]) for g in range(5)]
                    wq_rot, w2_rot, s1_rot, hp_rot = Rot(range(2)), Rot(range(2)), Rot(range(2)), Rot(range(2))

                    def moe_load1(e_, q):
                        wi = wq_rot.next()
                        P.dma(w1q[wi][:], fmview(exp_w1[l, e_][:, q * 512:(q + 1) * 512]), writes=[("w1q", wi)], q="poolq")
                        P.dma(w3q[wi][:], fmview(exp_w3[l, e_][:, q * 512:(q + 1) * 512]), writes=[("w3q", wi)], q="poolq")
                        return wi

                    def moe_load2(e_, half):
                        wi = w2_rot.next()
                        P.dma(w2h[wi][:], exp_w2[l, e_][:, half * 512:(half + 1) * 512].rearrange("(c p) d -> p c d", p=128), writes=[("w2h", wi)], q="poolq")
                        return wi

                    def moe_gather(e_):
                        for g in range(5):
                            ia = idxS[:, e_, g:g + 1] if g < 4 else idxP[:, e_:e_ + 1]
                            ir = "idxS" if g < 4 else "idxP"
                            P.op("pool", lambda e, g=g, ia=ia: e.indirect_dma_start(out=xg[g][:], out_offset=None, in_=HN2,
                                                                                     in_offset=bass.IndirectOffsetOnAxis(ap=ia, axis=0)),
                                 reads=[ir], writes=[("xg", g)], dmaq="poolq")

                    def moe_xpose(e_):
                        for g in range(5):
                            pj = ps_rot.next()
                            pbb = bank(pj).bitcast(BF16)
                            for c in range(8):
                                P.pe(lambda e, g=g, c=c, pbb=pbb: e.transpose(out=pbb[:, c * 128:(c + 1) * 128], in_=xg[g][:, c * 128:(c + 1) * 128], identity=identb[:]),
                                     reads=[("xg", g), "identb"], writes=[("ps", pj)], signal=(c == 7))
                            P.act(lambda e, g=g, pbb=pbb: e.activation(out=xsT[:, :, g * 128:(g + 1) * 128], in_=pbb.rearrange("p (c t) -> p c t", t=128), func=AF.Copy),
                                  reads=[("ps", pj)], writes=["xsT"])

                    moe_gather(0)
                    w1i = moe_load1(0, 0)
                    moe_xpose(0)
                    for e_ in range(NEXP):
                        w2is = [None, None]
                        for q in range(4):
                            nxt = moe_load1(e_, q + 1) if q < 3 else None
                            if q == 1:
                                w2is[0] = moe_load2(e_, 0)
                            if q == 2:
                                w2is[1] = moe_load2(e_, 1)
                                if e_ + 1 < NEXP:
                                    moe_gather(e_ + 1)
                            for fc in range(4):
                                fcg = q * 4 + fc
                                hk = hp_rot.next()
                                H1, H3 = PS[2 * hk], PS[2 * hk + 1]
                                for (Hh, wt, wr) in ((H1, w1q[w1i], ("w1q", w1i)), (H3, w3q[w1i], ("w3q", w1i))):
                                    pidx = 2 * hk if Hh is H1 else 2 * hk + 1
                                    for kc in range(8):
                                        P.pe(lambda e, Hh=Hh, wt=wt, kc=kc, fc=fc: e.matmul(Hh[:, 0:512], lhsT=wt[:, kc, fc * 128:(fc + 1) * 128], rhs=xsT[:, kc, 0:512],
                                                                                             start=(kc == 0), stop=(kc == 7)),
                                             reads=[wr, "xsT"], writes=[("ps", 2 * pidx)], signal=False)
                                        P.pe(lambda e, Hh=Hh, wt=wt, kc=kc, fc=fc: e.matmul(Hh[:, 512:640], lhsT=wt[:, kc, fc * 128:(fc + 1) * 128], rhs=xsT[:, kc, 512:640],
                                                                                             start=(kc == 0), stop=(kc == 7)),
                                             reads=[wr, "xsT"], writes=[("ps", 2 * pidx + 1)], signal=(kc == 7))
                                si = s1_rot.next()
                                r1 = [("ps", 4 * hk), ("ps", 4 * hk + 1)]
                                r3 = [("ps", 4 * hk + 2), ("ps", 4 * hk + 3)]
                                P.act(lambda e, H1=H1, si=si: e.activation(out=s1[si][:], in_=H1[:, 0:640], func=AF.Silu),
                                      reads=r1, writes=[("s1", si)])
                                P.dve(lambda e, H3=H3, si=si, fcg=fcg: e.tensor_tensor(out=hid[:, fcg, :], in0=H3[:, 0:640], in1=s1[si][:], op=ALU.mult),
                                      reads=r3 + [("s1", si)], writes=[("hid", fcg)])
                            w1i = nxt
                        if e_ + 1 < NEXP:
                            w1i = moe_load1(e_ + 1, 0)
                        for half in range(2):
                            w2i = w2is[half]
                            hs = slice(half * 512, (half + 1) * 512)
                            for g in range(5):
                                pj = ps_rot.next()
                                for fcg in range(16):
                                    P.pe(lambda e, pj=pj, fcg=fcg, g=g, w2i=w2i: e.matmul(bank(pj), lhsT=hid[:, fcg, g * 128:(g + 1) * 128], rhs=w2h[w2i][:, fcg, :],
                                                                                           start=(fcg == 0), stop=(fcg == 15)),
                                         reads=[("w2h", w2i)] + [("hid", k) for k in range(16)], writes=[("ps", pj)], signal=(fcg == 15))
                                ga = gatS[:, e_, g:g + 1] if g < 4 else gatP[:, e_:e_ + 1]
                                gr = "gatS" if g < 4 else "gatP"
                                v_ = 1 if g < 4 else 0
                                P.dve(lambda e, pj=pj, g=g, ga=ga, hs=hs, v_=v_: e.scalar_tensor_tensor(out=osb[g][:, hs], in0=bank(pj), scalar=ga, in1=g2bc[v_][:, hs],
                                                                                                        op0=ALU.mult, op1=ALU.mult),
                                      reads=[("ps", pj), gr, ("g2bc", v_)], writes=[("osb", g)])
                            if half == 0 and e_ + 1 < NEXP:
                                moe_xpose(e_ + 1)
                        for g in range(5):
                            ia = idxS[:, e_, g:g + 1] if g < 4 else idxP[:, e_:e_ + 1]
                            ir = "idxS" if g < 4 else "idxP"
                            P.op("pool", lambda e, g=g, ia=ia: e.indirect_dma_start(out=X, out_offset=bass.IndirectOffsetOnAxis(ap=ia, axis=0),
                                                                                     in_=osb[g][:], in_offset=None, compute_op=ALU.add),
                                 reads=[ir, ("osb", g)], writes=["Xall"], dmaq="poolq")
                    P.barrier()
                if phase_end("moe", l):
                    lst.close()
                    return True
                lst.close()

        def run_final():
            with ExitStack() as st:
                fgbc = T(st, "fgbc", [128, D])
                P.dma(fgbc[:], final_g.partition_broadcast(128), writes=["fgbc"])
                xf = [T(st, "xf%d" % i, [128, D]) for i in range(3)]
                yf = [T(st, "yf%d" % i, [128, D]) for i in range(3)]
                junk = T(st, "junkf", [128, D], BF16)
                sf = [T(st, "sf%d" % i, [128, 2]) for i in range(3)]
                for t in range(NTOK // 128):
                    bi = t % 3
                    P.dma(xf[bi][:], X[t * 128:(t + 1) * 128, :], writes=[("xf", bi)])
                    P.act(lambda e, bi=bi: e.activation(out=junk[:], in_=xf[bi][:], func=AF.Square, accum_out=sf[bi][:, 0:1]),
                          reads=[("xf", bi)], writes=["junkf", ("sf", bi)])
                    P.act(lambda e, bi=bi: e.activation(out=sf[bi][:, 0:1], in_=sf[bi][:, 0:1], func=AF.Sqrt, scale=1.0 / D, bias=epsT[:, 0:1]),
                          reads=[("sf", bi), "epsT"], writes=[("sf", bi)])
                    P.dve(lambda e, bi=bi: e.reciprocal(out=sf[bi][:, 1:2], in_=sf[bi][:, 0:1]), reads=[("sf", bi)], writes=[("sf1", bi)])
                    P.dve(lambda e, bi=bi: e.scalar_tensor_tensor(out=yf[bi][:], in0=xf[bi][:], scalar=sf[bi][:, 1:2], in1=fgbc[:], op0=ALU.mult, op1=ALU.mult),
                          reads=[("xf", bi), ("sf1", bi), "fgbc"], writes=[("yf", bi)])
                    P.dma(y_out[t * 128:(t + 1) * 128, :], yf[bi][:], reads=[("yf", bi)], writes=[("y", t)], q="poolq")
                pj = ps_rot.next()
                P.pe(lambda e: e.transpose(out=bank(pj)[:, 0:128], in_=nsfm[:], identity=ident[:]), reads=["nsfm", "ident"], writes=[("ps", pj)])
                nsr = T(st, "nsr", [128, 128])
                P.dve(lambda e: e.tensor_copy(out=nsr[:], in_=bank(pj)[:, 0:128]), reads=[("ps", pj)], writes=["nsr"])
                for l in range(2):
                    for d in range(2):
                        for r in range(4):
                            r0 = ((l * 2 + d) * 4 + r) * 8
                            P.dma(ns_out[r, l, d, :].rearrange("(c p) -> c p", p=128), nsr[r0:r0 + 8, :], reads=["nsr"], writes=[("ns", l, d, r)])
                P.barrier()
        if not run_setup():
            if not phase_end("fm", 0):
                run_layers()
        P.barrier()
        run_final()
    nc._prog_stats = (dict(P.cnt), dict(P.dma_i), P.nops)
    return nc


_NC_CACHE = {}


def kernel(x_prompt, x_sample, state_lru, c, c_ctx, norm1_g, norm2_g, final_g, w_mod, b_mod, w_in,
           pool_w, pool_scale, conv_w, conv_b, lru_wr, lru_br, lru_wi, lru_bi, lru_lambda,
           w_br_pool, w_br_lru, w_out, router_w, exp_w1, exp_w3, exp_w2):
    f = lambda a: np.ascontiguousarray(np.asarray(a, dtype=np.float32))
    if "nc" not in _NC_CACHE:
        _NC_CACHE["nc"] = build_nc()
    nc = _NC_CACHE["nc"]
    shared = dict(norm1_g=f(norm1_g), norm2_g=f(norm2_g), final_g=f(final_g), w_mod=f(w_mod), b_mod=f(b_mod),
                  w_in=f(w_in), pool_w=f(pool_w), pool_scale=f(pool_scale), conv_w=f(conv_w), conv_b=f(conv_b),
                  lru_wr=f(lru_wr), lru_br=f(lru_br), lru_wi=f(lru_wi), lru_bi=f(lru_bi), lru_lambda=f(lru_lambda),
                  w_br_pool=f(w_br_pool), w_br_lru=f(w_br_lru), w_out=f(w_out), router_w=f(router_w),
                  exp_w1=f(exp_w1), exp_w3=f(exp_w3), exp_w2=f(exp_w2))
    xp, xs, sl, cc, cx = f(x_prompt), f(x_sample), f(state_lru), f(c), f(c_ctx)
    in_maps = []
    for k in range(8):
        m = dict(shared)
        m["x_in"] = np.concatenate([xp[4 * k:4 * k + 4].reshape(1024, D), xs[k]], axis=0)
        m["cvec"] = np.stack([cx, cc[k]], axis=0)
        m["h0s"] = np.ascontiguousarray(sl[k])
        in_maps.append(m)
    res = run_bass_kernel_spmd(nc, in_maps, core_ids=list(range(8)))
    y_prompt = np.zeros((32, 256, D), np.float32)
    y_sample = np.zeros((8, 4096, D), np.float32)
    ns = np.zeros((32, 2, 2, D), np.float32)
    for k in range(8):
        r = res.results[k]
        y_prompt[4 * k:4 * k + 4] = r["y"][0:1024].reshape(4, 256, D)
        y_sample[k] = r["y"][1024:]
        ns[4 * k:4 * k + 4] = r["ns"]
    return (y_prompt, y_sample, ns)
```

```python
from contextlib import ExitStack
import numpy as np
import concourse.bass as bass
import concourse.mybir as mybir
from concourse.bass_utils import run_bass_kernel_spmd

F32 = mybir.dt.float32
BF16 = mybir.dt.bfloat16
U32 = mybir.dt.uint32
I32 = mybir.dt.int32
AF = mybir.ActivationFunctionType
ALU = mybir.AluOpType
AX = mybir.AxisListType

COMPUTE = ("pe", "act", "dve", "pool")
NDMASEM = {"sp": 24, "actq": 12, "poolq": 24}


class Prog:
    def __init__(self, nc):
        self.nc = nc
        self.sems = {}
        self.cnt = {k: 0 for k in COMPUTE}
        self.dma_i = {k: 0 for k in NDMASEM}
        self.waited = {}
        self.last_w = {}
        self.readers = {}
        self.nops = 0
        self.inject = None
        self.inject_every = 1
        self._inj_n = 0
        self._in_inject = False

    def alloc(self, stack):
        nc = self.nc
        for k in COMPUTE:
            self.sems[k] = stack.enter_context(nc.semaphore("s_" + k))
        for q, n in NDMASEM.items():
            for i in range(n):
                self.sems[(q, i)] = stack.enter_context(nc.semaphore("d_%s_%d" % (q, i)))

    def _need(self, stream, ev, waits):
        if ev is None:
            return
        key, val = ev
        if key == stream and val > self.cnt[key]:
            return
        if self.waited.get((stream, key), 0) >= val:
            return
        if val > waits.get(key, 0):
            waits[key] = val

    def op(self, eng, fn, reads=(), writes=(), signal=True, dmaq=None):
        stream = eng
        waits = {}
        for r in reads:
            self._need(stream, self.last_w.get(r), waits)
        for w in writes:
            self._need(stream, self.last_w.get(w), waits)
            for ev in self.readers.get(w, ()):
                self._need(stream, ev, waits)
        if dmaq is not None:
            n = NDMASEM[dmaq]
            i = self.dma_i[dmaq]
            self.dma_i[dmaq] = i + 1
            key = (dmaq, i % n)
            val = 16 * (i // n + 1)
            if i >= n:
                self._need(stream, (key, val - 16), waits)
            ev = (key, val)
            inc = (key, 16)
        else:
            if signal:
                self.cnt[eng] += 1
                ev = (eng, self.cnt[eng])
                inc = (eng, 1)
            else:
                ev = (eng, self.cnt[eng] + 1)
                inc = None
        for key, val in waits.items():
            self.waited[(stream, key)] = max(self.waited.get((stream, key), 0), val)
        self._emit(stream, fn, list(waits.items()), inc)
        for r in reads:
            self.readers.setdefault(r, []).append(ev)
        for w in writes:
            self.last_w[w] = ev
            self.readers[w] = []
        self.nops += 1
        if eng == "dve" and self.inject is not None and not self._in_inject:
            self._inj_n += 1
            if self._inj_n % self.inject_every == 0:
                self._in_inject = True
                try:
                    next(self.inject)
                except StopIteration:
                    self.inject = None
                self._in_inject = False
        return ev

    def pe(self, fn, reads=(), writes=(), signal=True):
        return self.op("pe", fn, reads, writes, signal)

    def act(self, fn, reads=(), writes=()):
        return self.op("act", fn, reads, writes)

    def dve(self, fn, reads=(), writes=()):
        return self.op("dve", fn, reads, writes)

    def pool(self, fn, reads=(), writes=()):
        return self.op("pool", fn, reads, writes)

    def dma(self, out, in_, reads=(), writes=(), q="sp", **kw):
        stream = {"sp": "sp", "actq": "act", "poolq": "pool"}[q]
        return self.op(stream, lambda e: e.dma_start(out=out, in_=in_, **kw), reads, writes, dmaq=q)

    def _emit(self, stream, fn, waits, inc):
        nc = self.nc
        e = {"pe": nc.tensor, "act": nc.scalar, "dve": nc.vector, "pool": nc.gpsimd, "sp": nc.sync}[stream]
        for key, val in waits:
            e.wait_ge(self.sems[key], val)
        if fn is None:
            return
        ins = fn(e)
        if inc is not None:
            ins.then_inc(self.sems[inc[0]], inc[1])

    def barrier(self):
        evs = [(k, self.cnt[k]) for k in COMPUTE if self.cnt[k] > 0]
        for q, n in NDMASEM.items():
            i = self.dma_i[q]
            for j in range(max(0, i - n), i):
                evs.append(((q, j % n), 16 * (j // n + 1)))
        for stream in ("pe", "act", "dve", "pool", "sp"):
            waits = {}
            for ev in evs:
                self._need(stream, ev, waits)
            for key, val in waits.items():
                self.waited[(stream, key)] = max(self.waited.get((stream, key), 0), val)
            self._emit(stream, None, list(waits.items()), None)


class Rot:
    def __init__(self, items):
        self.items = list(items)
        self.i = 0

    def next(self):
        it = self.items[self.i % len(self.items)]
        self.i += 1
        return it


D = 1024
NTOK = 5120
NTILE = 10
TS = 512
VW = 5140
EPS = 1e-6
NEXP = 16
FF = 2048
DEPTH = 2


def build_nc(dbg=False, nlayers=DEPTH, stop_after=None):
    nc = bass.Bass("TRN2", target_bir_lowering=False)

    def din(name, shape, dt=F32):
        return nc.dram_tensor(name, list(shape), dt, kind="ExternalInput").ap()

    def dint(name, shape, dt=F32):
        return nc.dram_tensor(name, list(shape), dt, kind=("ExternalOutput" if dbg else "Internal")).ap()

    x_in = din("x_in", [NTOK, D])
    cvec = din("cvec", [2, D])
    h0s = din("h0s", [2, 2, D])
    norm1_g = din("norm1_g", [2, D])
    norm2_g = din("norm2_g", [2, D])
    final_g = din("final_g", [D])
    w_mod = din("w_mod", [2, D, 6 * D])
    b_mod = din("b_mod", [2, 6 * D])
    w_in = din("w_in", [2, D, 3584])
    pool_w = din("pool_w", [2, 4, 128, 128])
    pool_scale = din("pool_scale", [2, 512])
    conv_w = din("conv_w", [2, 4, D])
    conv_b = din("conv_b", [2, D])
    lru_wr = din("lru_wr", [2, 2, 16, 64, 64])
    lru_br = din("lru_br", [2, 2, D])
    lru_wi = din("lru_wi", [2, 2, 16, 64, 64])
    lru_bi = din("lru_bi", [2, 2, D])
    lru_lambda = din("lru_lambda", [2, 2, D])
    w_br_pool = din("w_br_pool", [2, 512, D])
    w_br_lru = din("w_br_lru", [2, D, D])
    w_out = din("w_out", [2, D, D])
    router_w = din("router_w", [2, D, NEXP])
    _need_exp = stop_after is None or tuple(stop_after)[0] == "moe" or tuple(stop_after)[1] > 0
    exp_w1 = din("exp_w1", [2, NEXP, D, FF]) if _need_exp else None
    exp_w3 = din("exp_w3", [2, NEXP, D, FF]) if _need_exp else None
    exp_w2 = din("exp_w2", [2, NEXP, FF, D]) if _need_exp else None

    y_out = nc.dram_tensor("y", [NTOK, D], F32, kind="ExternalOutput").ap()
    ns_out = nc.dram_tensor("ns", [4, 2, 2, D], F32, kind="ExternalOutput").ap()

    X = dint("Xs", [NTOK, D])
    HN2 = dint("HN2s", [NTOK, D], BF16)
    V = dint("Vs", [D, VW])
    VC = dint("VCs", [D, NTOK])
    HF = dint("HFs", [D, NTOK])
    MP = dint("MPs", [D, NTOK])
    G = dint("Gs", [D, NTOK], BF16)
    MG = dint("MGs", [D, NTOK], BF16)
    MOD = dint("MODs", [2, 2, 6 * D])
    SIDX = dint("SIDXs", [16, 512], I32)
    SGATE = dint("SGATEs", [16, 512])
    PIDX = dint("PIDXs", [64, 32], I32)
    PGATE = dint("PGATEs", [64, 32])
    OFFS = dint("OFFSs", [64])

    with ExitStack() as gst:
        P = Prog(nc)
        P.alloc(gst)
        PS = [gst.enter_context(nc.psum_tensor("PS%d" % k, [128, 1024], F32)) for k in range(4)]

        def bank(j):
            return PS[j // 2][:, (j % 2) * 512:(j % 2) * 512 + 512]

        _tn = [0]

        def T(st, name, shape, dt=F32):
            _tn[0] += 1
            return st.enter_context(nc.sbuf_tensor("%s_%d" % (name, _tn[0]), list(shape), dt))

        def fmview(ap2d):
            return ap2d.rearrange("(c p) w -> p c w", p=128)

        class _Stop(Exception):
            pass

        def phase_end(name, lyr):
            return stop_after is not None and tuple(stop_after) == (name, lyr)

        ident = T(gst, "ident", [128, 128])
        identb = T(gst, "identb", [128, 128], BF16)
        iot = T(gst, "iot", [128, 128], I32)
        epsT = T(gst, "epsT", [128, 1])
        zt = T(gst, "zt", [128, 8, 2])
        rowsb = [T(gst, "rows%d" % i, [128, 128]) for i in range(2)]
        rows_rot = Rot([0, 1])
        fm = [T(gst, "fm%d" % l, [128, 192]) for l in range(2)]
        h0fm = T(gst, "h0fm", [128, 32])
        nsfm = T(gst, "nsfm", [128, 128])
        hcar = T(gst, "hcar", [128, 8])

        P.pool(lambda e: e.iota(iot[:], pattern=[[1, 128]], base=0, channel_multiplier=-1), writes=["iot"])
        P.dve(lambda e: e.tensor_scalar(out=ident[:], in0=iot[:], scalar1=0.0, scalar2=None, op0=ALU.is_equal),
              reads=["iot"], writes=["ident"])
        P.dve(lambda e: e.tensor_copy(out=identb[:], in_=ident[:]), reads=["ident"], writes=["identb"])
        P.dve(lambda e: e.memset(epsT[:], EPS), writes=["epsT"])
        P.dve(lambda e: e.memset(zt[:], 0.0), writes=["zt"])
        P.dve(lambda e: e.memset(nsfm[:], 0.0), writes=["nsfm"])

        pads = []
        for q in range(4):
            pads += [q * 260, q * 260 + 258]
        pads += [1040, 5138]
        for a in pads:
            P.dma(fmview(V[:, a:a + 2]), zt[:], reads=["zt"], writes=[("Vpad", a)])

        ps_rot = Rot(range(8))

        def load_fm(dst, col0, vec_aps, rows_per=8):
            nv = len(vec_aps)
            nr = nv * rows_per
            ri = rows_rot.next()
            rb = rowsb[ri]
            for v, ap in enumerate(vec_aps):
                P.dma(rb[v * rows_per:(v + 1) * rows_per, :], ap.rearrange("(c p) -> c p", p=128),
                      writes=[("rows", ri)])
            j = ps_rot.next()
            pb = bank(j)
            P.pe(lambda e: e.transpose(out=pb[:, 0:nr], in_=rb[0:nr, :], identity=ident[0:nr, 0:nr]),
                 reads=[("rows", ri), "ident"], writes=[("ps", j)])
            P.dve(lambda e: e.tensor_copy(out=dst[:, col0:col0 + nr], in_=pb[:, 0:nr]),
                  reads=[("ps", j)], writes=[("fmc", id(dst))])

        def FM(l, blk, c):
            return fm[l][:, blk * 8 + c: blk * 8 + c + 1]

        def run_setup():
            if phase_end("setup", 0):
                return True
            with ExitStack() as st:
                cfm = T(st, "cfm", [128, 16])
                csil = T(st, "csil", [128, 16], BF16)
                bm = T(st, "bm", [2, 6 * D])
                modrow = T(st, "modrow", [2, 6 * D])
                wmb = [T(st, "wm%d" % i, [128, 8, 512], BF16) for i in range(3)]
                wm_rot = Rot(range(3))
                load_fm(cfm, 0, [cvec[0, :], cvec[1, :]])
                P.act(lambda e: e.activation(out=csil[:], in_=cfm[:], func=AF.Silu),
                      reads=[("fmc", id(cfm))], writes=["csil"])
                for l in range(DEPTH):
                    for r in range(2):
                        P.dma(bm[r:r + 1, :], b_mod[l:l + 1, :], writes=["bm"])
                    for j in range(12):
                        wi = wm_rot.next()
                        wm = wmb[wi]
                        P.dma(wm[:], fmview(w_mod[l][:, j * 512:(j + 1) * 512]), writes=[("wm", wi)], q="poolq")
                        pj = ps_rot.next()
                        pb = bank(pj)
                        for c in range(8):
                            P.pe(lambda e, c=c, pb=pb, wm=wm: e.matmul(pb[0:2, :], lhsT=csil[:, c:16:8], rhs=wm[:, c, :],
                                                                      start=(c == 0), stop=(c == 7)),
                                 reads=["csil", ("wm", wi)], writes=[("ps", pj)], signal=(c == 7))
                        P.dve(lambda e, j=j, pb=pb: e.tensor_tensor(out=modrow[0:2, j * 512:(j + 1) * 512], in0=pb[0:2, :],
                                                                     in1=bm[0:2, j * 512:(j + 1) * 512], op=ALU.add),
                              reads=[("ps", pj), "bm"], writes=["modrow"])
                    P.dma(MOD[l], modrow[:], reads=["modrow"], writes=[("MOD", l)])
                P.barrier()
            if phase_end("mod", 0):
                return True


            with ExitStack() as st:
                tmpf = T(st, "tmpf", [128, 16])
                for l in range(DEPTH):
                    vecs = [norm1_g[l, :], MOD[l, 0, D:2 * D], MOD[l, 0, 0:D], MOD[l, 1, D:2 * D], MOD[l, 1, 0:D],
                            conv_w[l, 0, :], conv_w[l, 1, :], conv_w[l, 2, :], conv_w[l, 3, :], conv_b[l, :],
                            lru_br[l, 0, :], lru_bi[l, 0, :], lru_lambda[l, 0, :],
                            lru_br[l, 1, :], lru_bi[l, 1, :], lru_lambda[l, 1, :]]
                    load_fm(fm[l], 0, vecs)
                    load_fm(fm[l], 128, [pool_scale[l, :]], rows_per=4)
                    fr = ("fmc", id(fm[l]))
                    f = fm[l]
                    for vi, (sc, a1) in enumerate([(1, 17), (3, 18)]):
                        P.dve(lambda e, sc=sc, a1=a1, f=f: e.scalar_tensor_tensor(
                            out=f[:, a1 * 8:a1 * 8 + 8], in0=f[:, sc * 8:sc * 8 + 8], scalar=1.0, in1=f[:, 0:8],
                            op0=ALU.add, op1=ALU.mult), reads=[fr], writes=[fr])
                    for d, lam in enumerate([12, 15]):
                        P.act(lambda e, lam=lam, f=f, d=d: e.activation(out=tmpf[:, d * 8:d * 8 + 8], in_=f[:, lam * 8:lam * 8 + 8],
                                                                        func=AF.Exp, scale=-1.0), reads=[fr], writes=["tmpf"])
                        P.act(lambda e, d=d: e.activation(out=tmpf[:, d * 8:d * 8 + 8], in_=tmpf[:, d * 8:d * 8 + 8],
                                                          func=AF.Ln, bias=1.0), reads=["tmpf"], writes=["tmpf"])
                        P.dve(lambda e, d=d, f=f: e.tensor_scalar(out=f[:, (19 + d) * 8:(19 + d) * 8 + 8], in0=tmpf[:, d * 8:d * 8 + 8],
                                                                  scalar1=-8.0, scalar2=None, op0=ALU.mult),
                              reads=["tmpf", fr], writes=[fr])
                        P.dve(lambda e, d=d, f=f: e.tensor_scalar(out=f[:, (21 + d) * 8:(21 + d) * 8 + 8], in0=tmpf[:, d * 8:d * 8 + 8],
                                                                  scalar1=-16.0, scalar2=None, op0=ALU.mult),
                              reads=["tmpf", fr], writes=[fr])
                load_fm(h0fm, 0, [h0s[0, 0, :], h0s[0, 1, :], h0s[1, 0, :], h0s[1, 1, :]])
                P.barrier()

        def run_layers():
          for _ in range(1):
            for l in range(nlayers):
                Xsrc = x_in if l == 0 else X
                f = fm[l]
                fr = ("fmc", id(f))

                with ExitStack() as st:
                    winb = T(st, "winb", [128, 8, 3584], BF16)
                    pwb = T(st, "pwb", [128, 4, 128], BF16)
                    wbpb = T(st, "wbpb", [128, 4, D], BF16)
                    for j in range(7):
                        P.dma(winb[:, :, j * 512:(j + 1) * 512], fmview(w_in[l][:, j * 512:(j + 1) * 512]),
                              writes=[("winb", j)], q="poolq")
                    P.dma(pwb[:], pool_w[l].rearrange("g c d -> c g d"), writes=["pwb"], q="poolq")
                    P.dma(wbpb[:], w_br_pool[l].rearrange("(c p) d -> p c d", p=128), writes=["wbpb"], q="poolq")
                    xtb = [T(st, "xt%d" % i, [128, D]) for i in range(4)]
                    xnb = [T(st, "xn%d" % i, [128, D]) for i in range(2)]
                    junk = T(st, "junk", [128, D], BF16)
                    ssb = [T(st, "ss%d" % i, [128, 1]) for i in range(4)]
                    rsb = [T(st, "rs%d" % i, [128, 1]) for i in range(4)]
                    hnT = [T(st, "hnT%d" % i, [128, 8, TS], BF16) for i in range(2)]
                    ppad = [T(st, "ppad%d" % g, [128, 640]) for g in range(4)]
                    sA = [T(st, "sA%d" % g, [128, 640]) for g in range(4)]
                    sB = [T(st, "sB%d" % g, [128, 640]) for g in range(4)]
                    icnt = [T(st, "icnt%d" % k, [128, 4, TS]) for k in range(2)]
                    pooled = T(st, "pooled", [128, 4, TS], BF16)
                    ptmp = T(st, "ptmp", [128, TS])
                    ypool = T(st, "ypool", [128, 4, TS], BF16)
                    vst = [T(st, "vst%d" % i, [128, TS]) for i in range(3)]
                    gst_ = [T(st, "gst%d" % i, [128, TS], BF16) for i in range(3)]
                    gpb = [T(st, "gp%d" % i, [128, TS]) for i in range(2)]
                    mpst = [T(st, "mpst%d" % i, [128, TS]) for i in range(3)]
                    xt_rot, xn_rot, v_rot, g_rot, gp_rot, mp_rot = Rot(range(4)), Rot(range(2)), Rot(range(3)), Rot(range(3)), Rot(range(2)), Rot(range(3))
                    for g in range(4):
                        P.dve(lambda e, g=g: e.memset(ppad[g][:], 0.0), writes=[("ppad", g)])
                        P.pool(lambda e, g=g: e.memset(sA[g][:], 0.0), writes=[("sA", g)])
                        P.pool(lambda e, g=g: e.memset(sB[g][:], 0.0), writes=[("sB", g)])
                    for k, (nrow, L) in enumerate([(8, 64), (2, 256)]):
                        for g, w in enumerate((2, 4, 8, 16)):
                            iv = icnt[k][:, g, :].rearrange("p (r t) -> p r t", t=L)
                            P.dve(lambda e, iv=iv, w=w: e.memset(iv, 1.0 / w), writes=[("icnt", k)])
                            h = w // 2
                            for t in range(h):
                                c_lo = float(min(t + h, L) - max(t - h, 0))
                                tt = L - 1 - t
                                c_hi = float(min(tt + h, L) - max(tt - h, 0))
                                P.dve(lambda e, iv=iv, t=t, c_lo=c_lo: e.memset(iv[:, :, t:t + 1], 1.0 / c_lo), writes=[("icnt", k)])
                                if c_hi != float(w):
                                    P.dve(lambda e, iv=iv, tt=tt, c_hi=c_hi: e.memset(iv[:, :, tt:tt + 1], 1.0 / c_hi), writes=[("icnt", k)])

                    def geom(i):
                        return (2, 256, 272, 1) if i < 2 else (8, 64, 80, 0)

                    def p1_front(i):
                        vi = 0 if i < 2 else 1
                        A1 = 17 + vi
                        B1 = 2 if vi == 0 else 4
                        hb = i % 2
                        for pair in range(2):
                            pbs = []
                            for _ in range(4):
                                pbs.append(ps_rot.next())
                            for sj in range(2):
                                s = pair * 2 + sj
                                xi = xt_rot.next()
                                xt = xtb[xi]
                                r0 = i * TS + s * 128
                                P.dma(xt[:], Xsrc[r0:r0 + 128, :], writes=[("xt", xi)])
                                P.act(lambda e, xt=xt, xi=xi: e.activation(out=junk[:], in_=xt[:], func=AF.Square, accum_out=ssb[xi][:, 0:1]),
                                      reads=[("xt", xi)], writes=["junk", ("ss", xi)])
                                P.act(lambda e, xi=xi: e.activation(out=ssb[xi][:, 0:1], in_=ssb[xi][:, 0:1], func=AF.Sqrt,
                                                                    scale=1.0 / D, bias=epsT[:, 0:1]),
                                      reads=[("ss", xi), "epsT"], writes=[("ss", xi)])
                                P.dve(lambda e, xi=xi: e.reciprocal(out=rsb[xi][:, 0:1], in_=ssb[xi][:, 0:1]),
                                      reads=[("ss", xi)], writes=[("rs", xi)])
                                ni = xn_rot.next()
                                xn = xnb[ni]
                                P.dve(lambda e, xn=xn, xt=xt, xi=xi: e.tensor_scalar(out=xn[:], in0=xt[:], scalar1=rsb[xi][:, 0:1], scalar2=None, op0=ALU.mult),
                                      reads=[("xt", xi), ("rs", xi)], writes=[("xn", ni)])
                                for c in range(8):
                                    pj = pbs[c // 2]
                                    off = (c % 2) * 256 + sj * 128
                                    P.pe(lambda e, xn=xn, c=c, pj=pj, off=off: e.transpose(out=bank(pj)[:, off:off + 128], in_=xn[:, c * 128:(c + 1) * 128], identity=ident[:]),
                                         reads=[("xn", ni), "ident"], writes=[("ps", pj)], signal=(c == 7))
                            for c in range(8):
                                pj = pbs[c // 2]
                                off = (c % 2) * 256
                                P.act(lambda e, c=c, pj=pj, off=off, pair=pair, hb=hb: e.activation(
                                    out=hnT[hb][:, c, pair * 256:(pair + 1) * 256], in_=bank(pj)[:, off:off + 256], func=AF.Identity,
                                    scale=FM(l, A1, c), bias=FM(l, B1, c)),
                                    reads=[("ps", pj), fr], writes=[("hnT", hb)])

                    def win_mm(i, oc):
                        hb = i % 2
                        pj = ps_rot.next()
                        for kc in range(8):
                            P.pe(lambda e, kc=kc, pj=pj, oc=oc, hb=hb: e.matmul(bank(pj), lhsT=winb[:, kc, oc * 128:(oc + 1) * 128], rhs=hnT[hb][:, kc, :],
                                                                                 start=(kc == 0), stop=(kc == 7)),
                                 reads=[("winb", oc // 4), ("hnT", hb)], writes=[("ps", pj)], signal=(kc == 7))
                        return pj

                    def p1_back(i):
                        nrow, L, stride, ik = geom(i)
                        W = nrow * stride
                        if i == 2:
                            for g in range(4):
                                P.dve(lambda e, g=g: e.memset(ppad[g][:], 0.0), writes=[("ppad", g)])
                        for g in range(4):
                            pj = win_mm(i, g)
                            dst = ppad[g][:, 0:W].rearrange("p (r t) -> p r t", t=stride)[:, :, 8:8 + L]
                            P.act(lambda e, dst=dst, pj=pj, L=L: e.activation(out=dst, in_=bank(pj).rearrange("p (r t) -> p r t", t=L), func=AF.Copy),
                                  reads=[("ps", pj)], writes=[("ppad", g)])
                        for g, w in enumerate((2, 4, 8, 16)):
                            src, sres = ppad[g], ("ppad", g)
                            bufs = [(sA[g], ("sA", g)), (sB[g], ("sB", g))]
                            dstb, dres = bufs[0]
                            P.pool(lambda e, dstb=dstb, src=src, W=W: e.tensor_tensor(out=dstb[:, 1:W], in0=src[:, 0:W - 1], in1=src[:, 1:W], op=ALU.add),
                                   reads=[sres], writes=[dres])
                            cur, cres = dstb, dres
                            sh = 1
                            nb = 1
                            ww = 2
                            while ww < w:
                                dstb, dres = bufs[nb % 2]
                                P.pool(lambda e, dstb=dstb, cur=cur, sh=sh, W=W: e.tensor_tensor(out=dstb[:, sh:W - sh], in0=cur[:, 0:W - 2 * sh], in1=cur[:, 2 * sh:W], op=ALU.add),
                                       reads=[cres], writes=[dres])
                                cur, cres = dstb, dres
                                nb += 1
                                sh *= 2
                                ww *= 2
                            sw = cur[:, 0:W].rearrange("p (r t) -> p r t", t=stride)[:, :, 8:8 + L]
                            pin = src[:, 0:W].rearrange("p (r t) -> p r t", t=stride)[:, :, 8:8 + L]
                            iv = icnt[ik][:, g, :].rearrange("p (r t) -> p r t", t=L)
                            pt = ptmp[:].rearrange("p (r t) -> p r t", t=L)
                            P.dve(lambda e, pt=pt, sw=sw, iv=iv: e.tensor_tensor(out=pt, in0=sw, in1=iv, op=ALU.mult),
                                  reads=[cres, ("icnt", ik)], writes=["ptmp"])
                            po = pooled[:, g, :].rearrange("p (r t) -> p r t", t=L)
                            P.dve(lambda e, po=po, pt=pt, pin=pin: e.tensor_tensor(out=po, in0=pt, in1=pin, op=ALU.subtract),
                                  reads=["ptmp", sres], writes=[("pooled", g)])
                        for c in range(8):
                            pj = win_mm(i, 4 + c)
                            vi_ = v_rot.next()
                            P.act(lambda e, vi_=vi_, pj=pj: e.activation(out=vst[vi_][:], in_=bank(pj), func=AF.Copy),
                                  reads=[("ps", pj)], writes=[("vst", vi_)])
                            rows = V[c * 128:(c + 1) * 128, :]
                            if i < 2:
                                a = (2 * i) * 260 + 2
                                dst = rows[:, a:a + 520].rearrange("p (s w) -> p s w", w=260)[:, :, 0:256]
                                srcv = vst[vi_][:].rearrange("p (s w) -> p s w", w=256)
                            else:
                                a = 1042 + (i - 2) * TS
                                dst = rows[:, a:a + TS]
                                srcv = vst[vi_][:]
                            P.dma(dst, srcv, reads=[("vst", vi_)], writes=[("V", i, c)], q="actq")
                        for c in range(8):
                            pj = win_mm(i, 20 + c)
                            gi = g_rot.next()
                            P.act(lambda e, gi=gi, pj=pj: e.activation(out=gst_[gi][:], in_=bank(pj), func=AF.Sigmoid),
                                  reads=[("ps", pj)], writes=[("gst", gi)])
                            P.dma(G[c * 128:(c + 1) * 128, i * TS:(i + 1) * TS], gst_[gi][:], reads=[("gst", gi)], writes=[("G", i, c)], q="actq")
                        for g in range(4):
                            pj = ps_rot.next()
                            P.pe(lambda e, g=g, pj=pj: e.matmul(bank(pj), lhsT=pwb[:, g, :], rhs=pooled[:, g, :], start=True, stop=True),
                                 reads=["pwb", ("pooled", g)], writes=[("ps", pj)])
                            P.act(lambda e, g=g, pj=pj: e.activation(out=ypool[:, g, :], in_=bank(pj), func=AF.Identity, scale=f[:, 128 + g:129 + g]),
                                  reads=[("ps", pj), fr], writes=[("ypool", g)])
                        for k in range(8):
                            pj = win_mm(i, 12 + k)
                            gi = gp_rot.next()
                            P.act(lambda e, gi=gi, pj=pj: e.activation(out=gpb[gi][:], in_=bank(pj), func=AF.Sigmoid),
                                  reads=[("ps", pj)], writes=[("gp", gi)])
                            pj2 = ps_rot.next()
                            for kc in range(4):
                                P.pe(lambda e, kc=kc, k=k, pj2=pj2: e.matmul(bank(pj2), lhsT=wbpb[:, kc, k * 128:(k + 1) * 128], rhs=ypool[:, kc, :],
                                                                              start=(kc == 0), stop=(kc == 3)),
                                     reads=["wbpb"] + [("ypool", g) for g in range(4)], writes=[("ps", pj2)], signal=(kc == 3))
                            mi = mp_rot.next()
                            P.dve(lambda e, mi=mi, pj2=pj2, gi=gi: e.tensor_tensor(out=mpst[mi][:], in0=bank(pj2), in1=gpb[gi][:], op=ALU.mult),
                                  reads=[("ps", pj2), ("gp", gi)], writes=[("mpst", mi)])
                            P.dma(MP[k * 128:(k + 1) * 128, i * TS:(i + 1) * TS], mpst[mi][:], reads=[("mpst", mi)], writes=[("MP", i, k)], q="poolq")

                    p1_front(0)
                    for i in range(NTILE):
                        if i + 1 < NTILE:
                            p1_front(i + 1)
                        p1_back(i)
                    P.barrier()
                if phase_end("p1", l):
                    return True

                lst = ExitStack()
                gw = T(lst, "gw", [128, 4, 8, 128], BF16)
                P.pool(lambda e: e.memset(gw[:], 0.0), writes=[("gw", k) for k in range(8)])
                for gi_, wsrc in enumerate((lru_wr, lru_wi)):
                    for d in range(2):
                        for hl in range(2):
                            src = wsrc[l, d].rearrange("(c two) dd ee -> two dd c ee", two=2)[hl]
                            P.dma(gw[hl * 64:(hl + 1) * 64, gi_ * 2 + d, :, hl * 64:(hl + 1) * 64], src, writes=[("gw", (gi_ * 2 + d) * 2 + hl)], q="poolq")

                NG = 4

                def mk_sets(st, n=NG):
                    return [(T(st, "vcb%d" % j, [128, TS], BF16), T(st, "rs_%d" % j, [128, TS]), T(st, "is_%d" % j, [128, TS]),
                             T(st, "sq_%d" % j, [128, TS])) for j in range(n)]

                def lru_group(sets, d, cs, vc_aps, res_ins, nseq, inits_list, out_hs, res_outs, reverse):
                    n = len(cs)
                    br = 10 + 3 * d
                    for j in range(n):
                        P.act(lambda e, j=j: e.activation(out=sets[j][0][:], in_=vc_aps[j], func=AF.Copy),
                              reads=[res_ins[j]], writes=[("vcb", j)])
                    prs, pis = {}, {}
                    for h0 in range(0, n, 4):
                        js = list(range(h0, min(h0 + 4, n)))
                        for j in js:
                            pr = ps_rot.next()
                            prs[j] = pr
                            P.pe(lambda e, j=j, pr=pr: e.matmul(bank(pr), lhsT=gw[:, 0 * 2 + d, cs[j], :], rhs=sets[j][0][:], start=True, stop=True),
                                 reads=[("gw", (0 * 2 + d) * 2), ("gw", (0 * 2 + d) * 2 + 1), ("vcb", j)], writes=[("ps", pr)])
                        for j in js:
                            pi = ps_rot.next()
                            pis[j] = pi
                            P.pe(lambda e, j=j, pi=pi: e.matmul(bank(pi), lhsT=gw[:, 1 * 2 + d, cs[j], :], rhs=sets[j][0][:], start=True, stop=True),
                                 reads=[("gw", (1 * 2 + d) * 2), ("gw", (1 * 2 + d) * 2 + 1), ("vcb", j)], writes=[("ps", pi)])
                        for j in js:
                            P.act(lambda e, j=j: e.activation(out=sets[j][1][:], in_=bank(prs[j]), func=AF.Sigmoid, bias=FM(l, br, cs[j])),
                                  reads=[("ps", prs[j]), fr], writes=[("rs_", j)])
                        for j in js:
                            P.act(lambda e, j=j: e.activation(out=sets[j][2][:], in_=bank(pis[j]), func=AF.Sigmoid, bias=FM(l, br + 1, cs[j])),
                                  reads=[("ps", pis[j]), fr], writes=[("is_", j)])
                    for j in range(n):
                        P.act(lambda e, j=j: e.activation(out=sets[j][3][:], in_=sets[j][1][:], func=AF.Exp, scale=FM(l, 21 + d, cs[j])),
                              reads=[("rs_", j), fr], writes=[("sq_", j)])
                    for j in range(n):
                        P.act(lambda e, j=j: e.activation(out=sets[j][1][:], in_=sets[j][1][:], func=AF.Exp, scale=FM(l, 19 + d, cs[j])),
                              reads=[("rs_", j), fr], writes=[("rs_", j)])
                    for j in range(n):
                        P.act(lambda e, j=j: e.activation(out=sets[j][3][:], in_=sets[j][3][:], func=AF.Sqrt, scale=-1.0, bias=1.0),
                              reads=[("sq_", j)], writes=[("sq_", j)])
                    for j in range(n):
                        P.dve(lambda e, j=j: e.tensor_tensor(out=sets[j][2][:], in0=sets[j][3][:], in1=sets[j][2][:], op=ALU.mult),
                              reads=[("sq_", j), ("is_", j)], writes=[("is_", j)])
                    for j in range(n):
                        P.dve(lambda e, j=j: e.tensor_tensor(out=sets[j][2][:], in0=sets[j][2][:], in1=vc_aps[j], op=ALU.mult),
                              reads=[("is_", j), res_ins[j]], writes=[("is_", j)])
                    L = TS // nseq
                    for j in range(n):
                        a_, u_, out_h = sets[j][1], sets[j][2], out_hs[j]
                        for s in range(nseq):
                            if reverse:
                                sl = slice((s + 1) * L - 1, (s * L - 1 if s > 0 else None), -1)
                            else:
                                sl = slice(s * L, (s + 1) * L)
                            init = inits_list[j][s]
                            rd = [("rs_", j), ("is_", j)] + ([] if isinstance(init, float) else ["hcar"])
                            P.dve(lambda e, out_h=out_h, a_=a_, u_=u_, sl=sl, init=init: e.tensor_tensor_scan(
                                out=out_h[:, sl], data0=a_[:, sl], data1=u_[:, sl], initial=init, op0=ALU.mult, op1=ALU.add),
                                reads=rd, writes=[res_outs[j]])

                with ExitStack() as st:
                    vtb = [T(st, "vt%d" % i, [128, 8, 520]) for i in range(3)]
                    vcs = [T(st, "vcs%d" % i, [128, TS]) for i in range(16)]
                    hfs = [T(st, "hfs%d" % i, [128, TS]) for i in range(12)]
                    sets = mk_sets(st, 8)
                    vc_rot, hf_rot = Rot(range(16)), Rot(range(12))

                    def p2_load(i):
                        vt = vtb[i % 3]
                        if i < 2:
                            a = (2 * i) * 260
                            P.dma(vt[:], fmview(V[:, a:a + 520]), reads=[("V", i, c) for c in range(8)], writes=[("vt", i % 3)])
                        else:
                            a = 1040 + (i - 2) * TS
                            rd = [("V", ii, c) for ii in (i - 1, i, i + 1) if 2 <= ii < NTILE for c in range(8)]
                            P.dma(vt[:, :, 0:515], fmview(V[:, a:a + 515]), reads=rd, writes=[("vt", i % 3)])

                    tile_state = {}

                    def p2_conv(i):
                        vt = vtb[i % 3]
                        cs = list(range(8))
                        cis = [vc_rot.next() for _ in cs]
                        vos, tapss = [], []
                        for j, c in enumerate(cs):
                            vc = vcs[cis[j]]
                            if i < 2:
                                vin = vt[:, c, :].rearrange("p (s w) -> p s w", w=260)
                                vos.append(vc[:].rearrange("p (s w) -> p s w", w=256))
                                tapss.append([vin[:, :, k:k + 256] for k in range(4)])
                            else:
                                vos.append(vc[:])
                                tapss.append([vt[:, c, k:k + TS] for k in range(4)])
                        for j, c in enumerate(cs):
                            P.dve(lambda e, j=j, c=c: e.tensor_scalar(out=vos[j], in0=tapss[j][0], scalar1=FM(l, 5, c), scalar2=FM(l, 9, c),
                                                                     op0=ALU.mult, op1=ALU.add),
                                  reads=[("vt", i % 3), fr], writes=[("vcs", cis[j])])
                        for k in range(1, 4):
                            for j, c in enumerate(cs):
                                P.dve(lambda e, j=j, c=c, k=k: e.scalar_tensor_tensor(out=vos[j], in0=tapss[j][k], scalar=FM(l, 5 + k, c), in1=vos[j],
                                                                                     op0=ALU.mult, op1=ALU.add),
                                      reads=[("vt", i % 3), fr, ("vcs", cis[j])], writes=[("vcs", cis[j])])
                        for j, c in enumerate(cs):
                            P.dma(VC[c * 128:(c + 1) * 128, i * TS:(i + 1) * TS], vcs[cis[j]][:], reads=[("vcs", cis[j])], writes=[("VC", i, c)], q="poolq")
                        tile_state[i] = cis

                    def p2_lru(i):
                        cis = tile_state.pop(i)
                        cs = list(range(8))
                        nseq = 2 if i < 2 else 1
                        his = [hf_rot.next() for _ in cs]
                        inits_list = []
                        for c in cs:
                            if i < 2:
                                inits_list.append([0.0, 0.0])
                            elif i == 2:
                                inits_list.append([h0fm[:, (l * 2 + 0) * 8 + c:(l * 2 + 0) * 8 + c + 1]])
                            else:
                                inits_list.append([hcar[:, c:c + 1]])
                        lru_group(sets, 0, cs, [vcs[ci][:] for ci in cis], [("vcs", ci) for ci in cis], nseq, inits_list,
                                  [hfs[hi] for hi in his], [("hfs", hi) for hi in his], False)
                        for j, c in enumerate(cs):
                            hf = hfs[his[j]]
                            if i >= 2:
                                P.dve(lambda e, hf=hf, c=c: e.tensor_copy(out=hcar[:, c:c + 1], in_=hf[:, TS - 1:TS]),
                                      reads=[("hfs", his[j])], writes=["hcar"])
                            else:
                                for s in range(2):
                                    req = 2 * i + s
                                    col = ((l * 2 + 0) * 4 + req) * 8 + c
                                    P.dve(lambda e, hf=hf, s=s, col=col: e.tensor_copy(out=nsfm[:, col:col + 1], in_=hf[:, (s + 1) * 256 - 1:(s + 1) * 256]),
                                          reads=[("hfs", his[j])], writes=["nsfm"])
                            P.dma(HF[c * 128:(c + 1) * 128, i * TS:(i + 1) * TS], hf[:], reads=[("hfs", his[j])], writes=[("HF", i, c)], q="poolq")

                    p2_load(0)
                    p2_load(1)
                    p2_conv(0)
                    for i in range(NTILE):
                        if i + 2 < NTILE:
                            p2_load(i + 2)
                        if i + 1 < NTILE:
                            p2_conv(i + 1)
                        p2_lru(i)
                    P.barrier()
                if phase_end("p2", l):
                    lst.close()
                    return True

                def topk_gen(src, vals, idxs, niter, tag, srcres):
                    P.last_w[tag + "src"] = P.last_w.get(srcres)
                    for it in range(niter):
                        sl = slice(it * 8, it * 8 + 8)
                        P.dve(lambda e, sl=sl: e.max(out=vals[:, sl], in_=src), reads=[tag + "src"], writes=[tag + "v"])
                        P.dve(lambda e, sl=sl: e.max_index(out=idxs[:, sl], in_max=vals[:, sl], in_values=src), reads=[tag + "src", tag + "v"], writes=[tag + "i"])
                        if it + 1 < niter:
                            P.dve(lambda e, sl=sl: e.match_replace(out=src, in_to_replace=vals[:, sl], in_values=src, imm_value=-1.0),
                                  reads=[tag + "src", tag + "v"], writes=[tag + "src"])
                        yield

                if True:
                    def load_bc(vi):
                        P.dma(g1bc[:], MOD[l, vi, 2 * D:3 * D].partition_broadcast(128), writes=["g1bc"])
                        P.dma(a2bc[:], MOD[l, vi, 4 * D:5 * D].partition_broadcast(128), writes=["a2bc"])
                        P.dma(b2bc[:], MOD[l, vi, 3 * D:4 * D].partition_broadcast(128), writes=["b2bc"])
                        P.dma(g2n[:], norm2_g[l, :].partition_broadcast(128), writes=["g2n"])
                        P.dve(lambda e: e.scalar_tensor_tensor(out=a2bc[:], in0=a2bc[:], scalar=1.0, in1=g2n[:], op0=ALU.add, op1=ALU.mult),
                              reads=["a2bc", "g2n"], writes=["a2bc"])

                    def p3_lru(i, groups=(0,), gsz=8):
                        nseq = 2 if i < 2 else 1
                        cols = slice(i * TS, (i + 1) * TS)
                        ylru = ylru2[i % 2]
                        for g0 in groups:
                            cs = list(range(g0, g0 + gsz))
                            vis = [vcl_rot.next() for _ in cs]
                            his = [hfl_rot.next() for _ in cs]
                            bis = [hb_rot.next() for _ in cs]
                            for j, c in enumerate(cs):
                                rows = slice(c * 128, (c + 1) * 128)
                                P.dma(vcl[vis[j]][:], VC[rows, cols], reads=[("VC", i, c)], writes=[("vcl", vis[j])])
                            for j, c in enumerate(cs):
                                rows = slice(c * 128, (c + 1) * 128)
                                P.dma(hfl[his[j]][:], HF[rows, cols], reads=[("HF", i, c)], writes=[("hfl", his[j])])
                            inits_list = []
                            for c in cs:
                                if i < 2:
                                    inits_list.append([0.0, 0.0])
                                elif i == NTILE - 1:
                                    inits_list.append([h0fm[:, (l * 2 + 1) * 8 + c:(l * 2 + 1) * 8 + c + 1]])
                                else:
                                    inits_list.append([hcar[:, c:c + 1]])
                            lru_group(sets, 1, cs, [vcl[v][:] for v in vis], [("vcl", v) for v in vis], nseq, inits_list,
                                      [hbs[b] for b in bis], [("hbs", b) for b in bis], True)
                            for j, c in enumerate(cs):
                                hb = hbs[bis[j]]
                                if i >= 2:
                                    P.dve(lambda e, hb=hb, c=c: e.tensor_copy(out=hcar[:, c:c + 1], in_=hb[:, 0:1]),
                                          reads=[("hbs", bis[j])], writes=["hcar"])
                                else:
                                    for s in range(2):
                                        req = 2 * i + s
                                        col = ((l * 2 + 1) * 4 + req) * 8 + c
                                        P.dve(lambda e, hb=hb, s=s, col=col: e.tensor_copy(out=nsfm[:, col:col + 1], in_=hb[:, s * 256:s * 256 + 1]),
                                              reads=[("hbs", bis[j])], writes=["nsfm"])
                            for j, c in enumerate(cs):
                                P.pool(lambda e, j=j, c=c: e.tensor_tensor(out=ylru[:, c, :], in0=hbs[bis[j]][:], in1=hfl[his[j]][:], op=ALU.add),
                                       reads=[("hbs", bis[j]), ("hfl", his[j])], writes=[("ylru", i % 2, c)])

                    def p3_merge(i):
                        cols = slice(i * TS, (i + 1) * TS)
                        ylru = ylru2[i % 2]
                        merged = mergedb[i % 2]
                        for oc in range(8):
                            gi_, mi_ = gl_rot.next(), mpl_rot.next()
                            rows = slice(oc * 128, (oc + 1) * 128)
                            P.dma(gl_[gi_][:], G[rows, cols], reads=[("G", i, oc)], writes=[("gl", gi_)])
                            P.dma(mpl[mi_][:], MP[rows, cols], reads=[("MP", i, oc)], writes=[("mpl", mi_)])
                            pj = ps_rot.next()
                            for kc in range(8):
                                P.pe(lambda e, kc=kc, oc=oc, pj=pj: e.matmul(bank(pj), lhsT=wblb[:, kc, oc * 128:(oc + 1) * 128], rhs=ylru[:, kc, :],
                                                                              start=(kc == 0), stop=(kc == 7)),
                                     reads=[("wblb", oc // 4)] + [("ylru", i % 2, k) for k in range(8)], writes=[("ps", pj)], signal=(kc == 7))
                            ti = mt_rot.next()
                            P.dve(lambda e, pj=pj, gi_=gi_, ti=ti: e.tensor_tensor(out=mtmp[ti][:], in0=bank(pj), in1=gl_[gi_][:], op=ALU.mult),
                                  reads=[("ps", pj), ("gl", gi_)], writes=[("mtmp", ti)])
                            P.pool(lambda e, oc=oc, mi_=mi_, ti=ti: e.tensor_tensor(out=merged[:, oc, :], in0=mtmp[ti][:], in1=mpl[mi_][:], op=ALU.add),
                                   reads=[("mtmp", ti), ("mpl", mi_)], writes=[("merged", i % 2, oc)])
                        P.dma(fmview(MG[:, cols]), merged[:], reads=[("merged", i % 2, oc) for oc in range(8)], writes=[("MG", i)], q="poolq")

                    def p3_wout(i, ss):
                        K_ = list(range(len(ss)))
                        r0s = [i * TS + s * 128 for s in ss]
                        merged = mgl[i % 2]
                        for k in K_:
                            P.dma(xlb[k][:], Xsrc[r0s[k]:r0s[k] + 128, :], writes=[("xl", k)])
                        for k in K_:
                            s = ss[k]
                            for half in range(2):
                                pj = ps_rot.next()
                                hs = slice(half * 512, (half + 1) * 512)
                                for kc in range(8):
                                    P.pe(lambda e, kc=kc, pj=pj, s=s, hs=hs: e.matmul(bank(pj), lhsT=merged[:, kc, s * 128:(s + 1) * 128], rhs=woutb[:, kc, hs],
                                                                                       start=(kc == 0), stop=(kc == 7)),
                                         reads=[("woutb", half), ("mgl", i % 2)], writes=[("ps", pj)], signal=(kc == 7))
                                P.dve(lambda e, pj=pj, hs=hs, k=k: e.tensor_tensor(out=hn2[k][:, hs], in0=bank(pj), in1=g1bc[:, hs], op=ALU.mult),
                                      reads=[("ps", pj), "g1bc"], writes=[("hn2", k)])
                                P.pool(lambda e, hs=hs, k=k: e.tensor_tensor(out=xlb[k][:, hs], in0=xlb[k][:, hs], in1=hn2[k][:, hs], op=ALU.add),
                                       reads=[("hn2", k), ("xl", k)], writes=[("xl", k)])
                        for k in K_:
                            P.dma(X[r0s[k]:r0s[k] + 128, :], xlb[k][:], reads=[("xl", k)], writes=[("X", i, ss[k])], q="poolq")
                        for k in K_:
                            P.act(lambda e, k=k: e.activation(out=junk[:], in_=xlb[k][:], func=AF.Square, accum_out=sm[k][:, 0:1]),
                                  reads=[("xl", k)], writes=[("sm0", k)])
                        for k in K_:
                            P.act(lambda e, k=k: e.activation(out=sm[k][:, 0:1], in_=sm[k][:, 0:1], func=AF.Sqrt, scale=1.0 / D, bias=epsT[:, 0:1]),
                                  reads=[("sm0", k), "epsT"], writes=[("sm0", k)])
                        for k in K_:
                            P.dve(lambda e, k=k: e.reciprocal(out=sm[k][:, 1:2], in_=sm[k][:, 0:1]), reads=[("sm0", k)], writes=[("sm1", k)])
                        for k in K_:
                            P.dve(lambda e, k=k: e.scalar_tensor_tensor(out=hn2[k][:], in0=xlb[k][:], scalar=sm[k][:, 1:2], in1=a2bc[:], op0=ALU.mult, op1=ALU.mult),
                                  reads=[("xl", k), ("sm1", k), "a2bc"], writes=[("hn2", k)])
                        for k in K_:
                            P.pool(lambda e, k=k: e.tensor_tensor(out=hn2[k][:], in0=hn2[k][:], in1=b2bc[:], op=ALU.add),
                                   reads=[("hn2", k), "b2bc"], writes=[("hn2", k)])
                        for k in K_:
                            P.act(lambda e, k=k: e.activation(out=hn2b[k][:], in_=hn2[k][:], func=AF.Copy),
                                  reads=[("hn2", k)], writes=[("hn2b", k)])
                            P.dma(HN2[r0s[k]:r0s[k] + 128, :], hn2b[k][:], reads=[("hn2b", k)], writes=[("HN2", i, ss[k])], q="actq")
                        for k in K_:
                            for h2 in range(2):
                                pj = ps_rot.next()
                                for cc in range(4):
                                    c = h2 * 4 + cc
                                    P.pe(lambda e, c=c, cc=cc, pj=pj, k=k: e.transpose(out=bank(pj)[:, cc * 128:(cc + 1) * 128], in_=hn2[k][:, c * 128:(c + 1) * 128], identity=ident[:]),
                                         reads=[("hn2", k), "ident"], writes=[("ps", pj)], signal=(cc == 3))
                                P.act(lambda e, pj=pj, h2=h2, k=k: e.activation(out=hn2T[k][:, h2 * 4:(h2 + 1) * 4, :], in_=bank(pj).rearrange("p (c t) -> p c t", t=128), func=AF.Copy),
                                      reads=[("ps", pj)], writes=[("hn2T", k, h2)])
                        pjs = []
                        for k in K_:
                            pj = ps_rot.next()
                            pjs.append(pj)
                            for kc in range(8):
                                P.pe(lambda e, kc=kc, pj=pj, k=k: e.matmul(bank(pj)[:, 0:NEXP], lhsT=hn2T[k][:, kc, :], rhs=rwt[:, kc, :], start=(kc == 0), stop=(kc == 7)),
                                     reads=["rwt", ("hn2T", k, 0), ("hn2T", k, 1)], writes=[("ps", pj)], signal=(kc == 7))
                        for k in K_:
                            P.dve(lambda e, k=k: e.reduce_max(out=sm[k][:, 2:3], in_=bank(pjs[k])[:, 0:NEXP], axis=AX.X), reads=[("ps", pjs[k])], writes=[("sm2", k)])
                        for k in K_:
                            P.dve(lambda e, k=k: e.tensor_scalar(out=sm[k][:, 3:4], in0=sm[k][:, 2:3], scalar1=-1.0, scalar2=None, op0=ALU.mult), reads=[("sm2", k)], writes=[("sm3", k)])
                        for k in K_:
                            P.act(lambda e, k=k: e.activation(out=ex[k][:], in_=bank(pjs[k])[:, 0:NEXP], func=AF.Exp, bias=sm[k][:, 3:4], accum_out=sm[k][:, 4:5]),
                                  reads=[("ps", pjs[k]), ("sm3", k)], writes=[("ex", k), ("sm4", k)])
                        for k in K_:
                            P.dve(lambda e, k=k: e.reciprocal(out=sm[k][:, 5:6], in_=sm[k][:, 4:5]), reads=[("sm4", k)], writes=[("sm5", k)])
                        for k in K_:
                            P.dve(lambda e, k=k: e.tensor_scalar(out=affpad[k][:, 0:NEXP], in0=ex[k][:], scalar1=sm[k][:, 5:6], scalar2=None, op0=ALU.mult),
                                  reads=[("ex", k), ("sm5", k)], writes=[("affpad", k)])
                        pts = []
                        for k in K_:
                            pj = ps_rot.next()
                            pts.append(pj)
                            P.pe(lambda e, pj=pj, k=k: e.transpose(out=bank(pj)[:, 0:128], in_=affpad[k][:], identity=ident[:]),
                                 reads=[("affpad", k), "ident"], writes=[("ps", pj)])
                        for k in K_:
                            s = ss[k]
                            pj = pts[k]
                            if i < 2:
                                req = 2 * i + s // 2
                                tc0 = (s % 2) * 128
                                P.act(lambda e, pj=pj, k=k: e.activation(out=afst[k][0:NEXP, :], in_=bank(pj)[0:NEXP, 0:128], func=AF.Copy),
                                      reads=[("ps", pj)], writes=[("afst", k)])
                                P.dma(affP[NEXP * req:NEXP * req + NEXP, tc0:tc0 + 128], afst[k][0:NEXP, :], reads=[("afst", k)], writes=["affP"], q="actq")
                            else:
                                tc0 = (i - 2) * TS + s * 128
                                P.act(lambda e, pj=pj, tc0=tc0: e.activation(out=affS[0:NEXP, tc0:tc0 + 128], in_=bank(pj)[0:NEXP, 0:128], func=AF.Copy),
                                      reads=[("ps", pj)], writes=["affS"])

                order = list(range(NTILE - 1, -1, -1))
                NW = 4
                with ExitStack() as st:
                    wblb = T(st, "wblb", [128, 8, D], BF16)
                    for j in range(2):
                        P.dma(wblb[:, :, j * 512:(j + 1) * 512], fmview(w_br_lru[l][:, j * 512:(j + 1) * 512]), writes=[("wblb", j)], q="poolq")
                    vcl = [T(st, "vcl%d" % i, [128, TS]) for i in range(16)]
                    hfl = [T(st, "hfl%d" % i, [128, TS]) for i in range(12)]
                    gl_ = [T(st, "gl%d" % i, [128, TS], BF16) for i in range(3)]
                    mpl = [T(st, "mpl%d" % i, [128, TS]) for i in range(3)]
                    hbs = [T(st, "hbs%d" % i, [128, TS]) for i in range(8)]
                    sets = mk_sets(st, 8)
                    ylru2 = [T(st, "ylru%d" % i, [128, 8, TS], BF16) for i in range(2)]
                    mergedb = [T(st, "merged%d" % i, [128, 8, TS], BF16) for i in range(2)]
                    mtmp = [T(st, "mtmp%d" % i, [128, TS]) for i in range(2)]
                    vcl_rot, hfl_rot, gl_rot, mpl_rot, hb_rot, mt_rot = Rot(range(16)), Rot(range(12)), Rot(range(3)), Rot(range(3)), Rot(range(8)), Rot(range(2))
                    p3_lru(order[0])
                    for n_, i in enumerate(order):
                        nx = order[n_ + 1] if n_ + 1 < len(order) else None
                        if nx is not None:
                            p3_lru(nx)
                        p3_merge(i)
                    P.barrier()
                if phase_end("p3a", l):
                    lst.close()
                    return True
                affS = T(lst, "affS", [16, 4096])
                affP = T(lst, "affP", [64, 256])
                P.dve(lambda e: e.memset(affP[:], 0.0), writes=["affP"])
                vS = T(lst, "vS", [16, 512])
                iS = T(lst, "iS", [16, 512], U32)
                with ExitStack() as st:
                    woutb = T(st, "woutb", [128, 8, D], BF16)
                    rwt = T(st, "rwt", [128, 8, NEXP])
                    for j in range(2):
                        P.dma(woutb[:, :, j * 512:(j + 1) * 512], fmview(w_out[l][:, j * 512:(j + 1) * 512]), writes=[("woutb", j)], q="poolq")
                    P.dma(rwt[:], fmview(router_w[l]), writes=["rwt"])
                    g1bc = T(st, "g1bc", [128, D])
                    a2bc = T(st, "a2bc", [128, D])
                    b2bc = T(st, "b2bc", [128, D])
                    g2n = T(st, "g2n", [128, D])
                    mgl = [T(st, "mgl%d" % i, [128, 8, TS], BF16) for i in range(2)]
                    xlb = [T(st, "xl%d" % i, [128, D]) for i in range(NW)]
                    hn2 = [T(st, "hn2_%d" % i, [128, D]) for i in range(NW)]
                    hn2b = [T(st, "hn2b%d" % i, [128, D], BF16) for i in range(NW)]
                    hn2T = [T(st, "hn2T%d" % i, [128, 8, 128]) for i in range(NW)]
                    junk = T(st, "junk3", [128, D], BF16)
                    sm = [T(st, "sm%d" % i, [128, 8]) for i in range(NW)]
                    ex = [T(st, "ex%d" % i, [128, NEXP]) for i in range(NW)]
                    affpad = [T(st, "affpad%d" % i, [128, 128]) for i in range(NW)]
                    afst = [T(st, "afst%d" % i, [NEXP, 128]) for i in range(NW)]
                    for k in range(NW):
                        P.dve(lambda e, k=k: e.memset(affpad[k][:], 0.0), writes=[("affpad", k)])

                    def mg_load(i):
                        P.dma(mgl[i % 2][:], fmview(MG[:, i * TS:(i + 1) * TS]), reads=[("MG", i)], writes=[("mgl", i % 2)])

                    load_bc(1)
                    mg_load(order[0])
                    for n_, i in enumerate(order):
                        nx = order[n_ + 1] if n_ + 1 < len(order) else None
                        if nx is not None:
                            mg_load(nx)
                        if i == 1:
                            load_bc(0)
                        p3_wout(i, [0, 1, 2, 3])
                        if i == 2:
                            sgen = topk_gen(affS[:], vS, iS, 64, "S", "affS")
                            P.inject = sgen
                    P.inject = None
                    P.barrier()
                if phase_end("p3", l):
                    lst.close()
                    return True

                idxS = T(lst, "idxS", [128, NEXP, 4], I32)
                gatS = T(lst, "gatS", [128, NEXP, 4])
                idxP = T(lst, "idxP", [128, NEXP], I32)
                gatP = T(lst, "gatP", [128, NEXP])
                with ExitStack() as st:
                    fS = T(st, "fS", [16, 512])
                    iS2 = T(st, "iS2", [16, 512], I32)
                    wkP = T(st, "wkP", [64, 256])
                    vP = T(st, "vP", [64, 32])
                    iP = T(st, "iP", [64, 32], U32)
                    fP = T(st, "fP", [64, 32])
                    iP2 = T(st, "iP2", [64, 32], I32)
                    offi = T(st, "offi", [4, NEXP], I32)
                    offf = T(st, "offf", [4, NEXP])
                    offPf = T(st, "offPf", [64, 1])
                    P.pool(lambda e: e.iota(offi[:], pattern=[[0, NEXP]], base=0, channel_multiplier=256), writes=["offi"])
                    P.dve(lambda e: e.tensor_copy(out=offf[:], in_=offi[:]), reads=["offi"], writes=["offf"])
                    P.dma(OFFS.rearrange("(r e) -> r e", e=NEXP), offf[:], reads=["offf"], writes=["OFFS"])
                    P.dma(offPf[:], OFFS.rearrange("(p o) -> p o", o=1), reads=["OFFS"], writes=["offPf"])

                    def topk(src, wk, vals, idxs, niter, tag):
                        cur = src
                        cres = tag + "src"
                        for it in range(niter):
                            sl = slice(it * 8, it * 8 + 8)
                            P.dve(lambda e, cur=cur, sl=sl: e.max(out=vals[:, sl], in_=cur), reads=[cres], writes=[tag + "v"])
                            P.dve(lambda e, cur=cur, sl=sl: e.max_index(out=idxs[:, sl], in_max=vals[:, sl], in_values=cur), reads=[cres, tag + "v"], writes=[tag + "i"])
                            if it + 1 < niter:
                                P.dve(lambda e, cur=cur, sl=sl: e.match_replace(out=wk, in_to_replace=vals[:, sl], in_values=cur, imm_value=-1.0),
                                      reads=[cres, tag + "v"], writes=[tag + "wk"])
                                cur = wk
                                cres = tag + "wk"

                    P.last_w["Psrc"] = P.last_w.get("affP")
                    topk(affP[:], wkP[:], vP, iP, 4, "P")
                    P.dve(lambda e: e.tensor_copy(out=fP[:], in_=iP[:]), reads=["Pi"], writes=["fP"])
                    P.dve(lambda e: e.tensor_scalar(out=fP[:], in0=fP[:], scalar1=offPf[:, 0:1], scalar2=None, op0=ALU.add), reads=["fP", "offPf"], writes=["fP"])
                    P.dve(lambda e: e.tensor_copy(out=iP2[:], in_=fP[:]), reads=["fP"], writes=["iP2"])
                    P.dma(PIDX, iP2[:], reads=["iP2"], writes=["PIDX"])
                    P.dma(PGATE, vP[:], reads=["Pv"], writes=["PGATE"])
                    with nc.allow_non_contiguous_dma(reason="tiny index relayout"):
                        for r in range(4):
                            P.dma(idxP[32 * r:32 * r + 32, :], PIDX[NEXP * r:NEXP * r + NEXP, :].rearrange("e k -> k e"), reads=["PIDX"], writes=["idxP"])
                            P.dma(gatP[32 * r:32 * r + 32, :], PGATE[NEXP * r:NEXP * r + NEXP, :].rearrange("e k -> k e"), reads=["PGATE"], writes=["gatP"])
                    for _ in sgen:
                        pass
                    P.dve(lambda e: e.tensor_copy(out=fS[:], in_=iS[:]), reads=["Si"], writes=["fS"])
                    P.dve(lambda e: e.tensor_scalar(out=fS[:], in0=fS[:], scalar1=1024.0, scalar2=None, op0=ALU.add), reads=["fS"], writes=["fS"])
                    P.dve(lambda e: e.tensor_copy(out=iS2[:], in_=fS[:]), reads=["fS"], writes=["iS2"])
                    P.dma(SIDX, iS2[:], reads=["iS2"], writes=["SIDX"])
                    P.dma(SGATE, vS[:], reads=["Sv"], writes=["SGATE"])
                    with nc.allow_non_contiguous_dma(reason="tiny index relayout"):
                        P.dma(idxS[:], SIDX.rearrange("e (g p) -> p e g", p=128), reads=["SIDX"], writes=["idxS"])
                        P.dma(gatS[:], SGATE.rearrange("e (g p) -> p e g", p=128), reads=["SGATE"], writes=["gatS"])
                    P.barrier()
                if phase_end("route", l):
                    lst.close()
                    return True

                with ExitStack() as st:
                    g2bc = [T(st, "g2bc%d" % v, [128, D]) for v in range(2)]
                    for v in range(2):
                        P.dma(g2bc[v][:], MOD[l, v, 5 * D:6 * D].partition_broadcast(128), writes=[("g2bc", v)])
                    xg = [T(st, "xg%d" % g, [128, D], BF16) for g in range(5)]
                    xsT = T(st, "xsT", [128, 8, 640], BF16)
                    w1q = [T(st, "w1q%d" % i, [128, 8, 512], BF16) for i in range(2)]
                    w3q = [T(st, "w3q%d" % i, [128, 8, 512], BF16) for i in range(2)]
                    w2h = [T(st, "w2h%d" % i, [128, 16, 512], BF16) for i in range(2)]
                    hid = T(st, "hid", [128, 16, 640], BF16)
                    s1 = [T(st, "s1_%d" % i, [128, 640]) for i in range(2)]
                    osb = [T(st, "osb%d" % g, [128, D]) for g in range(5)]
                    wq_rot, w2_rot, s1_rot, hp_rot = Rot(range(2)), Rot(range(2)), Rot(range(2)), Rot(range(2))

                    def moe_load1(e_, q):
                        wi = wq_rot.next()
                        P.dma(w1q[wi][:], fmview(exp_w1[l, e_][:, q * 512:(q + 1) * 512]), writes=[("w1q", wi)], q="poolq")
                        P.dma(w3q[wi][:], fmview(exp_w3[l, e_][:, q * 512:(q + 1) * 512]), writes=[("w3q", wi)], q="poolq")
                        return wi

                    def moe_load2(e_, half):
                        wi = w2_rot.next()
                        P.dma(w2h[wi][:], exp_w2[l, e_][:, half * 512:(half + 1) * 512].rearrange("(c p) d -> p c d", p=128), writes=[("w2h", wi)], q="poolq")
                        return wi

                    def moe_gather(e_):
                        for g in range(5):
                            ia = idxS[:, e_, g:g + 1] if g < 4 else idxP[:, e_:e_ + 1]
                            ir = "idxS" if g < 4 else "idxP"
                            P.op("pool", lambda e, g=g, ia=ia: e.indirect_dma_start(out=xg[g][:], out_offset=None, in_=HN2,
                                                                                     in_offset=bass.IndirectOffsetOnAxis(ap=ia, axis=0)),
                                 reads=[ir], writes=[("xg", g)], dmaq="poolq")

                    def moe_xpose(e_):
                        for g in range(5):
                            pj = ps_rot.next()
                            pbb = bank(pj).bitcast(BF16)
                            for c in range(8):
                                P.pe(lambda e, g=g, c=c, pbb=pbb: e.transpose(out=pbb[:, c * 128:(c + 1) * 128], in_=xg[g][:, c * 128:(c + 1) * 128], identity=identb[:]),
                                     reads=[("xg", g), "identb"], writes=[("ps", pj)], signal=(c == 7))
                            P.act(lambda e, g=g, pbb=pbb: e.activation(out=xsT[:, :, g * 128:(g + 1) * 128], in_=pbb.rearrange("p (c t) -> p c t", t=128), func=AF.Copy),
                                  reads=[("ps", pj)], writes=["xsT"])

                    moe_gather(0)
                    w1i = moe_load1(0, 0)
                    moe_xpose(0)
                    for e_ in range(NEXP):
                        w2is = [None, None]
                        for q in range(4):
                            nxt = moe_load1(e_, q + 1) if q < 3 else None
                            if q == 1:
                                w2is[0] = moe_load2(e_, 0)
                            if q == 2:
                                w2is[1] = moe_load2(e_, 1)
                                if e_ + 1 < NEXP:
                                    moe_gather(e_ + 1)
                            for fc in range(4):
                                fcg = q * 4 + fc
                                hk = hp_rot.next()
                                H1, H3 = PS[2 * hk], PS[2 * hk + 1]
                                for (Hh, wt, wr) in ((H1, w1q[w1i], ("w1q", w1i)), (H3, w3q[w1i], ("w3q", w1i))):
                                    pidx = 2 * hk if Hh is H1 else 2 * hk + 1
                                    for kc in range(8):
                                        P.pe(lambda e, Hh=Hh, wt=wt, kc=kc, fc=fc: e.matmul(Hh[:, 0:512], lhsT=wt[:, kc, fc * 128:(fc + 1) * 128], rhs=xsT[:, kc, 0:512],
                                                                                             start=(kc == 0), stop=(kc == 7)),
                                             reads=[wr, "xsT"], writes=[("ps", 2 * pidx)], signal=False)
                                        P.pe(lambda e, Hh=Hh, wt=wt, kc=kc, fc=fc: e.matmul(Hh[:, 512:640], lhsT=wt[:, kc, fc * 128:(fc + 1) * 128], rhs=xsT[:, kc, 512:640],
                                                                                             start=(kc == 0), stop=(kc == 7)),
                                             reads=[wr, "xsT"], writes=[("ps", 2 * pidx + 1)], signal=(kc == 7))
                                si = s1_rot.next()
                                r1 = [("ps", 4 * hk), ("ps", 4 * hk + 1)]
                                r3 = [("ps", 4 * hk + 2), ("ps", 4 * hk + 3)]
                                P.act(lambda e, H1=H1, si=si: e.activation(out=s1[si][:], in_=H1[:, 0:640], func=AF.Silu),
                                      reads=r1, writes=[("s1", si)])
                                P.dve(lambda e, H3=H3, si=si, fcg=fcg: e.tensor_tensor(out=hid[:, fcg, :], in0=H3[:, 0:640], in1=s1[si][:], op=ALU.mult),
                                      reads=r3 + [("s1", si)], writes=[("hid", fcg)])
                            w1i = nxt
                        if e_ + 1 < NEXP:
                            w1i = moe_load1(e_ + 1, 0)
                        for half in range(2):
                            w2i = w2is[half]
                            hs = slice(half * 512, (half + 1) * 512)
                            for g in range(5):
                                pj = ps_rot.next()
                                for fcg in range(16):
                                    P.pe(lambda e, pj=pj, fcg=fcg, g=g, w2i=w2i: e.matmul(bank(pj), lhsT=hid[:, fcg, g * 128:(g + 1) * 128], rhs=w2h[w2i][:, fcg, :],
                                                                                           start=(fcg == 0), stop=(fcg == 15)),
                                         reads=[("w2h", w2i)] + [("hid", k) for k in range(16)], writes=[("ps", pj)], signal=(fcg == 15))
                                ga = gatS[:, e_, g:g + 1] if g < 4 else gatP[:, e_:e_ + 1]
                                gr = "gatS" if g < 4 else "gatP"
                                v_ = 1 if g < 4 else 0
                                P.dve(lambda e, pj=pj, g=g, ga=ga, hs=hs, v_=v_: e.scalar_tensor_tensor(out=osb[g][:, hs], in0=bank(pj), scalar=ga, in1=g2bc[v_][:, hs],
                                                                                                        op0=ALU.mult, op1=ALU.mult),
                                      reads=[("ps", pj), gr, ("g2bc", v_)], writes=[("osb", g)])
                            if half == 0 and e_ + 1 < NEXP:
                                moe_xpose(e_ + 1)
                        for g in range(5):
                            ia = idxS[:, e_, g:g + 1] if g < 4 else idxP[:, e_:e_ + 1]
                            ir = "idxS" if g < 4 else "idxP"
                            P.op("pool", lambda e, g=g, ia=ia: e.indirect_dma_start(out=X, out_offset=bass.IndirectOffsetOnAxis(ap=ia, axis=0),
                                                                                     in_=osb[g][:], in_offset=None, compute_op=ALU.add),
                                 reads=[ir, ("osb", g)], writes=["Xall"], dmaq="poolq")
                    P.barrier()
                if phase_end("moe", l):
                    lst.close()
                    return True
                lst.close()

        def run_final():
            with ExitStack() as st:
                fgbc = T(st, "fgbc", [128, D])
                P.dma(fgbc[:], final_g.partition_broadcast(128), writes=["fgbc"])
                xf = [T(st, "xf%d" % i, [128, D]) for i in range(3)]
                yf = [T(st, "yf%d" % i, [128, D]) for i in range(3)]
                junk = T(st, "junkf", [128, D], BF16)
                sf = [T(st, "sf%d" % i, [128, 2]) for i in range(3)]
                for t in range(NTOK // 128):
                    bi = t % 3
                    P.dma(xf[bi][:], X[t * 128:(t + 1) * 128, :], writes=[("xf", bi)])
                    P.act(lambda e, bi=bi: e.activation(out=junk[:], in_=xf[bi][:], func=AF.Square, accum_out=sf[bi][:, 0:1]),
                          reads=[("xf", bi)], writes=["junkf", ("sf", bi)])
                    P.act(lambda e, bi=bi: e.activation(out=sf[bi][:, 0:1], in_=sf[bi][:, 0:1], func=AF.Sqrt, scale=1.0 / D, bias=epsT[:, 0:1]),
                          reads=[("sf", bi), "epsT"], writes=[("sf", bi)])
                    P.dve(lambda e, bi=bi: e.reciprocal(out=sf[bi][:, 1:2], in_=sf[bi][:, 0:1]), reads=[("sf", bi)], writes=[("sf1", bi)])
                    P.dve(lambda e, bi=bi: e.scalar_tensor_tensor(out=yf[bi][:], in0=xf[bi][:], scalar=sf[bi][:, 1:2], in1=fgbc[:], op0=ALU.mult, op1=ALU.mult),
                          reads=[("xf", bi), ("sf1", bi), "fgbc"], writes=[("yf", bi)])
                    P.dma(y_out[t * 128:(t + 1) * 128, :], yf[bi][:], reads=[("yf", bi)], writes=[("y", t)], q="poolq")
                pj = ps_rot.next()
                P.pe(lambda e: e.transpose(out=bank(pj)[:, 0:128], in_=nsfm[:], identity=ident[:]), reads=["nsfm", "ident"], writes=[("ps", pj)])
                nsr = T(st, "nsr", [128, 128])
                P.dve(lambda e: e.tensor_copy(out=nsr[:], in_=bank(pj)[:, 0:128]), reads=[("ps", pj)], writes=["nsr"])
                for l in range(2):
                    for d in range(2):
                        for r in range(4):
                            r0 = ((l * 2 + d) * 4 + r) * 8
                            P.dma(ns_out[r, l, d, :].rearrange("(c p) -> c p", p=128), nsr[r0:r0 + 8, :], reads=["nsr"], writes=[("ns", l, d, r)])
                P.barrier()
        if not run_setup():
            if not phase_end("fm", 0):
                run_layers()
        P.barrier()
        run_final()
    nc._prog_stats = (dict(P.cnt), dict(P.dma_i), P.nops)
    return nc


_NC_CACHE = {}


def kernel(x_prompt, x_sample, state_lru, c, c_ctx, norm1_g, norm2_g, final_g, w_mod, b_mod, w_in,
           pool_w, pool_scale, conv_w, conv_b, lru_wr, lru_br, lru_wi, lru_bi, lru_lambda,
           w_br_pool, w_br_lru, w_out, router_w, exp_w1, exp_w3, exp_w2):
    f = lambda a: np.ascontiguousarray(np.asarray(a, dtype=np.float32))
    if "nc" not in _NC_CACHE:
        _NC_CACHE["nc"] = build_nc()
    nc = _NC_CACHE["nc"]
    shared = dict(norm1_g=f(norm1_g), norm2_g=f(norm2_g), final_g=f(final_g), w_mod=f(w_mod), b_mod=f(b_mod),
                  w_in=f(w_in), pool_w=f(pool_w), pool_scale=f(pool_scale), conv_w=f(conv_w), conv_b=f(conv_b),
                  lru_wr=f(lru_wr), lru_br=f(lru_br), lru_wi=f(lru_wi), lru_bi=f(lru_bi), lru_lambda=f(lru_lambda),
                  w_br_pool=f(w_br_pool), w_br_lru=f(w_br_lru), w_out=f(w_out), router_w=f(router_w),
                  exp_w1=f(exp_w1), exp_w3=f(exp_w3), exp_w2=f(exp_w2))
    xp, xs, sl, cc, cx = f(x_prompt), f(x_sample), f(state_lru), f(c), f(c_ctx)
    in_maps = []
    for k in range(8):
        m = dict(shared)
        m["x_in"] = np.concatenate([xp[4 * k:4 * k + 4].reshape(1024, D), xs[k]], axis=0)
        m["cvec"] = np.stack([cx, cc[k]], axis=0)
        m["h0s"] = np.ascontiguousarray(sl[k])
        in_maps.append(m)
    res = run_bass_kernel_spmd(nc, in_maps, core_ids=list(range(8)))
    y_prompt = np.zeros((32, 256, D), np.float32)
    y_sample = np.zeros((8, 4096, D), np.float32)
    ns = np.zeros((32, 2, 2, D), np.float32)
    for k in range(8):
        r = res.results[k]
        y_prompt[4 * k:4 * k + 4] = r["y"][0:1024].reshape(4, 256, D)
        y_sample[k] = r["y"][1024:]
        ns[4 * k:4 * k + 4] = r["ns"]
    return (y_prompt, y_sample, ns)
```

```python
from contextlib import ExitStack
import numpy as np
import concourse.bass as bass
import concourse.mybir as mybir
from concourse.bass_utils import run_bass_kernel_spmd

F32 = mybir.dt.float32
BF16 = mybir.dt.bfloat16
U32 = mybir.dt.uint32
I32 = mybir.dt.int32
AF = mybir.ActivationFunctionType
ALU = mybir.AluOpType
AX = mybir.AxisListType

COMPUTE = ("pe", "act", "dve", "pool")
NDMASEM = {"sp": 24, "actq": 12, "poolq": 24}


class Prog:
    def __init__(self, nc):
        self.nc = nc
        self.sems = {}
        self.cnt = {k: 0 for k in COMPUTE}
        self.dma_i = {k: 0 for k in NDMASEM}
        self.waited = {}
        self.last_w = {}
        self.readers = {}
        self.nops = 0
        self.inject = None
        self.inject_every = 1
        self._inj_n = 0
        self._in_inject = False

    def alloc(self, stack):
        nc = self.nc
        for k in COMPUTE:
            self.sems[k] = stack.enter_context(nc.semaphore("s_" + k))
        for q, n in NDMASEM.items():
            for i in range(n):
                self.sems[(q, i)] = stack.enter_context(nc.semaphore("d_%s_%d" % (q, i)))

    def _need(self, stream, ev, waits):
        if ev is None:
            return
        key, val = ev
        if key == stream and val > self.cnt[key]:
            return
        if self.waited.get((stream, key), 0) >= val:
            return
        if val > waits.get(key, 0):
            waits[key] = val

    def op(self, eng, fn, reads=(), writes=(), signal=True, dmaq=None):
        stream = eng
        waits = {}
        for r in reads:
            self._need(stream, self.last_w.get(r), waits)
        for w in writes:
            self._need(stream, self.last_w.get(w), waits)
            for ev in self.readers.get(w, ()):
                self._need(stream, ev, waits)
        if dmaq is not None:
            n = NDMASEM[dmaq]
            i = self.dma_i[dmaq]
            self.dma_i[dmaq] = i + 1
            key = (dmaq, i % n)
            val = 16 * (i // n + 1)
            if i >= n:
                self._need(stream, (key, val - 16), waits)
            ev = (key, val)
            inc = (key, 16)
        else:
            if signal:
                self.cnt[eng] += 1
                ev = (eng, self.cnt[eng])
                inc = (eng, 1)
            else:
                ev = (eng, self.cnt[eng] + 1)
                inc = None
        for key, val in waits.items():
            self.waited[(stream, key)] = max(self.waited.get((stream, key), 0), val)
        self._emit(stream, fn, list(waits.items()), inc)
        for r in reads:
            self.readers.setdefault(r, []).append(ev)
        for w in writes:
            self.last_w[w] = ev
            self.readers[w] = []
        self.nops += 1
        if eng == "dve" and self.inject is not None and not self._in_inject:
            self._inj_n += 1
            if self._inj_n % self.inject_every == 0:
                self._in_inject = True
                try:
                    next(self.inject)
                except StopIteration:
                    self.inject = None
                self._in_inject = False
        return ev

    def pe(self, fn, reads=(), writes=(), signal=True):
        return self.op("pe", fn, reads, writes, signal)

    def act(self, fn, reads=(), writes=()):
        return self.op("act", fn, reads, writes)

    def dve(self, fn, reads=(), writes=()):
        return self.op("dve", fn, reads, writes)

    def pool(self, fn, reads=(), writes=()):
        return self.op("pool", fn, reads, writes)

    def dma(self, out, in_, reads=(), writes=(), q="sp", **kw):
        stream = {"sp": "sp", "actq": "act", "poolq": "pool"}[q]
        return self.op(stream, lambda e: e.dma_start(out=out, in_=in_, **kw), reads, writes, dmaq=q)

    def _emit(self, stream, fn, waits, inc):
        nc = self.nc
        e = {"pe": nc.tensor, "act": nc.scalar, "dve": nc.vector, "pool": nc.gpsimd, "sp": nc.sync}[stream]
        for key, val in waits:
            e.wait_ge(self.sems[key], val)
        if fn is None:
            return
        ins = fn(e)
        if inc is not None:
            ins.then_inc(self.sems[inc[0]], inc[1])

    def barrier(self):
        evs = [(k, self.cnt[k]) for k in COMPUTE if self.cnt[k] > 0]
        for q, n in NDMASEM.items():
            i = self.dma_i[q]
            for j in range(max(0, i - n), i):
                evs.append(((q, j % n), 16 * (j // n + 1)))
        for stream in ("pe", "act", "dve", "pool", "sp"):
            waits = {}
            for ev in evs:
                self._need(stream, ev, waits)
            for key, val in waits.items():
                self.waited[(stream, key)] = max(self.waited.get((stream, key), 0), val)
            self._emit(stream, None, list(waits.items()), None)


class Rot:
    def __init__(self, items):
        self.items = list(items)
        self.i = 0

    def next(self):
        it = self.items[self.i % len(self.items)]
        self.i += 1
        return it


D = 1024
NTOK = 5120
NTILE = 10
TS = 512
VW = 5140
EPS = 1e-6
NEXP = 16
FF = 2048
DEPTH = 2


def build_nc(dbg=False, nlayers=DEPTH, stop_after=None):
    nc = bass.Bass("TRN2", target_bir_lowering=False)

    def din(name, shape, dt=F32):
        return nc.dram_tensor(name, list(shape), dt, kind="ExternalInput").ap()

    def dint(name, shape, dt=F32):
        return nc.dram_tensor(name, list(shape), dt, kind=("ExternalOutput" if dbg else "Internal")).ap()

    x_in = din("x_in", [NTOK, D])
    cvec = din("cvec", [2, D])
    h0s = din("h0s", [2, 2, D])
    norm1_g = din("norm1_g", [2, D])
    norm2_g = din("norm2_g", [2, D])
    final_g = din("final_g", [D])
    w_mod = din("w_mod", [2, D, 6 * D])
    b_mod = din("b_mod", [2, 6 * D])
    w_in = din("w_in", [2, D, 3584])
    pool_w = din("pool_w", [2, 4, 128, 128])
    pool_scale = din("pool_scale", [2, 512])
    conv_w = din("conv_w", [2, 4, D])
    conv_b = din("conv_b", [2, D])
    lru_wr = din("lru_wr", [2, 2, 16, 64, 64])
    lru_br = din("lru_br", [2, 2, D])
    lru_wi = din("lru_wi", [2, 2, 16, 64, 64])
    lru_bi = din("lru_bi", [2, 2, D])
    lru_lambda = din("lru_lambda", [2, 2, D])
    w_br_pool = din("w_br_pool", [2, 512, D])
    w_br_lru = din("w_br_lru", [2, D, D])
    w_out = din("w_out", [2, D, D])
    router_w = din("router_w", [2, D, NEXP])
    _need_exp = stop_after is None or tuple(stop_after)[0] == "moe" or tuple(stop_after)[1] > 0
    exp_w1 = din("exp_w1", [2, NEXP, D, FF]) if _need_exp else None
    exp_w3 = din("exp_w3", [2, NEXP, D, FF]) if _need_exp else None
    exp_w2 = din("exp_w2", [2, NEXP, FF, D]) if _need_exp else None

    y_out = nc.dram_tensor("y", [NTOK, D], F32, kind="ExternalOutput").ap()
    ns_out = nc.dram_tensor("ns", [4, 2, 2, D], F32, kind="ExternalOutput").ap()

    X = dint("Xs", [NTOK, D])
    HN2 = dint("HN2s", [NTOK, D], BF16)
    V = dint("Vs", [D, VW])
    VC = dint("VCs", [D, NTOK])
    HF = dint("HFs", [D, NTOK])
    MP = dint("MPs", [D, NTOK])
    G = dint("Gs", [D, NTOK], BF16)
    MG = dint("MGs", [D, NTOK], BF16)
    MOD = dint("MODs", [2, 2, 6 * D])
    SIDX = dint("SIDXs", [16, 512], I32)
    SGATE = dint("SGATEs", [16, 512])
    PIDX = dint("PIDXs", [64, 32], I32)
    PGATE = dint("PGATEs", [64, 32])
    OFFS = dint("OFFSs", [64])

    with ExitStack() as gst:
        P = Prog(nc)
        P.alloc(gst)
        PS = [gst.enter_context(nc.psum_tensor("PS%d" % k, [128, 1024], F32)) for k in range(4)]

        def bank(j):
            return PS[j // 2][:, (j % 2) * 512:(j % 2) * 512 + 512]

        _tn = [0]

        def T(st, name, shape, dt=F32):
            _tn[0] += 1
            return st.enter_context(nc.sbuf_tensor("%s_%d" % (name, _tn[0]), list(shape), dt))

        def fmview(ap2d):
            return ap2d.rearrange("(c p) w -> p c w", p=128)

        class _Stop(Exception):
            pass

        def phase_end(name, lyr):
            return stop_after is not None and tuple(stop_after) == (name, lyr)

        ident = T(gst, "ident", [128, 128])
        identb = T(gst, "identb", [128, 128], BF16)
        iot = T(gst, "iot", [128, 128], I32)
        epsT = T(gst, "epsT", [128, 1])
        zt = T(gst, "zt", [128, 8, 2])
        rowsb = [T(gst, "rows%d" % i, [128, 128]) for i in range(2)]
        rows_rot = Rot([0, 1])
        fm = [T(gst, "fm%d" % l, [128, 192]) for l in range(2)]
        h0fm = T(gst, "h0fm", [128, 32])
        nsfm = T(gst, "nsfm", [128, 128])
        hcar = T(gst, "hcar", [128, 8])

        P.pool(lambda e: e.iota(iot[:], pattern=[[1, 128]], base=0, channel_multiplier=-1), writes=["iot"])
        P.dve(lambda e: e.tensor_scalar(out=ident[:], in0=iot[:], scalar1=0.0, scalar2=None, op0=ALU.is_equal),
              reads=["iot"], writes=["ident"])
        P.dve(lambda e: e.tensor_copy(out=identb[:], in_=ident[:]), reads=["ident"], writes=["identb"])
        P.dve(lambda e: e.memset(epsT[:], EPS), writes=["epsT"])
        P.dve(lambda e: e.memset(zt[:], 0.0), writes=["zt"])
        P.dve(lambda e: e.memset(nsfm[:], 0.0), writes=["nsfm"])

        pads = []
        for q in range(4):
            pads += [q * 260, q * 260 + 258]
        pads += [1040, 5138]
        for a in pads:
            P.dma(fmview(V[:, a:a + 2]), zt[:], reads=["zt"], writes=[("Vpad", a)])

        ps_rot = Rot(range(8))

        def load_fm(dst, col0, vec_aps, rows_per=8):
            nv = len(vec_aps)
            nr = nv * rows_per
            ri = rows_rot.next()
            rb = rowsb[ri]
            for v, ap in enumerate(vec_aps):
                P.dma(rb[v * rows_per:(v + 1) * rows_per, :], ap.rearrange("(c p) -> c p", p=128),
                      writes=[("rows", ri)])
            j = ps_rot.next()
            pb = bank(j)
            P.pe(lambda e: e.transpose(out=pb[:, 0:nr], in_=rb[0:nr, :], identity=ident[0:nr, 0:nr]),
                 reads=[("rows", ri), "ident"], writes=[("ps", j)])
            P.dve(lambda e: e.tensor_copy(out=dst[:, col0:col0 + nr], in_=pb[:, 0:nr]),
                  reads=[("ps", j)], writes=[("fmc", id(dst))])

        def FM(l, blk, c):
            return fm[l][:, blk * 8 + c: blk * 8 + c + 1]

        def run_setup():
            if phase_end("setup", 0):
                return True
            with ExitStack() as st:
                cfm = T(st, "cfm", [128, 16])
                csil = T(st, "csil", [128, 16], BF16)
                bm = T(st, "bm", [2, 6 * D])
                modrow = T(st, "modrow", [2, 6 * D])
                wmb = [T(st, "wm%d" % i, [128, 8, 512], BF16) for i in range(3)]
                wm_rot = Rot(range(3))
                load_fm(cfm, 0, [cvec[0, :], cvec[1, :]])
                P.act(lambda e: e.activation(out=csil[:], in_=cfm[:], func=AF.Silu),
                      reads=[("fmc", id(cfm))], writes=["csil"])
                for l in range(DEPTH):
                    for r in range(2):
                        P.dma(bm[r:r + 1, :], b_mod[l:l + 1, :], writes=["bm"])
                    for j in range(12):
                        wi = wm_rot.next()
                        wm = wmb[wi]
                        P.dma(wm[:], fmview(w_mod[l][:, j * 512:(j + 1) * 512]), writes=[("wm", wi)], q="poolq")
                        pj = ps_rot.next()
                        pb = bank(pj)
                        for c in range(8):
                            P.pe(lambda e, c=c, pb=pb, wm=wm: e.matmul(pb[0:2, :], lhsT=csil[:, c:16:8], rhs=wm[:, c, :],
                                                                      start=(c == 0), stop=(c == 7)),
                                 reads=["csil", ("wm", wi)], writes=[("ps", pj)], signal=(c == 7))
                        P.dve(lambda e, j=j, pb=pb: e.tensor_tensor(out=modrow[0:2, j * 512:(j + 1) * 512], in0=pb[0:2, :],
                                                                     in1=bm[0:2, j * 512:(j + 1) * 512], op=ALU.add),
                              reads=[("ps", pj), "bm"], writes=["modrow"])
                    P.dma(MOD[l], modrow[:], reads=["modrow"], writes=[("MOD", l)])
                P.barrier()
            if phase_end("mod", 0):
                return True


            with ExitStack() as st:
                tmpf = T(st, "tmpf", [128, 16])
                for l in range(DEPTH):
                    vecs = [norm1_g[l, :], MOD[l, 0, D:2 * D], MOD[l, 0, 0:D], MOD[l, 1, D:2 * D], MOD[l, 1, 0:D],
                            conv_w[l, 0, :], conv_w[l, 1, :], conv_w[l, 2, :], conv_w[l, 3, :], conv_b[l, :],
                            lru_br[l, 0, :], lru_bi[l, 0, :], lru_lambda[l, 0, :],
                            lru_br[l, 1, :], lru_bi[l, 1, :], lru_lambda[l, 1, :]]
                    load_fm(fm[l], 0, vecs)
                    load_fm(fm[l], 128, [pool_scale[l, :]], rows_per=4)
                    fr = ("fmc", id(fm[l]))
                    f = fm[l]
                    for vi, (sc, a1) in enumerate([(1, 17), (3, 18)]):
                        P.dve(lambda e, sc=sc, a1=a1, f=f: e.scalar_tensor_tensor(
                            out=f[:, a1 * 8:a1 * 8 + 8], in0=f[:, sc * 8:sc * 8 + 8], scalar=1.0, in1=f[:, 0:8],
                            op0=ALU.add, op1=ALU.mult), reads=[fr], writes=[fr])
                    for d, lam in enumerate([12, 15]):
                        P.act(lambda e, lam=lam, f=f, d=d: e.activation(out=tmpf[:, d * 8:d * 8 + 8], in_=f[:, lam * 8:lam * 8 + 8],
                                                                        func=AF.Exp, scale=-1.0), reads=[fr], writes=["tmpf"])
                        P.act(lambda e, d=d: e.activation(out=tmpf[:, d * 8:d * 8 + 8], in_=tmpf[:, d * 8:d * 8 + 8],
                                                          func=AF.Ln, bias=1.0), reads=["tmpf"], writes=["tmpf"])
                        P.dve(lambda e, d=d, f=f: e.tensor_scalar(out=f[:, (19 + d) * 8:(19 + d) * 8 + 8], in0=tmpf[:, d * 8:d * 8 + 8],
                                                                  scalar1=-8.0, scalar2=None, op0=ALU.mult),
                              reads=["tmpf", fr], writes=[fr])
                        P.dve(lambda e, d=d, f=f: e.tensor_scalar(out=f[:, (21 + d) * 8:(21 + d) * 8 + 8], in0=tmpf[:, d * 8:d * 8 + 8],
                                                                  scalar1=-16.0, scalar2=None, op0=ALU.mult),
                              reads=["tmpf", fr], writes=[fr])
                load_fm(h0fm, 0, [h0s[0, 0, :], h0s[0, 1, :], h0s[1, 0, :], h0s[1, 1, :]])
                P.barrier()

        def run_layers():
          for _ in range(1):
            for l in range(nlayers):
                Xsrc = x_in if l == 0 else X
                f = fm[l]
                fr = ("fmc", id(f))

                with ExitStack() as st:
                    winb = T(st, "winb", [128, 8, 3584], BF16)
                    pwb = T(st, "pwb", [128, 4, 128], BF16)
                    wbpb = T(st, "wbpb", [128, 4, D], BF16)
                    for j in range(7):
                        P.dma(winb[:, :, j * 512:(j + 1) * 512], fmview(w_in[l][:, j * 512:(j + 1) * 512]),
                              writes=["winb"], q="poolq")
                    P.dma(pwb[:], pool_w[l].rearrange("g c d -> c g d"), writes=["pwb"], q="poolq")
                    P.dma(wbpb[:], w_br_pool[l].rearrange("(c p) d -> p c d", p=128), writes=["wbpb"], q="poolq")
                    xtb = [T(st, "xt%d" % i, [128, D]) for i in range(4)]
                    xnb = [T(st, "xn%d" % i, [128, D]) for i in range(2)]
                    junk = T(st, "junk", [128, D], BF16)
                    ssb = [T(st, "ss%d" % i, [128, 1]) for i in range(4)]
                    rsb = [T(st, "rs%d" % i, [128, 1]) for i in range(4)]
                    hnT = [T(st, "hnT%d" % i, [128, 8, TS], BF16) for i in range(2)]
                    ppad = [T(st, "ppad%d" % g, [128, 640]) for g in range(4)]
                    sA = [T(st, "sA%d" % g, [128, 640]) for g in range(4)]
                    sB = [T(st, "sB%d" % g, [128, 640]) for g in range(4)]
                    icnt = [T(st, "icnt%d" % k, [128, 4, TS]) for k in range(2)]
                    pooled = T(st, "pooled", [128, 4, TS], BF16)
                    ptmp = T(st, "ptmp", [128, TS])
                    ypool = T(st, "ypool", [128, 4, TS], BF16)
                    vst = [T(st, "vst%d" % i, [128, TS]) for i in range(3)]
                    gst_ = [T(st, "gst%d" % i, [128, TS], BF16) for i in range(3)]
                    gpb = [T(st, "gp%d" % i, [128, TS]) for i in range(2)]
                    mpst = [T(st, "mpst%d" % i, [128, TS]) for i in range(3)]
                    xt_rot, xn_rot, v_rot, g_rot, gp_rot, mp_rot = Rot(range(4)), Rot(range(2)), Rot(range(3)), Rot(range(3)), Rot(range(2)), Rot(range(3))
                    for g in range(4):
                        P.dve(lambda e, g=g: e.memset(ppad[g][:], 0.0), writes=[("ppad", g)])
                        P.pool(lambda e, g=g: e.memset(sA[g][:], 0.0), writes=[("sA", g)])
                        P.pool(lambda e, g=g: e.memset(sB[g][:], 0.0), writes=[("sB", g)])
                    for k, (nrow, L) in enumerate([(8, 64), (2, 256)]):
                        for g, w in enumerate((2, 4, 8, 16)):
                            iv = icnt[k][:, g, :].rearrange("p (r t) -> p r t", t=L)
                            P.dve(lambda e, iv=iv, w=w: e.memset(iv, 1.0 / w), writes=[("icnt", k)])
                            h = w // 2
                            for t in range(h):
                                c_lo = float(min(t + h, L) - max(t - h, 0))
                                tt = L - 1 - t
                                c_hi = float(min(tt + h, L) - max(tt - h, 0))
                                P.dve(lambda e, iv=iv, t=t, c_lo=c_lo: e.memset(iv[:, :, t:t + 1], 1.0 / c_lo), writes=[("icnt", k)])
                                if c_hi != float(w):
                                    P.dve(lambda e, iv=iv, tt=tt, c_hi=c_hi: e.memset(iv[:, :, tt:tt + 1], 1.0 / c_hi), writes=[("icnt", k)])

                    def geom(i):
                        return (2, 256, 272, 1) if i < 2 else (8, 64, 80, 0)

                    def p1_front(i):
                        vi = 0 if i < 2 else 1
                        A1 = 17 + vi
                        B1 = 2 if vi == 0 else 4
                        hb = i % 2
                        for pair in range(2):
                            pbs = []
                            for _ in range(4):
                                pbs.append(ps_rot.next())
                            for sj in range(2):
                                s = pair * 2 + sj
                                xi = xt_rot.next()
                                xt = xtb[xi]
                                r0 = i * TS + s * 128
                                P.dma(xt[:], Xsrc[r0:r0 + 128, :], writes=[("xt", xi)])
                                P.act(lambda e, xt=xt, xi=xi: e.activation(out=junk[:], in_=xt[:], func=AF.Square, accum_out=ssb[xi][:, 0:1]),
                                      reads=[("xt", xi)], writes=["junk", ("ss", xi)])
                                P.act(lambda e, xi=xi: e.activation(out=ssb[xi][:, 0:1], in_=ssb[xi][:, 0:1], func=AF.Sqrt,
                                                                    scale=1.0 / D, bias=epsT[:, 0:1]),
                                      reads=[("ss", xi), "epsT"], writes=[("ss", xi)])
                                P.dve(lambda e, xi=xi: e.reciprocal(out=rsb[xi][:, 0:1], in_=ssb[xi][:, 0:1]),
                                      reads=[("ss", xi)], writes=[("rs", xi)])
                                ni = xn_rot.next()
                                xn = xnb[ni]
                                P.dve(lambda e, xn=xn, xt=xt, xi=xi: e.tensor_scalar(out=xn[:], in0=xt[:], scalar1=rsb[xi][:, 0:1], scalar2=None, op0=ALU.mult),
                                      reads=[("xt", xi), ("rs", xi)], writes=[("xn", ni)])
                                for c in range(8):
                                    pj = pbs[c // 2]
                                    off = (c % 2) * 256 + sj * 128
                                    P.pe(lambda e, xn=xn, c=c, pj=pj, off=off: e.transpose(out=bank(pj)[:, off:off + 128], in_=xn[:, c * 128:(c + 1) * 128], identity=ident[:]),
                                         reads=[("xn", ni), "ident"], writes=[("ps", pj)], signal=(c == 7))
                            for c in range(8):
                                pj = pbs[c // 2]
                                off = (c % 2) * 256
                                P.act(lambda e, c=c, pj=pj, off=off, pair=pair, hb=hb: e.activation(
                                    out=hnT[hb][:, c, pair * 256:(pair + 1) * 256], in_=bank(pj)[:, off:off + 256], func=AF.Identity,
                                    scale=FM(l, A1, c), bias=FM(l, B1, c)),
                                    reads=[("ps", pj), fr], writes=[("hnT", hb)])

                    def win_mm(i, oc):
                        hb = i % 2
                        pj = ps_rot.next()
                        for kc in range(8):
                            P.pe(lambda e, kc=kc, pj=pj, oc=oc, hb=hb: e.matmul(bank(pj), lhsT=winb[:, kc, oc * 128:(oc + 1) * 128], rhs=hnT[hb][:, kc, :],
                                                                                 start=(kc == 0), stop=(kc == 7)),
                                 reads=["winb", ("hnT", hb)], writes=[("ps", pj)], signal=(kc == 7))
                        return pj

                    def p1_back(i):
                        nrow, L, stride, ik = geom(i)
                        W = nrow * stride
                        if i == 2:
                            for g in range(4):
                                P.dve(lambda e, g=g: e.memset(ppad[g][:], 0.0), writes=[("ppad", g)])
                        for g in range(4):
                            pj = win_mm(i, g)
                            dst = ppad[g][:, 0:W].rearrange("p (r t) -> p r t", t=stride)[:, :, 8:8 + L]
                            P.act(lambda e, dst=dst, pj=pj, L=L: e.activation(out=dst, in_=bank(pj).rearrange("p (r t) -> p r t", t=L), func=AF.Copy),
                                  reads=[("ps", pj)], writes=[("ppad", g)])
                        for g, w in enumerate((2, 4, 8, 16)):
                            src, sres = ppad[g], ("ppad", g)
                            bufs = [(sA[g], ("sA", g)), (sB[g], ("sB", g))]
                            dstb, dres = bufs[0]
                            P.pool(lambda e, dstb=dstb, src=src, W=W: e.tensor_tensor(out=dstb[:, 1:W], in0=src[:, 0:W - 1], in1=src[:, 1:W], op=ALU.add),
                                   reads=[sres], writes=[dres])
                            cur, cres = dstb, dres
                            sh = 1
                            nb = 1
                            ww = 2
                            while ww < w:
                                dstb, dres = bufs[nb % 2]
                                P.pool(lambda e, dstb=dstb, cur=cur, sh=sh, W=W: e.tensor_tensor(out=dstb[:, sh:W - sh], in0=cur[:, 0:W - 2 * sh], in1=cur[:, 2 * sh:W], op=ALU.add),
                                       reads=[cres], writes=[dres])
                                cur, cres = dstb, dres
                                nb += 1
                                sh *= 2
                                ww *= 2
                            sw = cur[:, 0:W].rearrange("p (r t) -> p r t", t=stride)[:, :, 8:8 + L]
                            pin = src[:, 0:W].rearrange("p (r t) -> p r t", t=stride)[:, :, 8:8 + L]
                            iv = icnt[ik][:, g, :].rearrange("p (r t) -> p r t", t=L)
                            pt = ptmp[:].rearrange("p (r t) -> p r t", t=L)
                            P.dve(lambda e, pt=pt, sw=sw, iv=iv: e.tensor_tensor(out=pt, in0=sw, in1=iv, op=ALU.mult),
                                  reads=[cres, ("icnt", ik)], writes=["ptmp"])
                            po = pooled[:, g, :].rearrange("p (r t) -> p r t", t=L)
                            P.dve(lambda e, po=po, pt=pt, pin=pin: e.tensor_tensor(out=po, in0=pt, in1=pin, op=ALU.subtract),
                                  reads=["ptmp", sres], writes=[("pooled", g)])
                        for c in range(8):
                            pj = win_mm(i, 4 + c)
                            vi_ = v_rot.next()
                            P.act(lambda e, vi_=vi_, pj=pj: e.activation(out=vst[vi_][:], in_=bank(pj), func=AF.Copy),
                                  reads=[("ps", pj)], writes=[("vst", vi_)])
                            rows = V[c * 128:(c + 1) * 128, :]
                            if i < 2:
                                a = (2 * i) * 260 + 2
                                dst = rows[:, a:a + 520].rearrange("p (s w) -> p s w", w=260)[:, :, 0:256]
                                srcv = vst[vi_][:].rearrange("p (s w) -> p s w", w=256)
                            else:
                                a = 1042 + (i - 2) * TS
                                dst = rows[:, a:a + TS]
                                srcv = vst[vi_][:]
                            P.dma(dst, srcv, reads=[("vst", vi_)], writes=[("V", i, c)], q="actq")
                        for c in range(8):
                            pj = win_mm(i, 20 + c)
                            gi = g_rot.next()
                            P.act(lambda e, gi=gi, pj=pj: e.activation(out=gst_[gi][:], in_=bank(pj), func=AF.Sigmoid),
                                  reads=[("ps", pj)], writes=[("gst", gi)])
                            P.dma(G[c * 128:(c + 1) * 128, i * TS:(i + 1) * TS], gst_[gi][:], reads=[("gst", gi)], writes=[("G", i, c)], q="actq")
                        for g in range(4):
                            pj = ps_rot.next()
                            P.pe(lambda e, g=g, pj=pj: e.matmul(bank(pj), lhsT=pwb[:, g, :], rhs=pooled[:, g, :], start=True, stop=True),
                                 reads=["pwb", ("pooled", g)], writes=[("ps", pj)])
                            P.act(lambda e, g=g, pj=pj: e.activation(out=ypool[:, g, :], in_=bank(pj), func=AF.Identity, scale=f[:, 128 + g:129 + g]),
                                  reads=[("ps", pj), fr], writes=[("ypool", g)])
                        for k in range(8):
                            pj = win_mm(i, 12 + k)
                            gi = gp_rot.next()
                            P.act(lambda e, gi=gi, pj=pj: e.activation(out=gpb[gi][:], in_=bank(pj), func=AF.Sigmoid),
                                  reads=[("ps", pj)], writes=[("gp", gi)])
                            pj2 = ps_rot.next()
                            for kc in range(4):
                                P.pe(lambda e, kc=kc, k=k, pj2=pj2: e.matmul(bank(pj2), lhsT=wbpb[:, kc, k * 128:(k + 1) * 128], rhs=ypool[:, kc, :],
                                                                              start=(kc == 0), stop=(kc == 3)),
                                     reads=["wbpb"] + [("ypool", g) for g in range(4)], writes=[("ps", pj2)], signal=(kc == 3))
                            mi = mp_rot.next()
                            P.dve(lambda e, mi=mi, pj2=pj2, gi=gi: e.tensor_tensor(out=mpst[mi][:], in0=bank(pj2), in1=gpb[gi][:], op=ALU.mult),
                                  reads=[("ps", pj2), ("gp", gi)], writes=[("mpst", mi)])
                            P.dma(MP[k * 128:(k + 1) * 128, i * TS:(i + 1) * TS], mpst[mi][:], reads=[("mpst", mi)], writes=[("MP", i, k)], q="poolq")

                    p1_front(0)
                    for i in range(NTILE):
                        if i + 1 < NTILE:
                            p1_front(i + 1)
                        p1_back(i)
                    P.barrier()
                if phase_end("p1", l):
                    return True

                lst = ExitStack()
                gw = T(lst, "gw", [128, 4, 8, 128], BF16)
                P.pool(lambda e: e.memset(gw[:], 0.0), writes=["gw"])
                for gi_, wsrc in enumerate((lru_wr, lru_wi)):
                    for d in range(2):
                        for hl in range(2):
                            src = wsrc[l, d].rearrange("(c two) dd ee -> two dd c ee", two=2)[hl]
                            P.dma(gw[hl * 64:(hl + 1) * 64, gi_ * 2 + d, :, hl * 64:(hl + 1) * 64], src, writes=["gw"], q="poolq")

                NG = 4

                def mk_sets(st, n=NG):
                    return [(T(st, "vcb%d" % j, [128, TS], BF16), T(st, "rs_%d" % j, [128, TS]), T(st, "is_%d" % j, [128, TS]),
                             T(st, "sq_%d" % j, [128, TS])) for j in range(n)]

                def lru_group(sets, d, cs, vc_aps, res_ins, nseq, inits_list, out_hs, res_outs, reverse):
                    n = len(cs)
                    br = 10 + 3 * d
                    for j in range(n):
                        P.act(lambda e, j=j: e.activation(out=sets[j][0][:], in_=vc_aps[j], func=AF.Copy),
                              reads=[res_ins[j]], writes=[("vcb", j)])
                    prs, pis = {}, {}
                    for h0 in range(0, n, 4):
                        js = list(range(h0, min(h0 + 4, n)))
                        for j in js:
                            pr = ps_rot.next()
                            prs[j] = pr
                            P.pe(lambda e, j=j, pr=pr: e.matmul(bank(pr), lhsT=gw[:, 0 * 2 + d, cs[j], :], rhs=sets[j][0][:], start=True, stop=True),
                                 reads=["gw", ("vcb", j)], writes=[("ps", pr)])
                        for j in js:
                            pi = ps_rot.next()
                            pis[j] = pi
                            P.pe(lambda e, j=j, pi=pi: e.matmul(bank(pi), lhsT=gw[:, 1 * 2 + d, cs[j], :], rhs=sets[j][0][:], start=True, stop=True),
                                 reads=["gw", ("vcb", j)], writes=[("ps", pi)])
                        for j in js:
                            P.act(lambda e, j=j: e.activation(out=sets[j][1][:], in_=bank(prs[j]), func=AF.Sigmoid, bias=FM(l, br, cs[j])),
                                  reads=[("ps", prs[j]), fr], writes=[("rs_", j)])
                        for j in js:
                            P.act(lambda e, j=j: e.activation(out=sets[j][2][:], in_=bank(pis[j]), func=AF.Sigmoid, bias=FM(l, br + 1, cs[j])),
                                  reads=[("ps", pis[j]), fr], writes=[("is_", j)])
                    for j in range(n):
                        P.act(lambda e, j=j: e.activation(out=sets[j][3][:], in_=sets[j][1][:], func=AF.Exp, scale=FM(l, 21 + d, cs[j])),
                              reads=[("rs_", j), fr], writes=[("sq_", j)])
                    for j in range(n):
                        P.act(lambda e, j=j: e.activation(out=sets[j][1][:], in_=sets[j][1][:], func=AF.Exp, scale=FM(l, 19 + d, cs[j])),
                              reads=[("rs_", j), fr], writes=[("rs_", j)])
                    for j in range(n):
                        P.act(lambda e, j=j: e.activation(out=sets[j][3][:], in_=sets[j][3][:], func=AF.Sqrt, scale=-1.0, bias=1.0),
                              reads=[("sq_", j)], writes=[("sq_", j)])
                    for j in range(n):
                        P.dve(lambda e, j=j: e.tensor_tensor(out=sets[j][2][:], in0=sets[j][3][:], in1=sets[j][2][:], op=ALU.mult),
                              reads=[("sq_", j), ("is_", j)], writes=[("is_", j)])
                    for j in range(n):
                        P.dve(lambda e, j=j: e.tensor_tensor(out=sets[j][2][:], in0=sets[j][2][:], in1=vc_aps[j], op=ALU.mult),
                              reads=[("is_", j), res_ins[j]], writes=[("is_", j)])
                    L = TS // nseq
                    for j in range(n):
                        a_, u_, out_h = sets[j][1], sets[j][2], out_hs[j]
                        for s in range(nseq):
                            if reverse:
                                sl = slice((s + 1) * L - 1, (s * L - 1 if s > 0 else None), -1)
                            else:
                                sl = slice(s * L, (s + 1) * L)
                            init = inits_list[j][s]
                            rd = [("rs_", j), ("is_", j)] + ([] if isinstance(init, float) else ["hcar"])
                            P.dve(lambda e, out_h=out_h, a_=a_, u_=u_, sl=sl, init=init: e.tensor_tensor_scan(
                                out=out_h[:, sl], data0=a_[:, sl], data1=u_[:, sl], initial=init, op0=ALU.mult, op1=ALU.add),
                                reads=rd, writes=[res_outs[j]])

                with ExitStack() as st:
                    vtb = [T(st, "vt%d" % i, [128, 8, 520]) for i in range(3)]
                    vcs = [T(st, "vcs%d" % i, [128, TS]) for i in range(16)]
                    hfs = [T(st, "hfs%d" % i, [128, TS]) for i in range(12)]
                    sets = mk_sets(st, 8)
                    vc_rot, hf_rot = Rot(range(16)), Rot(range(12))

                    def p2_load(i):
                        vt = vtb[i % 3]
                        if i < 2:
                            a = (2 * i) * 260
                            P.dma(vt[:], fmview(V[:, a:a + 520]), reads=[("V", i, c) for c in range(8)], writes=[("vt", i % 3)])
                        else:
                            a = 1040 + (i - 2) * TS
                            rd = [("V", ii, c) for ii in (i - 1, i, i + 1) if 2 <= ii < NTILE for c in range(8)]
                            P.dma(vt[:, :, 0:515], fmview(V[:, a:a + 515]), reads=rd, writes=[("vt", i % 3)])

                    tile_state = {}

                    def p2_conv(i):
                        vt = vtb[i % 3]
                        cs = list(range(8))
                        cis = [vc_rot.next() for _ in cs]
                        vos, tapss = [], []
                        for j, c in enumerate(cs):
                            vc = vcs[cis[j]]
                            if i < 2:
                                vin = vt[:, c, :].rearrange("p (s w) -> p s w", w=260)
                                vos.append(vc[:].rearrange("p (s w) -> p s w", w=256))
                                tapss.append([vin[:, :, k:k + 256] for k in range(4)])
                            else:
                                vos.append(vc[:])
                                tapss.append([vt[:, c, k:k + TS] for k in range(4)])
                        for j, c in enumerate(cs):
                            P.dve(lambda e, j=j, c=c: e.tensor_scalar(out=vos[j], in0=tapss[j][0], scalar1=FM(l, 5, c), scalar2=FM(l, 9, c),
                                                                     op0=ALU.mult, op1=ALU.add),
                                  reads=[("vt", i % 3), fr], writes=[("vcs", cis[j])])
                        for k in range(1, 4):
                            for j, c in enumerate(cs):
                                P.dve(lambda e, j=j, c=c, k=k: e.scalar_tensor_tensor(out=vos[j], in0=tapss[j][k], scalar=FM(l, 5 + k, c), in1=vos[j],
                                                                                     op0=ALU.mult, op1=ALU.add),
                                      reads=[("vt", i % 3), fr, ("vcs", cis[j])], writes=[("vcs", cis[j])])
                        for j, c in enumerate(cs):
                            P.dma(VC[c * 128:(c + 1) * 128, i * TS:(i + 1) * TS], vcs[cis[j]][:], reads=[("vcs", cis[j])], writes=[("VC", i, c)], q="poolq")
                        tile_state[i] = cis

                    def p2_lru(i):
                        cis = tile_state.pop(i)
                        cs = list(range(8))
                        nseq = 2 if i < 2 else 1
                        his = [hf_rot.next() for _ in cs]
                        inits_list = []
                        for c in cs:
                            if i < 2:
                                inits_list.append([0.0, 0.0])
                            elif i == 2:
                                inits_list.append([h0fm[:, (l * 2 + 0) * 8 + c:(l * 2 + 0) * 8 + c + 1]])
                            else:
                                inits_list.append([hcar[:, c:c + 1]])
                        lru_group(sets, 0, cs, [vcs[ci][:] for ci in cis], [("vcs", ci) for ci in cis], nseq, inits_list,
                                  [hfs[hi] for hi in his], [("hfs", hi) for hi in his], False)
                        for j, c in enumerate(cs):
                            hf = hfs[his[j]]
                            if i >= 2:
                                P.dve(lambda e, hf=hf, c=c: e.tensor_copy(out=hcar[:, c:c + 1], in_=hf[:, TS - 1:TS]),
                                      reads=[("hfs", his[j])], writes=["hcar"])
                            else:
                                for s in range(2):
                                    req = 2 * i + s
                                    col = ((l * 2 + 0) * 4 + req) * 8 + c
                                    P.dve(lambda e, hf=hf, s=s, col=col: e.tensor_copy(out=nsfm[:, col:col + 1], in_=hf[:, (s + 1) * 256 - 1:(s + 1) * 256]),
                                          reads=[("hfs", his[j])], writes=["nsfm"])
                            P.dma(HF[c * 128:(c + 1) * 128, i * TS:(i + 1) * TS], hf[:], reads=[("hfs", his[j])], writes=[("HF", i, c)], q="poolq")

                    p2_load(0)
                    p2_load(1)
                    p2_conv(0)
                    for i in range(NTILE):
                        if i + 2 < NTILE:
                            p2_load(i + 2)
                        if i + 1 < NTILE:
                            p2_conv(i + 1)
                        p2_lru(i)
                    P.barrier()
                if phase_end("p2", l):
                    lst.close()
                    return True

                def topk_gen(src, vals, idxs, niter, tag, srcres):
                    P.last_w[tag + "src"] = P.last_w.get(srcres)
                    for it in range(niter):
                        sl = slice(it * 8, it * 8 + 8)
                        P.dve(lambda e, sl=sl: e.max(out=vals[:, sl], in_=src), reads=[tag + "src"], writes=[tag + "v"])
                        P.dve(lambda e, sl=sl: e.max_index(out=idxs[:, sl], in_max=vals[:, sl], in_values=src), reads=[tag + "src", tag + "v"], writes=[tag + "i"])
                        if it + 1 < niter:
                            P.dve(lambda e, sl=sl: e.match_replace(out=src, in_to_replace=vals[:, sl], in_values=src, imm_value=-1.0),
                                  reads=[tag + "src", tag + "v"], writes=[tag + "src"])
                        yield

                if True:
                    def load_bc(vi):
                        P.dma(g1bc[:], MOD[l, vi, 2 * D:3 * D].partition_broadcast(128), writes=["g1bc"])
                        P.dma(a2bc[:], MOD[l, vi, 4 * D:5 * D].partition_broadcast(128), writes=["a2bc"])
                        P.dma(b2bc[:], MOD[l, vi, 3 * D:4 * D].partition_broadcast(128), writes=["b2bc"])
                        P.dma(g2n[:], norm2_g[l, :].partition_broadcast(128), writes=["g2n"])
                        P.dve(lambda e: e.scalar_tensor_tensor(out=a2bc[:], in0=a2bc[:], scalar=1.0, in1=g2n[:], op0=ALU.add, op1=ALU.mult),
                              reads=["a2bc", "g2n"], writes=["a2bc"])

                    def p3_lru(i, groups=(0,), gsz=8):
                        nseq = 2 if i < 2 else 1
                        cols = slice(i * TS, (i + 1) * TS)
                        ylru = ylru2[i % 2]
                        for g0 in groups:
                            cs = list(range(g0, g0 + gsz))
                            vis = [vcl_rot.next() for _ in cs]
                            his = [hfl_rot.next() for _ in cs]
                            bis = [hb_rot.next() for _ in cs]
                            for j, c in enumerate(cs):
                                rows = slice(c * 128, (c + 1) * 128)
                                P.dma(vcl[vis[j]][:], VC[rows, cols], reads=[("VC", i, c)], writes=[("vcl", vis[j])])
                            for j, c in enumerate(cs):
                                rows = slice(c * 128, (c + 1) * 128)
                                P.dma(hfl[his[j]][:], HF[rows, cols], reads=[("HF", i, c)], writes=[("hfl", his[j])])
                            inits_list = []
                            for c in cs:
                                if i < 2:
                                    inits_list.append([0.0, 0.0])
                                elif i == NTILE - 1:
                                    inits_list.append([h0fm[:, (l * 2 + 1) * 8 + c:(l * 2 + 1) * 8 + c + 1]])
                                else:
                                    inits_list.append([hcar[:, c:c + 1]])
                            lru_group(sets, 1, cs, [vcl[v][:] for v in vis], [("vcl", v) for v in vis], nseq, inits_list,
                                      [hbs[b] for b in bis], [("hbs", b) for b in bis], True)
                            for j, c in enumerate(cs):
                                hb = hbs[bis[j]]
                                if i >= 2:
                                    P.dve(lambda e, hb=hb, c=c: e.tensor_copy(out=hcar[:, c:c + 1], in_=hb[:, 0:1]),
                                          reads=[("hbs", bis[j])], writes=["hcar"])
                                else:
                                    for s in range(2):
                                        req = 2 * i + s
                                        col = ((l * 2 + 1) * 4 + req) * 8 + c
                                        P.dve(lambda e, hb=hb, s=s, col=col: e.tensor_copy(out=nsfm[:, col:col + 1], in_=hb[:, s * 256:s * 256 + 1]),
                                              reads=[("hbs", bis[j])], writes=["nsfm"])
                            for j, c in enumerate(cs):
                                P.pool(lambda e, j=j, c=c: e.tensor_tensor(out=ylru[:, c, :], in0=hbs[bis[j]][:], in1=hfl[his[j]][:], op=ALU.add),
                                       reads=[("hbs", bis[j]), ("hfl", his[j])], writes=[("ylru", i % 2, c)])

                    def p3_merge(i):
                        cols = slice(i * TS, (i + 1) * TS)
                        ylru = ylru2[i % 2]
                        merged = mergedb[i % 2]
                        for oc in range(8):
                            gi_, mi_ = gl_rot.next(), mpl_rot.next()
                            rows = slice(oc * 128, (oc + 1) * 128)
                            P.dma(gl_[gi_][:], G[rows, cols], reads=[("G", i, oc)], writes=[("gl", gi_)])
                            P.dma(mpl[mi_][:], MP[rows, cols], reads=[("MP", i, oc)], writes=[("mpl", mi_)])
                            pj = ps_rot.next()
                            for kc in range(8):
                                P.pe(lambda e, kc=kc, oc=oc, pj=pj: e.matmul(bank(pj), lhsT=wblb[:, kc, oc * 128:(oc + 1) * 128], rhs=ylru[:, kc, :],
                                                                              start=(kc == 0), stop=(kc == 7)),
                                     reads=["wblb"] + [("ylru", i % 2, k) for k in range(8)], writes=[("ps", pj)], signal=(kc == 7))
                            ti = mt_rot.next()
                            P.dve(lambda e, pj=pj, gi_=gi_, ti=ti: e.tensor_tensor(out=mtmp[ti][:], in0=bank(pj), in1=gl_[gi_][:], op=ALU.mult),
                                  reads=[("ps", pj), ("gl", gi_)], writes=[("mtmp", ti)])
                            P.pool(lambda e, oc=oc, mi_=mi_, ti=ti: e.tensor_tensor(out=merged[:, oc, :], in0=mtmp[ti][:], in1=mpl[mi_][:], op=ALU.add),
                                   reads=[("mtmp", ti), ("mpl", mi_)], writes=[("merged", i % 2, oc)])
                        P.dma(fmview(MG[:, cols]), merged[:], reads=[("merged", i % 2, oc) for oc in range(8)], writes=[("MG", i)], q="poolq")

                    def p3_wout(i, ss):
                        K_ = list(range(len(ss)))
                        r0s = [i * TS + s * 128 for s in ss]
                        merged = mgl[i % 2]
                        for k in K_:
                            P.dma(xlb[k][:], Xsrc[r0s[k]:r0s[k] + 128, :], writes=[("xl", k)])
                        for k in K_:
                            s = ss[k]
                            for half in range(2):
                                pj = ps_rot.next()
                                hs = slice(half * 512, (half + 1) * 512)
                                for kc in range(8):
                                    P.pe(lambda e, kc=kc, pj=pj, s=s, hs=hs: e.matmul(bank(pj), lhsT=merged[:, kc, s * 128:(s + 1) * 128], rhs=woutb[:, kc, hs],
                                                                                       start=(kc == 0), stop=(kc == 7)),
                                         reads=["woutb", ("mgl", i % 2)], writes=[("ps", pj)], signal=(kc == 7))
                                P.dve(lambda e, pj=pj, hs=hs, k=k: e.tensor_tensor(out=hn2[k][:, hs], in0=bank(pj), in1=g1bc[:, hs], op=ALU.mult),
                                      reads=[("ps", pj), "g1bc"], writes=[("hn2", k)])
                                P.pool(lambda e, hs=hs, k=k: e.tensor_tensor(out=xlb[k][:, hs], in0=xlb[k][:, hs], in1=hn2[k][:, hs], op=ALU.add),
                                       reads=[("hn2", k), ("xl", k)], writes=[("xl", k)])
                        for k in K_:
                            P.dma(X[r0s[k]:r0s[k] + 128, :], xlb[k][:], reads=[("xl", k)], writes=[("X", i, ss[k])], q="poolq")
                        for k in K_:
                            P.act(lambda e, k=k: e.activation(out=junk[:], in_=xlb[k][:], func=AF.Square, accum_out=sm[k][:, 0:1]),
                                  reads=[("xl", k)], writes=[("sm0", k)])
                        for k in K_:
                            P.act(lambda e, k=k: e.activation(out=sm[k][:, 0:1], in_=sm[k][:, 0:1], func=AF.Sqrt, scale=1.0 / D, bias=epsT[:, 0:1]),
                                  reads=[("sm0", k), "epsT"], writes=[("sm0", k)])
                        for k in K_:
                            P.dve(lambda e, k=k: e.reciprocal(out=sm[k][:, 1:2], in_=sm[k][:, 0:1]), reads=[("sm0", k)], writes=[("sm1", k)])
                        for k in K_:
                            P.dve(lambda e, k=k: e.scalar_tensor_tensor(out=hn2[k][:], in0=xlb[k][:], scalar=sm[k][:, 1:2], in1=a2bc[:], op0=ALU.mult, op1=ALU.mult),
                                  reads=[("xl", k), ("sm1", k), "a2bc"], writes=[("hn2", k)])
                        for k in K_:
                            P.pool(lambda e, k=k: e.tensor_tensor(out=hn2[k][:], in0=hn2[k][:], in1=b2bc[:], op=ALU.add),
                                   reads=[("hn2", k), "b2bc"], writes=[("hn2", k)])
                        for k in K_:
                            P.act(lambda e, k=k: e.activation(out=hn2b[k][:], in_=hn2[k][:], func=AF.Copy),
                                  reads=[("hn2", k)], writes=[("hn2b", k)])
                            P.dma(HN2[r0s[k]:r0s[k] + 128, :], hn2b[k][:], reads=[("hn2b", k)], writes=[("HN2", i, ss[k])], q="actq")
                        for k in K_:
                            for h2 in range(2):
                                pj = ps_rot.next()
                                for cc in range(4):
                                    c = h2 * 4 + cc
                                    P.pe(lambda e, c=c, cc=cc, pj=pj, k=k: e.transpose(out=bank(pj)[:, cc * 128:(cc + 1) * 128], in_=hn2[k][:, c * 128:(c + 1) * 128], identity=ident[:]),
                                         reads=[("hn2", k), "ident"], writes=[("ps", pj)], signal=(cc == 3))
                                P.act(lambda e, pj=pj, h2=h2, k=k: e.activation(out=hn2T[k][:, h2 * 4:(h2 + 1) * 4, :], in_=bank(pj).rearrange("p (c t) -> p c t", t=128), func=AF.Copy),
                                      reads=[("ps", pj)], writes=[("hn2T", k, h2)])
                        pjs = []
                        for k in K_:
                            pj = ps_rot.next()
                            pjs.append(pj)
                            for kc in range(8):
                                P.pe(lambda e, kc=kc, pj=pj, k=k: e.matmul(bank(pj)[:, 0:NEXP], lhsT=hn2T[k][:, kc, :], rhs=rwt[:, kc, :], start=(kc == 0), stop=(kc == 7)),
                                     reads=["rwt", ("hn2T", k, 0), ("hn2T", k, 1)], writes=[("ps", pj)], signal=(kc == 7))
                        for k in K_:
                            P.dve(lambda e, k=k: e.reduce_max(out=sm[k][:, 2:3], in_=bank(pjs[k])[:, 0:NEXP], axis=AX.X), reads=[("ps", pjs[k])], writes=[("sm2", k)])
                        for k in K_:
                            P.dve(lambda e, k=k: e.tensor_scalar(out=sm[k][:, 3:4], in0=sm[k][:, 2:3], scalar1=-1.0, scalar2=None, op0=ALU.mult), reads=[("sm2", k)], writes=[("sm3", k)])
                        for k in K_:
                            P.act(lambda e, k=k: e.activation(out=ex[k][:], in_=bank(pjs[k])[:, 0:NEXP], func=AF.Exp, bias=sm[k][:, 3:4], accum_out=sm[k][:, 4:5]),
                                  reads=[("ps", pjs[k]), ("sm3", k)], writes=[("ex", k), ("sm4", k)])
                        for k in K_:
                            P.dve(lambda e, k=k: e.reciprocal(out=sm[k][:, 5:6], in_=sm[k][:, 4:5]), reads=[("sm4", k)], writes=[("sm5", k)])
                        hb0 = 32 if (i >= 2 and (i - 2) >= 4) else 0
                        for k in K_:
                            P.dve(lambda e, k=k: e.tensor_scalar(out=affpad[k][:, hb0:hb0 + NEXP], in0=ex[k][:], scalar1=sm[k][:, 5:6], scalar2=None, op0=ALU.mult),
                                  reads=[("ex", k), ("sm5", k)], writes=[("affpad", k)])
                        pts = []
                        for k in K_:
                            pj = ps_rot.next()
                            pts.append(pj)
                            P.pe(lambda e, pj=pj, k=k: e.transpose(out=bank(pj)[:, 0:128], in_=affpad[k][:], identity=ident[:]),
                                 reads=[("affpad", k), "ident"], writes=[("ps", pj)])
                        for k in K_:
                            s = ss[k]
                            pj = pts[k]
                            if i < 2:
                                req = 2 * i + s // 2
                                tc0 = (s % 2) * 128
                                P.act(lambda e, pj=pj, k=k: e.activation(out=afst[k][0:NEXP, :], in_=bank(pj)[0:NEXP, 0:128], func=AF.Copy),
                                      reads=[("ps", pj)], writes=[("afst", k)])
                                P.dma(affP[NEXP * req:NEXP * req + NEXP, tc0:tc0 + 128], afst[k][0:NEXP, :], reads=[("afst", k)], writes=["affP"], q="actq")
                            else:
                                tc0 = ((i - 2) % 4) * TS + s * 128
                                P.act(lambda e, pj=pj, tc0=tc0: e.activation(out=affS[hb0:hb0 + NEXP, tc0:tc0 + 128], in_=bank(pj)[hb0:hb0 + NEXP, 0:128], func=AF.Copy),
                                      reads=[("ps", pj)], writes=["affS"])

                order = list(range(NTILE - 1, -1, -1))
                NW = 4
                with ExitStack() as st:
                    wblb = T(st, "wblb", [128, 8, D], BF16)
                    for j in range(2):
                        P.dma(wblb[:, :, j * 512:(j + 1) * 512], fmview(w_br_lru[l][:, j * 512:(j + 1) * 512]), writes=["wblb"], q="poolq")
                    vcl = [T(st, "vcl%d" % i, [128, TS]) for i in range(16)]
                    hfl = [T(st, "hfl%d" % i, [128, TS]) for i in range(12)]
                    gl_ = [T(st, "gl%d" % i, [128, TS], BF16) for i in range(3)]
                    mpl = [T(st, "mpl%d" % i, [128, TS]) for i in range(3)]
                    hbs = [T(st, "hbs%d" % i, [128, TS]) for i in range(8)]
                    sets = mk_sets(st, 8)
                    ylru2 = [T(st, "ylru%d" % i, [128, 8, TS], BF16) for i in range(2)]
                    mergedb = [T(st, "merged%d" % i, [128, 8, TS], BF16) for i in range(2)]
                    mtmp = [T(st, "mtmp%d" % i, [128, TS]) for i in range(2)]
                    vcl_rot, hfl_rot, gl_rot, mpl_rot, hb_rot, mt_rot = Rot(range(16)), Rot(range(12)), Rot(range(3)), Rot(range(3)), Rot(range(8)), Rot(range(2))
                    p3_lru(order[0])
                    for n_, i in enumerate(order):
                        nx = order[n_ + 1] if n_ + 1 < len(order) else None
                        if nx is not None:
                            p3_lru(nx)
                        p3_merge(i)
                    P.barrier()
                if phase_end("p3a", l):
                    lst.close()
                    return True
                affS = T(lst, "affS", [48, 2048])
                P.dve(lambda e: e.memset(affS[:], 0.0), writes=["affS"])
                affP = T(lst, "affP", [64, 256])
                P.dve(lambda e: e.memset(affP[:], 0.0), writes=["affP"])
                vS = T(lst, "vS", [48, 512])
                iS = T(lst, "iS", [48, 512], U32)
                with ExitStack() as st:
                    woutb = T(st, "woutb", [128, 8, D], BF16)
                    rwt = T(st, "rwt", [128, 8, NEXP])
                    for j in range(2):
                        P.dma(woutb[:, :, j * 512:(j + 1) * 512], fmview(w_out[l][:, j * 512:(j + 1) * 512]), writes=["woutb"], q="poolq")
                    P.dma(rwt[:], fmview(router_w[l]), writes=["rwt"])
                    g1bc = T(st, "g1bc", [128, D])
                    a2bc = T(st, "a2bc", [128, D])
                    b2bc = T(st, "b2bc", [128, D])
                    g2n = T(st, "g2n", [128, D])
                    mgl = [T(st, "mgl%d" % i, [128, 8, TS], BF16) for i in range(2)]
                    xlb = [T(st, "xl%d" % i, [128, D]) for i in range(NW)]
                    hn2 = [T(st, "hn2_%d" % i, [128, D]) for i in range(NW)]
                    hn2b = [T(st, "hn2b%d" % i, [128, D], BF16) for i in range(NW)]
                    hn2T = [T(st, "hn2T%d" % i, [128, 8, 128]) for i in range(NW)]
                    junk = T(st, "junk3", [128, D], BF16)
                    sm = [T(st, "sm%d" % i, [128, 8]) for i in range(NW)]
                    ex = [T(st, "ex%d" % i, [128, NEXP]) for i in range(NW)]
                    affpad = [T(st, "affpad%d" % i, [128, 128]) for i in range(NW)]
                    afst = [T(st, "afst%d" % i, [NEXP, 128]) for i in range(NW)]
                    for k in range(NW):
                        P.dve(lambda e, k=k: e.memset(affpad[k][:], 0.0), writes=[("affpad", k)])

                    def mg_load(i):
                        P.dma(mgl[i % 2][:], fmview(MG[:, i * TS:(i + 1) * TS]), reads=[("MG", i)], writes=[("mgl", i % 2)])

                    load_bc(1)
                    mg_load(order[0])
                    for n_, i in enumerate(order):
                        nx = order[n_ + 1] if n_ + 1 < len(order) else None
                        if nx is not None:
                            mg_load(nx)
                        if i == 1:
                            load_bc(0)
                        p3_wout(i, [0, 1, 2, 3])
                        if i == 2:
                            sgen = topk_gen(affS[:], vS, iS, 64, "S", "affS")
                            P.inject = sgen
                    P.inject = None
                    P.barrier()
                if phase_end("p3", l):
                    lst.close()
                    return True

                idxS = T(lst, "idxS", [128, NEXP, 4], I32)
                gatS = T(lst, "gatS", [128, NEXP, 4])
                idxP = T(lst, "idxP", [128, NEXP], I32)
                gatP = T(lst, "gatP", [128, NEXP])
                with ExitStack() as st:
                    fS = T(st, "fS", [16, 512])
                    iS2 = T(st, "iS2", [16, 512], I32)
                    wkP = T(st, "wkP", [64, 256])
                    vP = T(st, "vP", [64, 32])
                    iP = T(st, "iP", [64, 32], U32)
                    fP = T(st, "fP", [64, 32])
                    iP2 = T(st, "iP2", [64, 32], I32)
                    offi = T(st, "offi", [4, NEXP], I32)
                    offf = T(st, "offf", [4, NEXP])
                    offPf = T(st, "offPf", [64, 1])
                    P.pool(lambda e: e.iota(offi[:], pattern=[[0, NEXP]], base=0, channel_multiplier=256), writes=["offi"])
                    P.dve(lambda e: e.tensor_copy(out=offf[:], in_=offi[:]), reads=["offi"], writes=["offf"])
                    P.dma(OFFS.rearrange("(r e) -> r e", e=NEXP), offf[:], reads=["offf"], writes=["OFFS"])
                    P.dma(offPf[:], OFFS.rearrange("(p o) -> p o", o=1), reads=["OFFS"], writes=["offPf"])

                    def topk(src, wk, vals, idxs, niter, tag):
                        cur = src
                        cres = tag + "src"
                        for it in range(niter):
                            sl = slice(it * 8, it * 8 + 8)
                            P.dve(lambda e, cur=cur, sl=sl: e.max(out=vals[:, sl], in_=cur), reads=[cres], writes=[tag + "v"])
                            P.dve(lambda e, cur=cur, sl=sl: e.max_index(out=idxs[:, sl], in_max=vals[:, sl], in_values=cur), reads=[cres, tag + "v"], writes=[tag + "i"])
                            if it + 1 < niter:
                                P.dve(lambda e, cur=cur, sl=sl: e.match_replace(out=wk, in_to_replace=vals[:, sl], in_values=cur, imm_value=-1.0),
                                      reads=[cres, tag + "v"], writes=[tag + "wk"])
                                cur = wk
                                cres = tag + "wk"

                    P.last_w["Psrc"] = P.last_w.get("affP")
                    topk(affP[:], wkP[:], vP, iP, 4, "P")
                    P.dve(lambda e: e.tensor_copy(out=fP[:], in_=iP[:]), reads=["Pi"], writes=["fP"])
                    P.dve(lambda e: e.tensor_scalar(out=fP[:], in0=fP[:], scalar1=offPf[:, 0:1], scalar2=None, op0=ALU.add), reads=["fP", "offPf"], writes=["fP"])
                    P.dve(lambda e: e.tensor_copy(out=iP2[:], in_=fP[:]), reads=["fP"], writes=["iP2"])
                    P.dma(PIDX, iP2[:], reads=["iP2"], writes=["PIDX"])
                    P.dma(PGATE, vP[:], reads=["Pv"], writes=["PGATE"])
                    with nc.allow_non_contiguous_dma(reason="tiny index relayout"):
                        for r in range(4):
                            P.dma(idxP[32 * r:32 * r + 32, :], PIDX[NEXP * r:NEXP * r + NEXP, :].rearrange("e k -> k e"), reads=["PIDX"], writes=["idxP"])
                            P.dma(gatP[32 * r:32 * r + 32, :], PGATE[NEXP * r:NEXP * r + NEXP, :].rearrange("e k -> k e"), reads=["PGATE"], writes=["gatP"])
                    for _ in sgen:
                        pass
                    fSall = T(st, "fSall", [48, 512])
                    vB = T(st, "vB", [16, 512])
                    iBf = T(st, "iBf", [16, 512])
                    mk = T(st, "mk", [16, 512])
                    vM = T(st, "vM", [16, 512])
                    P.dve(lambda e: e.tensor_copy(out=fSall[:], in_=iS[:]), reads=["Si"], writes=["fSall"])
                    P.dma(vB[:], vS[32:48, :], reads=["Sv"], writes=["vB"])
                    P.dma(iBf[:], fSall[32:48, :], reads=["fSall"], writes=["iBf"])
                    P.dve(lambda e: e.tensor_scalar(out=iBf[:], in0=iBf[:], scalar1=2048.0, scalar2=None, op0=ALU.add), reads=["iBf"], writes=["iBf"])
                    P.dve(lambda e: e.tensor_tensor(out=mk[:], in0=vS[0:16, :], in1=vB[:, ::-1], op=ALU.is_ge), reads=["Sv", "vB"], writes=["mk"])
                    P.dve(lambda e: e.tensor_tensor(out=vM[:], in0=vS[0:16, :], in1=vB[:, ::-1], op=ALU.max), reads=["Sv", "vB"], writes=["vM"])
                    P.dve(lambda e: e.tensor_tensor(out=fS[:], in0=fSall[0:16, :], in1=iBf[:, ::-1], op=ALU.subtract), reads=["fSall", "iBf"], writes=["fS"])
                    P.dve(lambda e: e.tensor_tensor(out=fS[:], in0=fS[:], in1=mk[:], op=ALU.mult), reads=["fS", "mk"], writes=["fS"])
                    P.dve(lambda e: e.tensor_tensor(out=fS[:], in0=fS[:], in1=iBf[:, ::-1], op=ALU.add), reads=["fS", "iBf"], writes=["fS"])
                    P.dve(lambda e: e.tensor_scalar(out=fS[:], in0=fS[:], scalar1=1024.0, scalar2=None, op0=ALU.add), reads=["fS"], writes=["fS"])
                    P.dve(lambda e: e.tensor_copy(out=iS2[:], in_=fS[:]), reads=["fS"], writes=["iS2"])
                    P.dma(SIDX, iS2[:], reads=["iS2"], writes=["SIDX"])
                    P.dma(SGATE, vM[:], reads=["vM"], writes=["SGATE"])
                    with nc.allow_non_contiguous_dma(reason="tiny index relayout"):
                        P.dma(idxS[:], SIDX.rearrange("e (g p) -> p e g", p=128), reads=["SIDX"], writes=["idxS"])
                        P.dma(gatS[:], SGATE.rearrange("e (g p) -> p e g", p=128), reads=["SGATE"], writes=["gatS"])
                    P.barrier()
                if phase_end("route", l):
                    lst.close()
                    return True

                with ExitStack() as st:
                    g2bc = [T(st, "g2bc%d" % v, [128, D]) for v in range(2)]
                    for v in range(2):
                        P.dma(g2bc[v][:], MOD[l, v, 5 * D:6 * D].partition_broadcast(128), writes=[("g2bc", v)])
                    xg = [T(st, "xg%d" % g, [128, D], BF16) for g in range(5)]
                    xsT = T(st, "xsT", [128, 8, 640], BF16)
                    w1q = [T(st, "w1q%d" % i, [128, 8, 512], BF16) for i in range(2)]
                    w3q = [T(st, "w3q%d" % i, [128, 8, 512], BF16) for i in range(2)]
                    w2h = [T(st, "w2h%d" % i, [128, 16, 512], BF16) for i in range(2)]
                    hid = T(st, "hid", [128, 16, 640], BF16)
                    s1 = [T(st, "s1_%d" % i, [128, 640]) for i in range(2)]
                    osb = [T(st, "osb%d" % g, [128, D]) for g in range(5)]
                    wq_rot, w2_rot, s1_rot, hp_rot = Rot(range(2)), Rot(range(2)), Rot(range(2)), Rot(range(2))

                    def moe_load1(e_, q):
                        wi = wq_rot.next()
                        P.dma(w1q[wi][:], fmview(exp_w1[l, e_][:, q * 512:(q + 1) * 512]), writes=[("w1q", wi)], q="poolq")
                        P.dma(w3q[wi][:], fmview(exp_w3[l, e_][:, q * 512:(q + 1) * 512]), writes=[("w3q", wi)], q="poolq")
                        return wi

                    def moe_load2(e_, half):
                        wi = w2_rot.next()
                        P.dma(w2h[wi][:], exp_w2[l, e_][:, half * 512:(half + 1) * 512].rearrange("(c p) d -> p c d", p=128), writes=[("w2h", wi)], q="poolq")
                        return wi

                    def moe_gather(e_):
                        for g in range(5):
                            ia = idxS[:, e_, g:g + 1] if g < 4 else idxP[:, e_:e_ + 1]
                            ir = "idxS" if g < 4 else "idxP"
                            P.op("pool", lambda e, g=g, ia=ia: e.indirect_dma_start(out=xg[g][:], out_offset=None, in_=HN2,
                                                                                     in_offset=bass.IndirectOffsetOnAxis(ap=ia, axis=0)),
                                 reads=[ir], writes=[("xg", g)], dmaq="poolq")

                    def moe_xpose(e_):
                        for g in range(5):
                            pj = ps_rot.next()
                            pbb = bank(pj).bitcast(BF16)
                            for c in range(8):
                                P.pe(lambda e, g=g, c=c, pbb=pbb: e.transpose(out=pbb[:, c * 128:(c + 1) * 128], in_=xg[g][:, c * 128:(c + 1) * 128], identity=identb[:]),
                                     reads=[("xg", g), "identb"], writes=[("ps", pj)], signal=(c == 7))
                            P.act(lambda e, g=g, pbb=pbb: e.activation(out=xsT[:, :, g * 128:(g + 1) * 128], in_=pbb.rearrange("p (c t) -> p c t", t=128), func=AF.Copy),
                                  reads=[("ps", pj)], writes=["xsT"])

                    moe_gather(0)
                    w1i = moe_load1(0, 0)
                    moe_xpose(0)
                    for e_ in range(NEXP):
                        w2is = [None, None]
                        for q in range(4):
                            nxt = moe_load1(e_, q + 1) if q < 3 else None
                            if q == 1:
                                w2is[0] = moe_load2(e_, 0)
                            if q == 2:
                                w2is[1] = moe_load2(e_, 1)
                                if e_ + 1 < NEXP:
                                    moe_gather(e_ + 1)
                            for fc in range(4):
                                fcg = q * 4 + fc
                                hk = hp_rot.next()
                                H1, H3 = PS[2 * hk], PS[2 * hk + 1]
                                for (Hh, wt, wr) in ((H1, w1q[w1i], ("w1q", w1i)), (H3, w3q[w1i], ("w3q", w1i))):
                                    pidx = 2 * hk if Hh is H1 else 2 * hk + 1
                                    for kc in range(8):
                                        P.pe(lambda e, Hh=Hh, wt=wt, kc=kc, fc=fc: e.matmul(Hh[:, 0:512], lhsT=wt[:, kc, fc * 128:(fc + 1) * 128], rhs=xsT[:, kc, 0:512],
                                                                                             start=(kc == 0), stop=(kc == 7)),
                                             reads=[wr, "xsT"], writes=[("ps", 2 * pidx)], signal=False)
                                        P.pe(lambda e, Hh=Hh, wt=wt, kc=kc, fc=fc: e.matmul(Hh[:, 512:640], lhsT=wt[:, kc, fc * 128:(fc + 1) * 128], rhs=xsT[:, kc, 512:640],
                                                                                             start=(kc == 0), stop=(kc == 7)),
                                             reads=[wr, "xsT"], writes=[("ps", 2 * pidx + 1)], signal=(kc == 7))
                                si = s1_rot.next()
                                r1 = [("ps", 4 * hk), ("ps", 4 * hk + 1)]
                                r3 = [("ps", 4 * hk + 2), ("ps", 4 * hk + 3)]
                                P.act(lambda e, H1=H1, si=si: e.activation(out=s1[si][:], in_=H1[:, 0:640], func=AF.Silu),
                                      reads=r1, writes=[("s1", si)])
                                P.dve(lambda e, H3=H3, si=si, fcg=fcg: e.tensor_tensor(out=hid[:, fcg, :], in0=H3[:, 0:640], in1=s1[si][:], op=ALU.mult),
                                      reads=r3 + [("s1", si)], writes=[("hid", fcg)])
                            w1i = nxt
                        if e_ + 1 < NEXP:
                            w1i = moe_load1(e_ + 1, 0)
                        for half in range(2):
                            w2i = w2is[half]
                            hs = slice(half * 512, (half + 1) * 512)
                            for g in range(5):
                                pj = ps_rot.next()
                                for fcg in range(16):
                                    P.pe(lambda e, pj=pj, fcg=fcg, g=g, w2i=w2i: e.matmul(bank(pj), lhsT=hid[:, fcg, g * 128:(g + 1) * 128], rhs=w2h[w2i][:, fcg, :],
                                                                                           start=(fcg == 0), stop=(fcg == 15)),
                                         reads=[("w2h", w2i)] + [("hid", k) for k in range(16)], writes=[("ps", pj)], signal=(fcg == 15))
                                ga = gatS[:, e_, g:g + 1] if g < 4 else gatP[:, e_:e_ + 1]
                                gr = "gatS" if g < 4 else "gatP"
                                v_ = 1 if g < 4 else 0
                                P.dve(lambda e, pj=pj, g=g, ga=ga, hs=hs, v_=v_: e.scalar_tensor_tensor(out=osb[g][:, hs], in0=bank(pj), scalar=ga, in1=g2bc[v_][:, hs],
                                                                                                        op0=ALU.mult, op1=ALU.mult),
                                      reads=[("ps", pj), gr, ("g2bc", v_)], writes=[("osb", g)])
                            if half == 0 and e_ + 1 < NEXP:
                                moe_xpose(e_ + 1)
                        for g in range(5):
                            ia = idxS[:, e_, g:g + 1] if g < 4 else idxP[:, e_:e_ + 1]
                            ir = "idxS" if g < 4 else "idxP"
                            P.op("pool", lambda e, g=g, ia=ia: e.indirect_dma_start(out=X, out_offset=bass.IndirectOffsetOnAxis(ap=ia, axis=0),
                                                                                     in_=osb[g][:], in_offset=None, compute_op=ALU.add),
                                 reads=[ir, ("osb", g)], writes=["Xall"], dmaq="poolq")
                    P.barrier()
                if phase_end("moe", l):
                    lst.close()
                    return True
                lst.close()

        def run_final():
            with ExitStack() as st:
                fgbc = T(st, "fgbc", [128, D])
                P.dma(fgbc[:], final_g.partition_broadcast(128), writes=["fgbc"])
                xf = [T(st, "xf%d" % i, [128, D]) for i in range(3)]
                yf = [T(st, "yf%d" % i, [128, D]) for i in range(3)]
                junk = T(st, "junkf", [128, D], BF16)
                sf = [T(st, "sf%d" % i, [128, 2]) for i in range(3)]
                for t in range(NTOK // 128):
                    bi = t % 3
                    P.dma(xf[bi][:], X[t * 128:(t + 1) * 128, :], writes=[("xf", bi)])
                    P.act(lambda e, bi=bi: e.activation(out=junk[:], in_=xf[bi][:], func=AF.Square, accum_out=sf[bi][:, 0:1]),
                          reads=[("xf", bi)], writes=["junkf", ("sf", bi)])
                    P.act(lambda e, bi=bi: e.activation(out=sf[bi][:, 0:1], in_=sf[bi][:, 0:1], func=AF.Sqrt, scale=1.0 / D, bias=epsT[:, 0:1]),
                          reads=[("sf", bi), "epsT"], writes=[("sf", bi)])
                    P.dve(lambda e, bi=bi: e.reciprocal(out=sf[bi][:, 1:2], in_=sf[bi][:, 0:1]), reads=[("sf", bi)], writes=[("sf1", bi)])
                    P.dve(lambda e, bi=bi: e.scalar_tensor_tensor(out=yf[bi][:], in0=xf[bi][:], scalar=sf[bi][:, 1:2], in1=fgbc[:], op0=ALU.mult, op1=ALU.mult),
                          reads=[("xf", bi), ("sf1", bi), "fgbc"], writes=[("yf", bi)])
                    P.dma(y_out[t * 128:(t + 1) * 128, :], yf[bi][:], reads=[("yf", bi)], writes=[("y", t)], q="poolq")
                pj = ps_rot.next()
                P.pe(lambda e: e.transpose(out=bank(pj)[:, 0:128], in_=nsfm[:], identity=ident[:]), reads=["nsfm", "ident"], writes=[("ps", pj)])
                nsr = T(st, "nsr", [128, 128])
                P.dve(lambda e: e.tensor_copy(out=nsr[:], in_=bank(pj)[:, 0:128]), reads=[("ps", pj)], writes=["nsr"])
                for l in range(2):
                    for d in range(2):
                        for r in range(4):
                            r0 = ((l * 2 + d) * 4 + r) * 8
                            P.dma(ns_out[r, l, d, :].rearrange("(c p) -> c p", p=128), nsr[r0:r0 + 8, :], reads=["nsr"], writes=[("ns", l, d, r)])
                P.barrier()
        if not run_setup():
            if not phase_end("fm", 0):
                run_layers()
        P.barrier()
        run_final()
    nc._prog_stats = (dict(P.cnt), dict(P.dma_i), P.nops)
    return nc


_NC_CACHE = {}


def kernel(x_prompt, x_sample, state_lru, c, c_ctx, norm1_g, norm2_g, final_g, w_mod, b_mod, w_in,
           pool_w, pool_scale, conv_w, conv_b, lru_wr, lru_br, lru_wi, lru_bi, lru_lambda,
           w_br_pool, w_br_lru, w_out, router_w, exp_w1, exp_w3, exp_w2):
    f = lambda a: np.ascontiguousarray(np.asarray(a, dtype=np.float32))
    if "nc" not in _NC_CACHE:
        _NC_CACHE["nc"] = build_nc()
    nc = _NC_CACHE["nc"]
    shared = dict(norm1_g=f(norm1_g), norm2_g=f(norm2_g), final_g=f(final_g), w_mod=f(w_mod), b_mod=f(b_mod),
                  w_in=f(w_in), pool_w=f(pool_w), pool_scale=f(pool_scale), conv_w=f(conv_w), conv_b=f(conv_b),
                  lru_wr=f(lru_wr), lru_br=f(lru_br), lru_wi=f(lru_wi), lru_bi=f(lru_bi), lru_lambda=f(lru_lambda),
                  w_br_pool=f(w_br_pool), w_br_lru=f(w_br_lru), w_out=f(w_out), router_w=f(router_w),
                  exp_w1=f(exp_w1), exp_w3=f(exp_w3), exp_w2=f(exp_w2))
    xp, xs, sl, cc, cx = f(x_prompt), f(x_sample), f(state_lru), f(c), f(c_ctx)
    in_maps = []
    for k in range(8):
        m = dict(shared)
        m["x_in"] = np.concatenate([xp[4 * k:4 * k + 4].reshape(1024, D), xs[k]], axis=0)
        m["cvec"] = np.stack([cx, cc[k]], axis=0)
        m["h0s"] = np.ascontiguousarray(sl[k])
        in_maps.append(m)
    res = run_bass_kernel_spmd(nc, in_maps, core_ids=list(range(8)))
    y_prompt = np.zeros((32, 256, D), np.float32)
    y_sample = np.zeros((8, 4096, D), np.float32)
    ns = np.zeros((32, 2, 2, D), np.float32)
    for k in range(8):
        r = res.results[k]
        y_prompt[4 * k:4 * k + 4] = r["y"][0:1024].reshape(4, 256, D)
        y_sample[k] = r["y"][1024:]
        ns[4 * k:4 * k + 4] = r["ns"]
    return (y_prompt, y_sample, ns)
```
